# Optimizing a Trainium2 kernel written in Bass

```python
import jax
import jax.numpy as jnp
from jax import lax
import numpy as np

D_MODEL = 1024
BATCH = 8
SEQ = 4096
DEPTH = 4

N_A = DEPTH // 2
N_B = DEPTH - N_A
ALPHA = (2.0 * DEPTH) ** 0.25
BETA = (8.0 * DEPTH) ** -0.25
LN_EPS = 1e-5

SSM_EXPAND = 2
D_INNER = SSM_EXPAND * D_MODEL
SSM_HEAD_DIM = 64
SSM_HEADS = D_INNER // SSM_HEAD_DIM
SSM_GROUPS = 4
SSM_HPG = SSM_HEADS // SSM_GROUPS
SSM_STATE = 128
CONV_WIDTH = 4
SSM_CHUNK = 128
SSM_BC_DIM = SSM_GROUPS * SSM_STATE
CONV_DIM = D_INNER + 2 * SSM_BC_DIM
SSM_IN_DIM = D_INNER + CONV_DIM + SSM_HEADS
DT_MIN = 1e-3
DT_MAX = 1e-1

NSA_HEAD_DIM = 64
NSA_Q_HEADS = D_MODEL // NSA_HEAD_DIM
NSA_KV_HEADS = 4
NSA_Q_PER_KV = NSA_Q_HEADS // NSA_KV_HEADS
NSA_N_BRANCH = 3
NSA_Q_DIM = NSA_Q_HEADS * NSA_HEAD_DIM
NSA_KV_DIM = NSA_KV_HEADS * NSA_HEAD_DIM
CMP_BLOCK = 32
CMP_STRIDE = 16
SLC_BLOCK = 64
N_SELECT = 16
WINDOW = 512
PHI_HIDDEN = 4 * NSA_HEAD_DIM
SLC_QUERY_BLOCK = 32
WIN_QUERY_BLOCK = 128
ROPE_THETA = 10000.0
ATTN_SCALE = NSA_HEAD_DIM ** -0.5
FORCED_SCORE = 1e9

N_EXPERTS = 32
TOP_K = 4
D_EXPERT = D_MODEL
SWIGLU_LIMIT = 7.0
SWIGLU_ALPHA = 1.702
MOE_ROW_BLOCK = 256

kernel_name = "yoco_mamba2_nsa_moe_deepnorm_adaln"


def _normal(key, shape, scale):
    return jax.random.normal(key, shape, jnp.float32) * scale


def setup_inputs(seed: int = 0) -> dict:
    key = jax.random.key(seed)
    it = iter(jax.random.split(key, 40))
    nk = lambda: next(it)
    D = D_MODEL
    x = _normal(nk(), (BATCH, SEQ, D), 1.0)
    c = _normal(nk(), (BATCH, D), 1.0)
    pos = (jax.random.randint(nk(), (BATCH, 1), 0, 1024, dtype=jnp.int32)
           + jnp.arange(SEQ, dtype=jnp.int32)[None, :])
    ada_w = _normal(nk(), (DEPTH, D, 6 * D), 0.2 * D ** -0.5)
    ada_b = _normal(nk(), (DEPTH, 6 * D), 0.01)
    ln_g = 1.0 + _normal(nk(), (DEPTH, 2, D), 0.01)
    ln_b = _normal(nk(), (DEPTH, 2, D), 0.01)
    ssm_in_w = _normal(nk(), (N_A, D, SSM_IN_DIM), D ** -0.5)
    ssm_conv_w = _normal(nk(), (N_A, CONV_WIDTH, CONV_DIM), CONV_WIDTH ** -0.5)
    ssm_conv_b = _normal(nk(), (N_A, CONV_DIM), 0.01)
    u = jax.random.uniform(nk(), (N_A, SSM_HEADS), jnp.float32)
    dt0 = jnp.exp(u * (np.log(DT_MAX) - np.log(DT_MIN)) + np.log(DT_MIN))
    ssm_dt_bias = dt0 + jnp.log(-jnp.expm1(-dt0))
    ssm_a_log = jnp.log(jax.random.uniform(nk(), (N_A, SSM_HEADS), jnp.float32, 1.0, 16.0))
    ssm_d = 1.0 + _normal(nk(), (N_A, SSM_HEADS), 0.1)
    ssm_norm_w = 1.0 + _normal(nk(), (N_A, D_INNER), 0.01)
    ssm_out_w = _normal(nk(), (N_A, D_INNER, D), BETA * D_INNER ** -0.5)
    kv_ada_w = _normal(nk(), (D, 2 * D), 0.2 * D ** -0.5)
    kv_ada_b = _normal(nk(), (2 * D,), 0.01)
    kv_w = _normal(nk(), (D, 2 * NSA_N_BRANCH * NSA_KV_DIM), D ** -0.5)
    cmp_pos = _normal(nk(), (CMP_BLOCK, NSA_HEAD_DIM), 0.1)
    phi_in = CMP_BLOCK * NSA_HEAD_DIM
    phi_k_w1 = _normal(nk(), (phi_in, PHI_HIDDEN), phi_in ** -0.5)
    phi_k_w2 = _normal(nk(), (PHI_HIDDEN, NSA_HEAD_DIM), PHI_HIDDEN ** -0.5)
    phi_v_w1 = _normal(nk(), (phi_in, PHI_HIDDEN), phi_in ** -0.5)
    phi_v_w2 = _normal(nk(), (PHI_HIDDEN, NSA_HEAD_DIM), PHI_HIDDEN ** -0.5)
    nsa_q_w = _normal(nk(), (N_B, D, NSA_Q_DIM + NSA_N_BRANCH * NSA_Q_HEADS), D ** -0.5)
    nsa_o_w = _normal(nk(), (N_B, NSA_Q_DIM, D), BETA * NSA_Q_DIM ** -0.5)
    router_w = _normal(nk(), (DEPTH, D, N_EXPERTS), D ** -0.5)
    router_b = _normal(nk(), (DEPTH, N_EXPERTS), 0.01)
    moe_w_up = _normal(nk(), (DEPTH, N_EXPERTS, D, 2 * D_EXPERT), D ** -0.5)
    moe_b_up = _normal(nk(), (DEPTH, N_EXPERTS, 2 * D_EXPERT), 0.01)
    moe_w_down = _normal(nk(), (DEPTH, N_EXPERTS, D_EXPERT, D), BETA * D_EXPERT ** -0.5)
    moe_b_down = _normal(nk(), (DEPTH, N_EXPERTS, D), 0.01)
    return {"x": x, "c": c, "pos": pos, "ada_w": ada_w, "ada_b": ada_b, "ln_g": ln_g, "ln_b": ln_b,
            "ssm_in_w": ssm_in_w, "ssm_conv_w": ssm_conv_w, "ssm_conv_b": ssm_conv_b,
            "ssm_dt_bias": ssm_dt_bias, "ssm_a_log": ssm_a_log, "ssm_d": ssm_d,
            "ssm_norm_w": ssm_norm_w, "ssm_out_w": ssm_out_w,
            "kv_ada_w": kv_ada_w, "kv_ada_b": kv_ada_b, "kv_w": kv_w, "cmp_pos": cmp_pos,
            "phi_k_w1": phi_k_w1, "phi_k_w2": phi_k_w2, "phi_v_w1": phi_v_w1, "phi_v_w2": phi_v_w2,
            "nsa_q_w": nsa_q_w, "nsa_o_w": nsa_o_w, "router_w": router_w, "router_b": router_b,
            "moe_w_up": moe_w_up, "moe_b_up": moe_b_up, "moe_w_down": moe_w_down,
            "moe_b_down": moe_b_down}


def layer_norm(x, g, b):
    xf = x.astype(jnp.float32)
    mu = jnp.mean(xf, axis=-1, keepdims=True)
    var = jnp.mean(jnp.square(xf - mu), axis=-1, keepdims=True)
    return ((xf - mu) * lax.rsqrt(var + LN_EPS) * g + b).astype(x.dtype)


def modulate(x, shift, scale):
    return x * (1.0 + scale[:, None, :]) + shift[:, None, :]


def post_norm(x, y, gate, g, b):
    return layer_norm(ALPHA * x + (1.0 + gate[:, None, :]) * y, g, b)


def rope(x, pos):
    half = x.shape[-1] // 2
    inv = ROPE_THETA ** (-jnp.arange(half, dtype=jnp.float32) / half)
    ang = pos.astype(jnp.float32)[..., None] * inv
    cos = jnp.cos(ang)[:, :, None, :]
    sin = jnp.sin(ang)[:, :, None, :]
    x1 = x[..., :half].astype(jnp.float32)
    x2 = x[..., half:].astype(jnp.float32)
    return jnp.concatenate([x1 * cos - x2 * sin, x2 * cos + x1 * sin], axis=-1).astype(x.dtype)


def masked_softmax(s, mask):
    s = jnp.where(mask, s.astype(jnp.float32), -jnp.inf)
    m = jnp.max(s, axis=-1, keepdims=True)
    m = jnp.where(jnp.isfinite(m), m, 0.0)
    p = jnp.exp(s - m)
    return p / jnp.maximum(jnp.sum(p, axis=-1, keepdims=True), jnp.finfo(jnp.float32).tiny)


def ssd_chunked(xs, dt, a_neg, bm, cm):
    bsz, seq = xs.shape[:2]
    nc, L = seq // SSM_CHUNK, SSM_CHUNK
    G, R, P, N = SSM_GROUPS, SSM_HPG, SSM_HEAD_DIM, SSM_STATE
    xdt = (xs.astype(jnp.float32) * dt[..., None]).reshape(bsz, nc, L, G, R, P)
    a_cs = jnp.cumsum((dt * a_neg).reshape(bsz, nc, L, G, R), axis=2)
    bc = bm.astype(jnp.float32).reshape(bsz, nc, L, G, N)
    cc = cm.astype(jnp.float32).reshape(bsz, nc, L, G, N)
    seg = a_cs[:, :, :, None] - a_cs[:, :, None, :]
    tri = jnp.tril(jnp.ones((L, L), dtype=bool))
    decay = jnp.exp(jnp.where(tri[:, :, None, None], seg, -jnp.inf))
    cb = jnp.einsum("bclgn,bcsgn->bclsg", cc, bc)
    y_diag = jnp.einsum("bclsg,bclsgr,bcsgrp->bclgrp", cb, decay, xdt)
    decay_end = jnp.exp(a_cs[:, :, -1:] - a_cs)
    states = jnp.einsum("bclgn,bclgr,bclgrp->bcgrpn", bc, decay_end, xdt)
    chunk_decay = jnp.exp(a_cs[:, :, -1])

    def step(carry, inp):
        st, dec = inp
        return carry * dec[..., None, None] + st, carry

    init = jnp.zeros((bsz, G, R, P, N), jnp.float32)
    _, prev = lax.scan(step, init, (jnp.swapaxes(states, 0, 1), jnp.swapaxes(chunk_decay, 0, 1)))
    prev = jnp.swapaxes(prev, 0, 1)
    y_off = jnp.einsum("bclgn,bcgrpn,bclgr->bclgrp", cc, prev, jnp.exp(a_cs))
    return (y_diag + y_off).reshape(bsz, seq, G, R, P)


def mamba2_mixer(h, in_w, conv_w, conv_b, dt_bias, a_log, d_skip, norm_w, out_w):
    bsz, seq, _ = h.shape
    G, R, P, N = SSM_GROUPS, SSM_HPG, SSM_HEAD_DIM, SSM_STATE
    zxbcdt = h @ in_w
    z = zxbcdt[..., :D_INNER]
    xbc = zxbcdt[..., D_INNER:D_INNER + CONV_DIM]
    dt_raw = zxbcdt[..., D_INNER + CONV_DIM:]
    xbc = lax.conv_general_dilated(xbc, conv_w[:, None, :], window_strides=(1,),
                                   padding=[(CONV_WIDTH - 1, 0)],
                                   dimension_numbers=("NWC", "WIO", "NWC"),
                                   feature_group_count=CONV_DIM)
    xbc = jax.nn.silu(xbc + conv_b)
    xs = xbc[..., :D_INNER].reshape(bsz, seq, G, R, P)
    bm = xbc[..., D_INNER:D_INNER + SSM_BC_DIM].reshape(bsz, seq, G, N)
    cm = xbc[..., D_INNER + SSM_BC_DIM:].reshape(bsz, seq, G, N)
    dt = jax.nn.softplus(dt_raw.astype(jnp.float32) + dt_bias).reshape(bsz, seq, G, R)
    a_neg = -jnp.exp(a_log.astype(jnp.float32)).reshape(G, R)
    y = ssd_chunked(xs, dt, a_neg, bm, cm) + xs.astype(jnp.float32) * d_skip.reshape(G, R)[:, :, None]
    y = y.reshape(bsz, seq, D_INNER) * jax.nn.silu(z.astype(jnp.float32))
    yg = y.reshape(bsz, seq, G, D_INNER // G)
    yg = yg * lax.rsqrt(jnp.mean(jnp.square(yg), axis=-1, keepdims=True) + LN_EPS)
    y = (yg.reshape(bsz, seq, D_INNER) * norm_w).astype(h.dtype)
    return y @ out_w


def compress_blocks(k, cmp_pos, w1, w2):
    bsz, seq = k.shape[:2]
    sub = k.reshape(bsz, seq // CMP_STRIDE, CMP_STRIDE, NSA_KV_HEADS, NSA_HEAD_DIM)
    blocks = jnp.concatenate([sub[:, :-1], sub[:, 1:]], axis=2) + cmp_pos[:, None, :]
    n_cmp = blocks.shape[1]
    flat = blocks.transpose(0, 1, 3, 2, 4).reshape(bsz, n_cmp, NSA_KV_HEADS, CMP_BLOCK * NSA_HEAD_DIM)
    return jax.nn.gelu(flat @ w1) @ w2


def nsa_shared_kv(h, c_act, pos, kv_ada_w, kv_ada_b, kv_w, cmp_pos,
                  phi_k_w1, phi_k_w2, phi_v_w1, phi_v_w2):
    bsz, seq, _ = h.shape
    shift, scale = jnp.split(c_act @ kv_ada_w + kv_ada_b, 2, axis=-1)
    kv = (modulate(h, shift, scale) @ kv_w).reshape(bsz, seq, 2 * NSA_N_BRANCH, NSA_KV_HEADS, NSA_HEAD_DIM)
    k_cmp = compress_blocks(rope(kv[:, :, 0], pos), cmp_pos, phi_k_w1, phi_k_w2)
    v_cmp = compress_blocks(kv[:, :, 1], cmp_pos, phi_v_w1, phi_v_w2)
    k_slc = rope(kv[:, :, 2], pos)
    v_slc = kv[:, :, 3]
    k_win = rope(kv[:, :, 4], pos)
    v_win = kv[:, :, 5]
    return (k_cmp, v_cmp, k_slc, v_slc, k_win, v_win)


def cmp_slc_overlap(n_cmp, n_slc):
    cs = jnp.arange(n_cmp)[:, None] * CMP_STRIDE
    js = jnp.arange(n_slc)[None, :] * SLC_BLOCK
    ov = jnp.minimum(cs + CMP_BLOCK, js + SLC_BLOCK) - jnp.maximum(cs, js)
    return jnp.maximum(ov, 0).astype(jnp.float32) / CMP_BLOCK


def compressed_branch(q, k_cmp, v_cmp):
    seq = q.shape[1]
    n_cmp = k_cmp.shape[1]
    s = jnp.einsum("bsgrd,bngd->bgrsn", q, k_cmp) * ATTN_SCALE
    t = jnp.arange(seq)
    blk_end = jnp.arange(n_cmp) * CMP_STRIDE + CMP_BLOCK - 1
    p = masked_softmax(s, blk_end[None, :] <= t[:, None])
    o = jnp.einsum("bgrsn,bngd->bsgrd", p.astype(v_cmp.dtype), v_cmp)
    importance = jnp.einsum("bgrsn,nj->bgsj", p, cmp_slc_overlap(n_cmp, seq // SLC_BLOCK))
    return o, importance


def select_blocks(importance):
    seq, n_slc = importance.shape[-2:]
    n_sel = min(N_SELECT, n_slc)
    t_blk = jnp.arange(seq)[:, None] // SLC_BLOCK
    j = jnp.arange(n_slc)[None, :]
    forced = (j == 0) | (j == t_blk) | (j == t_blk - 1)
    score = jnp.where(forced, FORCED_SCORE, importance)
    score = jnp.where(j <= t_blk, score, -jnp.inf)
    top_v, idx = lax.top_k(score, n_sel)
    return idx, jnp.isfinite(top_v)


def selected_branch(q, k, v, blk_idx, blk_valid):
    bsz, seq = q.shape[:2]
    G, R, Dh = NSA_KV_HEADS, NSA_Q_PER_KV, NSA_HEAD_DIM
    n_slc = seq // SLC_BLOCK
    n_sel = blk_idx.shape[-1]
    QB = SLC_QUERY_BLOCK
    nq = seq // QB
    kb = k.reshape(bsz, n_slc, SLC_BLOCK, G, Dh).transpose(0, 3, 1, 2, 4)
    vb = v.reshape(bsz, n_slc, SLC_BLOCK, G, Dh).transpose(0, 3, 1, 2, 4)
    q_blocks = jnp.swapaxes(q.reshape(bsz, nq, QB, G, R, Dh), 0, 1)
    idx_blocks = blk_idx.reshape(bsz, G, nq, QB, n_sel).transpose(2, 0, 1, 3, 4)
    val_blocks = blk_valid.reshape(bsz, G, nq, QB, n_sel).transpose(2, 0, 1, 3, 4)
    t_blocks = jnp.arange(seq, dtype=jnp.int32).reshape(nq, QB)
    b_ix = jnp.arange(bsz)[:, None, None, None]
    g_ix = jnp.arange(G)[None, :, None, None]
    offs = jnp.arange(SLC_BLOCK, dtype=jnp.int32)

    def one_block(args):
        qb, ib, valb, tb = args
        kg = kb[b_ix, g_ix, ib]
        vg = vb[b_ix, g_ix, ib]
        s = jnp.einsum("bqgrd,bgqnkd->bgrqnk", qb, kg) * ATTN_SCALE
        kpos = ib[..., None] * SLC_BLOCK + offs
        mask = valb[..., None] & (kpos <= tb[None, None, :, None, None])
        p = masked_softmax(s.reshape(bsz, G, R, QB, n_sel * SLC_BLOCK),
                           mask.reshape(bsz, G, 1, QB, n_sel * SLC_BLOCK))
        p = p.reshape(bsz, G, R, QB, n_sel, SLC_BLOCK).astype(v.dtype)
        return jnp.einsum("bgrqnk,bgqnkd->bqgrd", p, vg)

    out = lax.map(one_block, (q_blocks, idx_blocks, val_blocks, t_blocks))
    return jnp.swapaxes(out, 0, 1).reshape(bsz, seq, G, R, Dh)


def window_branch(q, k, v):
    bsz, seq = q.shape[:2]
    G, R, Dh = NSA_KV_HEADS, NSA_Q_PER_KV, NSA_HEAD_DIM
    QB = WIN_QUERY_BLOCK
    nb = seq // QB
    span = WINDOW + QB
    k_pad = jnp.pad(k, ((0, 0), (WINDOW, 0), (0, 0), (0, 0)))
    v_pad = jnp.pad(v, ((0, 0), (WINDOW, 0), (0, 0), (0, 0)))
    q_off = jnp.arange(QB, dtype=jnp.int32)
    k_off = jnp.arange(span, dtype=jnp.int32)

    def one_block(i):
        start = i * QB
        qb = lax.dynamic_slice_in_dim(q, start, QB, axis=1)
        kw = lax.dynamic_slice_in_dim(k_pad, start, span, axis=1)
        vw = lax.dynamic_slice_in_dim(v_pad, start, span, axis=1)
        s = jnp.einsum("bqgrd,bkgd->bgrqk", qb, kw) * ATTN_SCALE
        t = start + q_off
        kpos = start - WINDOW + k_off
        diff = t[:, None] - kpos[None, :]
        mask = (diff >= 0) & (diff < WINDOW) & (kpos >= 0)[None, :]
        p = masked_softmax(s, mask).astype(v.dtype)
        return jnp.einsum("bgrqk,bkgd->bqgrd", p, vw)

    out = lax.map(one_block, jnp.arange(nb, dtype=jnp.int32))
    return jnp.swapaxes(out, 0, 1).reshape(bsz, seq, G, R, Dh)


def nsa_mixer(h, pos, q_w, o_w, k_cmp, v_cmp, k_slc, v_slc, k_win, v_win):
    bsz, seq, _ = h.shape
    G, R, Dh = NSA_KV_HEADS, NSA_Q_PER_KV, NSA_HEAD_DIM
    qg = h @ q_w
    q = rope(qg[..., :NSA_Q_DIM].reshape(bsz, seq, NSA_Q_HEADS, Dh), pos).reshape(bsz, seq, G, R, Dh)
    gates = jax.nn.sigmoid(qg[..., NSA_Q_DIM:].astype(jnp.float32)).reshape(bsz, seq, G, R, NSA_N_BRANCH)
    o_cmp, importance = compressed_branch(q, k_cmp, v_cmp)
    blk_idx, blk_valid = select_blocks(importance)
    o_slc = selected_branch(q, k_slc, v_slc, blk_idx, blk_valid)
    o_win = window_branch(q, k_win, v_win)
    o = gates[..., 0:1] * o_cmp + gates[..., 1:2] * o_slc + gates[..., 2:3] * o_win
    return o.astype(h.dtype).reshape(bsz, seq, NSA_Q_DIM) @ o_w


def clamped_swiglu(u):
    glu = jnp.minimum(u[..., :D_EXPERT], SWIGLU_LIMIT)
    lin = jnp.clip(u[..., D_EXPERT:], -SWIGLU_LIMIT, SWIGLU_LIMIT)
    return glu * jax.nn.sigmoid(SWIGLU_ALPHA * glu) * (lin + 1.0)


def moe(h, router_w, router_b, w_up, b_up, w_down, b_down):
    bsz, seq, D = h.shape
    T = bsz * seq
    xt = h.reshape(T, D)
    logits = (xt @ router_w + router_b).astype(jnp.float32)
    top_v, top_i = lax.top_k(logits, TOP_K)
    gate = jax.nn.softmax(top_v, axis=-1).astype(h.dtype)
    A = T * TOP_K
    eid = top_i.reshape(A)
    tok = jnp.arange(A, dtype=jnp.int32) // TOP_K
    wts = gate.reshape(A)
    order = jnp.argsort(eid)
    se = eid[order]
    counts = jnp.bincount(eid, length=N_EXPERTS)
    padded = (counts + MOE_ROW_BLOCK - 1) // MOE_ROW_BLOCK * MOE_ROW_BLOCK
    off = jnp.cumsum(counts) - counts
    poff = jnp.cumsum(padded) - padded
    dest = poff[se] + jnp.arange(A, dtype=jnp.int32) - off[se]
    R_rows = A + N_EXPERTS * MOE_ROW_BLOCK
    n_blk = R_rows // MOE_ROW_BLOCK
    row_tok = jnp.full((R_rows,), T, jnp.int32).at[dest].set(tok[order])
    row_w = jnp.zeros((R_rows,), h.dtype).at[dest].set(wts[order])
    blk_start = jnp.arange(n_blk, dtype=jnp.int32) * MOE_ROW_BLOCK
    blk_e = jnp.minimum(jnp.searchsorted(jnp.cumsum(padded), blk_start, side="right"), N_EXPERTS - 1)
    x_pad = jnp.concatenate([xt, jnp.zeros((1, D), h.dtype)], axis=0)
    xg = x_pad[row_tok].reshape(n_blk, MOE_ROW_BLOCK, D)

    def expert_rows(args):
        xb, e = args
        return clamped_swiglu(xb @ w_up[e] + b_up[e]) @ w_down[e] + b_down[e]

    y = lax.map(expert_rows, (xg, blk_e)).reshape(R_rows, D)
    out = jnp.zeros((T + 1, D), h.dtype).at[row_tok].add(y * row_w[:, None])[:T]
    return out.reshape(bsz, seq, D)


def reference(x, c, pos, ada_w, ada_b, ln_g, ln_b, ssm_in_w, ssm_conv_w, ssm_conv_b, ssm_dt_bias,
              ssm_a_log, ssm_d, ssm_norm_w, ssm_out_w, kv_ada_w, kv_ada_b, kv_w, cmp_pos,
              phi_k_w1, phi_k_w2, phi_v_w1, phi_v_w2, nsa_q_w, nsa_o_w, router_w, router_b,
              moe_w_up, moe_b_up, moe_w_down, moe_b_down):
    c_act = jax.nn.silu(c)
    shared = None
    for i in range(DEPTH):
        mod = c_act @ ada_w[i] + ada_b[i]
        sh_t, sc_t, g_t, sh_c, sc_c, g_c = jnp.split(mod, 6, axis=-1)
        h = modulate(x, sh_t, sc_t)
        if i < N_A:
            y = mamba2_mixer(h, ssm_in_w[i], ssm_conv_w[i], ssm_conv_b[i], ssm_dt_bias[i],
                             ssm_a_log[i], ssm_d[i], ssm_norm_w[i], ssm_out_w[i])
        else:
            j = i - N_A
            y = nsa_mixer(h, pos, nsa_q_w[j], nsa_o_w[j], *shared)
        x = post_norm(x, y, g_t, ln_g[i, 0], ln_b[i, 0])
        y = moe(modulate(x, sh_c, sc_c), router_w[i], router_b[i], moe_w_up[i], moe_b_up[i],
                moe_w_down[i], moe_b_down[i])
        x = post_norm(x, y, g_c, ln_g[i, 1], ln_b[i, 1])
        if i == N_A - 1:
            shared = nsa_shared_kv(x, c_act, pos, kv_ada_w, kv_ada_b, kv_w, cmp_pos,
                                   phi_k_w1, phi_k_w2, phi_v_w1, phi_v_w2)
    return x
```

```python
import contextlib
import math
import numpy as np
import ml_dtypes
import concourse.bass as bass
import concourse.mybir as mybir
from concourse.bass_utils import run_bass_kernel_spmd

F32 = mybir.dt.float32
BF16 = mybir.dt.bfloat16
I32 = mybir.dt.int32
AF = mybir.ActivationFunctionType
ALU = mybir.AluOpType

D = 1024
DEPTH = 4
ALPHA = (2.0 * DEPTH) ** 0.25
LN_EPS = 1e-5
EPS_A = LN_EPS / (ALPHA * ALPHA)
ATTN_SCALE = 0.125
TINY = 1e-30
SEM_LIMIT = 30000


class Buf:
    __slots__ = ("t", "name", "w", "rd")

    def __init__(self, t, name):
        self.t = t
        self.name = name
        self.w = None
        self.rd = {}

    def __getitem__(self, idx):
        return self.t[idx]


class Rot:
    def __init__(self, bufs):
        self.bufs = bufs
        self.i = 0

    def next(self):
        b = self.bufs[self.i % len(self.bufs)]
        self.i += 1
        return b


class Em:
    ENG = ("pe", "act", "dve", "pool", "sp")

    def __init__(self, nc, n_dma_sems=12, same_engine_sync=True):
        self.nc = nc
        self.es = contextlib.ExitStack()
        self.eng = {"pe": nc.tensor, "act": nc.scalar, "dve": nc.vector, "pool": nc.gpsimd, "sp": nc.sync}
        self.sem = {}
        self.cnt = {}
        self.cur = {}
        self.gen = {}
        self.owner = {}
        for e in self.ENG:
            self.gen[e] = 0
            self._new_sem(e)
        self.known = {e: {} for e in self.ENG}
        self.dma_pool = {}
        self.dma_idx = {}
        self.dma_uses = {}
        self.dma_gen = 0
        for q in ("sp", "pool", "act"):
            self.dma_pool[q] = []
            for i in range(n_dma_sems):
                self.dma_pool[q].append(self._new_dma_sem(q))
            self.dma_idx[q] = 0
        self.same = same_engine_sync
        self.phase_stack = None
        self.uid = 0
        self.n_wait = 0
        self.n_ins = 0

    def _new_sem(self, e):
        k = "%s_%d" % (e, self.gen[e])
        self.gen[e] += 1
        self.sem[k] = self.es.enter_context(self.nc.semaphore("s_" + k))
        self.cnt[k] = 0
        self.cur[e] = k
        self.owner[k] = e

    def _new_dma_sem(self, q):
        k = "d_%s_%d" % (q, self.dma_gen)
        self.dma_gen += 1
        self.sem[k] = self.es.enter_context(self.nc.semaphore(k))
        self.dma_uses[k] = 0
        self.owner[k] = "dma"
        return k

    def _stack(self, persist):
        return self.es if (persist or self.phase_stack is None) else self.phase_stack

    def sb(self, shape, dtype=F32, name=None, persist=False):
        self.uid += 1
        nm = "%s_%d" % (name or "t", self.uid)
        t = self._stack(persist).enter_context(self.nc.sbuf_tensor(nm, list(shape), dtype))
        return Buf(t, nm)

    def rot(self, n, shape, dtype=F32, name=None):
        return Rot([self.sb(shape, dtype, name) for _ in range(n)])

    def ps(self, shape, dtype=F32, name=None, persist=False):
        self.uid += 1
        nm = "%s_%d" % (name or "p", self.uid)
        t = self._stack(persist).enter_context(self.nc.psum_tensor(nm, list(shape), dtype))
        return Buf(t, nm)

    def dram(self, name, shape, dtype, kind="Internal"):
        t = self.nc.dram_tensor(name, list(shape), dtype, kind=kind)
        return t.ap()

    @contextlib.contextmanager
    def phase(self):
        assert self.phase_stack is None
        self.barrier()
        self.phase_stack = contextlib.ExitStack()
        try:
            with self.phase_stack:
                yield
                self.barrier()
        finally:
            self.phase_stack = None

    def _need(self, e, dep):
        if dep is None:
            return
        k, v = dep
        if self.owner[k] == e and not (self.same and e != "pe"):
            return
        if self.known[e].get(k, 0) >= v:
            return
        self.eng[e].wait_ge(self.sem[k], v)
        self.known[e][k] = v
        self.n_wait += 1

    def _deps(self, e, reads, writes):
        for b in reads:
            self._need(e, b.w)
        for b in writes:
            self._need(e, b.w)
            for k, v in b.rd.items():
                self._need(e, (k, v))

    def _record(self, tok, reads, writes):
        k, v = tok
        for b in reads:
            if b.rd.get(k, 0) < v:
                b.rd[k] = v
        for b in writes:
            b.w = tok
            b.rd = {}

    def op(self, e, ins_fn, reads=(), writes=()):
        self._deps(e, reads, writes)
        ins = ins_fn(self.eng[e])
        k = self.cur[e]
        self.cnt[k] += 1
        ins.then_inc(self.sem[k], 1)
        self._record((k, self.cnt[k]), reads, writes)
        self.n_ins += 1
        if self.cnt[k] >= SEM_LIMIT:
            self._new_sem(e)
        return ins

    def dma(self, q, out, in_, reads=(), writes=(), **kw):
        self._deps(q, reads, writes)
        pool = self.dma_pool[q]
        slot = self.dma_idx[q] % len(pool)
        k = pool[slot]
        self.dma_idx[q] += 1
        if 16 * (self.dma_uses[k] + 1) > SEM_LIMIT:
            k = self._new_dma_sem(q)
            pool[slot] = k
        if self.dma_uses[k] > 0:
            self._need(q, (k, 16 * self.dma_uses[k]))
        self.dma_uses[k] += 1
        ins = self.eng[q].dma_start(out=out, in_=in_, **kw)
        ins.then_inc(self.sem[k], 16)
        self._record((k, 16 * self.dma_uses[k]), reads, writes)
        self.n_ins += 1
        return ins

    def barrier(self):
        for e in self.ENG:
            for k, c in self.cnt.items():
                if c > 0:
                    self._need(e, (k, c))
            for k, u in self.dma_uses.items():
                if u > 0:
                    self._need(e, (k, 16 * u))

    def close(self):
        self.es.close()


def host_consts(T):
    c = {}
    c["ident"] = np.eye(128, dtype=np.float32)
    s = np.arange(128)
    c["tri"] = (s[:, None] <= s[None, :]).astype(np.float32)
    d = np.arange(64)
    inv = (10000.0 ** (-(np.arange(32, dtype=np.float32)) / np.float32(32))).astype(np.float32)
    c["ropec"] = np.stack([inv[d % 32], np.where(d < 32, -1.0, 1.0).astype(np.float32)], axis=1).astype(np.float32)
    n = np.arange(256)
    t = np.arange(T)
    m = ((16 * n[:, None] + 31) <= t[None, :]) & (n[:, None] < T // 16 - 1)
    c["mcmp"] = m.reshape(2, 128, T).astype(np.float32)
    n_cmp = T // 16 - 1
    n_slc = T // 64
    cs = np.arange(256)[:, None] * 16
    js = np.arange(64)[None, :] * 64
    ov = np.maximum(np.minimum(cs + 32, js + 64) - np.maximum(cs, js), 0).astype(np.float32) / 32.0
    ov[n_cmp:, :] = 0.0
    ov[:, n_slc:] = 0.0
    c["ovaug"] = np.concatenate([ov, np.ones((256, 1), np.float32)], axis=1).reshape(2, 128, 65)
    tb = (t // 64)[:, None]
    j = np.arange(64)[None, :]
    forced = ((j == 0) | (j == tb) | (j == tb - 1)).astype(np.float32)
    cb = (j <= tb).astype(np.float32)
    c["selc"] = np.stack([1.0 - forced, forced * (1e9 + 1024.0 * j), cb, (cb - 1.0) * 1e30], axis=1).astype(np.float32)
    E = np.zeros((64, 32, 128), np.float32)
    for kt in range(32):
        E[2 * kt, kt, :64] = 1.0
        E[2 * kt + 1, kt, 64:] = 1.0
    c["eall"] = E.astype(ml_dtypes.bfloat16)
    mm = np.arange(128)[:, None, None]
    dd = np.arange(4)[None, :, None]
    nn = np.arange(512)[None, None, :]
    c["cz"] = ((128 * dd + mm) <= nn).astype(ml_dtypes.bfloat16)
    rr = np.arange(8)[None, :, None] - 3
    diff = 128 * rr + nn - mm
    c["wm"] = ((diff >= 0) & (diff < 512)).astype(ml_dtypes.bfloat16)
    return c


def swap_halves(w, ncols):
    w = w[:, :ncols].reshape(w.shape[0], ncols // 64, 2, 32)
    return np.ascontiguousarray(w[:, :, ::-1, :].reshape(w.shape[0], ncols))


def build(T, stages=None, dbg=False):
    NT = T // 512
    NS = T // 128
    nc = bass.Bass("TRN2", target_bir_lowering=False)
    em = Em(nc)
    on = lambda s: stages is None or s in stages

    em.declared = []
    any_moe = stages is None or any(st.startswith("moe") for st in stages)

    def din(name, shape, dt=F32):
        if name in ("moe_w_up", "moe_w_down") and not any_moe:
            return None
        em.declared.append(name)
        return em.dram(name, shape, dt, kind="ExternalInput")

    x_in = din("x", [T, D])
    c_in = din("c", [128, 8])
    pos_in = din("pos", [1, T], I32)
    ada_w = din("ada_w", [4, D, 6 * D])
    ada_b = din("ada_b", [4, 6 * D])
    ln_g = din("ln_g", [8, D])
    ln_b = din("ln_b", [8, D])
    ssm_in_w = din("ssm_in_w", [2, D, 5152])
    ssm_conv_w = din("ssm_conv_w", [2, 4, 3072])
    ssm_conv_b = din("ssm_conv_b", [2, 3072])
    ssm_dt_bias = din("ssm_dt_bias", [2, 32])
    ssm_a_log = din("ssm_a_log", [2, 32])
    ssm_d = din("ssm_d", [2, 32])
    ssm_norm_w = din("ssm_norm_w", [2, 2048])
    ssm_out_w = din("ssm_out_w", [2, 2048, D])
    kv_ada_w = din("kv_ada_w", [D, 2 * D])
    kv_ada_b = din("kv_ada_b", [1, 2 * D])
    kv_w = din("kv_w", [D, 1536])
    kv_w_sw = din("kv_w_sw", [D, 768])
    cmp_pos = din("cmp_pos", [32, 64])
    phi_w1 = [din("phi_k_w1", [2048, 256]), din("phi_v_w1", [2048, 256])]
    phi_w2 = [din("phi_k_w2", [256, 64]), din("phi_v_w2", [256, 64])]
    nsa_q_w = din("nsa_q_w", [2, D, 1072])
    nsa_q_w_sw = din("nsa_q_w_sw", [2, D, 1024])
    nsa_o_w = din("nsa_o_w", [2, D, D])
    router_w = din("router_w", [4, D, 32])
    router_b = din("router_b", [4, 32])
    moe_w_up = din("moe_w_up", [4, 32, D, 2 * D])
    moe_b_up = din("moe_b_up", [4, 32, 2 * D])
    moe_w_down = din("moe_w_down", [4, 32, D, D])
    moe_b_down = din("moe_b_down", [4, 32, D])
    k_ident = din("k_ident", [128, 128])
    k_tri = din("k_tri", [128, 128])
    k_ropec = din("k_ropec", [64, 2])
    k_mcmp = din("k_mcmp", [2, 128, T])
    k_ovaug = din("k_ovaug", [2, 128, 65])
    k_selc = din("k_selc", [T, 4, 64])
    k_eall = din("k_eall", [64, 32, 128], BF16)
    k_cz = din("k_cz", [128, 4, 512], BF16)
    k_wm = din("k_wm", [128, 8, 512], BF16)

    y_out = em.dram("y", [T, D], F32, kind="ExternalOutput")
    sk = "ExternalOutput" if dbg else "Internal"
    xs_d = [em.dram("xA", [8, 128, T], F32, kind=sk), em.dram("xB", [8, 128, T], F32, kind=sk)]
    ymix_d = em.dram("ymix", [8, 128, T], F32, kind=sk)
    zs_d = em.dram("zs_tok", [T, 2048], BF16, kind=sk)
    dt_d = em.dram("dt_tok", [T, 32], F32, kind=sk)
    xbc_d = em.dram("xbcT", [24, 128, T], BF16, kind=sk)
    rope_d = em.dram("rope", [2, 64, T], F32, kind=sk)
    KT_d = em.dram("KT", [3, 4, 64, T], BF16, kind=sk)
    VT_d = em.dram("VT", [T, 512], BF16, kind=sk)
    KC_d = em.dram("KC", [4, 64, 256], BF16, kind=sk)
    VC_d = em.dram("VC", [4, 2, 128, 64], BF16, kind=sk)

    ident = em.sb([128, 128], F32, "ident", persist=True)
    identb = em.sb([128, 128], BF16, "identb", persist=True)
    ones32 = em.sb([128, 128], F32, "ones32", persist=True)
    onesb = em.sb([128, 64], BF16, "onesb", persist=True)
    mods = em.sb([128, 4, 48], F32, "mods", persist=True)
    kvmod = em.sb([128, 16], F32, "kvmod", persist=True)
    lng = em.sb([128, 8, 8], F32, "lng", persist=True)
    lnb = em.sb([128, 8, 8], F32, "lnb", persist=True)
    epsb = em.sb([128, 1], F32, "epsb", persist=True)
    eps5 = em.sb([128, 1], F32, "eps5", persist=True)

    em.dma("sp", ident[:], k_ident, writes=[ident])
    em.op("dve", lambda e: e.tensor_copy(identb[:], ident[:]), reads=[ident], writes=[identb])
    em.op("dve", lambda e: e.memset(ones32[:], 1.0), writes=[ones32])
    em.op("dve", lambda e: e.memset(onesb[:], 1.0), writes=[onesb])
    em.op("dve", lambda e: e.memset(epsb[:], EPS_A), writes=[epsb])
    em.op("dve", lambda e: e.memset(eps5[:], LN_EPS), writes=[eps5])

    def rowsT(dst_ap, src_rows_ap, R, C, pbuf, dstbuf, tmp=None, tmp_ap=None):
        if tmp is None:
            tmp = em.sb([R, C * 128], F32, "rowsT")
            tmp_ap = tmp[:]
        em.dma("sp", tmp_ap[0:R, 0:C * 128], src_rows_ap, writes=[tmp])
        for c in range(C):
            em.op("pe", lambda e: e.transpose(pbuf[:, c * R:(c + 1) * R], tmp_ap[0:R, c * 128:(c + 1) * 128], ident[0:R, 0:R]),
                  reads=[tmp, ident], writes=[pbuf])
        em.op("dve", lambda e: e.tensor_copy(dst_ap, pbuf[:, 0:C * R].rearrange("p (c r) -> p c r", r=R)),
              reads=[pbuf], writes=[dstbuf])

    if on("mod"):
        with em.phase():
            pm = em.ps([128, 512], F32)
            cT = em.sb([128, 8])
            cact = em.sb([128, 8])
            em.dma("sp", cT[:], c_in, writes=[cT])
            em.op("act", lambda e: e.activation(out=cact[:], in_=cT[:], func=AF.Silu), reads=[cT], writes=[cact])
            rowsT(lng[:], ln_g, 8, 8, pm, lng)
            rowsT(lnb[:], ln_b, 8, 8, pm, lnb)
            wrot = em.rot(2, [128, 8, 512], F32, "adaw")
            pmod = em.ps([128, 64], F32)
            bT = em.sb([128, 48, 4])
            rowsT(bT[:], ada_b, 4, 48, pm, bT)
            bTk = em.sb([128, 16, 1])
            rowsT(bTk[:], kv_ada_b, 1, 16, pm, bTk)
            for i in range(5):
                ncol = 48 if i < 4 else 16
                for cb in range(ncol // 4):
                    wk = wrot.next()
                    src = ada_w[i][:, cb * 512:(cb + 1) * 512] if i < 4 else kv_ada_w[:, cb * 512:(cb + 1) * 512]
                    em.dma("sp", wk[:], src.rearrange("(k p) f -> p k f", p=128), writes=[wk])
                    for o4 in range(4):
                        oc = cb * 4 + o4
                        for k in range(8):
                            em.op("pe", lambda e: e.matmul(pmod[:, oc:oc + 1], wk[:, k, o4 * 128:(o4 + 1) * 128], cact[:, k:k + 1],
                                                           start=(k == 0), stop=(k == 7)),
                                  reads=[wk, cact], writes=[pmod])
                dst = mods[:, i, :] if i < 4 else kvmod[:]
                dbuf = mods if i < 4 else kvmod
                bsl = bT[:, :, i] if i < 4 else bTk[:, :, 0]
                em.op("dve", lambda e: e.tensor_tensor(dst, pmod[:, 0:ncol], bsl, ALU.add),
                      reads=[pmod, bT, bTk], writes=[dbuf])
            for i in range(4):
                for c0 in (8, 32):
                    em.op("dve", lambda e: e.tensor_scalar_add(mods[:, i, c0:c0 + 8], mods[:, i, c0:c0 + 8], 1.0),
                          reads=[mods], writes=[mods])
                for c0 in (16, 40):
                    em.op("dve", lambda e: e.tensor_scalar(mods[:, i, c0:c0 + 8], mods[:, i, c0:c0 + 8], 1.0, 1.0 / ALPHA,
                                                           ALU.add, ALU.mult), reads=[mods], writes=[mods])
            em.op("dve", lambda e: e.tensor_scalar_add(kvmod[:, 8:16], kvmod[:, 8:16], 1.0), reads=[kvmod], writes=[kvmod])

    if dbg and on("mod"):
        dbg_mods = em.dram("dbg_mods", [128, 4, 48], F32, kind="ExternalOutput")
        em.dma("sp", dbg_mods, mods[:], reads=[mods])

    if on("in"):
        with em.phase():
            xin = em.rot(2, [128, 4, D], F32, "xin")
            xo = em.rot(2, [128, 8, 512], F32, "xo")
            pp = Rot([em.ps([128, 512], F32) for _ in range(4)])
            for i in range(NT):
                a = xin.next()
                em.dma("sp", a[:], x_in[i * 512:(i + 1) * 512, :].rearrange("(s p) f -> p s f", p=128), writes=[a])
                o = xo.next()
                for c in range(8):
                    p = pp.next()
                    for s in range(4):
                        em.op("pe", lambda e: e.transpose(p[:, s * 128:(s + 1) * 128], a[:, s, c * 128:(c + 1) * 128], ident[:]),
                              reads=[a, ident], writes=[p])
                    eng = "act" if c % 2 else "dve"
                    if eng == "act":
                        em.op("act", lambda e: e.copy(o[:, c, :], p[:]), reads=[p], writes=[o])
                    else:
                        em.op("dve", lambda e: e.tensor_copy(o[:, c, :], p[:]), reads=[p], writes=[o])
                em.dma("sp", xs_d[0][:, :, i * 512:(i + 1) * 512].rearrange("c p t -> p c t"), o[:], reads=[o])

    def load_mod_tile(xsrc, i, xt_r, dst, sc1, sh, dst_buf):
        for c in range(8):
            xt = xt_r.next()
            em.dma("sp", xt[:], xsrc[c, :, i * 512:(i + 1) * 512], writes=[xt])
            eng = "dve" if c % 2 == 0 else "pool"
            em.op(eng, lambda e: e.tensor_scalar(dst(c), xt[:], sc1[:, c:c + 1], sh[:, c:c + 1], ALU.mult, ALU.add),
                  reads=[xt, mods, kvmod], writes=[dst_buf])

    def post_norm(xsrc, ysrc, xdst, g1a, r, final):
        with em.phase():
            xt_r = em.rot(2, [128, 8, 512], F32, "lnx")
            yt_r = em.rot(2, [128, 8, 512], F32, "lny")
            z_r = em.rot(2, [128, 8, 512], F32, "lnz")
            sq_r = em.rot(1, [128, 8, 512], F32, "lnsq")
            xo_r = em.rot(2, [128, 8, 512], F32, "lno")
            psum_s = em.ps([128, 512], F32)
            psum_q = em.ps([128, 512], F32)
            mean = em.sb([128, 512]); msq = em.sb([128, 512]); var = em.sb([128, 512]); rstd = em.sb([128, 512])
            if final:
                pT = [em.ps([128, 512], F32), em.ps([128, 512], F32)]
                ot_r = em.rot(2, [128, 4, D], F32, "lnot")
            for i in range(NT):
                sl = slice(i * 512, (i + 1) * 512)
                xt = xt_r.next(); yt = yt_r.next(); z = z_r.next(); sq = sq_r.next(); xo = xo_r.next()
                em.dma("sp", xt[:], xsrc[:, :, sl].rearrange("c p t -> p c t"), writes=[xt])
                em.dma("sp", yt[:], ysrc[:, :, sl].rearrange("c p t -> p c t"), writes=[yt])
                for c in range(8):
                    eng = "dve"
                    em.op(eng, lambda e: e.scalar_tensor_tensor(z[:, c, :], yt[:, c, :], g1a[:, c:c + 1], xt[:, c, :], ALU.mult, ALU.add),
                          reads=[yt, xt, mods], writes=[z])
                em.op("act", lambda e: e.activation(out=sq[:], in_=z[:], func=AF.Square), reads=[z], writes=[sq])
                for c in range(8):
                    em.op("pe", lambda e: e.matmul(psum_s[:], ones32[:], z[:, c, :], start=(c == 0), stop=(c == 7)),
                          reads=[ones32, z], writes=[psum_s])
                for c in range(8):
                    em.op("pe", lambda e: e.matmul(psum_q[:], ones32[:], sq[:, c, :], start=(c == 0), stop=(c == 7)),
                          reads=[ones32, sq], writes=[psum_q])
                em.op("act", lambda e: e.mul(mean[:], psum_s[:], 1.0 / D), reads=[psum_s], writes=[mean])
                em.op("pool", lambda e: e.tensor_tensor(msq[:], mean[:], mean[:], ALU.mult), reads=[mean], writes=[msq])
                em.op("dve", lambda e: e.scalar_tensor_tensor(var[:], psum_q[:], 1.0 / D, msq[:], ALU.mult, ALU.subtract),
                      reads=[psum_q, msq], writes=[var])
                em.op("act", lambda e: e.activation(out=var[:], in_=var[:], func=AF.Sqrt, bias=epsb[:], scale=1.0),
                      reads=[var, epsb], writes=[var])
                em.op("dve", lambda e: e.reciprocal(rstd[:], var[:]), reads=[var], writes=[rstd])
                for c in range(8):
                    em.op("pool", lambda e: e.tensor_tensor(z[:, c, :], z[:, c, :], mean[:], ALU.subtract), reads=[z, mean], writes=[z])
                    em.op("dve", lambda e: e.tensor_tensor(z[:, c, :], z[:, c, :], rstd[:], ALU.mult), reads=[z, rstd], writes=[z])
                    em.op("act", lambda e: e.activation(out=xo[:, c, :], in_=z[:, c, :], func=AF.Identity,
                                                        bias=lnb[:, c, r:r + 1], scale=lng[:, c, r:r + 1]),
                          reads=[z, lng, lnb], writes=[xo])
                if not final:
                    em.dma("sp", xdst[:, :, sl].rearrange("c p t -> p c t"), xo[:], reads=[xo])
                else:
                    ot = ot_r.next()
                    for s in range(4):
                        for c in range(8):
                            p = pT[c // 4]
                            em.op("pe", lambda e: e.transpose(p[:, (c % 4) * 128:(c % 4 + 1) * 128], xo[:, c, s * 128:(s + 1) * 128], ident[:]),
                                  reads=[xo, ident], writes=[p])
                        em.op("act", lambda e: e.copy(ot[:, s, 0:512], pT[0][:]), reads=[pT[0]], writes=[ot])
                        em.op("dve", lambda e: e.tensor_copy(ot[:, s, 512:1024], pT[1][:]), reads=[pT[1]], writes=[ot])
                    em.dma("sp", y_out[sl, :].rearrange("(s p) f -> p s f", p=128), ot[:], reads=[ot])

    def moe_phase(l, xsrc, ydst):
        TB = min(1024, T)
        NJ = TB // 512
        sc1 = mods[:, l, 32:40]
        sh = mods[:, l, 24:32]
        with em.phase():
            rw = em.sb([128, 8, 32], F32, "rw")
            em.dma("sp", rw[:], router_w[l].rearrange("(k p) e -> p k e", p=128), writes=[rw])
            rb = em.sb([128, 32], F32, "rb")
            em.dma("sp", rb[:], router_b[l:l + 1, :].to_broadcast([128, 32]), writes=[rb])
            P = [em.ps([128, 512], F32) for _ in range(7)]
            bup = em.sb([128, 16, 32], F32, "bup")
            bdn = em.sb([32, D], BF16, "bdn")
            em.dma("pool", bdn[:], moe_b_down[l], writes=[bdn])
            acc = em.sb([128, 8, TB], F32, "acc")
            hb = em.sb([128, 8, TB], BF16, "hb")
            gT = em.sb([32, TB], BF16, "gT")
            h32_r = em.rot(1, [128, 8, 512], F32, "mh32")
            xt_r = em.rot(2, [128, 512], F32, "mx")
            rowsT(bup[:], moe_b_up[l], 32, 16, P[0], bup, tmp=h32_r.bufs[0], tmp_ap=h32_r.bufs[0][:].rearrange("p c t -> p (c t)"))
            wu_r = em.rot(2, [128, 8, 2048], BF16, "wu")
            wd_r = em.rot(2, [128, 8, 1024], BF16, "wd")
            hg_r = em.rot(2, [128, 8, 512], BF16, "hg")
            gsb_r = em.rot(1, [128, 512], F32, "gsb")
            glu_r = em.rot(1, [128, 512], F32, "glu")
            sig_r = em.rot(1, [128, 512], F32, "sig")
            lin_r = em.rot(1, [128, 512], F32, "lin")
            t1_r = em.rot(1, [128, 512], F32, "t1")
            sm = {k: em.sb([128, 32], F32, "sm" + k) for k in ("lg", "mask", "e", "g")}
            m8 = em.sb([128, 8]); nm = em.sb([128, 1]); ssum = em.sb([128, 1]); rs = em.sb([128, 1])
            pup = Rot(P[0:4]); pdn = Rot(P[4:6]); pg = P[6]
            for tb in range(T // TB):
                for j in range(NJ):
                    h32 = h32_r.next()
                    ti = tb * NJ + j
                    load_mod_tile(xsrc, ti, xt_r, lambda c: h32[:, c, :], sc1, sh, h32)
                    em.op("act", lambda e: e.copy(hb[:, :, j * 512:(j + 1) * 512], h32[:]), reads=[h32], writes=[hb])
                    for s in range(4):
                        pl = P[4]
                        for c in range(8):
                            em.op("pe", lambda e: e.matmul(pl[:, 0:32], h32[:, c, s * 128:(s + 1) * 128], rw[:, c, :], start=(c == 0), stop=(c == 7)),
                                  reads=[h32, rw], writes=[pl])
                        lg, mask, ee, g = sm["lg"], sm["mask"], sm["e"], sm["g"]
                        em.op("dve", lambda e: e.tensor_tensor(lg[:], pl[:, 0:32], rb[:], ALU.add), reads=[pl, rb], writes=[lg])
                        em.op("dve", lambda e: e.max(m8[:], lg[:]), reads=[lg], writes=[m8])
                        em.op("dve", lambda e: e.tensor_scalar(mask[:], lg[:], m8[:, 3:4], None, ALU.is_ge), reads=[lg, m8], writes=[mask])
                        em.op("dve", lambda e: e.tensor_scalar_mul(nm[:], m8[:, 0:1], -1.0), reads=[m8], writes=[nm])
                        em.op("act", lambda e: e.activation(out=ee[:], in_=lg[:], func=AF.Exp, bias=nm[:], scale=1.0), reads=[lg, nm], writes=[ee])
                        em.op("dve", lambda e: e.tensor_tensor(ee[:], ee[:], mask[:], ALU.mult), reads=[ee, mask], writes=[ee])
                        em.op("dve", lambda e: e.reduce_sum(ssum[:], ee[:], axis=mybir.AxisListType.X), reads=[ee], writes=[ssum])
                        em.op("dve", lambda e: e.reciprocal(rs[:], ssum[:]), reads=[ssum], writes=[rs])
                        em.op("dve", lambda e: e.tensor_scalar_mul(g[:], ee[:], rs[:, 0:1]), reads=[ee, rs], writes=[g])
                        pt = P[5]
                        em.op("pe", lambda e: e.transpose(pt[0:32, 0:128], g[:], ident[:]), reads=[g, ident], writes=[pt])
                        em.op("act", lambda e: e.copy(gT[:, j * 512 + s * 128: j * 512 + (s + 1) * 128], pt[0:32, 0:128]), reads=[pt], writes=[gT])
                for j in range(NJ):
                    for oc in range(8):
                        po = pdn.next()
                        em.op("pe", lambda e: e.matmul(po[:], bdn[:, oc * 128:(oc + 1) * 128], gT[:, j * 512:(j + 1) * 512], start=True, stop=True),
                              reads=[bdn, gT], writes=[po])
                        em.op("act", lambda e: e.copy(acc[:, oc, j * 512:(j + 1) * 512], po[:]), reads=[po], writes=[acc])
                for ex in range(32):
                    wu = wu_r.next(); wd = wd_r.next()
                    em.dma("pool", wu[:], moe_w_up[l, ex].rearrange("(k p) f -> p k f", p=128), writes=[wu])
                    em.dma("pool", wd[:], moe_w_down[l, ex].rearrange("(k p) f -> p k f", p=128), writes=[wd])
                    for j in range(NJ):
                        js = slice(j * 512, (j + 1) * 512)
                        gsb = gsb_r.next()
                        em.op("pe", lambda e: e.matmul(pg[:], identb[0:32, ex:ex + 1].to_broadcast([32, 128]), gT[:, js], start=True, stop=True), reads=[identb, gT], writes=[pg])
                        em.op("act", lambda e: e.copy(gsb[:], pg[:]), reads=[pg], writes=[gsb])
                        hg = hg_r.next()
                        for c in range(8):
                            p1 = pup.next(); p2 = pup.next()
                            for k in range(8):
                                em.op("pe", lambda e: e.matmul(p1[:], wu[:, k, c * 128:(c + 1) * 128], hb[:, k, js], start=(k == 0), stop=(k == 7)),
                                      reads=[wu, hb], writes=[p1])
                            for k in range(8):
                                em.op("pe", lambda e: e.matmul(p2[:], wu[:, k, 1024 + c * 128:1024 + (c + 1) * 128], hb[:, k, js], start=(k == 0), stop=(k == 7)),
                                      reads=[wu, hb], writes=[p2])
                            glu = glu_r.next(); sig = sig_r.next(); lin = lin_r.next(); t1 = t1_r.next()
                            em.op("dve", lambda e: e.tensor_scalar(glu[:], p1[:], bup[:, c, ex:ex + 1], 7.0, ALU.add, ALU.min), reads=[p1, bup], writes=[glu])
                            em.op("act", lambda e: e.activation(out=sig[:], in_=glu[:], func=AF.Sigmoid, scale=1.702), reads=[glu], writes=[sig])
                            em.op("dve", lambda e: e.tensor_scalar(lin[:], p2[:], bup[:, 8 + c, ex:ex + 1], 7.0, ALU.add, ALU.min), reads=[p2, bup], writes=[lin])
                            em.op("pool", lambda e: e.tensor_scalar(lin[:], lin[:], -7.0, 1.0, ALU.max, ALU.add), reads=[lin], writes=[lin])
                            em.op("pool", lambda e: e.tensor_tensor(t1[:], glu[:], sig[:], ALU.mult), reads=[glu, sig], writes=[t1])
                            em.op("pool", lambda e: e.tensor_tensor(lin[:], lin[:], gsb[:], ALU.mult), reads=[lin, gsb], writes=[lin])
                            em.op("dve", lambda e: e.tensor_tensor(hg[:, c, :], t1[:], lin[:], ALU.mult), reads=[t1, lin], writes=[hg])
                        for oc in range(8):
                            po = pdn.next()
                            for k in range(8):
                                em.op("pe", lambda e: e.matmul(po[:], wd[:, k, oc * 128:(oc + 1) * 128], hg[:, k, :], start=(k == 0), stop=(k == 7)),
                                      reads=[wd, hg], writes=[po])
                            em.op("dve", lambda e: e.tensor_tensor(acc[:, oc, js], po[:], acc[:, oc, js], ALU.add), reads=[po, acc], writes=[acc])
                em.dma("sp", ydst[:, :, tb * TB:(tb + 1) * TB].rearrange("c p t -> p c t"), acc[:], reads=[acc])

    def mamba_phase(l, xsrc, ydst):
        sc1 = mods[:, l, 8:16]
        sh = mods[:, l, 0:8]
        inw = ssm_in_w[l]

        def load_hb(hb):
            xt_r = em.rot(3, [128, 512], F32, "mbx")
            for i in range(NT):
                load_mod_tile(xsrc, i, xt_r, lambda c: hb[:, c, i * 512:(i + 1) * 512], sc1, sh, hb)

        with em.phase():
            hb = em.sb([128, 8, T], BF16, "hb")
            load_hb(hb)
            wz = em.sb([128, 8, 2048], BF16, "wz")
            em.dma("pool", wz[:], inw[:, 0:2048].rearrange("(k p) f -> p k f", p=128), writes=[wz])
            wdt = em.sb([128, 8, 32], BF16, "wdt")
            em.dma("pool", wdt[:], inw[:, 5120:5152].rearrange("(k p) f -> p k f", p=128), writes=[wdt])
            dtb = em.sb([128, 32], F32, "dtb")
            em.dma("sp", dtb[:], ssm_dt_bias[l:l + 1, :].to_broadcast([128, 32]), writes=[dtb])
            pz = Rot([em.ps([128, 512], F32) for _ in range(4)])
            pd = em.ps([128, 32], F32)
            zs_r = em.rot(2, [128, 2048], BF16, "zs")
            d_r = {k: em.rot(2, [128, 32], F32, "d" + k) for k in ("x", "a", "e", "r")}
            for s in range(NS):
                ss = slice(s * 128, (s + 1) * 128)
                zs = zs_r.next()
                for q in range(4):
                    p = pz.next()
                    for k in range(8):
                        em.op("pe", lambda e: e.matmul(p[:], hb[:, k, ss], wz[:, k, q * 512:(q + 1) * 512], start=(k == 0), stop=(k == 7)),
                              reads=[hb, wz], writes=[p])
                    em.op("act", lambda e: e.activation(out=zs[:, q * 512:(q + 1) * 512], in_=p[:], func=AF.Silu), reads=[p], writes=[zs])
                em.dma("sp", zs_d[ss, :], zs[:], reads=[zs])
                for k in range(8):
                    em.op("pe", lambda e: e.matmul(pd[:], hb[:, k, ss], wdt[:, k, :], start=(k == 0), stop=(k == 7)), reads=[hb, wdt], writes=[pd])
                dx = d_r["x"].next(); da = d_r["a"].next(); de = d_r["e"].next(); dr = d_r["r"].next()
                em.op("dve", lambda e: e.tensor_tensor(dx[:], pd[:], dtb[:], ALU.add), reads=[pd, dtb], writes=[dx])
                em.op("dve", lambda e: e.tensor_scalar_mul(da[:], dx[:], -1.0), reads=[dx], writes=[da])
                em.op("dve", lambda e: e.tensor_tensor(da[:], da[:], dx[:], ALU.max), reads=[dx, da], writes=[da])
                em.op("act", lambda e: e.activation(out=de[:], in_=da[:], func=AF.Exp, scale=-1.0), reads=[da], writes=[de])
                em.op("act", lambda e: e.activation(out=de[:], in_=de[:], func=AF.Ln, bias=ones32[:, 0:1], scale=1.0), reads=[de, ones32], writes=[de])
                em.op("dve", lambda e: e.scalar_tensor_tensor(dr[:], dx[:], 0.0, de[:], ALU.max, ALU.add), reads=[dx, de], writes=[dr])
                em.dma("sp", dt_d[ss, :], dr[:], reads=[dr])

        with em.phase():
            hb = em.sb([128, 8, T], BF16, "hb")
            load_hb(hb)
            wx = em.sb([128, 8, 3072], BF16, "wx")
            em.dma("pool", wx[:], inw[:, 2048:5120].rearrange("(k p) f -> p k f", p=128), writes=[wx])
            pm = em.ps([128, 512], F32)
            cw = em.sb([128, 24, 4], F32, "cw")
            cbias = em.sb([128, 24, 1], F32, "cb")
            pp = Rot([em.ps([128, 512], F32) for _ in range(4)])
            xpad_r = em.rot(2, [128, T + 3], F32, "xpad")
            rowsT(cw[:], ssm_conv_w[l], 4, 24, pm, cw, tmp=xpad_r.bufs[0], tmp_ap=xpad_r.bufs[0][:])
            rowsT(cbias[:], ssm_conv_b[l:l + 1, :], 1, 24, pm, cbias, tmp=xpad_r.bufs[1], tmp_ap=xpad_r.bufs[1][:])
            acc_r = em.rot(2, [128, T], F32, "cacc")
            ob_r = em.rot(1, [128, T], BF16, "cob")
            for ch in range(24):
                xp = xpad_r.next(); ac = acc_r.next(); ob = ob_r.next()
                em.op("pool", lambda e: e.memset(xp[:, 0:3], 0.0), writes=[xp])
                for i in range(NT):
                    p = pp.next()
                    for k in range(8):
                        em.op("pe", lambda e: e.matmul(p[:], wx[:, k, ch * 128:(ch + 1) * 128], hb[:, k, i * 512:(i + 1) * 512], start=(k == 0), stop=(k == 7)),
                              reads=[wx, hb], writes=[p])
                    if i % 2:
                        em.op("act", lambda e: e.copy(xp[:, 3 + i * 512:3 + (i + 1) * 512], p[:]), reads=[p], writes=[xp])
                    else:
                        em.op("dve", lambda e: e.tensor_copy(xp[:, 3 + i * 512:3 + (i + 1) * 512], p[:]), reads=[p], writes=[xp])
                em.op("dve", lambda e: e.tensor_scalar(ac[:], xp[:, 0:T], cw[:, ch, 0:1], None, ALU.mult), reads=[xp, cw], writes=[ac])
                for j in range(1, 4):
                    eng = "dve"
                    em.op(eng, lambda e: e.scalar_tensor_tensor(ac[:], xp[:, j:j + T], cw[:, ch, j:j + 1], ac[:], ALU.mult, ALU.add),
                          reads=[xp, cw, ac], writes=[ac])
                em.op("act", lambda e: e.activation(out=ob[:], in_=ac[:], func=AF.Silu, bias=cbias[:, ch, :], scale=1.0), reads=[ac, cbias], writes=[ob])
                em.dma("sp", xbc_d[ch], ob[:], reads=[ob])

        with em.phase():
            tri = em.sb([128, 128], F32, "tri")
            em.dma("sp", tri[:], k_tri, writes=[tri])
            aneg = em.sb([128, 32], F32, "aneg")
            em.dma("sp", aneg[:], ssm_a_log[l:l + 1, :].to_broadcast([128, 32]), writes=[aneg])
            em.op("act", lambda e: e.activation(out=aneg[:], in_=aneg[:], func=AF.Exp), reads=[aneg], writes=[aneg])
            em.op("dve", lambda e: e.tensor_scalar_mul(aneg[:], aneg[:], -1.0), reads=[aneg], writes=[aneg])
            dsk = em.sb([128, 32], F32, "dsk")
            em.dma("sp", dsk[:], ssm_d[l:l + 1, :].to_broadcast([128, 32]), writes=[dsk])
            nw = em.sb([128, 2048], F32, "nw")
            em.dma("sp", nw[:], ssm_norm_w[l:l + 1, :].to_broadcast([128, 2048]), writes=[nw])
            wout = em.sb([128, 16, D], BF16, "wout")
            em.dma("pool", wout[:], ssm_out_w[l].rearrange("(k p) o -> p k o", p=128), writes=[wout])
            st32 = [em.sb([128, 8, 64], F32, "st32") for _ in range(4)]
            stb = [em.sb([128, 8, 64], BF16, "stb") for _ in range(4)]
            for g in range(4):
                em.op("dve", lambda e: e.memset(st32[g][:], 0.0), writes=[st32[g]])
                em.op("dve", lambda e: e.memset(stb[g][:], 0.0), writes=[stb[g]])
            ynT = em.sb([128, 16, 512], BF16, "ynT")
            ptb = em.ps([128, 512], BF16)
            pmisc = em.ps([128, 512], F32)
            par = em.ps([128, 1024], F32)
            py = em.ps([128, 512], F32)
            psn = em.ps([128, 512], F32)
            pout = em.ps([128, 512], F32)
            xsT_r = em.rot(2, [128, 16, 128], BF16, "xsT")
            bT_r = em.rot(2, [128, 4, 128], BF16, "bT")
            cT_r = em.rot(2, [128, 4, 128], BF16, "cT")
            zs_r = em.rot(2, [128, 2048], BF16, "zsl")
            dt_r = em.rot(2, [128, 32], F32, "dtl")
            xs_r = em.rot(2, [128, 32, 64], BF16, "xs")
            bt_r = em.rot(2, [128, 512], BF16, "btok")
            xdt_r = em.rot(1, [128, 32, 64], BF16, "xdt")
            xdtd_r = em.rot(1, [128, 32, 64], BF16, "xdtd")
            arow_r = em.rot(1, [128, 32, 128], F32, "arow")
            seg_r = em.rot(2, [128, 8, 128], F32, "seg")
            ear_r = em.rot(2, [128, 8, 128], F32, "ear")
            Mh_r = em.rot(2, [128, 8, 128], BF16, "Mh")
            Cs_r = em.rot(2, [128, 8, 128], BF16, "Cs")
            cbm_r = em.rot(2, [128, 128], F32, "cbm")
            yz = em.sb([128, 4, 512], F32, "yz")
            tt_r = em.rot(2, [128, 8, 64], F32, "tt")
            junk = em.sb([128, 512], F32, "junk")
            yn = em.sb([128, 2048], BF16, "yn")
            yo_r = em.rot(1, [128, 8, 512], F32, "yo")
            sA = {k: em.sb([128, 32], F32, "s" + k) for k in ("a", "acs", "tot", "d1", "dend", "cdec", "dtd")}
            ss4 = em.sb([128, 4], F32); rstd4 = em.sb([128, 4], F32)
            for c in range(NS):
                cs_ = slice(c * 128, (c + 1) * 128)
                xsT = xsT_r.next(); bT = bT_r.next(); cT = cT_r.next(); zs = zs_r.next(); dt = dt_r.next()
                em.dma("sp", xsT[:], xbc_d[0:16, :, cs_].rearrange("k p t -> p k t"), writes=[xsT])
                em.dma("sp", bT[:], xbc_d[16:20, :, cs_].rearrange("k p t -> p k t"), writes=[bT])
                em.dma("sp", cT[:], xbc_d[20:24, :, cs_].rearrange("k p t -> p k t"), writes=[cT])
                em.dma("sp", zs[:], zs_d[cs_, :], writes=[zs])
                em.dma("sp", dt[:], dt_d[cs_, :], writes=[dt])
                xs = xs_r.next(); btok = bt_r.next()
                xsf = xs[:].rearrange("p h d -> p (h d)")
                for q in range(4):
                    for k in range(4):
                        em.op("pe", lambda e: e.transpose(ptb[:, k * 128:(k + 1) * 128], xsT[:, q * 4 + k, :], identb[:]), reads=[xsT, identb], writes=[ptb])
                    em.op("act", lambda e: e.copy(xsf[:, q * 512:(q + 1) * 512], ptb[:]), reads=[ptb], writes=[xs])
                for g in range(4):
                    em.op("pe", lambda e: e.transpose(ptb[:, g * 128:(g + 1) * 128], bT[:, g, :], identb[:]), reads=[bT, identb], writes=[ptb])
                em.op("dve", lambda e: e.tensor_copy(btok[:], ptb[:]), reads=[ptb], writes=[btok])
                a, acs, tot, d1, dend, cdec, dtd = (sA[k] for k in ("a", "acs", "tot", "d1", "dend", "cdec", "dtd"))
                em.op("dve", lambda e: e.tensor_tensor(a[:], dt[:], aneg[:], ALU.mult), reads=[dt, aneg], writes=[a])
                em.op("pe", lambda e: e.matmul(pmisc[:, 0:32], tri[:], a[:], start=True, stop=True), reads=[tri, a], writes=[pmisc])
                em.op("pe", lambda e: e.matmul(pmisc[:, 32:64], ones32[:], a[:], start=True, stop=True), reads=[ones32, a], writes=[pmisc])
                em.op("dve", lambda e: e.tensor_copy(acs[:], pmisc[:, 0:32]), reads=[pmisc], writes=[acs])
                em.op("dve", lambda e: e.tensor_copy(tot[:], pmisc[:, 32:64]), reads=[pmisc], writes=[tot])
                em.op("dve", lambda e: e.tensor_tensor(d1[:], tot[:], acs[:], ALU.subtract), reads=[tot, acs], writes=[d1])
                em.op("act", lambda e: e.activation(out=dend[:], in_=d1[:], func=AF.Exp), reads=[d1], writes=[dend])
                em.op("act", lambda e: e.activation(out=cdec[:], in_=tot[:], func=AF.Exp), reads=[tot], writes=[cdec])
                em.op("dve", lambda e: e.tensor_tensor(dtd[:], dt[:], dend[:], ALU.mult), reads=[dt, dend], writes=[dtd])
                xdt = xdt_r.next(); xdtd = xdtd_r.next()
                em.op("pool", lambda e: e.tensor_tensor(xdt[:], xs[:], dt[:].unsqueeze(2).to_broadcast([128, 32, 64]), ALU.mult), reads=[xs, dt], writes=[xdt])
                em.op("dve", lambda e: e.tensor_tensor(xdtd[:], xs[:], dtd[:].unsqueeze(2).to_broadcast([128, 32, 64]), ALU.mult), reads=[xs, dtd], writes=[xdtd])
                arow = arow_r.next()
                em.op("pool", lambda e: e.tensor_tensor(arow[:], tri[:].unsqueeze(1).to_broadcast([128, 32, 128]),
                                                        a[:].unsqueeze(2).to_broadcast([128, 32, 128]), ALU.mult), reads=[tri, a], writes=[arow])
                for g in range(4):
                    for h2 in range(2):
                        em.op("pe", lambda e: e.matmul(par[:, h2 * 512:(h2 + 1) * 512], ones32[:],
                                                       arow[:, g * 8 + h2 * 4:g * 8 + h2 * 4 + 4, :].rearrange("p h l -> p (h l)"), start=True, stop=True),
                              reads=[ones32, arow], writes=[par])
                    em.op("pe", lambda e: e.matmul(pmisc[:, 128:256], bT[:, g, :], cT[:, g, :], start=True, stop=True), reads=[bT, cT], writes=[pmisc])
                    cbm = cbm_r.next()
                    em.op("dve", lambda e: e.tensor_tensor(cbm[:], pmisc[:, 128:256], tri[:], ALU.mult), reads=[pmisc, tri], writes=[cbm])
                    seg = seg_r.next(); ear = ear_r.next(); Mh = Mh_r.next(); Cs = Cs_r.next()
                    for hh in range(8):
                        h = g * 8 + hh
                        em.op("dve", lambda e: e.tensor_scalar(seg[:, hh, :], par[:, hh * 128:(hh + 1) * 128], acs[:, h:h + 1], 0.0, ALU.subtract, ALU.min),
                              reads=[par, acs], writes=[seg])
                    em.op("act", lambda e: e.activation(out=seg[:], in_=seg[:], func=AF.Exp), reads=[seg], writes=[seg])
                    em.op("pool", lambda e: e.tensor_tensor(Mh[:], seg[:], cbm[:].unsqueeze(1).to_broadcast([128, 8, 128]), ALU.mult), reads=[seg, cbm], writes=[Mh])
                    em.op("act", lambda e: e.activation(out=ear[:], in_=par[:].rearrange("p (h l) -> p h l", l=128), func=AF.Exp), reads=[par], writes=[ear])
                    em.op("dve", lambda e: e.tensor_tensor(Cs[:], ear[:], cT[:, g, :].unsqueeze(1).to_broadcast([128, 8, 128]), ALU.mult), reads=[ear, cT], writes=[Cs])
                    for hh in range(8):
                        h = g * 8 + hh
                        em.op("pe", lambda e: e.matmul(py[:, hh * 64:(hh + 1) * 64], Mh[:, hh, :], xdt[:, h, :], start=True, stop=False), reads=[Mh, xdt], writes=[py])
                        em.op("pe", lambda e: e.matmul(py[:, hh * 64:(hh + 1) * 64], Cs[:, hh, :], stb[g][:, hh, :], start=False, stop=True), reads=[Cs, stb[g]], writes=[py])
                    tt = tt_r.next()
                    em.op("pool", lambda e: e.tensor_tensor(tt[:], xs[:, g * 8:(g + 1) * 8, :], dsk[:, g * 8:(g + 1) * 8].unsqueeze(2).to_broadcast([128, 8, 64]), ALU.mult),
                          reads=[xs, dsk], writes=[tt])
                    em.op("dve", lambda e: e.tensor_tensor(yz[:, g, :], py[:], tt[:].rearrange("p h d -> p (h d)"), ALU.add), reads=[py, tt], writes=[yz])
                    em.op("pool", lambda e: e.tensor_tensor(yz[:, g, :], yz[:, g, :], zs[:, g * 512:(g + 1) * 512], ALU.mult), reads=[yz, zs], writes=[yz])
                    em.op("act", lambda e: e.activation(out=junk[:], in_=yz[:, g, :], func=AF.Square, accum_out=ss4[:, g:g + 1]), reads=[yz], writes=[junk, ss4])
                    em.op("pe", lambda e: e.matmul(psn[:], btok[:, g * 128:(g + 1) * 128], xdtd[:, g * 8:(g + 1) * 8, :].rearrange("p h d -> p (h d)"), start=True, stop=True),
                          reads=[btok, xdtd], writes=[psn])
                    em.op("pool", lambda e: e.tensor_tensor(st32[g][:], st32[g][:], cdec[:, g * 8:(g + 1) * 8].unsqueeze(2).to_broadcast([128, 8, 64]), ALU.mult),
                          reads=[st32[g], cdec], writes=[st32[g]])
                    em.op("dve", lambda e: e.tensor_tensor(st32[g][:], st32[g][:], psn[:].rearrange("p (h d) -> p h d", d=64), ALU.add), reads=[st32[g], psn], writes=[st32[g]])
                    em.op("act", lambda e: e.copy(stb[g][:], st32[g][:]), reads=[st32[g]], writes=[stb[g]])
                em.op("dve", lambda e: e.tensor_scalar(rstd4[:], ss4[:], 1.0 / 512.0, None, ALU.mult), reads=[ss4], writes=[rstd4])
                em.op("act", lambda e: e.activation(out=rstd4[:], in_=rstd4[:], func=AF.Sqrt, bias=eps5[:], scale=1.0), reads=[rstd4, eps5], writes=[rstd4])
                em.op("dve", lambda e: e.reciprocal(rstd4[:], rstd4[:]), reads=[rstd4], writes=[rstd4])
                for g in range(4):
                    eng = "dve"
                    em.op(eng, lambda e: e.scalar_tensor_tensor(yn[:, g * 512:(g + 1) * 512], yz[:, g, :], rstd4[:, g:g + 1], nw[:, g * 512:(g + 1) * 512], ALU.mult, ALU.mult),
                          reads=[yz, rstd4, nw], writes=[yn])
                c4 = c % 4
                for q in range(4):
                    for k in range(4):
                        em.op("pe", lambda e: e.transpose(ptb[:, k * 128:(k + 1) * 128], yn[:, (q * 4 + k) * 128:(q * 4 + k + 1) * 128], identb[:]), reads=[yn, identb], writes=[ptb])
                    em.op("act", lambda e: e.copy(ynT[:, q * 4:(q + 1) * 4, c4 * 128:(c4 + 1) * 128], ptb[:].rearrange("p (k t) -> p k t", t=128)), reads=[ptb], writes=[ynT])
                if c4 == 3:
                    yo = yo_r.next()
                    for oc in range(8):
                        for k in range(16):
                            em.op("pe", lambda e: e.matmul(pout[:], wout[:, k, oc * 128:(oc + 1) * 128], ynT[:, k, :], start=(k == 0), stop=(k == 15)), reads=[wout, ynT], writes=[pout])
                        em.op("dve", lambda e: e.tensor_copy(yo[:, oc, :], pout[:]), reads=[pout], writes=[yo])
                    i = c // 4
                    em.dma("sp", ydst[:, :, i * 512:(i + 1) * 512].rearrange("c p t -> p c t"), yo[:], reads=[yo])

    def rope_phase():
        with em.phase():
            rc = em.sb([64, 2], F32, "rc")
            em.dma("sp", rc[:], k_ropec, writes=[rc])
            posi = em.sb([64, T], I32, "posi")
            em.dma("sp", posi[:], pos_in[0:1, :].to_broadcast([64, T]), writes=[posi])
            ang = em.sb([64, T], F32, "ang")
            em.op("dve", lambda e: e.tensor_copy(ang[:], posi[:]), reads=[posi], writes=[ang])
            em.op("dve", lambda e: e.tensor_scalar(ang[:], ang[:], rc[:, 0:1], None, ALU.mult), reads=[ang, rc], writes=[ang])
            MAGIC = 12582912.0
            C1 = 6.28125
            C2 = 2.0 * math.pi - 6.28125
            u = em.sb([64, T], F32, "u"); kk = em.sb([64, T], F32, "kk"); r = em.sb([64, T], F32, "r")
            for which, shift in ((0, math.pi / 2.0), (1, 0.0)):
                em.op("dve", lambda e: e.tensor_scalar_add(u[:], ang[:], shift), reads=[ang], writes=[u])
                em.op("dve", lambda e: e.tensor_scalar(kk[:], u[:], 1.0 / (2.0 * math.pi), MAGIC, ALU.mult, ALU.add), reads=[u], writes=[kk])
                em.op("dve", lambda e: e.tensor_scalar_add(kk[:], kk[:], -MAGIC), reads=[kk], writes=[kk])
                em.op("dve", lambda e: e.scalar_tensor_tensor(r[:], kk[:], -C1, u[:], ALU.mult, ALU.add), reads=[kk, u], writes=[r])
                em.op("dve", lambda e: e.scalar_tensor_tensor(r[:], kk[:], -C2, r[:], ALU.mult, ALU.add), reads=[kk, r], writes=[r])
                em.op("dve", lambda e: e.tensor_scalar(r[:], r[:], math.pi, -math.pi, ALU.min, ALU.max), reads=[r], writes=[r])
                em.op("act", lambda e: e.activation(out=r[:], in_=r[:], func=AF.Sin), reads=[r], writes=[r])
                if which == 1:
                    em.op("dve", lambda e: e.tensor_scalar(r[:], r[:], rc[:, 1:2], None, ALU.mult), reads=[r, rc], writes=[r])
                em.dma("sp", rope_d[which], r[:], reads=[r])

    def kv_phase(xsrc):
        with em.phase():
            hb = em.sb([128, 8, T], BF16, "hb")
            xt_r = em.rot(3, [128, 512], F32, "kvx")
            for i in range(NT):
                load_mod_tile(xsrc, i, xt_r, lambda c: hb[:, c, i * 512:(i + 1) * 512], kvmod[:, 8:16], kvmod[:, 0:8], hb)
            kvw = em.sb([128, 8, 1536], BF16, "kvw")
            em.dma("pool", kvw[:], kv_w.rearrange("(k p) f -> p k f", p=128), writes=[kvw])
            kvws = em.sb([128, 8, 768], BF16, "kvws")
            em.dma("pool", kvws[:], kv_w_sw.rearrange("(k p) f -> p k f", p=128), writes=[kvws])
            cos_r = em.rot(2, [64, 512], F32, "cos"); sin_r = em.rot(2, [64, 512], F32, "sin")
            P = [em.ps([128, 512], F32) for _ in range(6)]
            pdr = Rot(P[0:2]); psr = Rot(P[2:4])
            w1 = []; w2 = []; biasT = []
            cp = em.sb([32, 64], F32, "cp")
            em.dma("sp", cp[:], cmp_pos, writes=[cp])
            em.op("pe", lambda e: e.transpose(P[4][0:64, 0:32], cp[0:32, 0:64], ident[0:32, 0:32]), reads=[cp, ident], writes=[P[4]])
            cposT = em.sb([64, 32], BF16, "cposT")
            em.op("dve", lambda e: e.tensor_copy(cposT[:], P[4][0:64, 0:32]), reads=[P[4]], writes=[cposT])
            for m in range(2):
                a = em.sb([64, 32, 256], BF16, "w1")
                em.dma("pool", a[:], phi_w1[m].rearrange("(j d) f -> d j f", d=64), writes=[a])
                b = em.sb([128, 2, 64], BF16, "w2")
                em.dma("pool", b[:], phi_w2[m].rearrange("(c p) d -> p c d", p=128), writes=[b])
                w1.append(a); w2.append(b)
                bt = em.sb([128, 2], F32, "biasT")
                for hc in range(2):
                    for j in range(32):
                        em.op("pe", lambda e: e.matmul(P[5][:, hc:hc + 1], a[:, j, hc * 128:(hc + 1) * 128], cposT[:, j:j + 1], start=(j == 0), stop=(j == 31)),
                              reads=[a, cposT], writes=[P[5]])
                em.op("dve", lambda e: e.tensor_copy(bt[:], P[5][:, 0:2]), reads=[P[5]], writes=[bt])
                biasT.append(bt)
            kt_r = em.rot(2, [64, T], BF16, "kt")
            t1_r = em.rot(2, [64, 512], F32, "kt1")
            t2_r = em.rot(2, [64, 512], F32, "kt2")
            hid_r = em.rot(2, [128, 2, 256], BF16, "hid")
            u_r = em.rot(2, [128, 255], F32, "gu")
            u2_r = em.rot(2, [128, 255], F32, "gu2")
            kc_r = em.rot(2, [64, 256], BF16, "kc")
            vc_r = em.rot(2, [128, 64], BF16, "vc")

            def compress(srcT, m, g):
                hid = hid_r.next()
                for hc in range(2):
                    ph = P[4]
                    for j in range(32):
                        em.op("pe", lambda e: e.matmul(ph[:, 0:255], w1[m][:, j, hc * 128:(hc + 1) * 128], srcT[:, j:j + 16 * 254 + 1:16], start=(j == 0), stop=(j == 31)),
                              reads=[w1[m], srcT], writes=[ph])
                    u = u_r.next(); u2 = u2_r.next()
                    em.op("act", lambda e: e.activation(out=u[:], in_=ph[:, 0:255], func=AF.Identity, bias=biasT[m][:, hc:hc + 1], scale=1.0), reads=[ph, biasT[m]], writes=[u])
                    em.op("pool", lambda e: e.tensor_tensor(u2[:], u[:], u[:], ALU.mult), reads=[u], writes=[u2])
                    em.op("dve", lambda e: e.tensor_scalar(u2[:], u2[:], 0.044715, 1.0, ALU.mult, ALU.add), reads=[u2], writes=[u2])
                    em.op("pool", lambda e: e.tensor_tensor(u2[:], u2[:], u[:], ALU.mult), reads=[u2, u], writes=[u2])
                    em.op("act", lambda e: e.activation(out=u2[:], in_=u2[:], func=AF.Sigmoid, scale=1.5957691216057308), reads=[u2], writes=[u2])
                    em.op("dve", lambda e: e.tensor_tensor(hid[:, hc, 0:255], u[:], u2[:], ALU.mult), reads=[u, u2], writes=[hid])
                if m == 0:
                    pk = P[5]
                    for hc in range(2):
                        em.op("pe", lambda e: e.matmul(pk[0:64, 0:255], w2[0][:, hc, :], hid[:, hc, 0:255], start=(hc == 0), stop=(hc == 1)), reads=[w2[0], hid], writes=[pk])
                    kc = kc_r.next()
                    em.op("dve", lambda e: e.memset(kc[:], 0.0), writes=[kc])
                    em.op("dve", lambda e: e.tensor_copy(kc[:, 0:255], pk[0:64, 0:255]), reads=[pk], writes=[kc])
                    em.dma("sp", KC_d[g], kc[:], reads=[kc])
                else:
                    for ncn in range(2):
                        nn = 128 if ncn == 0 else 127
                        pv = P[5]
                        for hc in range(2):
                            em.op("pe", lambda e: e.matmul(pv[0:nn, 0:64], hid[:, hc, ncn * 128:ncn * 128 + nn], w2[1][:, hc, :], start=(hc == 0), stop=(hc == 1)), reads=[hid, w2[1]], writes=[pv])
                        vc = vc_r.next()
                        em.op("dve", lambda e: e.memset(vc[:], 0.0), writes=[vc])
                        em.op("dve", lambda e: e.tensor_copy(vc[0:nn, :], pv[0:nn, 0:64]), reads=[pv], writes=[vc])
                        em.dma("sp", VC_d[g, ncn], vc[:], reads=[vc])

            for si, slot in enumerate((0, 2, 4)):
                for g in range(4):
                    kt = kt_r.next()
                    for i in range(NT):
                        sl = slice(i * 512, (i + 1) * 512)
                        pd = pdr.next(); psw = psr.next()
                        for k in range(8):
                            em.op("pe", lambda e: e.matmul(pd[0:64, :], kvw[:, k, slot * 256 + g * 64:slot * 256 + g * 64 + 64], hb[:, k, sl], start=(k == 0), stop=(k == 7)), reads=[kvw, hb], writes=[pd])
                        for k in range(8):
                            em.op("pe", lambda e: e.matmul(psw[0:64, :], kvws[:, k, si * 256 + g * 64:si * 256 + g * 64 + 64], hb[:, k, sl], start=(k == 0), stop=(k == 7)), reads=[kvws, hb], writes=[psw])
                        t1 = t1_r.next(); t2 = t2_r.next()
                        cos = cos_r.next(); sin = sin_r.next()
                        em.dma("sp", cos[:], rope_d[0, :, sl], writes=[cos])
                        em.dma("sp", sin[:], rope_d[1, :, sl], writes=[sin])
                        em.op("dve", lambda e: e.tensor_tensor(t1[:], pd[0:64, :], cos[:], ALU.mult), reads=[pd, cos], writes=[t1])
                        em.op("dve", lambda e: e.tensor_tensor(t2[:], psw[0:64, :], sin[:], ALU.mult), reads=[psw, sin], writes=[t2])
                        em.op("pool", lambda e: e.tensor_tensor(kt[:, sl], t1[:], t2[:], ALU.add), reads=[t1, t2], writes=[kt])
                    em.dma("sp", KT_d[si, g], kt[:], reads=[kt])
                    if slot == 0:
                        compress(kt, 0, g)
            for g in range(4):
                vt = kt_r.next()
                for i in range(NT):
                    sl = slice(i * 512, (i + 1) * 512)
                    pd = pdr.next()
                    for k in range(8):
                        em.op("pe", lambda e: e.matmul(pd[0:64, :], kvw[:, k, 256 + g * 64:256 + g * 64 + 64], hb[:, k, sl], start=(k == 0), stop=(k == 7)), reads=[kvw, hb], writes=[pd])
                    em.op("act", lambda e: e.copy(vt[:, sl], pd[0:64, :]), reads=[pd], writes=[vt])
                compress(vt, 1, g)
            vt_r = em.rot(2, [128, 512], BF16, "vtok")
            for s in range(NS):
                ss = slice(s * 128, (s + 1) * 128)
                pv = pdr.next()
                for half, slot in enumerate((3, 5)):
                    for k in range(8):
                        em.op("pe", lambda e: e.matmul(pv[:, half * 256:(half + 1) * 256], hb[:, k, ss], kvw[:, k, slot * 256:(slot + 1) * 256], start=(k == 0), stop=(k == 7)), reads=[hb, kvw], writes=[pv])
                vtk = vt_r.next()
                em.op("act", lambda e: e.copy(vtk[:], pv[:]), reads=[pv], writes=[vtk])
                em.dma("sp", VT_d[ss, :], vtk[:], reads=[vtk])

    def nsa_phase(l, xsrc, ydst):
        jl = l - 2
        sc1 = mods[:, l, 8:16]
        sh = mods[:, l, 0:8]
        with em.phase():
            eall = em.sb([64, 32, 128], BF16, "eall"); em.dma("sp", eall[:], k_eall, writes=[eall])
            cz = em.sb([128, 4, 512], BF16, "cz"); em.dma("sp", cz[:], k_cz, writes=[cz])
            wm = em.sb([128, 8, 512], BF16, "wm"); em.dma("sp", wm[:], k_wm, writes=[wm])
            ovaug = em.sb([128, 2, 65], F32, "ovaug"); em.dma("sp", ovaug[:], k_ovaug.rearrange("c p j -> p c j"), writes=[ovaug])
            qw = em.sb([128, 8, 1072], BF16, "qw"); em.dma("pool", qw[:], nsa_q_w[jl].rearrange("(k p) f -> p k f", p=128), writes=[qw])
            qws = em.sb([128, 8, 1024], BF16, "qws"); em.dma("pool", qws[:], nsa_q_w_sw[jl].rearrange("(k p) f -> p k f", p=128), writes=[qws])
            ow = em.sb([64, 16, D], BF16, "ow"); em.dma("pool", ow[:], nsa_o_w[jl].rearrange("(h d) o -> d h o", d=64), writes=[ow])
            KC = em.sb([64, 4, 256], BF16, "KC"); em.dma("sp", KC[:], KC_d.rearrange("g d n -> d g n"), writes=[KC])
            VC = em.sb([128, 4, 2, 64], BF16, "VC"); em.dma("sp", VC[:], VC_d.rearrange("g c p d -> p g c d"), writes=[VC])
            P = [em.ps([128, 512], F32) for _ in range(8)]
            pS_r = Rot(P[0:2]); pM = P[2]; pO = P[3]; pD = P[4]; pI = P[5]; pOut = P[6]; pQ = P[7]
            xt_r = em.rot(2, [128, 512], F32, "ax")
            hbt_r = em.rot(1, [128, 8, 512], BF16, "ahb")
            cos_r = em.rot(1, [64, 512], F32, "acos"); sin_r = em.rot(1, [64, 512], F32, "asin")
            mc_r = em.rot(1, [128, 2, 512], F32, "amc")
            oT_r = em.rot(1, [64, 16, 512], BF16, "oT")
            KS_r = em.rot(1, [64, T], BF16, "KS"); VS_r = em.rot(1, [128, NS, 64], BF16, "VS")
            KW_r = em.rot(1, [64, 1024], BF16, "KW"); VW_r = em.rot(1, [128, 8, 64], BF16, "VW")
            qt_b = [em.sb([64, 512], BF16, "qt") for _ in range(4)]
            sig_r = em.rot(2, [64, 512], F32, "sig")
            gwr_b = [em.sb([128, 8, 3, 64], BF16, "gwr") for _ in range(4)]
            oc_b = [em.sb([64, 512], F32, "ocomb") for _ in range(4)]
            t1_r = em.rot(1, [64, 512], F32, "at1"); t2_r = em.rot(1, [64, 512], F32, "at2")
            e32_r = em.rot(1, [128, 512], F32, "e32")
            p32 = [em.sb([128, 512], F32, "p32") for _ in range(2)]
            pbc = [em.sb([128, 512], BF16, "pbc") for _ in range(2)]
            eb_r = em.rot(3, [128, 512], BF16, "eb"); pb_r = em.rot(3, [128, 512], BF16, "pb")
            rden_r = em.rot(2, [64, 512], F32, "rden")
            impg = em.sb([128, 4, 64], F32, "impg")
            rd_r = em.rot(2, [128, 1], F32, "rd")
            selc_r = em.rot(2, [128, 4, 64], F32, "selc")
            sc_r = em.rot(2, [128, 64], F32, "sc"); rep_r = em.rot(2, [128, 64], F32, "rep")
            m8a = em.sb([128, 8]); m8b = em.sb([128, 8])
            selT = em.sb([64, 512], BF16, "selT")
            yo_r = em.rot(2, [128, 512], F32, "ayo")

            cur_hbt = [None]

            def finish_branch(r, b, first):
                rden = rden_r.next(); sig = sig_r.next()
                for k in range(8):
                    em.op("pe", lambda e: e.matmul(pQ[0:64, :], gwr_b[r][:, k, b, :], cur_hbt[0][:, k, :], start=(k == 0), stop=(k == 7)), reads=[gwr_b[r], cur_hbt[0]], writes=[pQ])
                em.op("act", lambda e: e.activation(out=sig[:], in_=pQ[0:64, :], func=AF.Sigmoid), reads=[pQ], writes=[sig])
                em.op("dve", lambda e: e.tensor_scalar_max(rden[:], pD[0:64, :], TINY), reads=[pD], writes=[rden])
                em.op("dve", lambda e: e.reciprocal(rden[:], rden[:]), reads=[rden], writes=[rden])
                em.op("pool", lambda e: e.tensor_tensor(rden[:], rden[:], sig[:], ALU.mult), reads=[rden, sig], writes=[rden])
                if first:
                    em.op("dve", lambda e: e.tensor_tensor(oc_b[r][:], pO[0:64, :], rden[:], ALU.mult), reads=[pO, rden], writes=[oc_b[r]])
                else:
                    em.op("dve", lambda e: e.tensor_tensor(rden[:], pO[0:64, :], rden[:], ALU.mult), reads=[pO, rden], writes=[rden])
                    em.op("pool", lambda e: e.tensor_tensor(oc_b[r][:], oc_b[r][:], rden[:], ALU.add), reads=[oc_b[r], rden], writes=[oc_b[r]])

            for i in range(NT):
                sl = slice(i * 512, (i + 1) * 512)
                hbt = hbt_r.next()
                cur_hbt[0] = hbt
                for c in range(8):
                    xt = xt_r.next()
                    em.dma("sp", xt[:], xsrc[c, :, sl], writes=[xt])
                    eng = "dve" if c % 2 == 0 else "pool"
                    em.op(eng, lambda e: e.tensor_scalar(hbt[:, c, :], xt[:], sc1[:, c:c + 1], sh[:, c:c + 1], ALU.mult, ALU.add),
                          reads=[xt, mods], writes=[hbt])
                cos = cos_r.next(); sin = sin_r.next(); mc = mc_r.next()
                em.dma("sp", cos[:], rope_d[0, :, sl], writes=[cos])
                em.dma("sp", sin[:], rope_d[1, :, sl], writes=[sin])
                em.op("dve", lambda e: e.tensor_scalar_mul(cos[:], cos[:], ATTN_SCALE), reads=[cos], writes=[cos])
                em.op("dve", lambda e: e.tensor_scalar_mul(sin[:], sin[:], ATTN_SCALE), reads=[sin], writes=[sin])
                em.dma("sp", mc[:], k_mcmp[:, :, sl].rearrange("c p t -> p c t"), writes=[mc])
                oT = oT_r.next()
                nkt = 4 * (i + 1)
                w0 = max(0, 4 * i - 4)
                for g in range(4):
                    KS = KS_r.next(); VS = VS_r.next(); KW = KW_r.next(); VW = VW_r.next()
                    em.dma("sp", KS[:, 0:nkt * 128], KT_d[1, g, :, 0:nkt * 128], writes=[KS])
                    em.dma("sp", VS[:, 0:nkt, :], VT_d[0:nkt * 128, g * 64:(g + 1) * 64].rearrange("(k p) d -> p k d", p=128), writes=[VS])
                    nwk = nkt - w0
                    em.dma("sp", KW[:, 0:nwk * 128], KT_d[2, g, :, w0 * 128:nkt * 128], writes=[KW])
                    em.dma("sp", VW[:, 0:nwk, :], VT_d[w0 * 128:nkt * 128, 256 + g * 64:256 + (g + 1) * 64].rearrange("(k p) d -> p k d", p=128), writes=[VW])
                    for r in range(4):
                        h = g * 4 + r
                        pd = pS_r.next(); psw = pS_r.next()
                        for k in range(8):
                            em.op("pe", lambda e: e.matmul(pd[0:64, :], qw[:, k, h * 64:(h + 1) * 64], hbt[:, k, :], start=(k == 0), stop=(k == 7)), reads=[qw, hbt], writes=[pd])
                        for k in range(8):
                            em.op("pe", lambda e: e.matmul(psw[0:64, :], qws[:, k, h * 64:(h + 1) * 64], hbt[:, k, :], start=(k == 0), stop=(k == 7)), reads=[qws, hbt], writes=[psw])
                        t1 = t1_r.next(); t2 = t2_r.next()
                        em.op("dve", lambda e: e.tensor_tensor(t1[:], pd[0:64, :], cos[:], ALU.mult), reads=[pd, cos], writes=[t1])
                        em.op("dve", lambda e: e.tensor_tensor(t2[:], psw[0:64, :], sin[:], ALU.mult), reads=[psw, sin], writes=[t2])
                        em.op("pool", lambda e: e.tensor_tensor(qt_b[r][:], t1[:], t2[:], ALU.add), reads=[t1, t2], writes=[qt_b[r]])
                        gwr = gwr_b[r]
                        em.op("pool", lambda e: e.tensor_copy(gwr[:], qw[:, :, 1024 + h * 3:1024 + h * 3 + 3].unsqueeze(3).to_broadcast([128, 8, 3, 64])), reads=[qw], writes=[gwr])
                        for cn in range(2):
                            pS = pS_r.next()
                            em.op("pe", lambda e: e.matmul(pS[:], KC[:, g, cn * 128:(cn + 1) * 128], qt_b[r][:], start=True, stop=True), reads=[KC, qt_b[r]], writes=[pS])
                            e32 = e32_r.next()
                            em.op("act", lambda e: e.activation(out=e32[:], in_=pS[:], func=AF.Exp), reads=[pS], writes=[e32])
                            em.op("dve", lambda e: e.tensor_tensor(p32[cn][:], e32[:], mc[:, cn, :], ALU.mult), reads=[e32, mc], writes=[p32[cn]])
                            em.op("pool", lambda e: e.tensor_copy(pbc[cn][:], p32[cn][:]), reads=[p32[cn]], writes=[pbc[cn]])
                        for cn in range(2):
                            em.op("pe", lambda e: e.matmul(pO[0:64, :], VC[:, g, cn, :], pbc[cn][:], start=(cn == 0), stop=(cn == 1)), reads=[VC, pbc[cn]], writes=[pO])
                        for cn in range(2):
                            em.op("pe", lambda e: e.matmul(pD[0:64, :], onesb[:], pbc[cn][:], start=(cn == 0), stop=(cn == 1)), reads=[onesb, pbc[cn]], writes=[pD])
                        finish_branch(r, 0, True)
                        for s in range(4):
                            for cn in range(2):
                                em.op("pe", lambda e: e.matmul(pI[:, 0:65], p32[cn][:, s * 128:(s + 1) * 128], ovaug[:, cn, :], start=(cn == 0), stop=(cn == 1)), reads=[p32[cn], ovaug], writes=[pI])
                            rd = rd_r.next()
                            em.op("dve", lambda e: e.tensor_scalar_max(rd[:], pI[:, 64:65], TINY), reads=[pI], writes=[rd])
                            em.op("dve", lambda e: e.reciprocal(rd[:], rd[:]), reads=[rd], writes=[rd])
                            if r == 0:
                                em.op("dve", lambda e: e.tensor_scalar(impg[:, s, :], pI[:, 0:64], rd[:, 0:1], None, ALU.mult), reads=[pI, rd], writes=[impg])
                            else:
                                em.op("dve", lambda e: e.scalar_tensor_tensor(impg[:, s, :], pI[:, 0:64], rd[:, 0:1], impg[:, s, :], ALU.mult, ALU.add), reads=[pI, rd, impg], writes=[impg])
                    for s in range(4):
                        selc = selc_r.next()
                        t0 = i * 512 + s * 128
                        em.dma("sp", selc[:], k_selc[t0:t0 + 128], writes=[selc])
                        sc = sc_r.next(); rep = rep_r.next()
                        em.op("dve", lambda e: e.tensor_tensor(sc[:], impg[:, s, :], selc[:, 0, :], ALU.mult), reads=[impg, selc], writes=[sc])
                        em.op("dve", lambda e: e.tensor_tensor(sc[:], sc[:], selc[:, 1, :], ALU.add), reads=[sc, selc], writes=[sc])
                        em.op("dve", lambda e: e.tensor_tensor(sc[:], sc[:], selc[:, 2, :], ALU.mult), reads=[sc, selc], writes=[sc])
                        em.op("dve", lambda e: e.tensor_tensor(sc[:], sc[:], selc[:, 3, :], ALU.add), reads=[sc, selc], writes=[sc])
                        em.op("dve", lambda e: e.max(m8a[:], sc[:]), reads=[sc], writes=[m8a])
                        em.op("dve", lambda e: e.match_replace(rep[:], m8a[:], sc[:], -3e30), reads=[sc, m8a], writes=[rep])
                        em.op("dve", lambda e: e.max(m8b[:], rep[:]), reads=[rep], writes=[m8b])
                        em.op("dve", lambda e: e.tensor_scalar(rep[:], sc[:], m8b[:, 7:8], None, ALU.is_ge), reads=[sc, m8b], writes=[rep])
                        em.op("dve", lambda e: e.tensor_tensor(rep[:], rep[:], selc[:, 2, :], ALU.mult), reads=[rep, selc], writes=[rep])
                        em.op("pe", lambda e: e.transpose(pI[0:64, 128:256], rep[:], ident[:]), reads=[rep, ident], writes=[pI])
                        em.op("act", lambda e: e.copy(selT[:, s * 128:(s + 1) * 128], pI[0:64, 128:256]), reads=[pI], writes=[selT])
                    for r in range(4):
                        h = g * 4 + r
                        for kt in range(nkt):
                            pS = pS_r.next()
                            em.op("pe", lambda e: e.matmul(pS[:], KS[:, kt * 128:(kt + 1) * 128], qt_b[r][:], start=True, stop=True), reads=[KS, qt_b[r]], writes=[pS])
                            em.op("pe", lambda e: e.matmul(pM[:], eall[:, kt, :], selT[:], start=True, stop=True), reads=[eall, selT], writes=[pM])
                            eb = eb_r.next(); pb = pb_r.next()
                            em.op("act", lambda e: e.activation(out=eb[:], in_=pS[:], func=AF.Exp), reads=[pS], writes=[eb])
                            em.op("dve", lambda e: e.tensor_tensor(pb[:], eb[:], pM[:], ALU.mult), reads=[eb, pM], writes=[pb])
                            if kt >= 4 * i:
                                em.op("pool", lambda e: e.tensor_tensor(pb[:], pb[:], cz[:, kt - 4 * i, :], ALU.mult), reads=[pb, cz], writes=[pb])
                            em.op("pe", lambda e: e.matmul(pO[0:64, :], VS[:, kt, :], pb[:], start=(kt == 0), stop=(kt == nkt - 1)), reads=[VS, pb], writes=[pO])
                            em.op("pe", lambda e: e.matmul(pD[0:64, :], onesb[:], pb[:], start=(kt == 0), stop=(kt == nkt - 1)), reads=[onesb, pb], writes=[pD])
                        finish_branch(r, 1, False)
                        for kw in range(nwk):
                            kt = w0 + kw
                            pS = pS_r.next()
                            em.op("pe", lambda e: e.matmul(pS[:], KW[:, kw * 128:(kw + 1) * 128], qt_b[r][:], start=True, stop=True), reads=[KW, qt_b[r]], writes=[pS])
                            eb = eb_r.next(); pb = pb_r.next()
                            em.op("act", lambda e: e.activation(out=eb[:], in_=pS[:], func=AF.Exp), reads=[pS], writes=[eb])
                            em.op("pool", lambda e: e.tensor_tensor(pb[:], eb[:], wm[:, 4 * i - kt + 3, :], ALU.mult), reads=[eb, wm], writes=[pb])
                            em.op("pe", lambda e: e.matmul(pO[0:64, :], VW[:, kw, :], pb[:], start=(kw == 0), stop=(kw == nwk - 1)), reads=[VW, pb], writes=[pO])
                            em.op("pe", lambda e: e.matmul(pD[0:64, :], onesb[:], pb[:], start=(kw == 0), stop=(kw == nwk - 1)), reads=[onesb, pb], writes=[pD])
                        finish_branch(r, 2, False)
                        em.op("act", lambda e: e.copy(oT[:, h, :], oc_b[r][:]), reads=[oc_b[r]], writes=[oT])
                for oc in range(8):
                    yo = yo_r.next()
                    for h in range(16):
                        em.op("pe", lambda e: e.matmul(pOut[:], ow[:, h, oc * 128:(oc + 1) * 128], oT[:, h, :], start=(h == 0), stop=(h == 15)), reads=[ow, oT], writes=[pOut])
                    em.op("dve", lambda e: e.tensor_copy(yo[:], pOut[:]), reads=[pOut], writes=[yo])
                    em.dma("sp", ydst[oc, :, sl], yo[:], reads=[yo])

    cur = 0
    if on("rope"):
        rope_phase()
    for l in range(DEPTH):
        if on("mix%d" % l):
            if l < 2:
                mamba_phase(l, xs_d[cur], ymix_d)
            else:
                nsa_phase(l, xs_d[cur], ymix_d)
        if on("ln%da" % l):
            post_norm(xs_d[cur], ymix_d, xs_d[1 - cur], mods[:, l, 16:24], 2 * l, False)
        cur = 1 - cur
        if on("moe%d" % l):
            moe_phase(l, xs_d[cur], ymix_d)
        if on("ln%db" % l):
            post_norm(xs_d[cur], ymix_d, xs_d[1 - cur], mods[:, l, 40:48], 2 * l + 1, l == DEPTH - 1)
        cur = 1 - cur
        if l == 1 and on("kv"):
            kv_phase(xs_d[cur])
    em.barrier()
    em.close()
    return nc, em


def make_in_maps(inputs, T, cores):
    cst = host_consts(T)
    f = lambda a: np.ascontiguousarray(np.asarray(a, dtype=np.float32))
    shared = {
        "ada_w": f(inputs["ada_w"]), "ada_b": f(inputs["ada_b"]),
        "ln_g": f(inputs["ln_g"]).reshape(8, D), "ln_b": f(inputs["ln_b"]).reshape(8, D),
        "ssm_in_w": f(inputs["ssm_in_w"]), "ssm_conv_w": f(inputs["ssm_conv_w"]), "ssm_conv_b": f(inputs["ssm_conv_b"]),
        "ssm_dt_bias": f(inputs["ssm_dt_bias"]), "ssm_a_log": f(inputs["ssm_a_log"]), "ssm_d": f(inputs["ssm_d"]),
        "ssm_norm_w": f(inputs["ssm_norm_w"]), "ssm_out_w": f(inputs["ssm_out_w"]),
        "kv_ada_w": f(inputs["kv_ada_w"]), "kv_ada_b": f(inputs["kv_ada_b"]).reshape(1, 2 * D),
        "kv_w": f(inputs["kv_w"]),
        "cmp_pos": f(inputs["cmp_pos"]),
        "phi_k_w1": f(inputs["phi_k_w1"]), "phi_k_w2": f(inputs["phi_k_w2"]),
        "phi_v_w1": f(inputs["phi_v_w1"]), "phi_v_w2": f(inputs["phi_v_w2"]),
        "nsa_q_w": f(inputs["nsa_q_w"]), "nsa_o_w": f(inputs["nsa_o_w"]),
        "router_w": f(inputs["router_w"]), "router_b": f(inputs["router_b"]),
        "moe_w_up": f(inputs["moe_w_up"]), "moe_b_up": f(inputs["moe_b_up"]),
        "moe_w_down": f(inputs["moe_w_down"]), "moe_b_down": f(inputs["moe_b_down"]),
    }
    kvw = shared["kv_w"]
    shared["kv_w_sw"] = np.concatenate([swap_halves(kvw[:, s * 256:(s + 1) * 256], 256) for s in (0, 2, 4)], axis=1)
    shared["nsa_q_w_sw"] = np.stack([swap_halves(shared["nsa_q_w"][j], 1024) for j in range(2)], axis=0)
    for k, v in cst.items():
        shared["k_" + k] = v
    maps = []
    for b in cores:
        m = dict(shared)
        m["x"] = f(inputs["x"][b][:T])
        m["c"] = np.ascontiguousarray(f(inputs["c"][b]).reshape(8, 128).T)
        m["pos"] = np.ascontiguousarray(np.asarray(inputs["pos"][b][:T], dtype=np.int32).reshape(1, T))
        maps.append(m)
    return maps


_CACHE = {}


def kernel(**inputs):
    T = 4096
    if T not in _CACHE:
        _CACHE[T] = build(T)[0]
    nc = _CACHE[T]
    maps = make_in_maps(inputs, T, list(range(8)))
    res = run_bass_kernel_spmd(nc, maps, core_ids=list(range(8)))
    out = np.stack([np.asarray(r["y"], dtype=np.float32) for r in res.results], axis=0)
    return out
```

```python
import contextlib
import math
import numpy as np
import ml_dtypes
import concourse.bass as bass
import concourse.mybir as mybir
from concourse.bass_utils import run_bass_kernel_spmd

F32 = mybir.dt.float32
BF16 = mybir.dt.bfloat16
I32 = mybir.dt.int32
AF = mybir.ActivationFunctionType
ALU = mybir.AluOpType

D = 1024
DEPTH = 4
ALPHA = (2.0 * DEPTH) ** 0.25
LN_EPS = 1e-5
EPS_A = LN_EPS / (ALPHA * ALPHA)
ATTN_SCALE = 0.125
TINY = 1e-30
SEM_LIMIT = 30000


class Buf:
    __slots__ = ("t", "name", "w", "rd")

    def __init__(self, t, name):
        self.t = t
        self.name = name
        self.w = None
        self.rd = {}

    def __getitem__(self, idx):
        return self.t[idx]


class Rot:
    def __init__(self, bufs):
        self.bufs = bufs
        self.i = 0

    def next(self):
        b = self.bufs[self.i % len(self.bufs)]
        self.i += 1
        return b


class Em:
    ENG = ("pe", "act", "dve", "pool", "sp")

    def __init__(self, nc, n_dma_sems=12, same_engine_sync=True):
        self.nc = nc
        self.es = contextlib.ExitStack()
        self.eng = {"pe": nc.tensor, "act": nc.scalar, "dve": nc.vector, "pool": nc.gpsimd, "sp": nc.sync}
        self.sem = {}
        self.cnt = {}
        self.cur = {}
        self.gen = {}
        self.owner = {}
        for e in self.ENG:
            self.gen[e] = 0
            self._new_sem(e)
        self.known = {e: {} for e in self.ENG}
        self.dma_pool = {}
        self.dma_idx = {}
        self.dma_uses = {}
        self.dma_gen = 0
        for q in ("sp", "pool", "act"):
            self.dma_pool[q] = []
            for i in range(n_dma_sems):
                self.dma_pool[q].append(self._new_dma_sem(q))
            self.dma_idx[q] = 0
        self.same = same_engine_sync
        self.phase_stack = None
        self.uid = 0
        self.n_wait = 0
        self.n_ins = 0

    def _new_sem(self, e):
        k = "%s_%d" % (e, self.gen[e])
        self.gen[e] += 1
        self.sem[k] = self.es.enter_context(self.nc.semaphore("s_" + k))
        self.cnt[k] = 0
        self.cur[e] = k
        self.owner[k] = e

    def _new_dma_sem(self, q):
        k = "d_%s_%d" % (q, self.dma_gen)
        self.dma_gen += 1
        self.sem[k] = self.es.enter_context(self.nc.semaphore(k))
        self.dma_uses[k] = 0
        self.owner[k] = "dma"
        return k

    def _stack(self, persist):
        return self.es if (persist or self.phase_stack is None) else self.phase_stack

    def sb(self, shape, dtype=F32, name=None, persist=False):
        self.uid += 1
        nm = "%s_%d" % (name or "t", self.uid)
        t = self._stack(persist).enter_context(self.nc.sbuf_tensor(nm, list(shape), dtype))
        return Buf(t, nm)

    def rot(self, n, shape, dtype=F32, name=None):
        return Rot([self.sb(shape, dtype, name) for _ in range(n)])

    def ps(self, shape, dtype=F32, name=None, persist=False):
        self.uid += 1
        nm = "%s_%d" % (name or "p", self.uid)
        t = self._stack(persist).enter_context(self.nc.psum_tensor(nm, list(shape), dtype))
        return Buf(t, nm)

    def dram(self, name, shape, dtype, kind="Internal"):
        t = self.nc.dram_tensor(name, list(shape), dtype, kind=kind)
        return t.ap()

    @contextlib.contextmanager
    def phase(self):
        assert self.phase_stack is None
        self.barrier()
        self.phase_stack = contextlib.ExitStack()
        try:
            with self.phase_stack:
                yield
                self.barrier()
        finally:
            self.phase_stack = None

    def _need(self, e, dep):
        if dep is None:
            return
        k, v = dep
        if self.owner[k] == e and not (self.same and e != "pe"):
            return
        if self.known[e].get(k, 0) >= v:
            return
        self.eng[e].wait_ge(self.sem[k], v)
        self.known[e][k] = v
        self.n_wait += 1

    def _deps(self, e, reads, writes):
        for b in reads:
            self._need(e, b.w)
        for b in writes:
            self._need(e, b.w)
            for k, v in b.rd.items():
                self._need(e, (k, v))

    def _record(self, tok, reads, writes):
        k, v = tok
        for b in reads:
            if b.rd.get(k, 0) < v:
                b.rd[k] = v
        for b in writes:
            b.w = tok
            b.rd = {}

    def op(self, e, ins_fn, reads=(), writes=()):
        self._deps(e, reads, writes)
        ins = ins_fn(self.eng[e])
        k = self.cur[e]
        self.cnt[k] += 1
        ins.then_inc(self.sem[k], 1)
        self._record((k, self.cnt[k]), reads, writes)
        self.n_ins += 1
        if self.cnt[k] >= SEM_LIMIT:
            self._new_sem(e)
        return ins

    def dma(self, q, out, in_, reads=(), writes=(), **kw):
        self._deps(q, reads, writes)
        pool = self.dma_pool[q]
        slot = self.dma_idx[q] % len(pool)
        k = pool[slot]
        self.dma_idx[q] += 1
        if 16 * (self.dma_uses[k] + 1) > SEM_LIMIT:
            k = self._new_dma_sem(q)
            pool[slot] = k
        if self.dma_uses[k] > 0:
            self._need(q, (k, 16 * self.dma_uses[k]))
        self.dma_uses[k] += 1
        ins = self.eng[q].dma_start(out=out, in_=in_, **kw)
        ins.then_inc(self.sem[k], 16)
        self._record((k, 16 * self.dma_uses[k]), reads, writes)
        self.n_ins += 1
        return ins

    def barrier(self):
        for e in self.ENG:
            for k, c in self.cnt.items():
                if c > 0:
                    self._need(e, (k, c))
            for k, u in self.dma_uses.items():
                if u > 0:
                    self._need(e, (k, 16 * u))

    def close(self):
        self.es.close()


def host_consts(T):
    c = {}
    c["ident"] = np.eye(128, dtype=np.float32)
    s = np.arange(128)
    c["tri"] = (s[:, None] <= s[None, :]).astype(np.float32)
    d = np.arange(64)
    inv = (10000.0 ** (-(np.arange(32, dtype=np.float32)) / np.float32(32))).astype(np.float32)
    c["ropec"] = np.stack([inv[d % 32], np.where(d < 32, -1.0, 1.0).astype(np.float32)], axis=1).astype(np.float32)
    n = np.arange(256)
    t = np.arange(T)
    m = ((16 * n[:, None] + 31) <= t[None, :]) & (n[:, None] < T // 16 - 1)
    c["mcmp"] = m.reshape(2, 128, T).astype(np.float32)
    n_cmp = T // 16 - 1
    n_slc = T // 64
    cs = np.arange(256)[:, None] * 16
    js = np.arange(64)[None, :] * 64
    ov = np.maximum(np.minimum(cs + 32, js + 64) - np.maximum(cs, js), 0).astype(np.float32) / 32.0
    ov[n_cmp:, :] = 0.0
    ov[:, n_slc:] = 0.0
    c["ovaug"] = np.concatenate([ov, np.ones((256, 1), np.float32)], axis=1).reshape(2, 128, 65)
    tb = (t // 64)[:, None]
    j = np.arange(64)[None, :]
    forced = ((j == 0) | (j == tb) | (j == tb - 1)).astype(np.float32)
    cb = (j <= tb).astype(np.float32)
    c["selc"] = np.stack([1.0 - forced, forced * (1e9 + 1024.0 * j), cb, (cb - 1.0) * 1e30], axis=1).astype(np.float32)
    E = np.zeros((64, 32, 128), np.float32)
    for kt in range(32):
        E[2 * kt, kt, :64] = 1.0
        E[2 * kt + 1, kt, 64:] = 1.0
    c["eall"] = E.astype(ml_dtypes.bfloat16)
    mm = np.arange(128)[:, None, None]
    dd = np.arange(4)[None, :, None]
    nn = np.arange(512)[None, None, :]
    c["cz"] = ((128 * dd + mm) <= nn).astype(ml_dtypes.bfloat16)
    rr = np.arange(8)[None, :, None] - 3
    diff = 128 * rr + nn - mm
    c["wm"] = ((diff >= 0) & (diff < 512)).astype(ml_dtypes.bfloat16)
    c["stri"] = (s[:, None] < s[None, :]).astype(np.float32)
    c["iota"] = np.broadcast_to(np.arange(128, dtype=np.float32)[None, :], (128, 128)).copy()
    c["base8"] = (np.arange(8, dtype=np.float32)[None, :] * 128 + np.arange(128, dtype=np.float32)[:, None]).astype(np.float32)
    return c


def swap_halves(w, ncols):
    w = w[:, :ncols].reshape(w.shape[0], ncols // 64, 2, 32)
    return np.ascontiguousarray(w[:, :, ::-1, :].reshape(w.shape[0], ncols))


def build(T, stages=None, dbg=False):
    NT = T // 512
    NS = T // 128
    nc = bass.Bass("TRN2", target_bir_lowering=False)
    em = Em(nc)
    on = lambda s: stages is None or s in stages

    em.declared = []
    any_moe = stages is None or any(st.startswith("moe") for st in stages)

    def din(name, shape, dt=F32):
        if name in ("moe_w_up", "moe_w_down") and not any_moe:
            return None
        em.declared.append(name)
        return em.dram(name, shape, dt, kind="ExternalInput")

    x_in = din("x", [T, D])
    c_in = din("c", [128, 8])
    pos_in = din("pos", [1, T], I32)
    ada_w = din("ada_w", [4, D, 6 * D])
    ada_b = din("ada_b", [4, 6 * D])
    ln_g = din("ln_g", [8, D])
    ln_b = din("ln_b", [8, D])
    ssm_in_w = din("ssm_in_w", [2, D, 5152])
    ssm_conv_w = din("ssm_conv_w", [2, 4, 3072])
    ssm_conv_b = din("ssm_conv_b", [2, 3072])
    ssm_dt_bias = din("ssm_dt_bias", [2, 32])
    ssm_a_log = din("ssm_a_log", [2, 32])
    ssm_d = din("ssm_d", [2, 32])
    ssm_norm_w = din("ssm_norm_w", [2, 2048])
    ssm_out_w = din("ssm_out_w", [2, 2048, D])
    kv_ada_w = din("kv_ada_w", [D, 2 * D])
    kv_ada_b = din("kv_ada_b", [1, 2 * D])
    kv_w = din("kv_w", [D, 1536])
    kv_w_sw = din("kv_w_sw", [D, 768])
    cmp_pos = din("cmp_pos", [32, 64])
    phi_w1 = [din("phi_k_w1", [2048, 256]), din("phi_v_w1", [2048, 256])]
    phi_w2 = [din("phi_k_w2", [256, 64]), din("phi_v_w2", [256, 64])]
    nsa_q_w = din("nsa_q_w", [2, D, 1072])
    nsa_q_w_sw = din("nsa_q_w_sw", [2, D, 1024])
    nsa_o_w = din("nsa_o_w", [2, D, D])
    router_w = din("router_w", [4, D, 32])
    router_b = din("router_b", [4, 32])
    moe_w_up = din("moe_w_up", [4, 32, D, 2 * D])
    moe_b_up = din("moe_b_up", [4, 32, 2 * D])
    moe_w_down = din("moe_w_down", [4, 32, D, D])
    moe_b_down = din("moe_b_down", [4, 32, D])
    k_ident = din("k_ident", [128, 128])
    k_tri = din("k_tri", [128, 128])
    k_ropec = din("k_ropec", [64, 2])
    k_mcmp = din("k_mcmp", [2, 128, T])
    k_ovaug = din("k_ovaug", [2, 128, 65])
    k_selc = din("k_selc", [T, 4, 64])
    k_eall = din("k_eall", [64, 32, 128], BF16)
    k_cz = din("k_cz", [128, 4, 512], BF16)
    k_wm = din("k_wm", [128, 8, 512], BF16)
    k_stri = din("k_stri", [128, 128])
    k_iota = din("k_iota", [128, 128])
    k_base8 = din("k_base8", [128, 8])

    y_out = em.dram("y", [T, D], F32, kind="ExternalOutput")
    sk = "ExternalOutput" if dbg else "Internal"
    xs_d = [em.dram("xA", [8, 128, T], F32, kind=sk), em.dram("xB", [8, 128, T], F32, kind=sk)]
    ymix_d = em.dram("ymix", [8, 128, T], F32, kind=sk)
    zs_d = em.dram("zs_tok", [T, 2048], BF16, kind=sk)
    dt_d = em.dram("dt_tok", [T, 32], F32, kind=sk)
    xbc_d = em.dram("xbcT", [24, 128, T], BF16, kind=sk)
    rope_d = em.dram("rope", [2, 64, T], F32, kind=sk)
    KT_d = em.dram("KT", [3, 4, 64, T], BF16, kind=sk)
    VT_d = em.dram("VT", [T, 512], BF16, kind=sk)
    KC_d = em.dram("KC", [4, 64, 256], BF16, kind=sk)
    VC_d = em.dram("VC", [4, 2, 128, 64], BF16, kind=sk)
    NBLK = (4 * T + 32 * 512) // 512
    RROWS = NBLK * 512
    hs_d = em.dram("h_sorted", [RROWS, D], BF16)
    ys_d = em.dram("y_sorted", [RROWS, D], F32)

    ident = em.sb([128, 128], F32, "ident", persist=True)
    identb = em.sb([128, 128], BF16, "identb", persist=True)
    ones32 = em.sb([128, 128], F32, "ones32", persist=True)
    onesb = em.sb([128, 64], BF16, "onesb", persist=True)
    mods = em.sb([128, 4, 48], F32, "mods", persist=True)
    kvmod = em.sb([128, 16], F32, "kvmod", persist=True)
    lng = em.sb([128, 8, 8], F32, "lng", persist=True)
    lnb = em.sb([128, 8, 8], F32, "lnb", persist=True)
    epsb = em.sb([128, 1], F32, "epsb", persist=True)
    eps5 = em.sb([128, 1], F32, "eps5", persist=True)

    em.dma("sp", ident[:], k_ident, writes=[ident])
    em.op("dve", lambda e: e.tensor_copy(identb[:], ident[:]), reads=[ident], writes=[identb])
    em.op("dve", lambda e: e.memset(ones32[:], 1.0), writes=[ones32])
    em.op("dve", lambda e: e.memset(onesb[:], 1.0), writes=[onesb])
    em.op("dve", lambda e: e.memset(epsb[:], EPS_A), writes=[epsb])
    em.op("dve", lambda e: e.memset(eps5[:], LN_EPS), writes=[eps5])

    def rowsT(dst_ap, src_rows_ap, R, C, pbuf, dstbuf, tmp=None, tmp_ap=None):
        if tmp is None:
            tmp = em.sb([R, C * 128], F32, "rowsT")
            tmp_ap = tmp[:]
        em.dma("sp", tmp_ap[0:R, 0:C * 128], src_rows_ap, writes=[tmp])
        for c in range(C):
            em.op("pe", lambda e: e.transpose(pbuf[:, c * R:(c + 1) * R], tmp_ap[0:R, c * 128:(c + 1) * 128], ident[0:R, 0:R]),
                  reads=[tmp, ident], writes=[pbuf])
        em.op("dve", lambda e: e.tensor_copy(dst_ap, pbuf[:, 0:C * R].rearrange("p (c r) -> p c r", r=R)),
              reads=[pbuf], writes=[dstbuf])

    if on("mod"):
        with em.phase():
            pm = em.ps([128, 512], F32)
            cT = em.sb([128, 8])
            cact = em.sb([128, 8])
            em.dma("sp", cT[:], c_in, writes=[cT])
            em.op("act", lambda e: e.activation(out=cact[:], in_=cT[:], func=AF.Silu), reads=[cT], writes=[cact])
            rowsT(lng[:], ln_g, 8, 8, pm, lng)
            rowsT(lnb[:], ln_b, 8, 8, pm, lnb)
            wrot = em.rot(2, [128, 8, 512], F32, "adaw")
            pmod = em.ps([128, 64], F32)
            bT = em.sb([128, 48, 4])
            rowsT(bT[:], ada_b, 4, 48, pm, bT)
            bTk = em.sb([128, 16, 1])
            rowsT(bTk[:], kv_ada_b, 1, 16, pm, bTk)
            for i in range(5):
                ncol = 48 if i < 4 else 16
                for cb in range(ncol // 4):
                    wk = wrot.next()
                    src = ada_w[i][:, cb * 512:(cb + 1) * 512] if i < 4 else kv_ada_w[:, cb * 512:(cb + 1) * 512]
                    em.dma("sp", wk[:], src.rearrange("(k p) f -> p k f", p=128), writes=[wk])
                    for o4 in range(4):
                        oc = cb * 4 + o4
                        for k in range(8):
                            em.op("pe", lambda e: e.matmul(pmod[:, oc:oc + 1], wk[:, k, o4 * 128:(o4 + 1) * 128], cact[:, k:k + 1],
                                                           start=(k == 0), stop=(k == 7)),
                                  reads=[wk, cact], writes=[pmod])
                dst = mods[:, i, :] if i < 4 else kvmod[:]
                dbuf = mods if i < 4 else kvmod
                bsl = bT[:, :, i] if i < 4 else bTk[:, :, 0]
                em.op("dve", lambda e: e.tensor_tensor(dst, pmod[:, 0:ncol], bsl, ALU.add),
                      reads=[pmod, bT, bTk], writes=[dbuf])
            for i in range(4):
                for c0 in (8, 32):
                    em.op("dve", lambda e: e.tensor_scalar_add(mods[:, i, c0:c0 + 8], mods[:, i, c0:c0 + 8], 1.0),
                          reads=[mods], writes=[mods])
                for c0 in (16, 40):
                    em.op("dve", lambda e: e.tensor_scalar(mods[:, i, c0:c0 + 8], mods[:, i, c0:c0 + 8], 1.0, 1.0 / ALPHA,
                                                           ALU.add, ALU.mult), reads=[mods], writes=[mods])
            em.op("dve", lambda e: e.tensor_scalar_add(kvmod[:, 8:16], kvmod[:, 8:16], 1.0), reads=[kvmod], writes=[kvmod])

    if dbg and on("mod"):
        dbg_mods = em.dram("dbg_mods", [128, 4, 48], F32, kind="ExternalOutput")
        em.dma("sp", dbg_mods, mods[:], reads=[mods])

    if on("in"):
        with em.phase():
            xin = em.rot(2, [128, 4, D], F32, "xin")
            xo = em.rot(2, [128, 8, 512], F32, "xo")
            pp = Rot([em.ps([128, 512], F32) for _ in range(4)])
            for i in range(NT):
                a = xin.next()
                em.dma("sp", a[:], x_in[i * 512:(i + 1) * 512, :].rearrange("(s p) f -> p s f", p=128), writes=[a])
                o = xo.next()
                for c in range(8):
                    p = pp.next()
                    for s in range(4):
                        em.op("pe", lambda e: e.transpose(p[:, s * 128:(s + 1) * 128], a[:, s, c * 128:(c + 1) * 128], ident[:]),
                              reads=[a, ident], writes=[p])
                    eng = "act" if c % 2 else "dve"
                    if eng == "act":
                        em.op("act", lambda e: e.copy(o[:, c, :], p[:]), reads=[p], writes=[o])
                    else:
                        em.op("dve", lambda e: e.tensor_copy(o[:, c, :], p[:]), reads=[p], writes=[o])
                em.dma("sp", xs_d[0][:, :, i * 512:(i + 1) * 512].rearrange("c p t -> p c t"), o[:], reads=[o])

    def load_mod_tile(xsrc, i, xt_r, dst, sc1, sh, dst_buf):
        for c in range(8):
            xt = xt_r.next()
            em.dma("sp", xt[:], xsrc[c, :, i * 512:(i + 1) * 512], writes=[xt])
            eng = "dve" if c % 2 == 0 else "pool"
            em.op(eng, lambda e: e.tensor_scalar(dst(c), xt[:], sc1[:, c:c + 1], sh[:, c:c + 1], ALU.mult, ALU.add),
                  reads=[xt, mods, kvmod], writes=[dst_buf])

    def post_norm(xsrc, ysrc, xdst, g1a, r, final):
        with em.phase():
            xt_r = em.rot(2, [128, 8, 512], F32, "lnx")
            yt_r = em.rot(2, [128, 8, 512], F32, "lny")
            z_r = em.rot(2, [128, 8, 512], F32, "lnz")
            sq_r = em.rot(1, [128, 8, 512], F32, "lnsq")
            xo_r = em.rot(2, [128, 8, 512], F32, "lno")
            psum_s = em.ps([128, 512], F32)
            psum_q = em.ps([128, 512], F32)
            mean = em.sb([128, 512]); msq = em.sb([128, 512]); var = em.sb([128, 512]); rstd = em.sb([128, 512])
            if final:
                pT = [em.ps([128, 512], F32), em.ps([128, 512], F32)]
                ot_r = em.rot(2, [128, 4, D], F32, "lnot")
            for i in range(NT):
                sl = slice(i * 512, (i + 1) * 512)
                xt = xt_r.next(); yt = yt_r.next(); z = z_r.next(); sq = sq_r.next(); xo = xo_r.next()
                em.dma("sp", xt[:], xsrc[:, :, sl].rearrange("c p t -> p c t"), writes=[xt])
                em.dma("sp", yt[:], ysrc[:, :, sl].rearrange("c p t -> p c t"), writes=[yt])
                for c in range(8):
                    eng = "dve"
                    em.op(eng, lambda e: e.scalar_tensor_tensor(z[:, c, :], yt[:, c, :], g1a[:, c:c + 1], xt[:, c, :], ALU.mult, ALU.add),
                          reads=[yt, xt, mods], writes=[z])
                em.op("act", lambda e: e.activation(out=sq[:], in_=z[:], func=AF.Square), reads=[z], writes=[sq])
                for c in range(8):
                    em.op("pe", lambda e: e.matmul(psum_s[:], ones32[:], z[:, c, :], start=(c == 0), stop=(c == 7)),
                          reads=[ones32, z], writes=[psum_s])
                for c in range(8):
                    em.op("pe", lambda e: e.matmul(psum_q[:], ones32[:], sq[:, c, :], start=(c == 0), stop=(c == 7)),
                          reads=[ones32, sq], writes=[psum_q])
                em.op("act", lambda e: e.mul(mean[:], psum_s[:], 1.0 / D), reads=[psum_s], writes=[mean])
                em.op("pool", lambda e: e.tensor_tensor(msq[:], mean[:], mean[:], ALU.mult), reads=[mean], writes=[msq])
                em.op("dve", lambda e: e.scalar_tensor_tensor(var[:], psum_q[:], 1.0 / D, msq[:], ALU.mult, ALU.subtract),
                      reads=[psum_q, msq], writes=[var])
                em.op("act", lambda e: e.activation(out=var[:], in_=var[:], func=AF.Sqrt, bias=epsb[:], scale=1.0),
                      reads=[var, epsb], writes=[var])
                em.op("dve", lambda e: e.reciprocal(rstd[:], var[:]), reads=[var], writes=[rstd])
                for c in range(8):
                    em.op("pool", lambda e: e.tensor_tensor(z[:, c, :], z[:, c, :], mean[:], ALU.subtract), reads=[z, mean], writes=[z])
                    em.op("dve", lambda e: e.tensor_tensor(z[:, c, :], z[:, c, :], rstd[:], ALU.mult), reads=[z, rstd], writes=[z])
                    em.op("act", lambda e: e.activation(out=xo[:, c, :], in_=z[:, c, :], func=AF.Identity,
                                                        bias=lnb[:, c, r:r + 1], scale=lng[:, c, r:r + 1]),
                          reads=[z, lng, lnb], writes=[xo])
                if not final:
                    em.dma("sp", xdst[:, :, sl].rearrange("c p t -> p c t"), xo[:], reads=[xo])
                else:
                    ot = ot_r.next()
                    for s in range(4):
                        for c in range(8):
                            p = pT[c // 4]
                            em.op("pe", lambda e: e.transpose(p[:, (c % 4) * 128:(c % 4 + 1) * 128], xo[:, c, s * 128:(s + 1) * 128], ident[:]),
                                  reads=[xo, ident], writes=[p])
                        em.op("act", lambda e: e.copy(ot[:, s, 0:512], pT[0][:]), reads=[pT[0]], writes=[ot])
                        em.op("dve", lambda e: e.tensor_copy(ot[:, s, 512:1024], pT[1][:]), reads=[pT[1]], writes=[ot])
                    em.dma("sp", y_out[sl, :].rearrange("(s p) f -> p s f", p=128), ot[:], reads=[ot])

    def moe_phase_dense(l, xsrc, ydst):
        TB = min(1024, T)
        NJ = TB // 512
        sc1 = mods[:, l, 32:40]
        sh = mods[:, l, 24:32]
        with em.phase():
            rw = em.sb([128, 8, 32], F32, "rw")
            em.dma("sp", rw[:], router_w[l].rearrange("(k p) e -> p k e", p=128), writes=[rw])
            rb = em.sb([128, 32], F32, "rb")
            em.dma("sp", rb[:], router_b[l:l + 1, :].to_broadcast([128, 32]), writes=[rb])
            P = [em.ps([128, 512], F32) for _ in range(7)]
            bup = em.sb([128, 16, 32], F32, "bup")
            bdn = em.sb([32, D], BF16, "bdn")
            em.dma("pool", bdn[:], moe_b_down[l], writes=[bdn])
            acc = em.sb([128, 8, TB], F32, "acc")
            hb = em.sb([128, 8, TB], BF16, "hb")
            gT = em.sb([32, TB], BF16, "gT")
            h32_r = em.rot(1, [128, 8, 512], F32, "mh32")
            xt_r = em.rot(2, [128, 512], F32, "mx")
            rowsT(bup[:], moe_b_up[l], 32, 16, P[0], bup, tmp=h32_r.bufs[0], tmp_ap=h32_r.bufs[0][:].rearrange("p c t -> p (c t)"))
            wu_r = em.rot(2, [128, 8, 2048], BF16, "wu")
            wd_r = em.rot(2, [128, 8, 1024], BF16, "wd")
            hg_r = em.rot(2, [128, 8, 512], BF16, "hg")
            gsb_r = em.rot(1, [128, 512], F32, "gsb")
            glu_r = em.rot(1, [128, 512], F32, "glu")
            sig_r = em.rot(1, [128, 512], F32, "sig")
            lin_r = em.rot(1, [128, 512], F32, "lin")
            t1_r = em.rot(1, [128, 512], F32, "t1")
            sm = {k: em.sb([128, 32], F32, "sm" + k) for k in ("lg", "mask", "e", "g")}
            m8 = em.sb([128, 8]); nm = em.sb([128, 1]); ssum = em.sb([128, 1]); rs = em.sb([128, 1])
            pup = Rot(P[0:4]); pdn = Rot(P[4:6]); pg = P[6]
            for tb in range(T // TB):
                for j in range(NJ):
                    h32 = h32_r.next()
                    ti = tb * NJ + j
                    load_mod_tile(xsrc, ti, xt_r, lambda c: h32[:, c, :], sc1, sh, h32)
                    em.op("act", lambda e: e.copy(hb[:, :, j * 512:(j + 1) * 512], h32[:]), reads=[h32], writes=[hb])
                    for s in range(4):
                        pl = P[4]
                        for c in range(8):
                            em.op("pe", lambda e: e.matmul(pl[:, 0:32], h32[:, c, s * 128:(s + 1) * 128], rw[:, c, :], start=(c == 0), stop=(c == 7)),
                                  reads=[h32, rw], writes=[pl])
                        lg, mask, ee, g = sm["lg"], sm["mask"], sm["e"], sm["g"]
                        em.op("dve", lambda e: e.tensor_tensor(lg[:], pl[:, 0:32], rb[:], ALU.add), reads=[pl, rb], writes=[lg])
                        em.op("dve", lambda e: e.max(m8[:], lg[:]), reads=[lg], writes=[m8])
                        em.op("dve", lambda e: e.tensor_scalar(mask[:], lg[:], m8[:, 3:4], None, ALU.is_ge), reads=[lg, m8], writes=[mask])
                        em.op("dve", lambda e: e.tensor_scalar_mul(nm[:], m8[:, 0:1], -1.0), reads=[m8], writes=[nm])
                        em.op("act", lambda e: e.activation(out=ee[:], in_=lg[:], func=AF.Exp, bias=nm[:], scale=1.0), reads=[lg, nm], writes=[ee])
                        em.op("dve", lambda e: e.tensor_tensor(ee[:], ee[:], mask[:], ALU.mult), reads=[ee, mask], writes=[ee])
                        em.op("dve", lambda e: e.reduce_sum(ssum[:], ee[:], axis=mybir.AxisListType.X), reads=[ee], writes=[ssum])
                        em.op("dve", lambda e: e.reciprocal(rs[:], ssum[:]), reads=[ssum], writes=[rs])
                        em.op("dve", lambda e: e.tensor_scalar_mul(g[:], ee[:], rs[:, 0:1]), reads=[ee, rs], writes=[g])
                        pt = P[5]
                        em.op("pe", lambda e: e.transpose(pt[0:32, 0:128], g[:], ident[:]), reads=[g, ident], writes=[pt])
                        em.op("act", lambda e: e.copy(gT[:, j * 512 + s * 128: j * 512 + (s + 1) * 128], pt[0:32, 0:128]), reads=[pt], writes=[gT])
                for j in range(NJ):
                    for oc in range(8):
                        po = pdn.next()
                        em.op("pe", lambda e: e.matmul(po[:], bdn[:, oc * 128:(oc + 1) * 128], gT[:, j * 512:(j + 1) * 512], start=True, stop=True),
                              reads=[bdn, gT], writes=[po])
                        em.op("act", lambda e: e.copy(acc[:, oc, j * 512:(j + 1) * 512], po[:]), reads=[po], writes=[acc])
                def issue_w(ex_):
                    wu_ = wu_r.next(); wd_ = wd_r.next()
                    em.dma("pool", wu_[:], moe_w_up[l, ex_].rearrange("(k p) f -> p k f", p=128), writes=[wu_])
                    em.dma("pool", wd_[:], moe_w_down[l, ex_].rearrange("(k p) f -> p k f", p=128), writes=[wd_])
                    return wu_, wd_
                nxt = issue_w(0)
                for ex in range(32):
                    wu, wd = nxt
                    if ex + 1 < 32:
                        nxt = issue_w(ex + 1)
                    for j in range(NJ):
                        js = slice(j * 512, (j + 1) * 512)
                        gsb = gsb_r.next()
                        em.op("pe", lambda e: e.matmul(pg[:], identb[0:32, ex:ex + 1].to_broadcast([32, 128]), gT[:, js], start=True, stop=True), reads=[identb, gT], writes=[pg])
                        em.op("act", lambda e: e.copy(gsb[:], pg[:]), reads=[pg], writes=[gsb])
                        hg = hg_r.next()
                        for c in range(8):
                            p1 = pup.next(); p2 = pup.next()
                            for k in range(8):
                                em.op("pe", lambda e: e.matmul(p1[:], wu[:, k, c * 128:(c + 1) * 128], hb[:, k, js], start=(k == 0), stop=(k == 7)),
                                      reads=[wu, hb], writes=[p1])
                            for k in range(8):
                                em.op("pe", lambda e: e.matmul(p2[:], wu[:, k, 1024 + c * 128:1024 + (c + 1) * 128], hb[:, k, js], start=(k == 0), stop=(k == 7)),
                                      reads=[wu, hb], writes=[p2])
                            glu = glu_r.next(); sig = sig_r.next(); lin = lin_r.next(); t1 = t1_r.next()
                            em.op("dve", lambda e: e.tensor_scalar(glu[:], p1[:], bup[:, c, ex:ex + 1], 7.0, ALU.add, ALU.min), reads=[p1, bup], writes=[glu])
                            em.op("act", lambda e: e.activation(out=sig[:], in_=glu[:], func=AF.Sigmoid, scale=1.702), reads=[glu], writes=[sig])
                            em.op("dve", lambda e: e.tensor_scalar(lin[:], p2[:], bup[:, 8 + c, ex:ex + 1], 7.0, ALU.add, ALU.min), reads=[p2, bup], writes=[lin])
                            em.op("pool", lambda e: e.tensor_scalar(lin[:], lin[:], -7.0, 1.0, ALU.max, ALU.add), reads=[lin], writes=[lin])
                            em.op("pool", lambda e: e.tensor_tensor(t1[:], glu[:], sig[:], ALU.mult), reads=[glu, sig], writes=[t1])
                            em.op("pool", lambda e: e.tensor_tensor(lin[:], lin[:], gsb[:], ALU.mult), reads=[lin, gsb], writes=[lin])
                            em.op("dve", lambda e: e.tensor_tensor(hg[:, c, :], t1[:], lin[:], ALU.mult), reads=[t1, lin], writes=[hg])
                        for oc in range(8):
                            po = pdn.next()
                            for k in range(8):
                                em.op("pe", lambda e: e.matmul(po[:], wd[:, k, oc * 128:(oc + 1) * 128], hg[:, k, :], start=(k == 0), stop=(k == 7)),
                                      reads=[wd, hg], writes=[po])
                            em.op("dve", lambda e: e.tensor_tensor(acc[:, oc, js], po[:], acc[:, oc, js], ALU.add), reads=[po, acc], writes=[acc])
                em.dma("sp", ydst[:, :, tb * TB:(tb + 1) * TB].rearrange("c p t -> p c t"), acc[:], reads=[acc])

    U32 = mybir.dt.uint32
    hs_zeroed = [False]

    def idma(kind, out, in_, idx_ap, reads=(), writes=(), bound=None):
        q = "pool"
        em._deps(q, reads, writes)
        pool = em.dma_pool[q]
        slot = em.dma_idx[q] % len(pool)
        k = pool[slot]
        em.dma_idx[q] += 1
        if 16 * (em.dma_uses[k] + 1) > SEM_LIMIT:
            k = em._new_dma_sem(q)
            pool[slot] = k
        if em.dma_uses[k] > 0:
            em._need(q, (k, 16 * em.dma_uses[k]))
        em.dma_uses[k] += 1
        off = bass.IndirectOffsetOnAxis(ap=idx_ap, axis=0)
        if kind == "g":
            ins = nc.gpsimd.indirect_dma_start(out=out, out_offset=None, in_=in_, in_offset=off)
        else:
            ins = nc.gpsimd.indirect_dma_start(out=out, out_offset=off, in_=in_, in_offset=None)
        ins.then_inc(em.sem[k], 16)
        em._record((k, 16 * em.dma_uses[k]), reads, writes)
        em.n_ins += 1

    def moe_phase(l, xsrc, ydst):
        sc1 = mods[:, l, 32:40]
        sh = mods[:, l, 24:32]
        NQ = NS * 4
        MAGIC = 12582912.0
        desti = em.sb([128, NS, 4], I32, "desti", persist=True) if not hasattr(em, "_moe_p") else em._moe_p[0]
        eidi = em.sb([128, NS, 4], I32, "eidi", persist=True) if not hasattr(em, "_moe_p") else em._moe_p[1]
        g4 = em.sb([128, NS, 4], F32, "g4", persist=True) if not hasattr(em, "_moe_p") else em._moe_p[2]
        widx = em.sb([128, NBLK, 8], I32, "widx", persist=True) if not hasattr(em, "_moe_p") else em._moe_p[3]
        blki = em.sb([128, NBLK], I32, "blki", persist=True) if not hasattr(em, "_moe_p") else em._moe_p[4]
        em._moe_p = (desti, eidi, g4, widx, blki)

        with em.phase():
            rw = em.sb([128, 8, 32], F32, "rw")
            em.dma("sp", rw[:], router_w[l].rearrange("(k p) e -> p k e", p=128), writes=[rw])
            rb = em.sb([128, 32], F32, "rb")
            em.dma("sp", rb[:], router_b[l:l + 1, :].to_broadcast([128, 32]), writes=[rb])
            stri = em.sb([128, 128], F32, "stri"); em.dma("sp", stri[:], k_stri, writes=[stri])
            iota = em.sb([128, 128], F32, "iota"); em.dma("sp", iota[:], k_iota, writes=[iota])
            base8 = em.sb([128, 8], F32, "base8"); em.dma("sp", base8[:], k_base8, writes=[base8])
            if not hs_zeroed[0]:
                zt = em.sb([128, 4, D], BF16, "zt")
                em.op("dve", lambda e: e.memset(zt[:], 0.0), writes=[zt])
                for k in range(NBLK):
                    em.dma("sp", hs_d[k * 512:(k + 1) * 512, :].rearrange("(j p) f -> p j f", p=128), zt[:], reads=[zt])
                hs_zeroed[0] = True
            P = [em.ps([128, 512], F32) for _ in range(3)]
            ptb = [em.ps([128, 1024], BF16) for _ in range(2)]
            h32_r = em.rot(1, [128, 8, 512], F32, "mh32")
            hbt_r = em.rot(1, [128, 8, 512], BF16, "mhb")
            xt_r = em.rot(2, [128, 512], F32, "mx")
            htok = [em.sb([128, D], BF16, "htok") for _ in range(NS)]
            eidf = em.sb([128, NS, 4], F32, "eidf")
            rks = em.sb([128, NS, 4], F32, "rks")
            carry = em.sb([128, 32], F32, "carry")
            em.op("dve", lambda e: e.memset(carry[:], 0.0), writes=[carry])
            lg = em.sb([128, 32]); mask = em.sb([128, 32]); rank = em.sb([128, 32])
            m8 = em.sb([128, 8]); mi = em.sb([128, 8], U32); nm = em.sb([128, 1]); e4 = em.sb([128, 4]); ssum = em.sb([128, 1]); rs = em.sb([128, 1])
            oh4 = em.sb([128, 4, 32]);
            for j in range(NT):
                h32 = h32_r.next(); hbt = hbt_r.next()
                load_mod_tile(xsrc, j, xt_r, lambda c: h32[:, c, :], sc1, sh, h32)
                em.op("act", lambda e: e.copy(hbt[:], h32[:]), reads=[h32], writes=[hbt])
                for s_ in range(4):
                    ti = j * 4 + s_
                    ss = slice(s_ * 128, (s_ + 1) * 128)
                    pl = P[0]
                    for c in range(8):
                        em.op("pe", lambda e: e.matmul(pl[:, 0:32], h32[:, c, ss], rw[:, c, :], start=(c == 0), stop=(c == 7)), reads=[h32, rw], writes=[pl])
                    em.op("dve", lambda e: e.tensor_tensor(lg[:], pl[:, 0:32], rb[:], ALU.add), reads=[pl, rb], writes=[lg])
                    em.op("dve", lambda e: e.max(m8[:], lg[:]), reads=[lg], writes=[m8])
                    em.op("dve", lambda e: e.max_index(mi[:], m8[:], lg[:]), reads=[lg, m8], writes=[mi])
                    em.op("dve", lambda e: e.tensor_copy(eidf[:, ti, :], mi[:, 0:4]), reads=[mi], writes=[eidf])
                    em.op("dve", lambda e: e.tensor_scalar(mask[:], lg[:], m8[:, 3:4], None, ALU.is_ge), reads=[lg, m8], writes=[mask])
                    em.op("dve", lambda e: e.tensor_scalar_mul(nm[:], m8[:, 0:1], -1.0), reads=[m8], writes=[nm])
                    em.op("act", lambda e: e.activation(out=e4[:], in_=m8[:, 0:4], func=AF.Exp, bias=nm[:], scale=1.0), reads=[m8, nm], writes=[e4])
                    em.op("dve", lambda e: e.reduce_sum(ssum[:], e4[:], axis=mybir.AxisListType.X), reads=[e4], writes=[ssum])
                    em.op("dve", lambda e: e.reciprocal(rs[:], ssum[:]), reads=[ssum], writes=[rs])
                    em.op("dve", lambda e: e.tensor_scalar_mul(g4[:, ti, :], e4[:], rs[:, 0:1]), reads=[e4, rs], writes=[g4])
                    pr = P[1]
                    em.op("pe", lambda e: e.matmul(pr[:, 0:32], stri[:], mask[:], start=True, stop=True), reads=[stri, mask], writes=[pr])
                    em.op("pe", lambda e: e.matmul(pr[:, 32:64], ones32[:], mask[:], start=True, stop=True), reads=[ones32, mask], writes=[pr])
                    em.op("dve", lambda e: e.tensor_tensor(rank[:], pr[:, 0:32], carry[:], ALU.add), reads=[pr, carry], writes=[rank])
                    em.op("dve", lambda e: e.tensor_tensor(carry[:], carry[:], pr[:, 32:64], ALU.add), reads=[pr, carry], writes=[carry])
                    em.op("dve", lambda e: e.tensor_tensor(oh4[:], iota[:, 0:32].unsqueeze(1).to_broadcast([128, 4, 32]),
                                                           eidf[:, ti, :].unsqueeze(2).to_broadcast([128, 4, 32]), ALU.is_equal), reads=[iota, eidf], writes=[oh4])
                    em.op("dve", lambda e: e.tensor_tensor(oh4[:], oh4[:], rank[:].unsqueeze(1).to_broadcast([128, 4, 32]), ALU.mult), reads=[oh4, rank], writes=[oh4])
                    em.op("dve", lambda e: e.reduce_sum(rks[:, ti, :], oh4[:], axis=mybir.AxisListType.X), reads=[oh4], writes=[rks])
                    pt = ptb[ti % 2]
                    for c in range(8):
                        em.op("pe", lambda e: e.transpose(pt[:, c * 128:(c + 1) * 128], hbt[:, c, ss], identb[:]), reads=[hbt, identb], writes=[pt])
                    em.op("act", lambda e: e.copy(htok[ti][:], pt[:]), reads=[pt], writes=[htok[ti]])
            cnt = carry
            nb = em.sb([128, 32]); pad = em.sb([128, 32]); ca = em.sb([128, 32]); cb2 = em.sb([128, 32]); poff = em.sb([128, 32])
            em.op("dve", lambda e: e.tensor_scalar(nb[:], cnt[:], 511.0, 1.0 / 512.0, ALU.add, ALU.mult), reads=[cnt], writes=[nb])
            em.op("dve", lambda e: e.tensor_scalar(nb[:], nb[:], -0.5 + 1.0 / 1024.0, MAGIC, ALU.add, ALU.add), reads=[nb], writes=[nb])
            em.op("dve", lambda e: e.tensor_scalar_add(nb[:], nb[:], -MAGIC), reads=[nb], writes=[nb])
            em.op("dve", lambda e: e.tensor_scalar_mul(pad[:], nb[:], 512.0), reads=[nb], writes=[pad])
            em.op("dve", lambda e: e.tensor_copy(ca[:], pad[:]), reads=[pad], writes=[ca])
            src_, dst_ = ca, cb2
            for shf in (1, 2, 4, 8, 16):
                em.op("dve", lambda e: e.tensor_copy(dst_[:, 0:shf], src_[:, 0:shf]), reads=[src_], writes=[dst_])
                em.op("dve", lambda e: e.tensor_tensor(dst_[:, shf:32], src_[:, shf:32], src_[:, 0:32 - shf], ALU.add), reads=[src_, dst_], writes=[dst_])
                src_, dst_ = dst_, src_
            cum = src_
            em.op("dve", lambda e: e.tensor_tensor(poff[:], cum[:], pad[:], ALU.subtract), reads=[cum, pad], writes=[poff])
            oh = em.sb([128, NQ, 32], F32, "ohall")
            pofft = em.sb([128, NQ], F32, "pofft")
            em.op("dve", lambda e: e.tensor_tensor(oh[:], iota[:, 0:32].unsqueeze(1).to_broadcast([128, NQ, 32]),
                                                   eidf[:].rearrange("p a b -> p (a b)").unsqueeze(2).to_broadcast([128, NQ, 32]), ALU.is_equal), reads=[iota, eidf], writes=[oh])
            em.op("dve", lambda e: e.tensor_tensor(oh[:], oh[:], poff[:].unsqueeze(1).to_broadcast([128, NQ, 32]), ALU.mult), reads=[oh, poff], writes=[oh])
            em.op("dve", lambda e: e.reduce_sum(pofft[:], oh[:], axis=mybir.AxisListType.X), reads=[oh], writes=[pofft])
            em.op("dve", lambda e: e.tensor_tensor(pofft[:], pofft[:], rks[:].rearrange("p a b -> p (a b)"), ALU.add), reads=[pofft, rks], writes=[pofft])
            em.op("dve", lambda e: e.tensor_copy(desti[:].rearrange("p a b -> p (a b)"), pofft[:]), reads=[pofft], writes=[desti])
            em.op("dve", lambda e: e.tensor_copy(eidi[:], eidf[:]), reads=[eidf], writes=[eidi])
            cmpk = em.sb([128, NBLK, 32], F32, "cmpk")
            kst = em.sb([128, NBLK], F32, "kst")
            blkf = em.sb([128, NBLK], F32, "blkf")
            wxf = em.sb([128, NBLK, 8], F32, "wxf")
            em.op("dve", lambda e: e.tensor_scalar_mul(kst[:], iota[:, 0:NBLK], 512.0), reads=[iota], writes=[kst])
            em.op("dve", lambda e: e.tensor_tensor(cmpk[:], cum[:].unsqueeze(1).to_broadcast([128, NBLK, 32]),
                                                   kst[:].unsqueeze(2).to_broadcast([128, NBLK, 32]), ALU.is_le), reads=[cum, kst], writes=[cmpk])
            em.op("dve", lambda e: e.reduce_sum(blkf[:], cmpk[:], axis=mybir.AxisListType.X), reads=[cmpk], writes=[blkf])
            em.op("dve", lambda e: e.tensor_scalar_min(blkf[:], blkf[:], 31.0), reads=[blkf], writes=[blkf])
            em.op("dve", lambda e: e.tensor_scalar_add(kst[:], blkf[:], 32.0 * l), reads=[blkf], writes=[kst])
            em.op("dve", lambda e: e.tensor_copy(blki[:], kst[:]), reads=[kst], writes=[blki])
            em.op("dve", lambda e: e.tensor_scalar(blkf[:], blkf[:], 1024.0, 32768.0 * l, ALU.mult, ALU.add), reads=[blkf], writes=[blkf])
            em.op("dve", lambda e: e.tensor_tensor(wxf[:], blkf[:].unsqueeze(2).to_broadcast([128, NBLK, 8]),
                                                   base8[:].unsqueeze(1).to_broadcast([128, NBLK, 8]), ALU.add), reads=[blkf, base8], writes=[wxf])
            em.op("dve", lambda e: e.tensor_copy(widx[:], wxf[:]), reads=[wxf], writes=[widx])
            for ti in range(NS):
                for sl_ in range(4):
                    idma("s", hs_d[:, :], htok[ti][:, :], desti[:, ti, sl_:sl_ + 1], reads=[htok[ti], desti])

        import os as _os
        if _os.environ.get("MOE_CUT") == "A":
            return
        wup_rows = moe_w_up.rearrange("l e r f -> (l e r) f")
        wdn_rows = moe_w_down.rearrange("l e r f -> (l e r) f")
        with em.phase():
            P = [em.ps([128, 512], F32) for _ in range(6)]
            pup = Rot(P[0:4]); pdn = Rot(P[4:6])
            ptb = em.ps([128, 512], BF16)
            hblk_r = em.rot(1, [128, 4, D], BF16, "hblk")
            hT_r = em.rot(2, [128, 8, 512], BF16, "hT")
            wu_r = em.rot(2, [128, 8, 2048], BF16, "wu")
            wd_r = em.rot(2, [128, 8, 1024], BF16, "wd")
            stgu_r = em.rot(2, [128, 2048], F32, "stgu")
            stgd_r = em.rot(2, [128, 1024], F32, "stgd")
            bu32_r = em.rot(1, [2, 3072], F32, "bu32")
            bub_r = em.rot(2, [1, 3072], BF16, "bub")
            hg_r = em.rot(1, [128, 8, 512], BF16, "hg")
            glu_r = em.rot(1, [128, 512], F32, "glu"); sig_r = em.rot(1, [128, 512], F32, "sig")
            lin_r = em.rot(1, [128, 512], F32, "lin"); t1_r = em.rot(1, [128, 512], F32, "t1")
            yrow_r = em.rot(2, [128, D], F32, "yrow")
            onesrow = em.sb([1, 512], BF16, "onesrow")
            em.op("dve", lambda e: e.memset(onesrow[:], 1.0), writes=[onesrow])

            def gather_piece(k, c):
                su = stgu_r.next(); sd = stgd_r.next()
                idma("g", su[:, :], wup_rows, widx[:, k, c:c + 1], reads=[widx], writes=[su])
                idma("g", sd[:, :], wdn_rows, widx[:, k, c:c + 1], reads=[widx], writes=[sd])
                return su, sd

            def cast_piece(wu_, wd_, c, su, sd):
                em.op("act", lambda e: e.copy(wu_[:, c, :], su[:]), reads=[su], writes=[wu_])
                em.op("dve", lambda e: e.tensor_copy(wd_[:, c, :], sd[:]), reads=[sd], writes=[wd_])

            def bias_row(k):
                b32 = bu32_r.next(); bb_ = bub_r.next()
                idma("g", b32[0:2, 0:2048], moe_b_up.rearrange("l e f -> (l e) f"), blki[0:2, k:k + 1], reads=[blki], writes=[b32])
                idma("g", b32[0:2, 2048:3072], moe_b_down.rearrange("l e f -> (l e) f"), blki[0:2, k:k + 1], reads=[blki], writes=[b32])
                em.op("act", lambda e: e.copy(bb_[0:1, :], b32[0:1, :]), reads=[b32], writes=[bb_])
                return bb_

            wu = wu_r.next(); wd = wd_r.next()
            for c in range(8):
                su, sd = gather_piece(0, c)
                cast_piece(wu, wd, c, su, sd)
            bb = bias_row(0)
            for k in range(NBLK):
                hblk = hblk_r.next(); hT = hT_r.next()
                em.dma("sp", hblk[:], hs_d[k * 512:(k + 1) * 512, :].rearrange("(j p) f -> p j f", p=128), writes=[hblk])
                more = k + 1 < NBLK
                if more:
                    wu_n = wu_r.next(); wd_n = wd_r.next()
                    bb_n = bias_row(k + 1)
                    pend = gather_piece(k + 1, 0)
                for c in range(8):
                    for j in range(4):
                        em.op("pe", lambda e: e.transpose(ptb[:, j * 128:(j + 1) * 128], hblk[:, j, c * 128:(c + 1) * 128], identb[:]), reads=[hblk, identb], writes=[ptb])
                    if c % 2:
                        em.op("act", lambda e: e.copy(hT[:, c, :], ptb[:]), reads=[ptb], writes=[hT])
                    else:
                        em.op("dve", lambda e: e.tensor_copy(hT[:, c, :], ptb[:]), reads=[ptb], writes=[hT])
                hg = hg_r.next()
                for c in range(8):
                    if more and c + 1 < 8:
                        nxt_piece = gather_piece(k + 1, c + 1)
                    p1 = pup.next(); p2 = pup.next()
                    for k8 in range(8):
                        em.op("pe", lambda e: e.matmul(p1[:], wu[:, k8, c * 128:(c + 1) * 128], hT[:, k8, :], start=(k8 == 0), stop=False), reads=[wu, hT], writes=[p1])
                    em.op("pe", lambda e: e.matmul(p1[:], bb[0:1, c * 128:(c + 1) * 128], onesrow[:], start=False, stop=True), reads=[bb, onesrow], writes=[p1])
                    for k8 in range(8):
                        em.op("pe", lambda e: e.matmul(p2[:], wu[:, k8, 1024 + c * 128:1024 + (c + 1) * 128], hT[:, k8, :], start=(k8 == 0), stop=False), reads=[wu, hT], writes=[p2])
                    em.op("pe", lambda e: e.matmul(p2[:], bb[0:1, 1024 + c * 128:1024 + (c + 1) * 128], onesrow[:], start=False, stop=True), reads=[bb, onesrow], writes=[p2])
                    glu = glu_r.next(); sig = sig_r.next(); lin = lin_r.next(); t1 = t1_r.next()
                    em.op("dve", lambda e: e.tensor_scalar_min(glu[:], p1[:], 7.0), reads=[p1], writes=[glu])
                    em.op("act", lambda e: e.activation(out=sig[:], in_=glu[:], func=AF.Sigmoid, scale=1.702), reads=[glu], writes=[sig])
                    em.op("dve", lambda e: e.tensor_scalar(lin[:], p2[:], 7.0, -7.0, ALU.min, ALU.max), reads=[p2], writes=[lin])
                    em.op("pool", lambda e: e.tensor_tensor(t1[:], glu[:], sig[:], ALU.mult), reads=[glu, sig], writes=[t1])
                    em.op("dve", lambda e: e.scalar_tensor_tensor(hg[:, c, :], lin[:], 1.0, t1[:], ALU.add, ALU.mult), reads=[lin, t1], writes=[hg])
                    if more:
                        cast_piece(wu_n, wd_n, c, *pend)
                        if c + 1 < 8:
                            pend = nxt_piece
                for j in range(4):
                    yrow = yrow_r.next()
                    for half in range(2):
                        po = pdn.next()
                        for k8 in range(8):
                            em.op("pe", lambda e: e.matmul(po[:], hg[:, k8, j * 128:(j + 1) * 128], wd[:, k8, half * 512:(half + 1) * 512], start=(k8 == 0), stop=False), reads=[hg, wd], writes=[po])
                        em.op("pe", lambda e: e.matmul(po[:], onesrow[0:1, 0:128], bb[0:1, 2048 + half * 512:2048 + (half + 1) * 512], start=False, stop=True), reads=[onesrow, bb], writes=[po])
                        if half:
                            em.op("act", lambda e: e.copy(yrow[:, 512:1024], po[:]), reads=[po], writes=[yrow])
                        else:
                            em.op("dve", lambda e: e.tensor_copy(yrow[:, 0:512], po[:]), reads=[po], writes=[yrow])
                    em.dma("sp", ys_d[k * 512 + j * 128:k * 512 + (j + 1) * 128, :], yrow[:], reads=[yrow])
                if more:
                    wu, wd, bb = wu_n, wd_n, bb_n

        if _os.environ.get("MOE_CUT") == "B":
            return
        with em.phase():
            pT = [em.ps([128, 512], F32) for _ in range(2)]
            yr_r = em.rot(3, [128, D], F32, "cyr")
            acc_r = em.rot(2, [128, D], F32, "cacc")
            yo_r = em.rot(2, [128, 8, 512], F32, "cyo")
            yo = None
            for ti in range(NS):
                acc = acc_r.next()
                for sl_ in range(4):
                    yr = yr_r.next()
                    idma("g", yr[:, :], ys_d[:, :], desti[:, ti, sl_:sl_ + 1], reads=[desti], writes=[yr])
                    if sl_ == 0:
                        em.op("dve", lambda e: e.tensor_scalar(acc[:], yr[:], g4[:, ti, 0:1], None, ALU.mult), reads=[yr, g4], writes=[acc])
                    else:
                        em.op("dve", lambda e: e.scalar_tensor_tensor(acc[:], yr[:], g4[:, ti, sl_:sl_ + 1], acc[:], ALU.mult, ALU.add), reads=[yr, g4, acc], writes=[acc])
                s_ = ti % 4
                if s_ == 0:
                    yo = yo_r.next()
                for c in range(8):
                    p = pT[c % 2]
                    em.op("pe", lambda e: e.transpose(p[:, 0:128], acc[:, c * 128:(c + 1) * 128], ident[:]), reads=[acc, ident], writes=[p])
                    if c % 2:
                        em.op("act", lambda e: e.copy(yo[:, c, s_ * 128:(s_ + 1) * 128], p[:, 0:128]), reads=[p], writes=[yo])
                    else:
                        em.op("dve", lambda e: e.tensor_copy(yo[:, c, s_ * 128:(s_ + 1) * 128], p[:, 0:128]), reads=[p], writes=[yo])
                if s_ == 3:
                    j = ti // 4
                    em.dma("sp", ydst[:, :, j * 512:(j + 1) * 512].rearrange("c p t -> p c t"), yo[:], reads=[yo])

    def mamba_phase(l, xsrc, ydst):
        sc1 = mods[:, l, 8:16]
        sh = mods[:, l, 0:8]
        inw = ssm_in_w[l]

        def load_hb(hb):
            xt_r = em.rot(3, [128, 512], F32, "mbx")
            for i in range(NT):
                load_mod_tile(xsrc, i, xt_r, lambda c: hb[:, c, i * 512:(i + 1) * 512], sc1, sh, hb)

        with em.phase():
            hb = em.sb([128, 8, T], BF16, "hb")
            load_hb(hb)
            wz = em.sb([128, 8, 2048], BF16, "wz")
            em.dma("pool", wz[:], inw[:, 0:2048].rearrange("(k p) f -> p k f", p=128), writes=[wz])
            wdt = em.sb([128, 8, 32], BF16, "wdt")
            em.dma("pool", wdt[:], inw[:, 5120:5152].rearrange("(k p) f -> p k f", p=128), writes=[wdt])
            dtb = em.sb([128, 32], F32, "dtb")
            em.dma("sp", dtb[:], ssm_dt_bias[l:l + 1, :].to_broadcast([128, 32]), writes=[dtb])
            pz = Rot([em.ps([128, 512], F32) for _ in range(4)])
            pd = em.ps([128, 32], F32)
            zs_r = em.rot(2, [128, 2048], BF16, "zs")
            d_r = {k: em.rot(2, [128, 32], F32, "d" + k) for k in ("x", "a", "e", "r")}
            for s in range(NS):
                ss = slice(s * 128, (s + 1) * 128)
                zs = zs_r.next()
                for q in range(4):
                    p = pz.next()
                    for k in range(8):
                        em.op("pe", lambda e: e.matmul(p[:], hb[:, k, ss], wz[:, k, q * 512:(q + 1) * 512], start=(k == 0), stop=(k == 7)),
                              reads=[hb, wz], writes=[p])
                    em.op("act", lambda e: e.activation(out=zs[:, q * 512:(q + 1) * 512], in_=p[:], func=AF.Silu), reads=[p], writes=[zs])
                em.dma("sp", zs_d[ss, :], zs[:], reads=[zs])
                for k in range(8):
                    em.op("pe", lambda e: e.matmul(pd[:], hb[:, k, ss], wdt[:, k, :], start=(k == 0), stop=(k == 7)), reads=[hb, wdt], writes=[pd])
                dx = d_r["x"].next(); da = d_r["a"].next(); de = d_r["e"].next(); dr = d_r["r"].next()
                em.op("dve", lambda e: e.tensor_tensor(dx[:], pd[:], dtb[:], ALU.add), reads=[pd, dtb], writes=[dx])
                em.op("dve", lambda e: e.tensor_scalar_mul(da[:], dx[:], -1.0), reads=[dx], writes=[da])
                em.op("dve", lambda e: e.tensor_tensor(da[:], da[:], dx[:], ALU.max), reads=[dx, da], writes=[da])
                em.op("act", lambda e: e.activation(out=de[:], in_=da[:], func=AF.Exp, scale=-1.0), reads=[da], writes=[de])
                em.op("act", lambda e: e.activation(out=de[:], in_=de[:], func=AF.Ln, bias=ones32[:, 0:1], scale=1.0), reads=[de, ones32], writes=[de])
                em.op("dve", lambda e: e.scalar_tensor_tensor(dr[:], dx[:], 0.0, de[:], ALU.max, ALU.add), reads=[dx, de], writes=[dr])
                em.dma("sp", dt_d[ss, :], dr[:], reads=[dr])

        with em.phase():
            hb = em.sb([128, 8, T], BF16, "hb")
            load_hb(hb)
            wx = em.sb([128, 8, 3072], BF16, "wx")
            em.dma("pool", wx[:], inw[:, 2048:5120].rearrange("(k p) f -> p k f", p=128), writes=[wx])
            pm = em.ps([128, 512], F32)
            cw = em.sb([128, 24, 4], F32, "cw")
            cbias = em.sb([128, 24, 1], F32, "cb")
            pp = Rot([em.ps([128, 512], F32) for _ in range(4)])
            xpad_r = em.rot(2, [128, T + 3], F32, "xpad")
            rowsT(cw[:], ssm_conv_w[l], 4, 24, pm, cw, tmp=xpad_r.bufs[0], tmp_ap=xpad_r.bufs[0][:])
            rowsT(cbias[:], ssm_conv_b[l:l + 1, :], 1, 24, pm, cbias, tmp=xpad_r.bufs[1], tmp_ap=xpad_r.bufs[1][:])
            acc_r = em.rot(2, [128, T], F32, "cacc")
            ob_r = em.rot(1, [128, T], BF16, "cob")
            for ch in range(24):
                xp = xpad_r.next(); ac = acc_r.next(); ob = ob_r.next()
                em.op("pool", lambda e: e.memset(xp[:, 0:3], 0.0), writes=[xp])
                for i in range(NT):
                    p = pp.next()
                    for k in range(8):
                        em.op("pe", lambda e: e.matmul(p[:], wx[:, k, ch * 128:(ch + 1) * 128], hb[:, k, i * 512:(i + 1) * 512], start=(k == 0), stop=(k == 7)),
                              reads=[wx, hb], writes=[p])
                    if i % 2:
                        em.op("act", lambda e: e.copy(xp[:, 3 + i * 512:3 + (i + 1) * 512], p[:]), reads=[p], writes=[xp])
                    else:
                        em.op("dve", lambda e: e.tensor_copy(xp[:, 3 + i * 512:3 + (i + 1) * 512], p[:]), reads=[p], writes=[xp])
                em.op("dve", lambda e: e.tensor_scalar(ac[:], xp[:, 0:T], cw[:, ch, 0:1], None, ALU.mult), reads=[xp, cw], writes=[ac])
                for j in range(1, 4):
                    eng = "dve"
                    em.op(eng, lambda e: e.scalar_tensor_tensor(ac[:], xp[:, j:j + T], cw[:, ch, j:j + 1], ac[:], ALU.mult, ALU.add),
                          reads=[xp, cw, ac], writes=[ac])
                em.op("act", lambda e: e.activation(out=ob[:], in_=ac[:], func=AF.Silu, bias=cbias[:, ch, :], scale=1.0), reads=[ac, cbias], writes=[ob])
                em.dma("sp", xbc_d[ch], ob[:], reads=[ob])

        with em.phase():
            tri = em.sb([128, 128], F32, "tri")
            em.dma("sp", tri[:], k_tri, writes=[tri])
            aneg = em.sb([128, 32], F32, "aneg")
            em.dma("sp", aneg[:], ssm_a_log[l:l + 1, :].to_broadcast([128, 32]), writes=[aneg])
            em.op("act", lambda e: e.activation(out=aneg[:], in_=aneg[:], func=AF.Exp), reads=[aneg], writes=[aneg])
            em.op("dve", lambda e: e.tensor_scalar_mul(aneg[:], aneg[:], -1.0), reads=[aneg], writes=[aneg])
            dsk = em.sb([128, 32], F32, "dsk")
            em.dma("sp", dsk[:], ssm_d[l:l + 1, :].to_broadcast([128, 32]), writes=[dsk])
            nw = em.sb([128, 2048], F32, "nw")
            em.dma("sp", nw[:], ssm_norm_w[l:l + 1, :].to_broadcast([128, 2048]), writes=[nw])
            wout = em.sb([128, 16, D], BF16, "wout")
            em.dma("pool", wout[:], ssm_out_w[l].rearrange("(k p) o -> p k o", p=128), writes=[wout])
            st32 = [em.sb([128, 8, 64], F32, "st32") for _ in range(4)]
            stb = [em.sb([128, 8, 64], BF16, "stb") for _ in range(4)]
            for g in range(4):
                em.op("dve", lambda e: e.memset(st32[g][:], 0.0), writes=[st32[g]])
                em.op("dve", lambda e: e.memset(stb[g][:], 0.0), writes=[stb[g]])
            ynT = em.sb([128, 16, 512], BF16, "ynT")
            ptb = em.ps([128, 512], BF16)
            pmisc = em.ps([128, 512], F32)
            par = em.ps([128, 1024], F32)
            py = em.ps([128, 512], F32)
            psn = em.ps([128, 512], F32)
            pout = em.ps([128, 512], F32)
            xsT_r = em.rot(2, [128, 16, 128], BF16, "xsT")
            bT_r = em.rot(2, [128, 4, 128], BF16, "bT")
            cT_r = em.rot(2, [128, 4, 128], BF16, "cT")
            zs_r = em.rot(2, [128, 2048], BF16, "zsl")
            dt_r = em.rot(2, [128, 32], F32, "dtl")
            xs_r = em.rot(2, [128, 32, 64], BF16, "xs")
            bt_r = em.rot(2, [128, 512], BF16, "btok")
            xdt_r = em.rot(1, [128, 32, 64], BF16, "xdt")
            xdtd_r = em.rot(1, [128, 32, 64], BF16, "xdtd")
            arow_r = em.rot(1, [128, 32, 128], F32, "arow")
            seg_r = em.rot(2, [128, 8, 128], F32, "seg")
            ear_r = em.rot(2, [128, 8, 128], F32, "ear")
            Mh_r = em.rot(2, [128, 8, 128], BF16, "Mh")
            Cs_r = em.rot(2, [128, 8, 128], BF16, "Cs")
            cbm_r = em.rot(2, [128, 128], F32, "cbm")
            yz = em.sb([128, 4, 512], F32, "yz")
            tt_r = em.rot(2, [128, 8, 64], F32, "tt")
            junk = em.sb([128, 512], F32, "junk")
            yn = em.sb([128, 2048], BF16, "yn")
            yo_r = em.rot(1, [128, 8, 512], F32, "yo")
            sA = {k: em.sb([128, 32], F32, "s" + k) for k in ("a", "acs", "tot", "d1", "dend", "cdec", "dtd")}
            ss4 = em.sb([128, 4], F32); rstd4 = em.sb([128, 4], F32)
            for c in range(NS):
                cs_ = slice(c * 128, (c + 1) * 128)
                xsT = xsT_r.next(); bT = bT_r.next(); cT = cT_r.next(); zs = zs_r.next(); dt = dt_r.next()
                em.dma("sp", xsT[:], xbc_d[0:16, :, cs_].rearrange("k p t -> p k t"), writes=[xsT])
                em.dma("sp", bT[:], xbc_d[16:20, :, cs_].rearrange("k p t -> p k t"), writes=[bT])
                em.dma("sp", cT[:], xbc_d[20:24, :, cs_].rearrange("k p t -> p k t"), writes=[cT])
                em.dma("sp", zs[:], zs_d[cs_, :], writes=[zs])
                em.dma("sp", dt[:], dt_d[cs_, :], writes=[dt])
                xs = xs_r.next(); btok = bt_r.next()
                xsf = xs[:].rearrange("p h d -> p (h d)")
                for q in range(4):
                    for k in range(4):
                        em.op("pe", lambda e: e.transpose(ptb[:, k * 128:(k + 1) * 128], xsT[:, q * 4 + k, :], identb[:]), reads=[xsT, identb], writes=[ptb])
                    em.op("act", lambda e: e.copy(xsf[:, q * 512:(q + 1) * 512], ptb[:]), reads=[ptb], writes=[xs])
                for g in range(4):
                    em.op("pe", lambda e: e.transpose(ptb[:, g * 128:(g + 1) * 128], bT[:, g, :], identb[:]), reads=[bT, identb], writes=[ptb])
                em.op("dve", lambda e: e.tensor_copy(btok[:], ptb[:]), reads=[ptb], writes=[btok])
                a, acs, tot, d1, dend, cdec, dtd = (sA[k] for k in ("a", "acs", "tot", "d1", "dend", "cdec", "dtd"))
                em.op("dve", lambda e: e.tensor_tensor(a[:], dt[:], aneg[:], ALU.mult), reads=[dt, aneg], writes=[a])
                em.op("pe", lambda e: e.matmul(pmisc[:, 0:32], tri[:], a[:], start=True, stop=True), reads=[tri, a], writes=[pmisc])
                em.op("pe", lambda e: e.matmul(pmisc[:, 32:64], ones32[:], a[:], start=True, stop=True), reads=[ones32, a], writes=[pmisc])
                em.op("dve", lambda e: e.tensor_copy(acs[:], pmisc[:, 0:32]), reads=[pmisc], writes=[acs])
                em.op("dve", lambda e: e.tensor_copy(tot[:], pmisc[:, 32:64]), reads=[pmisc], writes=[tot])
                em.op("dve", lambda e: e.tensor_tensor(d1[:], tot[:], acs[:], ALU.subtract), reads=[tot, acs], writes=[d1])
                em.op("act", lambda e: e.activation(out=dend[:], in_=d1[:], func=AF.Exp), reads=[d1], writes=[dend])
                em.op("act", lambda e: e.activation(out=cdec[:], in_=tot[:], func=AF.Exp), reads=[tot], writes=[cdec])
                em.op("dve", lambda e: e.tensor_tensor(dtd[:], dt[:], dend[:], ALU.mult), reads=[dt, dend], writes=[dtd])
                xdt = xdt_r.next(); xdtd = xdtd_r.next()
                em.op("pool", lambda e: e.tensor_tensor(xdt[:], xs[:], dt[:].unsqueeze(2).to_broadcast([128, 32, 64]), ALU.mult), reads=[xs, dt], writes=[xdt])
                em.op("dve", lambda e: e.tensor_tensor(xdtd[:], xs[:], dtd[:].unsqueeze(2).to_broadcast([128, 32, 64]), ALU.mult), reads=[xs, dtd], writes=[xdtd])
                arow = arow_r.next()
                em.op("pool", lambda e: e.tensor_tensor(arow[:], tri[:].unsqueeze(1).to_broadcast([128, 32, 128]),
                                                        a[:].unsqueeze(2).to_broadcast([128, 32, 128]), ALU.mult), reads=[tri, a], writes=[arow])
                for g in range(4):
                    for h2 in range(2):
                        em.op("pe", lambda e: e.matmul(par[:, h2 * 512:(h2 + 1) * 512], ones32[:],
                                                       arow[:, g * 8 + h2 * 4:g * 8 + h2 * 4 + 4, :].rearrange("p h l -> p (h l)"), start=True, stop=True),
                              reads=[ones32, arow], writes=[par])
                    em.op("pe", lambda e: e.matmul(pmisc[:, 128:256], bT[:, g, :], cT[:, g, :], start=True, stop=True), reads=[bT, cT], writes=[pmisc])
                    cbm = cbm_r.next()
                    em.op("dve", lambda e: e.tensor_tensor(cbm[:], pmisc[:, 128:256], tri[:], ALU.mult), reads=[pmisc, tri], writes=[cbm])
                    seg = seg_r.next(); ear = ear_r.next(); Mh = Mh_r.next(); Cs = Cs_r.next()
                    for hh in range(8):
                        h = g * 8 + hh
                        em.op("dve", lambda e: e.tensor_scalar(seg[:, hh, :], par[:, hh * 128:(hh + 1) * 128], acs[:, h:h + 1], 0.0, ALU.subtract, ALU.min),
                              reads=[par, acs], writes=[seg])
                    em.op("act", lambda e: e.activation(out=seg[:], in_=seg[:], func=AF.Exp), reads=[seg], writes=[seg])
                    em.op("pool", lambda e: e.tensor_tensor(Mh[:], seg[:], cbm[:].unsqueeze(1).to_broadcast([128, 8, 128]), ALU.mult), reads=[seg, cbm], writes=[Mh])
                    em.op("act", lambda e: e.activation(out=ear[:], in_=par[:].rearrange("p (h l) -> p h l", l=128), func=AF.Exp), reads=[par], writes=[ear])
                    em.op("dve", lambda e: e.tensor_tensor(Cs[:], ear[:], cT[:, g, :].unsqueeze(1).to_broadcast([128, 8, 128]), ALU.mult), reads=[ear, cT], writes=[Cs])
                    for hh in range(8):
                        h = g * 8 + hh
                        em.op("pe", lambda e: e.matmul(py[:, hh * 64:(hh + 1) * 64], Mh[:, hh, :], xdt[:, h, :], start=True, stop=False), reads=[Mh, xdt], writes=[py])
                        em.op("pe", lambda e: e.matmul(py[:, hh * 64:(hh + 1) * 64], Cs[:, hh, :], stb[g][:, hh, :], start=False, stop=True), reads=[Cs, stb[g]], writes=[py])
                    tt = tt_r.next()
                    em.op("pool", lambda e: e.tensor_tensor(tt[:], xs[:, g * 8:(g + 1) * 8, :], dsk[:, g * 8:(g + 1) * 8].unsqueeze(2).to_broadcast([128, 8, 64]), ALU.mult),
                          reads=[xs, dsk], writes=[tt])
                    em.op("dve", lambda e: e.tensor_tensor(yz[:, g, :], py[:], tt[:].rearrange("p h d -> p (h d)"), ALU.add), reads=[py, tt], writes=[yz])
                    em.op("pool", lambda e: e.tensor_tensor(yz[:, g, :], yz[:, g, :], zs[:, g * 512:(g + 1) * 512], ALU.mult), reads=[yz, zs], writes=[yz])
                    em.op("act", lambda e: e.activation(out=junk[:], in_=yz[:, g, :], func=AF.Square, accum_out=ss4[:, g:g + 1]), reads=[yz], writes=[junk, ss4])
                    em.op("pe", lambda e: e.matmul(psn[:], btok[:, g * 128:(g + 1) * 128], xdtd[:, g * 8:(g + 1) * 8, :].rearrange("p h d -> p (h d)"), start=True, stop=True),
                          reads=[btok, xdtd], writes=[psn])
                    em.op("pool", lambda e: e.tensor_tensor(st32[g][:], st32[g][:], cdec[:, g * 8:(g + 1) * 8].unsqueeze(2).to_broadcast([128, 8, 64]), ALU.mult),
                          reads=[st32[g], cdec], writes=[st32[g]])
                    em.op("dve", lambda e: e.tensor_tensor(st32[g][:], st32[g][:], psn[:].rearrange("p (h d) -> p h d", d=64), ALU.add), reads=[st32[g], psn], writes=[st32[g]])
                    em.op("act", lambda e: e.copy(stb[g][:], st32[g][:]), reads=[st32[g]], writes=[stb[g]])
                em.op("dve", lambda e: e.tensor_scalar(rstd4[:], ss4[:], 1.0 / 512.0, None, ALU.mult), reads=[ss4], writes=[rstd4])
                em.op("act", lambda e: e.activation(out=rstd4[:], in_=rstd4[:], func=AF.Sqrt, bias=eps5[:], scale=1.0), reads=[rstd4, eps5], writes=[rstd4])
                em.op("dve", lambda e: e.reciprocal(rstd4[:], rstd4[:]), reads=[rstd4], writes=[rstd4])
                for g in range(4):
                    eng = "dve"
                    em.op(eng, lambda e: e.scalar_tensor_tensor(yn[:, g * 512:(g + 1) * 512], yz[:, g, :], rstd4[:, g:g + 1], nw[:, g * 512:(g + 1) * 512], ALU.mult, ALU.mult),
                          reads=[yz, rstd4, nw], writes=[yn])
                c4 = c % 4
                for q in range(4):
                    for k in range(4):
                        em.op("pe", lambda e: e.transpose(ptb[:, k * 128:(k + 1) * 128], yn[:, (q * 4 + k) * 128:(q * 4 + k + 1) * 128], identb[:]), reads=[yn, identb], writes=[ptb])
                    em.op("act", lambda e: e.copy(ynT[:, q * 4:(q + 1) * 4, c4 * 128:(c4 + 1) * 128], ptb[:].rearrange("p (k t) -> p k t", t=128)), reads=[ptb], writes=[ynT])
                if c4 == 3:
                    yo = yo_r.next()
                    for oc in range(8):
                        for k in range(16):
                            em.op("pe", lambda e: e.matmul(pout[:], wout[:, k, oc * 128:(oc + 1) * 128], ynT[:, k, :], start=(k == 0), stop=(k == 15)), reads=[wout, ynT], writes=[pout])
                        em.op("dve", lambda e: e.tensor_copy(yo[:, oc, :], pout[:]), reads=[pout], writes=[yo])
                    i = c // 4
                    em.dma("sp", ydst[:, :, i * 512:(i + 1) * 512].rearrange("c p t -> p c t"), yo[:], reads=[yo])

    def rope_phase():
        with em.phase():
            rc = em.sb([64, 2], F32, "rc")
            em.dma("sp", rc[:], k_ropec, writes=[rc])
            posi = em.sb([64, T], I32, "posi")
            em.dma("sp", posi[:], pos_in[0:1, :].to_broadcast([64, T]), writes=[posi])
            ang = em.sb([64, T], F32, "ang")
            em.op("dve", lambda e: e.tensor_copy(ang[:], posi[:]), reads=[posi], writes=[ang])
            em.op("dve", lambda e: e.tensor_scalar(ang[:], ang[:], rc[:, 0:1], None, ALU.mult), reads=[ang, rc], writes=[ang])
            MAGIC = 12582912.0
            C1 = 6.28125
            C2 = 2.0 * math.pi - 6.28125
            u = em.sb([64, T], F32, "u"); kk = em.sb([64, T], F32, "kk"); r = em.sb([64, T], F32, "r")
            for which, shift in ((0, math.pi / 2.0), (1, 0.0)):
                em.op("dve", lambda e: e.tensor_scalar_add(u[:], ang[:], shift), reads=[ang], writes=[u])
                em.op("dve", lambda e: e.tensor_scalar(kk[:], u[:], 1.0 / (2.0 * math.pi), MAGIC, ALU.mult, ALU.add), reads=[u], writes=[kk])
                em.op("dve", lambda e: e.tensor_scalar_add(kk[:], kk[:], -MAGIC), reads=[kk], writes=[kk])
                em.op("dve", lambda e: e.scalar_tensor_tensor(r[:], kk[:], -C1, u[:], ALU.mult, ALU.add), reads=[kk, u], writes=[r])
                em.op("dve", lambda e: e.scalar_tensor_tensor(r[:], kk[:], -C2, r[:], ALU.mult, ALU.add), reads=[kk, r], writes=[r])
                em.op("dve", lambda e: e.tensor_scalar(r[:], r[:], math.pi, -math.pi, ALU.min, ALU.max), reads=[r], writes=[r])
                em.op("act", lambda e: e.activation(out=r[:], in_=r[:], func=AF.Sin), reads=[r], writes=[r])
                if which == 1:
                    em.op("dve", lambda e: e.tensor_scalar(r[:], r[:], rc[:, 1:2], None, ALU.mult), reads=[r, rc], writes=[r])
                em.dma("sp", rope_d[which], r[:], reads=[r])

    def kv_phase(xsrc):
        with em.phase():
            hb = em.sb([128, 8, T], BF16, "hb")
            xt_r = em.rot(3, [128, 512], F32, "kvx")
            for i in range(NT):
                load_mod_tile(xsrc, i, xt_r, lambda c: hb[:, c, i * 512:(i + 1) * 512], kvmod[:, 8:16], kvmod[:, 0:8], hb)
            kvw = em.sb([128, 8, 1536], BF16, "kvw")
            em.dma("pool", kvw[:], kv_w.rearrange("(k p) f -> p k f", p=128), writes=[kvw])
            kvws = em.sb([128, 8, 768], BF16, "kvws")
            em.dma("pool", kvws[:], kv_w_sw.rearrange("(k p) f -> p k f", p=128), writes=[kvws])
            cos_r = em.rot(2, [64, 512], F32, "cos"); sin_r = em.rot(2, [64, 512], F32, "sin")
            P = [em.ps([128, 512], F32) for _ in range(6)]
            pdr = Rot(P[0:2]); psr = Rot(P[2:4])
            w1 = []; w2 = []; biasT = []
            cp = em.sb([32, 64], F32, "cp")
            em.dma("sp", cp[:], cmp_pos, writes=[cp])
            em.op("pe", lambda e: e.transpose(P[4][0:64, 0:32], cp[0:32, 0:64], ident[0:32, 0:32]), reads=[cp, ident], writes=[P[4]])
            cposT = em.sb([64, 32], BF16, "cposT")
            em.op("dve", lambda e: e.tensor_copy(cposT[:], P[4][0:64, 0:32]), reads=[P[4]], writes=[cposT])
            for m in range(2):
                a = em.sb([64, 32, 256], BF16, "w1")
                em.dma("pool", a[:], phi_w1[m].rearrange("(j d) f -> d j f", d=64), writes=[a])
                b = em.sb([128, 2, 64], BF16, "w2")
                em.dma("pool", b[:], phi_w2[m].rearrange("(c p) d -> p c d", p=128), writes=[b])
                w1.append(a); w2.append(b)
                bt = em.sb([128, 2], F32, "biasT")
                for hc in range(2):
                    for j in range(32):
                        em.op("pe", lambda e: e.matmul(P[5][:, hc:hc + 1], a[:, j, hc * 128:(hc + 1) * 128], cposT[:, j:j + 1], start=(j == 0), stop=(j == 31)),
                              reads=[a, cposT], writes=[P[5]])
                em.op("dve", lambda e: e.tensor_copy(bt[:], P[5][:, 0:2]), reads=[P[5]], writes=[bt])
                biasT.append(bt)
            kt_r = em.rot(2, [64, T], BF16, "kt")
            t1_r = em.rot(2, [64, 512], F32, "kt1")
            t2_r = em.rot(2, [64, 512], F32, "kt2")
            hid_r = em.rot(2, [128, 2, 256], BF16, "hid")
            u_r = em.rot(2, [128, 255], F32, "gu")
            u2_r = em.rot(2, [128, 255], F32, "gu2")
            kc_r = em.rot(2, [64, 256], BF16, "kc")
            vc_r = em.rot(2, [128, 64], BF16, "vc")

            def compress(srcT, m, g):
                hid = hid_r.next()
                for hc in range(2):
                    ph = P[4]
                    for j in range(32):
                        em.op("pe", lambda e: e.matmul(ph[:, 0:255], w1[m][:, j, hc * 128:(hc + 1) * 128], srcT[:, j:j + 16 * 254 + 1:16], start=(j == 0), stop=(j == 31)),
                              reads=[w1[m], srcT], writes=[ph])
                    u = u_r.next(); u2 = u2_r.next()
                    em.op("act", lambda e: e.activation(out=u[:], in_=ph[:, 0:255], func=AF.Identity, bias=biasT[m][:, hc:hc + 1], scale=1.0), reads=[ph, biasT[m]], writes=[u])
                    em.op("pool", lambda e: e.tensor_tensor(u2[:], u[:], u[:], ALU.mult), reads=[u], writes=[u2])
                    em.op("dve", lambda e: e.tensor_scalar(u2[:], u2[:], 0.044715, 1.0, ALU.mult, ALU.add), reads=[u2], writes=[u2])
                    em.op("pool", lambda e: e.tensor_tensor(u2[:], u2[:], u[:], ALU.mult), reads=[u2, u], writes=[u2])
                    em.op("act", lambda e: e.activation(out=u2[:], in_=u2[:], func=AF.Sigmoid, scale=1.5957691216057308), reads=[u2], writes=[u2])
                    em.op("dve", lambda e: e.tensor_tensor(hid[:, hc, 0:255], u[:], u2[:], ALU.mult), reads=[u, u2], writes=[hid])
                if m == 0:
                    pk = P[5]
                    for hc in range(2):
                        em.op("pe", lambda e: e.matmul(pk[0:64, 0:255], w2[0][:, hc, :], hid[:, hc, 0:255], start=(hc == 0), stop=(hc == 1)), reads=[w2[0], hid], writes=[pk])
                    kc = kc_r.next()
                    em.op("dve", lambda e: e.memset(kc[:], 0.0), writes=[kc])
                    em.op("dve", lambda e: e.tensor_copy(kc[:, 0:255], pk[0:64, 0:255]), reads=[pk], writes=[kc])
                    em.dma("sp", KC_d[g], kc[:], reads=[kc])
                else:
                    for ncn in range(2):
                        nn = 128 if ncn == 0 else 127
                        pv = P[5]
                        for hc in range(2):
                            em.op("pe", lambda e: e.matmul(pv[0:nn, 0:64], hid[:, hc, ncn * 128:ncn * 128 + nn], w2[1][:, hc, :], start=(hc == 0), stop=(hc == 1)), reads=[hid, w2[1]], writes=[pv])
                        vc = vc_r.next()
                        em.op("dve", lambda e: e.memset(vc[:], 0.0), writes=[vc])
                        em.op("dve", lambda e: e.tensor_copy(vc[0:nn, :], pv[0:nn, 0:64]), reads=[pv], writes=[vc])
                        em.dma("sp", VC_d[g, ncn], vc[:], reads=[vc])

            for si, slot in enumerate((0, 2, 4)):
                for g in range(4):
                    kt = kt_r.next()
                    for i in range(NT):
                        sl = slice(i * 512, (i + 1) * 512)
                        pd = pdr.next(); psw = psr.next()
                        for k in range(8):
                            em.op("pe", lambda e: e.matmul(pd[0:64, :], kvw[:, k, slot * 256 + g * 64:slot * 256 + g * 64 + 64], hb[:, k, sl], start=(k == 0), stop=(k == 7)), reads=[kvw, hb], writes=[pd])
                        for k in range(8):
                            em.op("pe", lambda e: e.matmul(psw[0:64, :], kvws[:, k, si * 256 + g * 64:si * 256 + g * 64 + 64], hb[:, k, sl], start=(k == 0), stop=(k == 7)), reads=[kvws, hb], writes=[psw])
                        t1 = t1_r.next(); t2 = t2_r.next()
                        cos = cos_r.next(); sin = sin_r.next()
                        em.dma("sp", cos[:], rope_d[0, :, sl], writes=[cos])
                        em.dma("sp", sin[:], rope_d[1, :, sl], writes=[sin])
                        em.op("dve", lambda e: e.tensor_tensor(t1[:], pd[0:64, :], cos[:], ALU.mult), reads=[pd, cos], writes=[t1])
                        em.op("dve", lambda e: e.tensor_tensor(t2[:], psw[0:64, :], sin[:], ALU.mult), reads=[psw, sin], writes=[t2])
                        em.op("pool", lambda e: e.tensor_tensor(kt[:, sl], t1[:], t2[:], ALU.add), reads=[t1, t2], writes=[kt])
                    em.dma("sp", KT_d[si, g], kt[:], reads=[kt])
                    if slot == 0:
                        compress(kt, 0, g)
            for g in range(4):
                vt = kt_r.next()
                for i in range(NT):
                    sl = slice(i * 512, (i + 1) * 512)
                    pd = pdr.next()
                    for k in range(8):
                        em.op("pe", lambda e: e.matmul(pd[0:64, :], kvw[:, k, 256 + g * 64:256 + g * 64 + 64], hb[:, k, sl], start=(k == 0), stop=(k == 7)), reads=[kvw, hb], writes=[pd])
                    em.op("act", lambda e: e.copy(vt[:, sl], pd[0:64, :]), reads=[pd], writes=[vt])
                compress(vt, 1, g)
            vt_r = em.rot(2, [128, 512], BF16, "vtok")
            for s in range(NS):
                ss = slice(s * 128, (s + 1) * 128)
                pv = pdr.next()
                for half, slot in enumerate((3, 5)):
                    for k in range(8):
                        em.op("pe", lambda e: e.matmul(pv[:, half * 256:(half + 1) * 256], hb[:, k, ss], kvw[:, k, slot * 256:(slot + 1) * 256], start=(k == 0), stop=(k == 7)), reads=[hb, kvw], writes=[pv])
                vtk = vt_r.next()
                em.op("act", lambda e: e.copy(vtk[:], pv[:]), reads=[pv], writes=[vtk])
                em.dma("sp", VT_d[ss, :], vtk[:], reads=[vtk])

    def nsa_phase(l, xsrc, ydst):
        jl = l - 2
        sc1 = mods[:, l, 8:16]
        sh = mods[:, l, 0:8]
        with em.phase():
            eall = em.sb([64, 32, 128], BF16, "eall"); em.dma("sp", eall[:], k_eall, writes=[eall])
            cz = em.sb([128, 4, 512], BF16, "cz"); em.dma("sp", cz[:], k_cz, writes=[cz])
            wm = em.sb([128, 8, 512], BF16, "wm"); em.dma("sp", wm[:], k_wm, writes=[wm])
            ovaug = em.sb([128, 2, 65], F32, "ovaug"); em.dma("sp", ovaug[:], k_ovaug.rearrange("c p j -> p c j"), writes=[ovaug])
            qw = em.sb([128, 8, 1072], BF16, "qw"); em.dma("pool", qw[:], nsa_q_w[jl].rearrange("(k p) f -> p k f", p=128), writes=[qw])
            qws = em.sb([128, 8, 1024], BF16, "qws"); em.dma("pool", qws[:], nsa_q_w_sw[jl].rearrange("(k p) f -> p k f", p=128), writes=[qws])
            ow = em.sb([64, 16, D], BF16, "ow"); em.dma("pool", ow[:], nsa_o_w[jl].rearrange("(h d) o -> d h o", d=64), writes=[ow])
            KC = em.sb([64, 4, 256], BF16, "KC"); em.dma("sp", KC[:], KC_d.rearrange("g d n -> d g n"), writes=[KC])
            VC = em.sb([128, 4, 2, 64], BF16, "VC"); em.dma("sp", VC[:], VC_d.rearrange("g c p d -> p g c d"), writes=[VC])
            P = [em.ps([128, 512], F32) for _ in range(8)]
            pS_r = Rot(P[0:2]); pM = P[2]; pO = P[3]; pD = P[4]; pI = P[5]; pOut = P[6]; pQ = P[7]
            xt_r = em.rot(2, [128, 512], F32, "ax")
            hbt_r = em.rot(1, [128, 8, 512], BF16, "ahb")
            cos_r = em.rot(1, [64, 512], F32, "acos"); sin_r = em.rot(1, [64, 512], F32, "asin")
            mc_r = em.rot(1, [128, 2, 512], F32, "amc")
            oT_r = em.rot(1, [64, 16, 512], BF16, "oT")
            KS_r = em.rot(1, [64, T], BF16, "KS"); VS_r = em.rot(1, [128, NS, 64], BF16, "VS")
            KW_r = em.rot(1, [64, 1024], BF16, "KW"); VW_r = em.rot(1, [128, 8, 64], BF16, "VW")
            qt_b = [em.sb([64, 512], BF16, "qt") for _ in range(4)]
            sig_r = em.rot(2, [64, 512], F32, "sig")
            gwr_b = [em.sb([128, 8, 3, 64], BF16, "gwr") for _ in range(4)]
            oc_b = [em.sb([64, 512], F32, "ocomb") for _ in range(4)]
            t1_r = em.rot(1, [64, 512], F32, "at1"); t2_r = em.rot(1, [64, 512], F32, "at2")
            e32_r = em.rot(1, [128, 512], F32, "e32")
            p32 = [em.sb([128, 512], F32, "p32") for _ in range(2)]
            pbc = [em.sb([128, 512], BF16, "pbc") for _ in range(2)]
            eb_r = em.rot(3, [128, 512], BF16, "eb"); pb_r = em.rot(3, [128, 512], BF16, "pb")
            rden_r = em.rot(2, [64, 512], F32, "rden")
            impg = em.sb([128, 4, 64], F32, "impg")
            rd_r = em.rot(2, [128, 1], F32, "rd")
            selc_r = em.rot(2, [128, 4, 64], F32, "selc")
            sc_r = em.rot(2, [128, 64], F32, "sc"); rep_r = em.rot(2, [128, 64], F32, "rep")
            m8a = em.sb([128, 8]); m8b = em.sb([128, 8])
            selT = em.sb([64, 512], BF16, "selT")
            yo_r = em.rot(2, [128, 512], F32, "ayo")

            cur_hbt = [None]

            def finish_branch(r, b, first):
                rden = rden_r.next(); sig = sig_r.next()
                for k in range(8):
                    em.op("pe", lambda e: e.matmul(pQ[0:64, :], gwr_b[r][:, k, b, :], cur_hbt[0][:, k, :], start=(k == 0), stop=(k == 7)), reads=[gwr_b[r], cur_hbt[0]], writes=[pQ])
                em.op("act", lambda e: e.activation(out=sig[:], in_=pQ[0:64, :], func=AF.Sigmoid), reads=[pQ], writes=[sig])
                em.op("dve", lambda e: e.tensor_scalar_max(rden[:], pD[0:64, :], TINY), reads=[pD], writes=[rden])
                em.op("dve", lambda e: e.reciprocal(rden[:], rden[:]), reads=[rden], writes=[rden])
                em.op("pool", lambda e: e.tensor_tensor(rden[:], rden[:], sig[:], ALU.mult), reads=[rden, sig], writes=[rden])
                if first:
                    em.op("dve", lambda e: e.tensor_tensor(oc_b[r][:], pO[0:64, :], rden[:], ALU.mult), reads=[pO, rden], writes=[oc_b[r]])
                else:
                    em.op("dve", lambda e: e.tensor_tensor(rden[:], pO[0:64, :], rden[:], ALU.mult), reads=[pO, rden], writes=[rden])
                    em.op("pool", lambda e: e.tensor_tensor(oc_b[r][:], oc_b[r][:], rden[:], ALU.add), reads=[oc_b[r], rden], writes=[oc_b[r]])

            for i in range(NT):
                sl = slice(i * 512, (i + 1) * 512)
                hbt = hbt_r.next()
                cur_hbt[0] = hbt
                for c in range(8):
                    xt = xt_r.next()
                    em.dma("sp", xt[:], xsrc[c, :, sl], writes=[xt])
                    eng = "dve" if c % 2 == 0 else "pool"
                    em.op(eng, lambda e: e.tensor_scalar(hbt[:, c, :], xt[:], sc1[:, c:c + 1], sh[:, c:c + 1], ALU.mult, ALU.add),
                          reads=[xt, mods], writes=[hbt])
                cos = cos_r.next(); sin = sin_r.next(); mc = mc_r.next()
                em.dma("sp", cos[:], rope_d[0, :, sl], writes=[cos])
                em.dma("sp", sin[:], rope_d[1, :, sl], writes=[sin])
                em.op("dve", lambda e: e.tensor_scalar_mul(cos[:], cos[:], ATTN_SCALE), reads=[cos], writes=[cos])
                em.op("dve", lambda e: e.tensor_scalar_mul(sin[:], sin[:], ATTN_SCALE), reads=[sin], writes=[sin])
                em.dma("sp", mc[:], k_mcmp[:, :, sl].rearrange("c p t -> p c t"), writes=[mc])
                oT = oT_r.next()
                nkt = 4 * (i + 1)
                w0 = max(0, 4 * i - 4)
                for g in range(4):
                    KS = KS_r.next(); VS = VS_r.next(); KW = KW_r.next(); VW = VW_r.next()
                    em.dma("sp", KS[:, 0:nkt * 128], KT_d[1, g, :, 0:nkt * 128], writes=[KS])
                    em.dma("sp", VS[:, 0:nkt, :], VT_d[0:nkt * 128, g * 64:(g + 1) * 64].rearrange("(k p) d -> p k d", p=128), writes=[VS])
                    nwk = nkt - w0
                    em.dma("sp", KW[:, 0:nwk * 128], KT_d[2, g, :, w0 * 128:nkt * 128], writes=[KW])
                    em.dma("sp", VW[:, 0:nwk, :], VT_d[w0 * 128:nkt * 128, 256 + g * 64:256 + (g + 1) * 64].rearrange("(k p) d -> p k d", p=128), writes=[VW])
                    for r in range(4):
                        h = g * 4 + r
                        pd = pS_r.next(); psw = pS_r.next()
                        for k in range(8):
                            em.op("pe", lambda e: e.matmul(pd[0:64, :], qw[:, k, h * 64:(h + 1) * 64], hbt[:, k, :], start=(k == 0), stop=(k == 7)), reads=[qw, hbt], writes=[pd])
                        for k in range(8):
                            em.op("pe", lambda e: e.matmul(psw[0:64, :], qws[:, k, h * 64:(h + 1) * 64], hbt[:, k, :], start=(k == 0), stop=(k == 7)), reads=[qws, hbt], writes=[psw])
                        t1 = t1_r.next(); t2 = t2_r.next()
                        em.op("dve", lambda e: e.tensor_tensor(t1[:], pd[0:64, :], cos[:], ALU.mult), reads=[pd, cos], writes=[t1])
                        em.op("dve", lambda e: e.tensor_tensor(t2[:], psw[0:64, :], sin[:], ALU.mult), reads=[psw, sin], writes=[t2])
                        em.op("pool", lambda e: e.tensor_tensor(qt_b[r][:], t1[:], t2[:], ALU.add), reads=[t1, t2], writes=[qt_b[r]])
                        gwr = gwr_b[r]
                        em.op("pool", lambda e: e.tensor_copy(gwr[:], qw[:, :, 1024 + h * 3:1024 + h * 3 + 3].unsqueeze(3).to_broadcast([128, 8, 3, 64])), reads=[qw], writes=[gwr])
                        for cn in range(2):
                            pS = pS_r.next()
                            em.op("pe", lambda e: e.matmul(pS[:], KC[:, g, cn * 128:(cn + 1) * 128], qt_b[r][:], start=True, stop=True), reads=[KC, qt_b[r]], writes=[pS])
                            e32 = e32_r.next()
                            em.op("act", lambda e: e.activation(out=e32[:], in_=pS[:], func=AF.Exp), reads=[pS], writes=[e32])
                            em.op("dve", lambda e: e.tensor_tensor(p32[cn][:], e32[:], mc[:, cn, :], ALU.mult), reads=[e32, mc], writes=[p32[cn]])
                            em.op("pool", lambda e: e.tensor_copy(pbc[cn][:], p32[cn][:]), reads=[p32[cn]], writes=[pbc[cn]])
                        for cn in range(2):
                            em.op("pe", lambda e: e.matmul(pO[0:64, :], VC[:, g, cn, :], pbc[cn][:], start=(cn == 0), stop=(cn == 1)), reads=[VC, pbc[cn]], writes=[pO])
                        for cn in range(2):
                            em.op("pe", lambda e: e.matmul(pD[0:64, :], onesb[:], pbc[cn][:], start=(cn == 0), stop=(cn == 1)), reads=[onesb, pbc[cn]], writes=[pD])
                        finish_branch(r, 0, True)
                        for s in range(4):
                            for cn in range(2):
                                em.op("pe", lambda e: e.matmul(pI[:, 0:65], p32[cn][:, s * 128:(s + 1) * 128], ovaug[:, cn, :], start=(cn == 0), stop=(cn == 1)), reads=[p32[cn], ovaug], writes=[pI])
                            rd = rd_r.next()
                            em.op("dve", lambda e: e.tensor_scalar_max(rd[:], pI[:, 64:65], TINY), reads=[pI], writes=[rd])
                            em.op("dve", lambda e: e.reciprocal(rd[:], rd[:]), reads=[rd], writes=[rd])
                            if r == 0:
                                em.op("dve", lambda e: e.tensor_scalar(impg[:, s, :], pI[:, 0:64], rd[:, 0:1], None, ALU.mult), reads=[pI, rd], writes=[impg])
                            else:
                                em.op("dve", lambda e: e.scalar_tensor_tensor(impg[:, s, :], pI[:, 0:64], rd[:, 0:1], impg[:, s, :], ALU.mult, ALU.add), reads=[pI, rd, impg], writes=[impg])
                    for s in range(4):
                        selc = selc_r.next()
                        t0 = i * 512 + s * 128
                        em.dma("sp", selc[:], k_selc[t0:t0 + 128], writes=[selc])
                        sc = sc_r.next(); rep = rep_r.next()
                        em.op("dve", lambda e: e.tensor_tensor(sc[:], impg[:, s, :], selc[:, 0, :], ALU.mult), reads=[impg, selc], writes=[sc])
                        em.op("dve", lambda e: e.tensor_tensor(sc[:], sc[:], selc[:, 1, :], ALU.add), reads=[sc, selc], writes=[sc])
                        em.op("dve", lambda e: e.tensor_tensor(sc[:], sc[:], selc[:, 2, :], ALU.mult), reads=[sc, selc], writes=[sc])
                        em.op("dve", lambda e: e.tensor_tensor(sc[:], sc[:], selc[:, 3, :], ALU.add), reads=[sc, selc], writes=[sc])
                        em.op("dve", lambda e: e.max(m8a[:], sc[:]), reads=[sc], writes=[m8a])
                        em.op("dve", lambda e: e.match_replace(rep[:], m8a[:], sc[:], -3e30), reads=[sc, m8a], writes=[rep])
                        em.op("dve", lambda e: e.max(m8b[:], rep[:]), reads=[rep], writes=[m8b])
                        em.op("dve", lambda e: e.tensor_scalar(rep[:], sc[:], m8b[:, 7:8], None, ALU.is_ge), reads=[sc, m8b], writes=[rep])
                        em.op("dve", lambda e: e.tensor_tensor(rep[:], rep[:], selc[:, 2, :], ALU.mult), reads=[rep, selc], writes=[rep])
                        em.op("pe", lambda e: e.transpose(pI[0:64, 128:256], rep[:], ident[:]), reads=[rep, ident], writes=[pI])
                        em.op("act", lambda e: e.copy(selT[:, s * 128:(s + 1) * 128], pI[0:64, 128:256]), reads=[pI], writes=[selT])
                    for r in range(4):
                        h = g * 4 + r
                        for kt in range(nkt):
                            pS = pS_r.next()
                            em.op("pe", lambda e: e.matmul(pS[:], KS[:, kt * 128:(kt + 1) * 128], qt_b[r][:], start=True, stop=True), reads=[KS, qt_b[r]], writes=[pS])
                            em.op("pe", lambda e: e.matmul(pM[:], eall[:, kt, :], selT[:], start=True, stop=True), reads=[eall, selT], writes=[pM])
                            eb = eb_r.next(); pb = pb_r.next()
                            em.op("act", lambda e: e.activation(out=eb[:], in_=pS[:], func=AF.Exp), reads=[pS], writes=[eb])
                            em.op("dve", lambda e: e.tensor_tensor(pb[:], eb[:], pM[:], ALU.mult), reads=[eb, pM], writes=[pb])
                            if kt >= 4 * i:
                                em.op("pool", lambda e: e.tensor_tensor(pb[:], pb[:], cz[:, kt - 4 * i, :], ALU.mult), reads=[pb, cz], writes=[pb])
                            em.op("pe", lambda e: e.matmul(pO[0:64, :], VS[:, kt, :], pb[:], start=(kt == 0), stop=(kt == nkt - 1)), reads=[VS, pb], writes=[pO])
                            em.op("pe", lambda e: e.matmul(pD[0:64, :], onesb[:], pb[:], start=(kt == 0), stop=(kt == nkt - 1)), reads=[onesb, pb], writes=[pD])
                        finish_branch(r, 1, False)
                        for kw in range(nwk):
                            kt = w0 + kw
                            pS = pS_r.next()
                            em.op("pe", lambda e: e.matmul(pS[:], KW[:, kw * 128:(kw + 1) * 128], qt_b[r][:], start=True, stop=True), reads=[KW, qt_b[r]], writes=[pS])
                            eb = eb_r.next(); pb = pb_r.next()
                            em.op("act", lambda e: e.activation(out=eb[:], in_=pS[:], func=AF.Exp), reads=[pS], writes=[eb])
                            em.op("pool", lambda e: e.tensor_tensor(pb[:], eb[:], wm[:, 4 * i - kt + 3, :], ALU.mult), reads=[eb, wm], writes=[pb])
                            em.op("pe", lambda e: e.matmul(pO[0:64, :], VW[:, kw, :], pb[:], start=(kw == 0), stop=(kw == nwk - 1)), reads=[VW, pb], writes=[pO])
                            em.op("pe", lambda e: e.matmul(pD[0:64, :], onesb[:], pb[:], start=(kw == 0), stop=(kw == nwk - 1)), reads=[onesb, pb], writes=[pD])
                        finish_branch(r, 2, False)
                        em.op("act", lambda e: e.copy(oT[:, h, :], oc_b[r][:]), reads=[oc_b[r]], writes=[oT])
                for oc in range(8):
                    yo = yo_r.next()
                    for h in range(16):
                        em.op("pe", lambda e: e.matmul(pOut[:], ow[:, h, oc * 128:(oc + 1) * 128], oT[:, h, :], start=(h == 0), stop=(h == 15)), reads=[ow, oT], writes=[pOut])
                    em.op("dve", lambda e: e.tensor_copy(yo[:], pOut[:]), reads=[pOut], writes=[yo])
                    em.dma("sp", ydst[oc, :, sl], yo[:], reads=[yo])

    cur = 0
    if on("rope"):
        rope_phase()
    for l in range(DEPTH):
        if on("mix%d" % l):
            if l < 2:
                mamba_phase(l, xs_d[cur], ymix_d)
            else:
                nsa_phase(l, xs_d[cur], ymix_d)
        if on("ln%da" % l):
            post_norm(xs_d[cur], ymix_d, xs_d[1 - cur], mods[:, l, 16:24], 2 * l, False)
        cur = 1 - cur
        if on("moe%d" % l):
            moe_phase(l, xs_d[cur], ymix_d)
        if on("ln%db" % l):
            post_norm(xs_d[cur], ymix_d, xs_d[1 - cur], mods[:, l, 40:48], 2 * l + 1, l == DEPTH - 1)
        cur = 1 - cur
        if l == 1 and on("kv"):
            kv_phase(xs_d[cur])
    em.barrier()
    em.close()
    return nc, em


def make_in_maps(inputs, T, cores):
    cst = host_consts(T)
    f = lambda a: np.ascontiguousarray(np.asarray(a, dtype=np.float32))
    shared = {
        "ada_w": f(inputs["ada_w"]), "ada_b": f(inputs["ada_b"]),
        "ln_g": f(inputs["ln_g"]).reshape(8, D), "ln_b": f(inputs["ln_b"]).reshape(8, D),
        "ssm_in_w": f(inputs["ssm_in_w"]), "ssm_conv_w": f(inputs["ssm_conv_w"]), "ssm_conv_b": f(inputs["ssm_conv_b"]),
        "ssm_dt_bias": f(inputs["ssm_dt_bias"]), "ssm_a_log": f(inputs["ssm_a_log"]), "ssm_d": f(inputs["ssm_d"]),
        "ssm_norm_w": f(inputs["ssm_norm_w"]), "ssm_out_w": f(inputs["ssm_out_w"]),
        "kv_ada_w": f(inputs["kv_ada_w"]), "kv_ada_b": f(inputs["kv_ada_b"]).reshape(1, 2 * D),
        "kv_w": f(inputs["kv_w"]),
        "cmp_pos": f(inputs["cmp_pos"]),
        "phi_k_w1": f(inputs["phi_k_w1"]), "phi_k_w2": f(inputs["phi_k_w2"]),
        "phi_v_w1": f(inputs["phi_v_w1"]), "phi_v_w2": f(inputs["phi_v_w2"]),
        "nsa_q_w": f(inputs["nsa_q_w"]), "nsa_o_w": f(inputs["nsa_o_w"]),
        "router_w": f(inputs["router_w"]), "router_b": f(inputs["router_b"]),
        "moe_w_up": f(inputs["moe_w_up"]), "moe_b_up": f(inputs["moe_b_up"]),
        "moe_w_down": f(inputs["moe_w_down"]), "moe_b_down": f(inputs["moe_b_down"]),
    }
    kvw = shared["kv_w"]
    shared["kv_w_sw"] = np.concatenate([swap_halves(kvw[:, s * 256:(s + 1) * 256], 256) for s in (0, 2, 4)], axis=1)
    shared["nsa_q_w_sw"] = np.stack([swap_halves(shared["nsa_q_w"][j], 1024) for j in range(2)], axis=0)
    for k, v in cst.items():
        shared["k_" + k] = v
    maps = []
    for b in cores:
        m = dict(shared)
        m["x"] = f(inputs["x"][b][:T])
        m["c"] = np.ascontiguousarray(f(inputs["c"][b]).reshape(8, 128).T)
        m["pos"] = np.ascontiguousarray(np.asarray(inputs["pos"][b][:T], dtype=np.int32).reshape(1, T))
        maps.append(m)
    return maps


_CACHE = {}


def kernel(**inputs):
    T = 4096
    if T not in _CACHE:
        _CACHE[T] = build(T)[0]
    nc = _CACHE[T]
    maps = make_in_maps(inputs, T, list(range(8)))
    res = run_bass_kernel_spmd(nc, maps, core_ids=list(range(8)))
    out = np.stack([np.asarray(r["y"], dtype=np.float32) for r in res.results], axis=0)
    return out
```

```python
import contextlib
import math
import numpy as np
import ml_dtypes
import concourse.bass as bass
import concourse.mybir as mybir
from concourse.bass_utils import run_bass_kernel_spmd

F32 = mybir.dt.float32
BF16 = mybir.dt.bfloat16
I32 = mybir.dt.int32
AF = mybir.ActivationFunctionType
ALU = mybir.AluOpType

D = 1024
DEPTH = 4
ALPHA = (2.0 * DEPTH) ** 0.25
LN_EPS = 1e-5
EPS_A = LN_EPS / (ALPHA * ALPHA)
ATTN_SCALE = 0.125
TINY = 1e-30
SEM_LIMIT = 30000


class Buf:
    __slots__ = ("t", "name", "w", "rd")

    def __init__(self, t, name):
        self.t = t
        self.name = name
        self.w = None
        self.rd = {}

    def __getitem__(self, idx):
        return self.t[idx]


class Rot:
    def __init__(self, bufs):
        self.bufs = bufs
        self.i = 0

    def next(self):
        b = self.bufs[self.i % len(self.bufs)]
        self.i += 1
        return b


class Em:
    ENG = ("pe", "act", "dve", "pool", "sp")

    def __init__(self, nc, n_dma_sems=12, same_engine_sync=True):
        self.nc = nc
        self.es = contextlib.ExitStack()
        self.eng = {"pe": nc.tensor, "act": nc.scalar, "dve": nc.vector, "pool": nc.gpsimd, "sp": nc.sync}
        self.sem = {}
        self.cnt = {}
        self.cur = {}
        self.gen = {}
        self.owner = {}
        for e in self.ENG:
            self.gen[e] = 0
            self._new_sem(e)
        self.known = {e: {} for e in self.ENG}
        self.dma_pool = {}
        self.dma_idx = {}
        self.dma_uses = {}
        self.dma_gen = 0
        for q in ("sp", "pool", "act"):
            self.dma_pool[q] = []
            for i in range(4 if q == "pool" else n_dma_sems):
                self.dma_pool[q].append(self._new_dma_sem(q))
            self.dma_idx[q] = 0
        self.same = same_engine_sync
        self.phase_stack = None
        self.uid = 0
        self.n_wait = 0
        self.n_ins = 0

    def _new_sem(self, e):
        k = "%s_%d" % (e, self.gen[e])
        self.gen[e] += 1
        self.sem[k] = self.es.enter_context(self.nc.semaphore("s_" + k))
        self.cnt[k] = 0
        self.cur[e] = k
        self.owner[k] = e

    def _new_dma_sem(self, q):
        k = "d_%s_%d" % (q, self.dma_gen)
        self.dma_gen += 1
        self.sem[k] = self.es.enter_context(self.nc.semaphore(k))
        self.dma_uses[k] = 0
        self.owner[k] = "dma"
        return k

    def _stack(self, persist):
        return self.es if (persist or self.phase_stack is None) else self.phase_stack

    def sb(self, shape, dtype=F32, name=None, persist=False):
        self.uid += 1
        nm = "%s_%d" % (name or "t", self.uid)
        t = self._stack(persist).enter_context(self.nc.sbuf_tensor(nm, list(shape), dtype))
        return Buf(t, nm)

    def rot(self, n, shape, dtype=F32, name=None):
        return Rot([self.sb(shape, dtype, name) for _ in range(n)])

    def ps(self, shape, dtype=F32, name=None, persist=False):
        self.uid += 1
        nm = "%s_%d" % (name or "p", self.uid)
        t = self._stack(persist).enter_context(self.nc.psum_tensor(nm, list(shape), dtype))
        return Buf(t, nm)

    def dram(self, name, shape, dtype, kind="Internal"):
        t = self.nc.dram_tensor(name, list(shape), dtype, kind=kind)
        return t.ap()

    @contextlib.contextmanager
    def phase(self):
        assert self.phase_stack is None
        self.barrier()
        self.phase_stack = contextlib.ExitStack()
        try:
            with self.phase_stack:
                yield
                self.barrier()
        finally:
            self.phase_stack = None

    def _need(self, e, dep):
        if dep is None:
            return
        k, v = dep
        if self.owner[k] == e and (e == "pe" or (e != "pool" and not self.same)):
            return
        if self.known[e].get(k, 0) >= v:
            return
        self.eng[e].wait_ge(self.sem[k], v)
        self.known[e][k] = v
        self.n_wait += 1

    def _deps(self, e, reads, writes):
        for b in reads:
            self._need(e, b.w)
        for b in writes:
            self._need(e, b.w)
            for k, v in b.rd.items():
                self._need(e, (k, v))

    def _record(self, tok, reads, writes):
        k, v = tok
        for b in reads:
            if b.rd.get(k, 0) < v:
                b.rd[k] = v
        for b in writes:
            b.w = tok
            b.rd = {}

    def op(self, e, ins_fn, reads=(), writes=()):
        self._deps(e, reads, writes)
        ins = ins_fn(self.eng[e])
        k = self.cur[e]
        self.cnt[k] += 1
        ins.then_inc(self.sem[k], 1)
        self._record((k, self.cnt[k]), reads, writes)
        self.n_ins += 1
        if self.cnt[k] >= SEM_LIMIT:
            self._new_sem(e)
        return ins

    def dma(self, q, out, in_, reads=(), writes=(), **kw):
        self._deps(q, reads, writes)
        pool = self.dma_pool[q]
        slot = self.dma_idx[q] % len(pool)
        k = pool[slot]
        self.dma_idx[q] += 1
        if 16 * (self.dma_uses[k] + 1) > SEM_LIMIT:
            k = self._new_dma_sem(q)
            pool[slot] = k
        if self.dma_uses[k] > 0:
            self._need(q, (k, 16 * self.dma_uses[k]))
        self.dma_uses[k] += 1
        ins = self.eng[q].dma_start(out=out, in_=in_, **kw)
        ins.then_inc(self.sem[k], 16)
        self._record((k, 16 * self.dma_uses[k]), reads, writes)
        self.n_ins += 1
        return ins

    def barrier(self):
        for e in self.ENG:
            for k, c in self.cnt.items():
                if c > 0:
                    self._need(e, (k, c))
            for k, u in self.dma_uses.items():
                if u > 0:
                    self._need(e, (k, 16 * u))

    def close(self):
        self.es.close()


def host_consts(T):
    c = {}
    c["ident"] = np.eye(128, dtype=np.float32)
    s = np.arange(128)
    c["tri"] = (s[:, None] <= s[None, :]).astype(np.float32)
    d = np.arange(64)
    inv = (10000.0 ** (-(np.arange(32, dtype=np.float32)) / np.float32(32))).astype(np.float32)
    c["ropec"] = np.stack([inv[d % 32], np.where(d < 32, -1.0, 1.0).astype(np.float32)], axis=1).astype(np.float32)
    n = np.arange(256)
    t = np.arange(T)
    m = ((16 * n[:, None] + 31) <= t[None, :]) & (n[:, None] < T // 16 - 1)
    c["mcmp"] = m.reshape(2, 128, T).astype(np.float32)
    n_cmp = T // 16 - 1
    n_slc = T // 64
    cs = np.arange(256)[:, None] * 16
    js = np.arange(64)[None, :] * 64
    ov = np.maximum(np.minimum(cs + 32, js + 64) - np.maximum(cs, js), 0).astype(np.float32) / 32.0
    ov[n_cmp:, :] = 0.0
    ov[:, n_slc:] = 0.0
    c["ovaug"] = np.concatenate([ov, np.ones((256, 1), np.float32)], axis=1).reshape(2, 128, 65)
    tb = (t // 64)[:, None]
    j = np.arange(64)[None, :]
    forced = ((j == 0) | (j == tb) | (j == tb - 1)).astype(np.float32)
    cb = (j <= tb).astype(np.float32)
    c["selc"] = np.stack([1.0 - forced, forced * (1e9 + 1024.0 * j), cb, (cb - 1.0) * 1e30], axis=1).astype(np.float32)
    E = np.zeros((64, 32, 128), np.float32)
    for kt in range(32):
        E[2 * kt, kt, :64] = 1.0
        E[2 * kt + 1, kt, 64:] = 1.0
    c["eall"] = E.astype(ml_dtypes.bfloat16)
    mm = np.arange(128)[:, None, None]
    dd = np.arange(4)[None, :, None]
    nn = np.arange(512)[None, None, :]
    c["cz"] = ((128 * dd + mm) <= nn).astype(ml_dtypes.bfloat16)
    rr = np.arange(8)[None, :, None] - 3
    diff = 128 * rr + nn - mm
    c["wm"] = ((diff >= 0) & (diff < 512)).astype(ml_dtypes.bfloat16)
    c["stri"] = (s[:, None] < s[None, :]).astype(np.float32)
    c["iota"] = np.broadcast_to(np.arange(128, dtype=np.float32)[None, :], (128, 128)).copy()
    c["base8"] = (np.arange(8, dtype=np.float32)[None, :] * 128 + np.arange(128, dtype=np.float32)[:, None]).astype(np.float32)
    return c


def swap_halves(w, ncols):
    w = w[:, :ncols].reshape(w.shape[0], ncols // 64, 2, 32)
    return np.ascontiguousarray(w[:, :, ::-1, :].reshape(w.shape[0], ncols))


def build(T, stages=None, dbg=False):
    NT = T // 512
    NS = T // 128
    nc = bass.Bass("TRN2", target_bir_lowering=False)
    import os as _os0
    em = Em(nc, same_engine_sync=(_os0.environ.get('SES', '1') == '1'))
    on = lambda s: stages is None or s in stages

    em.declared = []
    any_moe = stages is None or any(st.startswith("moe") for st in stages)

    def din(name, shape, dt=F32):
        if name in ("moe_w_up", "moe_w_down") and not any_moe:
            return None
        em.declared.append(name)
        return em.dram(name, shape, dt, kind="ExternalInput")

    x_in = din("x", [T, D])
    c_in = din("c", [128, 8])
    pos_in = din("pos", [1, T], I32)
    ada_w = din("ada_w", [4, D, 6 * D])
    ada_b = din("ada_b", [4, 6 * D])
    ln_g = din("ln_g", [8, D])
    ln_b = din("ln_b", [8, D])
    ssm_in_w = din("ssm_in_w", [2, D, 5152])
    ssm_conv_w = din("ssm_conv_w", [2, 4, 3072])
    ssm_conv_b = din("ssm_conv_b", [2, 3072])
    ssm_dt_bias = din("ssm_dt_bias", [2, 32])
    ssm_a_log = din("ssm_a_log", [2, 32])
    ssm_d = din("ssm_d", [2, 32])
    ssm_norm_w = din("ssm_norm_w", [2, 2048])
    ssm_out_w = din("ssm_out_w", [2, 2048, D])
    kv_ada_w = din("kv_ada_w", [D, 2 * D])
    kv_ada_b = din("kv_ada_b", [1, 2 * D])
    kv_w = din("kv_w", [D, 1536])
    kv_w_sw = din("kv_w_sw", [D, 768])
    cmp_pos = din("cmp_pos", [32, 64])
    phi_w1 = [din("phi_k_w1", [2048, 256]), din("phi_v_w1", [2048, 256])]
    phi_w2 = [din("phi_k_w2", [256, 64]), din("phi_v_w2", [256, 64])]
    nsa_q_w = din("nsa_q_w", [2, D, 1072])
    nsa_q_w_sw = din("nsa_q_w_sw", [2, D, 1024])
    nsa_o_w = din("nsa_o_w", [2, D, D])
    router_w = din("router_w", [4, D, 32])
    router_b = din("router_b", [4, 32])
    moe_w_up = din("moe_w_up", [4, 32, D, 2 * D])
    moe_b_up = din("moe_b_up", [4, 32, 2 * D])
    moe_w_down = din("moe_w_down", [4, 32, D, D])
    moe_b_down = din("moe_b_down", [4, 32, D])
    k_ident = din("k_ident", [128, 128])
    k_tri = din("k_tri", [128, 128])
    k_ropec = din("k_ropec", [64, 2])
    k_mcmp = din("k_mcmp", [2, 128, T])
    k_ovaug = din("k_ovaug", [2, 128, 65])
    k_selc = din("k_selc", [T, 4, 64])
    k_eall = din("k_eall", [64, 32, 128], BF16)
    k_cz = din("k_cz", [128, 4, 512], BF16)
    k_wm = din("k_wm", [128, 8, 512], BF16)
    k_stri = din("k_stri", [128, 128])
    k_iota = din("k_iota", [128, 128])
    k_base8 = din("k_base8", [128, 8])

    y_out = em.dram("y", [T, D], F32, kind="ExternalOutput")
    sk = "ExternalOutput" if dbg else "Internal"
    xs_d = [em.dram("xA", [8, 128, T], F32, kind=sk), em.dram("xB", [8, 128, T], F32, kind=sk)]
    ymix_d = em.dram("ymix", [8, 128, T], F32, kind=sk)
    zs_d = em.dram("zs_tok", [T, 2048], BF16, kind=sk)
    dt_d = em.dram("dt_tok", [T, 32], F32, kind=sk)
    xbc_d = em.dram("xbcT", [24, 128, T], BF16, kind=sk)
    rope_d = em.dram("rope", [2, 64, T], F32, kind=sk)
    KT_d = em.dram("KT", [3, 4, 64, T], BF16, kind=sk)
    VT_d = em.dram("VT", [T, 512], BF16, kind=sk)
    KC_d = em.dram("KC", [4, 64, 256], BF16, kind=sk)
    VC_d = em.dram("VC", [4, 2, 128, 64], BF16, kind=sk)
    NBLK = (4 * T + 32 * 512) // 512
    RROWS = NBLK * 512
    hs_d = em.dram("h_sorted", [RROWS, D], BF16)
    ys_d = em.dram("y_sorted", [RROWS, D], F32)

    ident = em.sb([128, 128], F32, "ident", persist=True)
    identb = em.sb([128, 128], BF16, "identb", persist=True)
    ones32 = em.sb([128, 128], F32, "ones32", persist=True)
    onesb = em.sb([128, 64], BF16, "onesb", persist=True)
    mods = em.sb([128, 4, 48], F32, "mods", persist=True)
    kvmod = em.sb([128, 16], F32, "kvmod", persist=True)
    lng = em.sb([128, 8, 8], F32, "lng", persist=True)
    lnb = em.sb([128, 8, 8], F32, "lnb", persist=True)
    epsb = em.sb([128, 1], F32, "epsb", persist=True)
    eps5 = em.sb([128, 1], F32, "eps5", persist=True)

    em.dma("sp", ident[:], k_ident, writes=[ident])
    em.op("dve", lambda e: e.tensor_copy(identb[:], ident[:]), reads=[ident], writes=[identb])
    em.op("dve", lambda e: e.memset(ones32[:], 1.0), writes=[ones32])
    em.op("dve", lambda e: e.memset(onesb[:], 1.0), writes=[onesb])
    em.op("dve", lambda e: e.memset(epsb[:], EPS_A), writes=[epsb])
    em.op("dve", lambda e: e.memset(eps5[:], LN_EPS), writes=[eps5])

    def rowsT(dst_ap, src_rows_ap, R, C, pbuf, dstbuf, tmp=None, tmp_ap=None):
        if tmp is None:
            tmp = em.sb([R, C * 128], F32, "rowsT")
            tmp_ap = tmp[:]
        em.dma("sp", tmp_ap[0:R, 0:C * 128], src_rows_ap, writes=[tmp])
        for c in range(C):
            em.op("pe", lambda e: e.transpose(pbuf[:, c * R:(c + 1) * R], tmp_ap[0:R, c * 128:(c + 1) * 128], ident[0:R, 0:R]),
                  reads=[tmp, ident], writes=[pbuf])
        em.op("dve", lambda e: e.tensor_copy(dst_ap, pbuf[:, 0:C * R].rearrange("p (c r) -> p c r", r=R)),
              reads=[pbuf], writes=[dstbuf])

    if on("mod"):
        with em.phase():
            pm = em.ps([128, 512], F32)
            cT = em.sb([128, 8])
            cact = em.sb([128, 8])
            em.dma("sp", cT[:], c_in, writes=[cT])
            em.op("act", lambda e: e.activation(out=cact[:], in_=cT[:], func=AF.Silu), reads=[cT], writes=[cact])
            rowsT(lng[:], ln_g, 8, 8, pm, lng)
            rowsT(lnb[:], ln_b, 8, 8, pm, lnb)
            wrot = em.rot(2, [128, 8, 512], F32, "adaw")
            pmod = em.ps([128, 64], F32)
            bT = em.sb([128, 48, 4])
            rowsT(bT[:], ada_b, 4, 48, pm, bT)
            bTk = em.sb([128, 16, 1])
            rowsT(bTk[:], kv_ada_b, 1, 16, pm, bTk)
            for i in range(5):
                ncol = 48 if i < 4 else 16
                for cb in range(ncol // 4):
                    wk = wrot.next()
                    src = ada_w[i][:, cb * 512:(cb + 1) * 512] if i < 4 else kv_ada_w[:, cb * 512:(cb + 1) * 512]
                    em.dma("sp", wk[:], src.rearrange("(k p) f -> p k f", p=128), writes=[wk])
                    for o4 in range(4):
                        oc = cb * 4 + o4
                        for k in range(8):
                            em.op("pe", lambda e: e.matmul(pmod[:, oc:oc + 1], wk[:, k, o4 * 128:(o4 + 1) * 128], cact[:, k:k + 1],
                                                           start=(k == 0), stop=(k == 7)),
                                  reads=[wk, cact], writes=[pmod])
                dst = mods[:, i, :] if i < 4 else kvmod[:]
                dbuf = mods if i < 4 else kvmod
                bsl = bT[:, :, i] if i < 4 else bTk[:, :, 0]
                em.op("dve", lambda e: e.tensor_tensor(dst, pmod[:, 0:ncol], bsl, ALU.add),
                      reads=[pmod, bT, bTk], writes=[dbuf])
            for i in range(4):
                for c0 in (8, 32):
                    em.op("dve", lambda e: e.tensor_scalar_add(mods[:, i, c0:c0 + 8], mods[:, i, c0:c0 + 8], 1.0),
                          reads=[mods], writes=[mods])
                for c0 in (16, 40):
                    em.op("dve", lambda e: e.tensor_scalar(mods[:, i, c0:c0 + 8], mods[:, i, c0:c0 + 8], 1.0, 1.0 / ALPHA,
                                                           ALU.add, ALU.mult), reads=[mods], writes=[mods])
            em.op("dve", lambda e: e.tensor_scalar_add(kvmod[:, 8:16], kvmod[:, 8:16], 1.0), reads=[kvmod], writes=[kvmod])

    if dbg and on("mod"):
        dbg_mods = em.dram("dbg_mods", [128, 4, 48], F32, kind="ExternalOutput")
        em.dma("sp", dbg_mods, mods[:], reads=[mods])

    if on("in"):
        with em.phase():
            xin = em.rot(2, [128, 4, D], F32, "xin")
            xo = em.rot(2, [128, 8, 512], F32, "xo")
            pp = Rot([em.ps([128, 512], F32) for _ in range(4)])
            for i in range(NT):
                a = xin.next()
                em.dma("sp", a[:], x_in[i * 512:(i + 1) * 512, :].rearrange("(s p) f -> p s f", p=128), writes=[a])
                o = xo.next()
                for c in range(8):
                    p = pp.next()
                    for s in range(4):
                        em.op("pe", lambda e: e.transpose(p[:, s * 128:(s + 1) * 128], a[:, s, c * 128:(c + 1) * 128], ident[:]),
                              reads=[a, ident], writes=[p])
                    eng = "act" if c % 2 else "dve"
                    if eng == "act":
                        em.op("act", lambda e: e.copy(o[:, c, :], p[:]), reads=[p], writes=[o])
                    else:
                        em.op("dve", lambda e: e.tensor_copy(o[:, c, :], p[:]), reads=[p], writes=[o])
                em.dma("sp", xs_d[0][:, :, i * 512:(i + 1) * 512].rearrange("c p t -> p c t"), o[:], reads=[o])

    def load_mod_tile(xsrc, i, xt_r, dst, sc1, sh, dst_buf):
        for c in range(8):
            xt = xt_r.next()
            em.dma("sp", xt[:], xsrc[c, :, i * 512:(i + 1) * 512], writes=[xt])
            eng = "dve" if c % 2 == 0 else "pool"
            em.op(eng, lambda e: e.tensor_scalar(dst(c), xt[:], sc1[:, c:c + 1], sh[:, c:c + 1], ALU.mult, ALU.add),
                  reads=[xt, mods, kvmod], writes=[dst_buf])

    def post_norm(xsrc, ysrc, xdst, g1a, r, final):
        with em.phase():
            xt_r = em.rot(2, [128, 8, 512], F32, "lnx")
            yt_r = em.rot(2, [128, 8, 512], F32, "lny")
            z_r = em.rot(2, [128, 8, 512], F32, "lnz")
            sq_r = em.rot(1, [128, 8, 512], F32, "lnsq")
            xo_r = em.rot(2, [128, 8, 512], F32, "lno")
            psum_s = em.ps([128, 512], F32)
            psum_q = em.ps([128, 512], F32)
            mean = em.sb([128, 512]); msq = em.sb([128, 512]); var = em.sb([128, 512]); rstd = em.sb([128, 512])
            if final:
                pT = [em.ps([128, 512], F32), em.ps([128, 512], F32)]
                ot_r = em.rot(2, [128, 4, D], F32, "lnot")
            for i in range(NT):
                sl = slice(i * 512, (i + 1) * 512)
                xt = xt_r.next(); yt = yt_r.next(); z = z_r.next(); sq = sq_r.next(); xo = xo_r.next()
                em.dma("sp", xt[:], xsrc[:, :, sl].rearrange("c p t -> p c t"), writes=[xt])
                em.dma("sp", yt[:], ysrc[:, :, sl].rearrange("c p t -> p c t"), writes=[yt])
                for c in range(8):
                    eng = "dve"
                    em.op(eng, lambda e: e.scalar_tensor_tensor(z[:, c, :], yt[:, c, :], g1a[:, c:c + 1], xt[:, c, :], ALU.mult, ALU.add),
                          reads=[yt, xt, mods], writes=[z])
                em.op("act", lambda e: e.activation(out=sq[:], in_=z[:], func=AF.Square), reads=[z], writes=[sq])
                for c in range(8):
                    em.op("pe", lambda e: e.matmul(psum_s[:], ones32[:], z[:, c, :], start=(c == 0), stop=(c == 7)),
                          reads=[ones32, z], writes=[psum_s])
                for c in range(8):
                    em.op("pe", lambda e: e.matmul(psum_q[:], ones32[:], sq[:, c, :], start=(c == 0), stop=(c == 7)),
                          reads=[ones32, sq], writes=[psum_q])
                em.op("act", lambda e: e.mul(mean[:], psum_s[:], 1.0 / D), reads=[psum_s], writes=[mean])
                em.op("pool", lambda e: e.tensor_tensor(msq[:], mean[:], mean[:], ALU.mult), reads=[mean], writes=[msq])
                em.op("dve", lambda e: e.scalar_tensor_tensor(var[:], psum_q[:], 1.0 / D, msq[:], ALU.mult, ALU.subtract),
                      reads=[psum_q, msq], writes=[var])
                em.op("act", lambda e: e.activation(out=var[:], in_=var[:], func=AF.Sqrt, bias=epsb[:], scale=1.0),
                      reads=[var, epsb], writes=[var])
                em.op("dve", lambda e: e.reciprocal(rstd[:], var[:]), reads=[var], writes=[rstd])
                for c in range(8):
                    em.op("pool", lambda e: e.tensor_tensor(z[:, c, :], z[:, c, :], mean[:], ALU.subtract), reads=[z, mean], writes=[z])
                    em.op("dve", lambda e: e.tensor_tensor(z[:, c, :], z[:, c, :], rstd[:], ALU.mult), reads=[z, rstd], writes=[z])
                    em.op("act", lambda e: e.activation(out=xo[:, c, :], in_=z[:, c, :], func=AF.Identity,
                                                        bias=lnb[:, c, r:r + 1], scale=lng[:, c, r:r + 1]),
                          reads=[z, lng, lnb], writes=[xo])
                if not final:
                    em.dma("sp", xdst[:, :, sl].rearrange("c p t -> p c t"), xo[:], reads=[xo])
                else:
                    ot = ot_r.next()
                    for s in range(4):
                        for c in range(8):
                            p = pT[c // 4]
                            em.op("pe", lambda e: e.transpose(p[:, (c % 4) * 128:(c % 4 + 1) * 128], xo[:, c, s * 128:(s + 1) * 128], ident[:]),
                                  reads=[xo, ident], writes=[p])
                        em.op("act", lambda e: e.copy(ot[:, s, 0:512], pT[0][:]), reads=[pT[0]], writes=[ot])
                        em.op("dve", lambda e: e.tensor_copy(ot[:, s, 512:1024], pT[1][:]), reads=[pT[1]], writes=[ot])
                    em.dma("sp", y_out[sl, :].rearrange("(s p) f -> p s f", p=128), ot[:], reads=[ot])

    def moe_phase_dense(l, xsrc, ydst):
        TB = min(1024, T)
        NJ = TB // 512
        sc1 = mods[:, l, 32:40]
        sh = mods[:, l, 24:32]
        with em.phase():
            rw = em.sb([128, 8, 32], F32, "rw")
            em.dma("sp", rw[:], router_w[l].rearrange("(k p) e -> p k e", p=128), writes=[rw])
            rb = em.sb([128, 32], F32, "rb")
            em.dma("sp", rb[:], router_b[l:l + 1, :].to_broadcast([128, 32]), writes=[rb])
            P = [em.ps([128, 512], F32) for _ in range(7)]
            bup = em.sb([128, 16, 32], F32, "bup")
            bdn = em.sb([32, D], BF16, "bdn")
            em.dma("pool", bdn[:], moe_b_down[l], writes=[bdn])
            acc = em.sb([128, 8, TB], F32, "acc")
            hb = em.sb([128, 8, TB], BF16, "hb")
            gT = em.sb([32, TB], BF16, "gT")
            h32_r = em.rot(1, [128, 8, 512], F32, "mh32")
            xt_r = em.rot(2, [128, 512], F32, "mx")
            rowsT(bup[:], moe_b_up[l], 32, 16, P[0], bup, tmp=h32_r.bufs[0], tmp_ap=h32_r.bufs[0][:].rearrange("p c t -> p (c t)"))
            wu_r = em.rot(2, [128, 8, 2048], BF16, "wu")
            wd_r = em.rot(2, [128, 8, 1024], BF16, "wd")
            hg_r = em.rot(2, [128, 8, 512], BF16, "hg")
            gsb_r = em.rot(1, [128, 512], F32, "gsb")
            glu_r = em.rot(1, [128, 512], F32, "glu")
            sig_r = em.rot(1, [128, 512], F32, "sig")
            lin_r = em.rot(1, [128, 512], F32, "lin")
            t1_r = em.rot(1, [128, 512], F32, "t1")
            sm = {k: em.sb([128, 32], F32, "sm" + k) for k in ("lg", "mask", "e", "g")}
            m8 = em.sb([128, 8]); nm = em.sb([128, 1]); ssum = em.sb([128, 1]); rs = em.sb([128, 1])
            pup = Rot(P[0:4]); pdn = Rot(P[4:6]); pg = P[6]
            for tb in range(T // TB):
                for j in range(NJ):
                    h32 = h32_r.next()
                    ti = tb * NJ + j
                    load_mod_tile(xsrc, ti, xt_r, lambda c: h32[:, c, :], sc1, sh, h32)
                    em.op("act", lambda e: e.copy(hb[:, :, j * 512:(j + 1) * 512], h32[:]), reads=[h32], writes=[hb])
                    for s in range(4):
                        pl = P[4]
                        for c in range(8):
                            em.op("pe", lambda e: e.matmul(pl[:, 0:32], h32[:, c, s * 128:(s + 1) * 128], rw[:, c, :], start=(c == 0), stop=(c == 7)),
                                  reads=[h32, rw], writes=[pl])
                        lg, mask, ee, g = sm["lg"], sm["mask"], sm["e"], sm["g"]
                        em.op("dve", lambda e: e.tensor_tensor(lg[:], pl[:, 0:32], rb[:], ALU.add), reads=[pl, rb], writes=[lg])
                        em.op("dve", lambda e: e.max(m8[:], lg[:]), reads=[lg], writes=[m8])
                        em.op("dve", lambda e: e.tensor_scalar(mask[:], lg[:], m8[:, 3:4], None, ALU.is_ge), reads=[lg, m8], writes=[mask])
                        em.op("dve", lambda e: e.tensor_scalar_mul(nm[:], m8[:, 0:1], -1.0), reads=[m8], writes=[nm])
                        em.op("act", lambda e: e.activation(out=ee[:], in_=lg[:], func=AF.Exp, bias=nm[:], scale=1.0), reads=[lg, nm], writes=[ee])
                        em.op("dve", lambda e: e.tensor_tensor(ee[:], ee[:], mask[:], ALU.mult), reads=[ee, mask], writes=[ee])
                        em.op("dve", lambda e: e.reduce_sum(ssum[:], ee[:], axis=mybir.AxisListType.X), reads=[ee], writes=[ssum])
                        em.op("dve", lambda e: e.reciprocal(rs[:], ssum[:]), reads=[ssum], writes=[rs])
                        em.op("dve", lambda e: e.tensor_scalar_mul(g[:], ee[:], rs[:, 0:1]), reads=[ee, rs], writes=[g])
                        pt = P[5]
                        em.op("pe", lambda e: e.transpose(pt[0:32, 0:128], g[:], ident[:]), reads=[g, ident], writes=[pt])
                        em.op("act", lambda e: e.copy(gT[:, j * 512 + s * 128: j * 512 + (s + 1) * 128], pt[0:32, 0:128]), reads=[pt], writes=[gT])
                for j in range(NJ):
                    for oc in range(8):
                        po = pdn.next()
                        em.op("pe", lambda e: e.matmul(po[:], bdn[:, oc * 128:(oc + 1) * 128], gT[:, j * 512:(j + 1) * 512], start=True, stop=True),
                              reads=[bdn, gT], writes=[po])
                        em.op("act", lambda e: e.copy(acc[:, oc, j * 512:(j + 1) * 512], po[:]), reads=[po], writes=[acc])
                def issue_w(ex_):
                    wu_ = wu_r.next(); wd_ = wd_r.next()
                    em.dma("pool", wu_[:], moe_w_up[l, ex_].rearrange("(k p) f -> p k f", p=128), writes=[wu_])
                    em.dma("pool", wd_[:], moe_w_down[l, ex_].rearrange("(k p) f -> p k f", p=128), writes=[wd_])
                    return wu_, wd_
                nxt = issue_w(0)
                for ex in range(32):
                    wu, wd = nxt
                    if ex + 1 < 32:
                        nxt = issue_w(ex + 1)
                    for j in range(NJ):
                        js = slice(j * 512, (j + 1) * 512)
                        gsb = gsb_r.next()
                        em.op("pe", lambda e: e.matmul(pg[:], identb[0:32, ex:ex + 1].to_broadcast([32, 128]), gT[:, js], start=True, stop=True), reads=[identb, gT], writes=[pg])
                        em.op("act", lambda e: e.copy(gsb[:], pg[:]), reads=[pg], writes=[gsb])
                        hg = hg_r.next()
                        for c in range(8):
                            p1 = pup.next(); p2 = pup.next()
                            for k in range(8):
                                em.op("pe", lambda e: e.matmul(p1[:], wu[:, k, c * 128:(c + 1) * 128], hb[:, k, js], start=(k == 0), stop=(k == 7)),
                                      reads=[wu, hb], writes=[p1])
                            for k in range(8):
                                em.op("pe", lambda e: e.matmul(p2[:], wu[:, k, 1024 + c * 128:1024 + (c + 1) * 128], hb[:, k, js], start=(k == 0), stop=(k == 7)),
                                      reads=[wu, hb], writes=[p2])
                            glu = glu_r.next(); sig = sig_r.next(); lin = lin_r.next(); t1 = t1_r.next()
                            em.op("dve", lambda e: e.tensor_scalar(glu[:], p1[:], bup[:, c, ex:ex + 1], 7.0, ALU.add, ALU.min), reads=[p1, bup], writes=[glu])
                            em.op("act", lambda e: e.activation(out=sig[:], in_=glu[:], func=AF.Sigmoid, scale=1.702), reads=[glu], writes=[sig])
                            em.op("dve", lambda e: e.tensor_scalar(lin[:], p2[:], bup[:, 8 + c, ex:ex + 1], 7.0, ALU.add, ALU.min), reads=[p2, bup], writes=[lin])
                            em.op("pool", lambda e: e.tensor_scalar(lin[:], lin[:], -7.0, 1.0, ALU.max, ALU.add), reads=[lin], writes=[lin])
                            em.op("pool", lambda e: e.tensor_tensor(t1[:], glu[:], sig[:], ALU.mult), reads=[glu, sig], writes=[t1])
                            em.op("pool", lambda e: e.tensor_tensor(lin[:], lin[:], gsb[:], ALU.mult), reads=[lin, gsb], writes=[lin])
                            em.op("dve", lambda e: e.tensor_tensor(hg[:, c, :], t1[:], lin[:], ALU.mult), reads=[t1, lin], writes=[hg])
                        for oc in range(8):
                            po = pdn.next()
                            for k in range(8):
                                em.op("pe", lambda e: e.matmul(po[:], wd[:, k, oc * 128:(oc + 1) * 128], hg[:, k, :], start=(k == 0), stop=(k == 7)),
                                      reads=[wd, hg], writes=[po])
                            em.op("dve", lambda e: e.tensor_tensor(acc[:, oc, js], po[:], acc[:, oc, js], ALU.add), reads=[po, acc], writes=[acc])
                em.dma("sp", ydst[:, :, tb * TB:(tb + 1) * TB].rearrange("c p t -> p c t"), acc[:], reads=[acc])

    U32 = mybir.dt.uint32
    hs_zeroed = [False]

    def idma(kind, out, in_, idx_ap, reads=(), writes=(), bound=None):
        q = "pool"
        em._deps(q, reads, writes)
        pool = em.dma_pool[q]
        slot = em.dma_idx[q] % len(pool)
        k = pool[slot]
        em.dma_idx[q] += 1
        if 16 * (em.dma_uses[k] + 1) > SEM_LIMIT:
            k = em._new_dma_sem(q)
            pool[slot] = k
        if em.dma_uses[k] > 0:
            em._need(q, (k, 16 * em.dma_uses[k]))
        em.dma_uses[k] += 1
        off = bass.IndirectOffsetOnAxis(ap=idx_ap, axis=0)
        if kind == "g":
            ins = nc.gpsimd.indirect_dma_start(out=out, out_offset=None, in_=in_, in_offset=off)
        else:
            ins = nc.gpsimd.indirect_dma_start(out=out, out_offset=off, in_=in_, in_offset=None)
        ins.then_inc(em.sem[k], 16)
        em._record((k, 16 * em.dma_uses[k]), reads, writes)
        em.n_ins += 1

    def moe_phase(l, xsrc, ydst):
        sc1 = mods[:, l, 32:40]
        sh = mods[:, l, 24:32]
        NQ = NS * 4
        MAGIC = 12582912.0
        desti = em.sb([128, NS, 4], I32, "desti", persist=True) if not hasattr(em, "_moe_p") else em._moe_p[0]
        eidi = em.sb([128, NS, 4], I32, "eidi", persist=True) if not hasattr(em, "_moe_p") else em._moe_p[1]
        g4 = em.sb([128, NS, 4], F32, "g4", persist=True) if not hasattr(em, "_moe_p") else em._moe_p[2]
        widx = em.sb([128, NBLK, 8], I32, "widx", persist=True) if not hasattr(em, "_moe_p") else em._moe_p[3]
        blki = em.sb([128, NBLK], I32, "blki", persist=True) if not hasattr(em, "_moe_p") else em._moe_p[4]
        em._moe_p = (desti, eidi, g4, widx, blki)

        with em.phase():
            rw = em.sb([128, 8, 32], F32, "rw")
            em.dma("sp", rw[:], router_w[l].rearrange("(k p) e -> p k e", p=128), writes=[rw])
            rb = em.sb([128, 32], F32, "rb")
            em.dma("sp", rb[:], router_b[l:l + 1, :].to_broadcast([128, 32]), writes=[rb])
            stri = em.sb([128, 128], F32, "stri"); em.dma("sp", stri[:], k_stri, writes=[stri])
            iota = em.sb([128, 128], F32, "iota"); em.dma("sp", iota[:], k_iota, writes=[iota])
            base8 = em.sb([128, 8], F32, "base8"); em.dma("sp", base8[:], k_base8, writes=[base8])
            if not hs_zeroed[0]:
                zt = em.sb([128, 4, D], BF16, "zt")
                em.op("dve", lambda e: e.memset(zt[:], 0.0), writes=[zt])
                for k in range(NBLK):
                    em.dma("sp", hs_d[k * 512:(k + 1) * 512, :].rearrange("(j p) f -> p j f", p=128), zt[:], reads=[zt])
                hs_zeroed[0] = True
            P = [em.ps([128, 512], F32) for _ in range(3)]
            ptb = [em.ps([128, 1024], BF16) for _ in range(2)]
            h32_r = em.rot(1, [128, 8, 512], F32, "mh32")
            hbt_r = em.rot(1, [128, 8, 512], BF16, "mhb")
            xt_r = em.rot(2, [128, 512], F32, "mx")
            htok = [em.sb([128, D], BF16, "htok") for _ in range(NS)]
            eidf = em.sb([128, NS, 4], F32, "eidf")
            rks = em.sb([128, NS, 4], F32, "rks")
            carry = em.sb([128, 32], F32, "carry")
            em.op("dve", lambda e: e.memset(carry[:], 0.0), writes=[carry])
            lg = em.sb([128, 32]); mask = em.sb([128, 32]); rank = em.sb([128, 32])
            m8 = em.sb([128, 8]); mi = em.sb([128, 8], U32); nm = em.sb([128, 1]); e4 = em.sb([128, 4]); ssum = em.sb([128, 1]); rs = em.sb([128, 1])
            oh4 = em.sb([128, 4, 32]);
            for j in range(NT):
                h32 = h32_r.next(); hbt = hbt_r.next()
                load_mod_tile(xsrc, j, xt_r, lambda c: h32[:, c, :], sc1, sh, h32)
                em.op("act", lambda e: e.copy(hbt[:], h32[:]), reads=[h32], writes=[hbt])
                for s_ in range(4):
                    ti = j * 4 + s_
                    ss = slice(s_ * 128, (s_ + 1) * 128)
                    pl = P[0]
                    for c in range(8):
                        em.op("pe", lambda e: e.matmul(pl[:, 0:32], h32[:, c, ss], rw[:, c, :], start=(c == 0), stop=(c == 7)), reads=[h32, rw], writes=[pl])
                    em.op("dve", lambda e: e.tensor_tensor(lg[:], pl[:, 0:32], rb[:], ALU.add), reads=[pl, rb], writes=[lg])
                    em.op("dve", lambda e: e.max(m8[:], lg[:]), reads=[lg], writes=[m8])
                    em.op("dve", lambda e: e.max_index(mi[:], m8[:], lg[:]), reads=[lg, m8], writes=[mi])
                    em.op("dve", lambda e: e.tensor_copy(eidf[:, ti, :], mi[:, 0:4]), reads=[mi], writes=[eidf])
                    em.op("dve", lambda e: e.tensor_scalar(mask[:], lg[:], m8[:, 3:4], None, ALU.is_ge), reads=[lg, m8], writes=[mask])
                    em.op("dve", lambda e: e.tensor_scalar_mul(nm[:], m8[:, 0:1], -1.0), reads=[m8], writes=[nm])
                    em.op("act", lambda e: e.activation(out=e4[:], in_=m8[:, 0:4], func=AF.Exp, bias=nm[:], scale=1.0), reads=[m8, nm], writes=[e4])
                    em.op("dve", lambda e: e.reduce_sum(ssum[:], e4[:], axis=mybir.AxisListType.X), reads=[e4], writes=[ssum])
                    em.op("dve", lambda e: e.reciprocal(rs[:], ssum[:]), reads=[ssum], writes=[rs])
                    em.op("dve", lambda e: e.tensor_scalar_mul(g4[:, ti, :], e4[:], rs[:, 0:1]), reads=[e4, rs], writes=[g4])
                    pr = P[1]
                    em.op("pe", lambda e: e.matmul(pr[:, 0:32], stri[:], mask[:], start=True, stop=True), reads=[stri, mask], writes=[pr])
                    em.op("pe", lambda e: e.matmul(pr[:, 32:64], ones32[:], mask[:], start=True, stop=True), reads=[ones32, mask], writes=[pr])
                    em.op("dve", lambda e: e.tensor_tensor(rank[:], pr[:, 0:32], carry[:], ALU.add), reads=[pr, carry], writes=[rank])
                    em.op("dve", lambda e: e.tensor_tensor(carry[:], carry[:], pr[:, 32:64], ALU.add), reads=[pr, carry], writes=[carry])
                    em.op("dve", lambda e: e.tensor_tensor(oh4[:], iota[:, 0:32].unsqueeze(1).to_broadcast([128, 4, 32]),
                                                           eidf[:, ti, :].unsqueeze(2).to_broadcast([128, 4, 32]), ALU.is_equal), reads=[iota, eidf], writes=[oh4])
                    em.op("dve", lambda e: e.tensor_tensor(oh4[:], oh4[:], rank[:].unsqueeze(1).to_broadcast([128, 4, 32]), ALU.mult), reads=[oh4, rank], writes=[oh4])
                    em.op("dve", lambda e: e.reduce_sum(rks[:, ti, :], oh4[:], axis=mybir.AxisListType.X), reads=[oh4], writes=[rks])
                    pt = ptb[ti % 2]
                    for c in range(8):
                        em.op("pe", lambda e: e.transpose(pt[:, c * 128:(c + 1) * 128], hbt[:, c, ss], identb[:]), reads=[hbt, identb], writes=[pt])
                    em.op("act", lambda e: e.copy(htok[ti][:], pt[:]), reads=[pt], writes=[htok[ti]])
            cnt = carry
            nb = em.sb([128, 32]); pad = em.sb([128, 32]); ca = em.sb([128, 32]); cb2 = em.sb([128, 32]); poff = em.sb([128, 32])
            em.op("dve", lambda e: e.tensor_scalar(nb[:], cnt[:], 511.0, 1.0 / 512.0, ALU.add, ALU.mult), reads=[cnt], writes=[nb])
            em.op("dve", lambda e: e.tensor_scalar(nb[:], nb[:], -0.5 + 1.0 / 1024.0, MAGIC, ALU.add, ALU.add), reads=[nb], writes=[nb])
            em.op("dve", lambda e: e.tensor_scalar_add(nb[:], nb[:], -MAGIC), reads=[nb], writes=[nb])
            em.op("dve", lambda e: e.tensor_scalar_mul(pad[:], nb[:], 512.0), reads=[nb], writes=[pad])
            em.op("dve", lambda e: e.tensor_copy(ca[:], pad[:]), reads=[pad], writes=[ca])
            src_, dst_ = ca, cb2
            for shf in (1, 2, 4, 8, 16):
                em.op("dve", lambda e: e.tensor_copy(dst_[:, 0:shf], src_[:, 0:shf]), reads=[src_], writes=[dst_])
                em.op("dve", lambda e: e.tensor_tensor(dst_[:, shf:32], src_[:, shf:32], src_[:, 0:32 - shf], ALU.add), reads=[src_, dst_], writes=[dst_])
                src_, dst_ = dst_, src_
            cum = src_
            em.op("dve", lambda e: e.tensor_tensor(poff[:], cum[:], pad[:], ALU.subtract), reads=[cum, pad], writes=[poff])
            oh = em.sb([128, NQ, 32], F32, "ohall")
            pofft = em.sb([128, NQ], F32, "pofft")
            em.op("dve", lambda e: e.tensor_tensor(oh[:], iota[:, 0:32].unsqueeze(1).to_broadcast([128, NQ, 32]),
                                                   eidf[:].rearrange("p a b -> p (a b)").unsqueeze(2).to_broadcast([128, NQ, 32]), ALU.is_equal), reads=[iota, eidf], writes=[oh])
            em.op("dve", lambda e: e.tensor_tensor(oh[:], oh[:], poff[:].unsqueeze(1).to_broadcast([128, NQ, 32]), ALU.mult), reads=[oh, poff], writes=[oh])
            em.op("dve", lambda e: e.reduce_sum(pofft[:], oh[:], axis=mybir.AxisListType.X), reads=[oh], writes=[pofft])
            em.op("dve", lambda e: e.tensor_tensor(pofft[:], pofft[:], rks[:].rearrange("p a b -> p (a b)"), ALU.add), reads=[pofft, rks], writes=[pofft])
            em.op("dve", lambda e: e.tensor_copy(desti[:].rearrange("p a b -> p (a b)"), pofft[:]), reads=[pofft], writes=[desti])
            em.op("dve", lambda e: e.tensor_copy(eidi[:], eidf[:]), reads=[eidf], writes=[eidi])
            cmpk = em.sb([128, NBLK, 32], F32, "cmpk")
            kst = em.sb([128, NBLK], F32, "kst")
            blkf = em.sb([128, NBLK], F32, "blkf")
            wxf = em.sb([128, NBLK, 8], F32, "wxf")
            em.op("dve", lambda e: e.tensor_scalar_mul(kst[:], iota[:, 0:NBLK], 512.0), reads=[iota], writes=[kst])
            em.op("dve", lambda e: e.tensor_tensor(cmpk[:], cum[:].unsqueeze(1).to_broadcast([128, NBLK, 32]),
                                                   kst[:].unsqueeze(2).to_broadcast([128, NBLK, 32]), ALU.is_le), reads=[cum, kst], writes=[cmpk])
            em.op("dve", lambda e: e.reduce_sum(blkf[:], cmpk[:], axis=mybir.AxisListType.X), reads=[cmpk], writes=[blkf])
            em.op("dve", lambda e: e.tensor_scalar_min(blkf[:], blkf[:], 31.0), reads=[blkf], writes=[blkf])
            em.op("dve", lambda e: e.tensor_scalar_add(kst[:], blkf[:], 32.0 * l), reads=[blkf], writes=[kst])
            em.op("dve", lambda e: e.tensor_copy(blki[:], kst[:]), reads=[kst], writes=[blki])
            em.op("dve", lambda e: e.tensor_scalar(blkf[:], blkf[:], 1024.0, 32768.0 * l, ALU.mult, ALU.add), reads=[blkf], writes=[blkf])
            em.op("dve", lambda e: e.tensor_tensor(wxf[:], blkf[:].unsqueeze(2).to_broadcast([128, NBLK, 8]),
                                                   base8[:].unsqueeze(1).to_broadcast([128, NBLK, 8]), ALU.add), reads=[blkf, base8], writes=[wxf])
            em.op("dve", lambda e: e.tensor_copy(widx[:], wxf[:]), reads=[wxf], writes=[widx])
            for ti in range(NS):
                for sl_ in range(4):
                    idma("s", hs_d[:, :], htok[ti][:, :], desti[:, ti, sl_:sl_ + 1], reads=[htok[ti], desti])

        import os as _os
        if _os.environ.get("MOE_CUT") == "A":
            return
        wup_rows = moe_w_up.rearrange("l e r f -> (l e r) f")
        wdn_rows = moe_w_down.rearrange("l e r f -> (l e r) f")
        with em.phase():
            P = [em.ps([128, 512], F32) for _ in range(6)]
            pup = Rot(P[0:4]); pdn = Rot(P[4:6])
            ptb = em.ps([128, 512], BF16)
            hblk_r = em.rot(1, [128, 4, D], BF16, "hblk")
            hT_r = em.rot(2, [128, 8, 512], BF16, "hT")
            wu_r = em.rot(2, [128, 8, 2048], BF16, "wu")
            wd_r = em.rot(2, [128, 8, 1024], BF16, "wd")
            stgu_r = em.rot(2, [128, 2048], F32, "stgu")
            stgd_r = em.rot(2, [128, 1024], F32, "stgd")
            bu32_r = em.rot(1, [2, 3072], F32, "bu32")
            bub_r = em.rot(2, [1, 3072], BF16, "bub")
            hg_r = em.rot(1, [128, 8, 512], BF16, "hg")
            glu_r = em.rot(1, [128, 512], F32, "glu"); sig_r = em.rot(1, [128, 512], F32, "sig")
            lin_r = em.rot(1, [128, 512], F32, "lin"); t1_r = em.rot(1, [128, 512], F32, "t1")
            yrow_r = em.rot(2, [128, D], F32, "yrow")
            onesrow = em.sb([1, 512], BF16, "onesrow")
            em.op("dve", lambda e: e.memset(onesrow[:], 1.0), writes=[onesrow])

            def gather_piece(k, c):
                su = stgu_r.next(); sd = stgd_r.next()
                idma("g", su[:, :], wup_rows, widx[:, k, c:c + 1], reads=[widx], writes=[su])
                idma("g", sd[:, :], wdn_rows, widx[:, k, c:c + 1], reads=[widx], writes=[sd])
                return su, sd

            def cast_piece(wu_, wd_, c, su, sd):
                em.op("act", lambda e: e.copy(wu_[:, c, :], su[:]), reads=[su], writes=[wu_])
                em.op("dve", lambda e: e.tensor_copy(wd_[:, c, :], sd[:]), reads=[sd], writes=[wd_])

            def bias_row(k):
                b32 = bu32_r.next(); bb_ = bub_r.next()
                idma("g", b32[0:2, 0:2048], moe_b_up.rearrange("l e f -> (l e) f"), blki[0:2, k:k + 1], reads=[blki], writes=[b32])
                idma("g", b32[0:2, 2048:3072], moe_b_down.rearrange("l e f -> (l e) f"), blki[0:2, k:k + 1], reads=[blki], writes=[b32])
                em.op("act", lambda e: e.copy(bb_[0:1, :], b32[0:1, :]), reads=[b32], writes=[bb_])
                return bb_

            wu = wu_r.next(); wd = wd_r.next()
            for c in range(8):
                su, sd = gather_piece(0, c)
                cast_piece(wu, wd, c, su, sd)
            bb = bias_row(0)
            for k in range(NBLK):
                hblk = hblk_r.next(); hT = hT_r.next()
                em.dma("sp", hblk[:], hs_d[k * 512:(k + 1) * 512, :].rearrange("(j p) f -> p j f", p=128), writes=[hblk])
                more = k + 1 < NBLK
                if more:
                    wu_n = wu_r.next(); wd_n = wd_r.next()
                    bb_n = bias_row(k + 1)
                    pend = gather_piece(k + 1, 0)
                for c in range(8):
                    for j in range(4):
                        em.op("pe", lambda e: e.transpose(ptb[:, j * 128:(j + 1) * 128], hblk[:, j, c * 128:(c + 1) * 128], identb[:]), reads=[hblk, identb], writes=[ptb])
                    if c % 2:
                        em.op("act", lambda e: e.copy(hT[:, c, :], ptb[:]), reads=[ptb], writes=[hT])
                    else:
                        em.op("dve", lambda e: e.tensor_copy(hT[:, c, :], ptb[:]), reads=[ptb], writes=[hT])
                hg = hg_r.next()
                for c in range(8):
                    if more and c + 1 < 8:
                        nxt_piece = gather_piece(k + 1, c + 1)
                    p1 = pup.next(); p2 = pup.next()
                    for k8 in range(8):
                        em.op("pe", lambda e: e.matmul(p1[:], wu[:, k8, c * 128:(c + 1) * 128], hT[:, k8, :], start=(k8 == 0), stop=False), reads=[wu, hT], writes=[p1])
                    em.op("pe", lambda e: e.matmul(p1[:], bb[0:1, c * 128:(c + 1) * 128], onesrow[:], start=False, stop=True), reads=[bb, onesrow], writes=[p1])
                    for k8 in range(8):
                        em.op("pe", lambda e: e.matmul(p2[:], wu[:, k8, 1024 + c * 128:1024 + (c + 1) * 128], hT[:, k8, :], start=(k8 == 0), stop=False), reads=[wu, hT], writes=[p2])
                    em.op("pe", lambda e: e.matmul(p2[:], bb[0:1, 1024 + c * 128:1024 + (c + 1) * 128], onesrow[:], start=False, stop=True), reads=[bb, onesrow], writes=[p2])
                    glu = glu_r.next(); sig = sig_r.next(); lin = lin_r.next(); t1 = t1_r.next()
                    em.op("dve", lambda e: e.tensor_scalar_min(glu[:], p1[:], 7.0), reads=[p1], writes=[glu])
                    em.op("act", lambda e: e.activation(out=sig[:], in_=glu[:], func=AF.Sigmoid, scale=1.702), reads=[glu], writes=[sig])
                    em.op("dve", lambda e: e.tensor_scalar(lin[:], p2[:], 7.0, -7.0, ALU.min, ALU.max), reads=[p2], writes=[lin])
                    em.op("dve", lambda e: e.tensor_tensor(t1[:], glu[:], sig[:], ALU.mult), reads=[glu, sig], writes=[t1])
                    em.op("dve", lambda e: e.scalar_tensor_tensor(hg[:, c, :], lin[:], 1.0, t1[:], ALU.add, ALU.mult), reads=[lin, t1], writes=[hg])
                    if more:
                        cast_piece(wu_n, wd_n, c, *pend)
                        if c + 1 < 8:
                            pend = nxt_piece
                for j in range(4):
                    yrow = yrow_r.next()
                    for half in range(2):
                        po = pdn.next()
                        for k8 in range(8):
                            em.op("pe", lambda e: e.matmul(po[:], hg[:, k8, j * 128:(j + 1) * 128], wd[:, k8, half * 512:(half + 1) * 512], start=(k8 == 0), stop=False), reads=[hg, wd], writes=[po])
                        em.op("pe", lambda e: e.matmul(po[:], onesrow[0:1, 0:128], bb[0:1, 2048 + half * 512:2048 + (half + 1) * 512], start=False, stop=True), reads=[onesrow, bb], writes=[po])
                        if half:
                            em.op("act", lambda e: e.copy(yrow[:, 512:1024], po[:]), reads=[po], writes=[yrow])
                        else:
                            em.op("dve", lambda e: e.tensor_copy(yrow[:, 0:512], po[:]), reads=[po], writes=[yrow])
                    em.dma("sp", ys_d[k * 512 + j * 128:k * 512 + (j + 1) * 128, :], yrow[:], reads=[yrow])
                if more:
                    wu, wd, bb = wu_n, wd_n, bb_n

        if _os.environ.get("MOE_CUT") == "B":
            return
        with em.phase():
            pT = [em.ps([128, 512], F32) for _ in range(2)]
            yr_r = em.rot(3, [128, D], F32, "cyr")
            acc_r = em.rot(2, [128, D], F32, "cacc")
            yo_r = em.rot(2, [128, 8, 512], F32, "cyo")
            yo = None
            for ti in range(NS):
                acc = acc_r.next()
                for sl_ in range(4):
                    yr = yr_r.next()
                    idma("g", yr[:, :], ys_d[:, :], desti[:, ti, sl_:sl_ + 1], reads=[desti], writes=[yr])
                    if sl_ == 0:
                        em.op("dve", lambda e: e.tensor_scalar(acc[:], yr[:], g4[:, ti, 0:1], None, ALU.mult), reads=[yr, g4], writes=[acc])
                    else:
                        em.op("dve", lambda e: e.scalar_tensor_tensor(acc[:], yr[:], g4[:, ti, sl_:sl_ + 1], acc[:], ALU.mult, ALU.add), reads=[yr, g4, acc], writes=[acc])
                s_ = ti % 4
                if s_ == 0:
                    yo = yo_r.next()
                for c in range(8):
                    p = pT[c % 2]
                    em.op("pe", lambda e: e.transpose(p[:, 0:128], acc[:, c * 128:(c + 1) * 128], ident[:]), reads=[acc, ident], writes=[p])
                    if c % 2:
                        em.op("act", lambda e: e.copy(yo[:, c, s_ * 128:(s_ + 1) * 128], p[:, 0:128]), reads=[p], writes=[yo])
                    else:
                        em.op("dve", lambda e: e.tensor_copy(yo[:, c, s_ * 128:(s_ + 1) * 128], p[:, 0:128]), reads=[p], writes=[yo])
                if s_ == 3:
                    j = ti // 4
                    em.dma("sp", ydst[:, :, j * 512:(j + 1) * 512].rearrange("c p t -> p c t"), yo[:], reads=[yo])

    def mamba_phase(l, xsrc, ydst):
        sc1 = mods[:, l, 8:16]
        sh = mods[:, l, 0:8]
        inw = ssm_in_w[l]

        def load_hb(hb):
            xt_r = em.rot(3, [128, 512], F32, "mbx")
            for i in range(NT):
                load_mod_tile(xsrc, i, xt_r, lambda c: hb[:, c, i * 512:(i + 1) * 512], sc1, sh, hb)

        with em.phase():
            hb = em.sb([128, 8, T], BF16, "hb")
            load_hb(hb)
            wz = em.sb([128, 8, 2048], BF16, "wz")
            em.dma("pool", wz[:], inw[:, 0:2048].rearrange("(k p) f -> p k f", p=128), writes=[wz])
            wdt = em.sb([128, 8, 32], BF16, "wdt")
            em.dma("pool", wdt[:], inw[:, 5120:5152].rearrange("(k p) f -> p k f", p=128), writes=[wdt])
            dtb = em.sb([128, 32], F32, "dtb")
            em.dma("sp", dtb[:], ssm_dt_bias[l:l + 1, :].to_broadcast([128, 32]), writes=[dtb])
            pz = Rot([em.ps([128, 512], F32) for _ in range(4)])
            pd = em.ps([128, 32], F32)
            zs_r = em.rot(2, [128, 2048], BF16, "zs")
            d_r = {k: em.rot(2, [128, 32], F32, "d" + k) for k in ("x", "a", "e", "r")}
            for s in range(NS):
                ss = slice(s * 128, (s + 1) * 128)
                zs = zs_r.next()
                for q in range(4):
                    p = pz.next()
                    for k in range(8):
                        em.op("pe", lambda e: e.matmul(p[:], hb[:, k, ss], wz[:, k, q * 512:(q + 1) * 512], start=(k == 0), stop=(k == 7)),
                              reads=[hb, wz], writes=[p])
                    em.op("act", lambda e: e.activation(out=zs[:, q * 512:(q + 1) * 512], in_=p[:], func=AF.Silu), reads=[p], writes=[zs])
                em.dma("sp", zs_d[ss, :], zs[:], reads=[zs])
                for k in range(8):
                    em.op("pe", lambda e: e.matmul(pd[:], hb[:, k, ss], wdt[:, k, :], start=(k == 0), stop=(k == 7)), reads=[hb, wdt], writes=[pd])
                dx = d_r["x"].next(); da = d_r["a"].next(); de = d_r["e"].next(); dr = d_r["r"].next()
                em.op("dve", lambda e: e.tensor_tensor(dx[:], pd[:], dtb[:], ALU.add), reads=[pd, dtb], writes=[dx])
                em.op("dve", lambda e: e.tensor_scalar_mul(da[:], dx[:], -1.0), reads=[dx], writes=[da])
                em.op("dve", lambda e: e.tensor_tensor(da[:], da[:], dx[:], ALU.max), reads=[dx, da], writes=[da])
                em.op("act", lambda e: e.activation(out=de[:], in_=da[:], func=AF.Exp, scale=-1.0), reads=[da], writes=[de])
                em.op("act", lambda e: e.activation(out=de[:], in_=de[:], func=AF.Ln, bias=ones32[:, 0:1], scale=1.0), reads=[de, ones32], writes=[de])
                em.op("dve", lambda e: e.scalar_tensor_tensor(dr[:], dx[:], 0.0, de[:], ALU.max, ALU.add), reads=[dx, de], writes=[dr])
                em.dma("sp", dt_d[ss, :], dr[:], reads=[dr])

        with em.phase():
            hb = em.sb([128, 8, T], BF16, "hb")
            load_hb(hb)
            wx = em.sb([128, 8, 3072], BF16, "wx")
            em.dma("pool", wx[:], inw[:, 2048:5120].rearrange("(k p) f -> p k f", p=128), writes=[wx])
            pm = em.ps([128, 512], F32)
            cw = em.sb([128, 24, 4], F32, "cw")
            cbias = em.sb([128, 24, 1], F32, "cb")
            pp = Rot([em.ps([128, 512], F32) for _ in range(4)])
            xpad_r = em.rot(2, [128, T + 3], F32, "xpad")
            rowsT(cw[:], ssm_conv_w[l], 4, 24, pm, cw, tmp=xpad_r.bufs[0], tmp_ap=xpad_r.bufs[0][:])
            rowsT(cbias[:], ssm_conv_b[l:l + 1, :], 1, 24, pm, cbias, tmp=xpad_r.bufs[1], tmp_ap=xpad_r.bufs[1][:])
            acc_r = em.rot(2, [128, T], F32, "cacc")
            ob_r = em.rot(1, [128, T], BF16, "cob")
            for ch in range(24):
                xp = xpad_r.next(); ac = acc_r.next(); ob = ob_r.next()
                em.op("pool", lambda e: e.memset(xp[:, 0:3], 0.0), writes=[xp])
                for i in range(NT):
                    p = pp.next()
                    for k in range(8):
                        em.op("pe", lambda e: e.matmul(p[:], wx[:, k, ch * 128:(ch + 1) * 128], hb[:, k, i * 512:(i + 1) * 512], start=(k == 0), stop=(k == 7)),
                              reads=[wx, hb], writes=[p])
                    if i % 2:
                        em.op("act", lambda e: e.copy(xp[:, 3 + i * 512:3 + (i + 1) * 512], p[:]), reads=[p], writes=[xp])
                    else:
                        em.op("dve", lambda e: e.tensor_copy(xp[:, 3 + i * 512:3 + (i + 1) * 512], p[:]), reads=[p], writes=[xp])
                em.op("dve", lambda e: e.tensor_scalar(ac[:], xp[:, 0:T], cw[:, ch, 0:1], None, ALU.mult), reads=[xp, cw], writes=[ac])
                for j in range(1, 4):
                    eng = "dve"
                    em.op(eng, lambda e: e.scalar_tensor_tensor(ac[:], xp[:, j:j + T], cw[:, ch, j:j + 1], ac[:], ALU.mult, ALU.add),
                          reads=[xp, cw, ac], writes=[ac])
                em.op("act", lambda e: e.activation(out=ob[:], in_=ac[:], func=AF.Silu, bias=cbias[:, ch, :], scale=1.0), reads=[ac, cbias], writes=[ob])
                em.dma("sp", xbc_d[ch], ob[:], reads=[ob])

        with em.phase():
            tri = em.sb([128, 128], F32, "tri")
            em.dma("sp", tri[:], k_tri, writes=[tri])
            aneg = em.sb([128, 32], F32, "aneg")
            em.dma("sp", aneg[:], ssm_a_log[l:l + 1, :].to_broadcast([128, 32]), writes=[aneg])
            em.op("act", lambda e: e.activation(out=aneg[:], in_=aneg[:], func=AF.Exp), reads=[aneg], writes=[aneg])
            em.op("dve", lambda e: e.tensor_scalar_mul(aneg[:], aneg[:], -1.0), reads=[aneg], writes=[aneg])
            dsk = em.sb([128, 32], F32, "dsk")
            em.dma("sp", dsk[:], ssm_d[l:l + 1, :].to_broadcast([128, 32]), writes=[dsk])
            nw = em.sb([128, 2048], F32, "nw")
            em.dma("sp", nw[:], ssm_norm_w[l:l + 1, :].to_broadcast([128, 2048]), writes=[nw])
            wout = em.sb([128, 16, D], BF16, "wout")
            em.dma("pool", wout[:], ssm_out_w[l].rearrange("(k p) o -> p k o", p=128), writes=[wout])
            st32 = [em.sb([128, 8, 64], F32, "st32") for _ in range(4)]
            stb = [em.sb([128, 8, 64], BF16, "stb") for _ in range(4)]
            for g in range(4):
                em.op("dve", lambda e: e.memset(st32[g][:], 0.0), writes=[st32[g]])
                em.op("dve", lambda e: e.memset(stb[g][:], 0.0), writes=[stb[g]])
            ynT = em.sb([128, 16, 512], BF16, "ynT")
            ptb = em.ps([128, 512], BF16)
            pmisc = em.ps([128, 512], F32)
            par = em.ps([128, 1024], F32)
            py = em.ps([128, 512], F32)
            psn = em.ps([128, 512], F32)
            pout = em.ps([128, 512], F32)
            xsT_r = em.rot(2, [128, 16, 128], BF16, "xsT")
            bT_r = em.rot(2, [128, 4, 128], BF16, "bT")
            cT_r = em.rot(2, [128, 4, 128], BF16, "cT")
            zs_r = em.rot(2, [128, 2048], BF16, "zsl")
            dt_r = em.rot(2, [128, 32], F32, "dtl")
            xs_r = em.rot(2, [128, 32, 64], BF16, "xs")
            bt_r = em.rot(2, [128, 512], BF16, "btok")
            xdt_r = em.rot(1, [128, 32, 64], BF16, "xdt")
            xdtd_r = em.rot(1, [128, 32, 64], BF16, "xdtd")
            arow_r = em.rot(1, [128, 32, 128], F32, "arow")
            seg_r = em.rot(2, [128, 8, 128], F32, "seg")
            ear_r = em.rot(2, [128, 8, 128], F32, "ear")
            Mh_r = em.rot(2, [128, 8, 128], BF16, "Mh")
            Cs_r = em.rot(2, [128, 8, 128], BF16, "Cs")
            cbm_r = em.rot(2, [128, 128], F32, "cbm")
            yz = em.sb([128, 4, 512], F32, "yz")
            tt_r = em.rot(2, [128, 8, 64], F32, "tt")
            junk = em.sb([128, 512], F32, "junk")
            yn = em.sb([128, 2048], BF16, "yn")
            yo_r = em.rot(1, [128, 8, 512], F32, "yo")
            sA = {k: em.sb([128, 32], F32, "s" + k) for k in ("a", "acs", "tot", "d1", "dend", "cdec", "dtd")}
            ss4 = em.sb([128, 4], F32); rstd4 = em.sb([128, 4], F32)
            for c in range(NS):
                cs_ = slice(c * 128, (c + 1) * 128)
                xsT = xsT_r.next(); bT = bT_r.next(); cT = cT_r.next(); zs = zs_r.next(); dt = dt_r.next()
                em.dma("sp", xsT[:], xbc_d[0:16, :, cs_].rearrange("k p t -> p k t"), writes=[xsT])
                em.dma("sp", bT[:], xbc_d[16:20, :, cs_].rearrange("k p t -> p k t"), writes=[bT])
                em.dma("sp", cT[:], xbc_d[20:24, :, cs_].rearrange("k p t -> p k t"), writes=[cT])
                em.dma("sp", zs[:], zs_d[cs_, :], writes=[zs])
                em.dma("sp", dt[:], dt_d[cs_, :], writes=[dt])
                xs = xs_r.next(); btok = bt_r.next()
                xsf = xs[:].rearrange("p h d -> p (h d)")
                for q in range(4):
                    for k in range(4):
                        em.op("pe", lambda e: e.transpose(ptb[:, k * 128:(k + 1) * 128], xsT[:, q * 4 + k, :], identb[:]), reads=[xsT, identb], writes=[ptb])
                    em.op("act", lambda e: e.copy(xsf[:, q * 512:(q + 1) * 512], ptb[:]), reads=[ptb], writes=[xs])
                for g in range(4):
                    em.op("pe", lambda e: e.transpose(ptb[:, g * 128:(g + 1) * 128], bT[:, g, :], identb[:]), reads=[bT, identb], writes=[ptb])
                em.op("dve", lambda e: e.tensor_copy(btok[:], ptb[:]), reads=[ptb], writes=[btok])
                a, acs, tot, d1, dend, cdec, dtd = (sA[k] for k in ("a", "acs", "tot", "d1", "dend", "cdec", "dtd"))
                em.op("dve", lambda e: e.tensor_tensor(a[:], dt[:], aneg[:], ALU.mult), reads=[dt, aneg], writes=[a])
                em.op("pe", lambda e: e.matmul(pmisc[:, 0:32], tri[:], a[:], start=True, stop=True), reads=[tri, a], writes=[pmisc])
                em.op("pe", lambda e: e.matmul(pmisc[:, 32:64], ones32[:], a[:], start=True, stop=True), reads=[ones32, a], writes=[pmisc])
                em.op("dve", lambda e: e.tensor_copy(acs[:], pmisc[:, 0:32]), reads=[pmisc], writes=[acs])
                em.op("dve", lambda e: e.tensor_copy(tot[:], pmisc[:, 32:64]), reads=[pmisc], writes=[tot])
                em.op("dve", lambda e: e.tensor_tensor(d1[:], tot[:], acs[:], ALU.subtract), reads=[tot, acs], writes=[d1])
                em.op("act", lambda e: e.activation(out=dend[:], in_=d1[:], func=AF.Exp), reads=[d1], writes=[dend])
                em.op("act", lambda e: e.activation(out=cdec[:], in_=tot[:], func=AF.Exp), reads=[tot], writes=[cdec])
                em.op("dve", lambda e: e.tensor_tensor(dtd[:], dt[:], dend[:], ALU.mult), reads=[dt, dend], writes=[dtd])
                xdt = xdt_r.next(); xdtd = xdtd_r.next()
                em.op("pool", lambda e: e.tensor_tensor(xdt[:], xs[:], dt[:].unsqueeze(2).to_broadcast([128, 32, 64]), ALU.mult), reads=[xs, dt], writes=[xdt])
                em.op("dve", lambda e: e.tensor_tensor(xdtd[:], xs[:], dtd[:].unsqueeze(2).to_broadcast([128, 32, 64]), ALU.mult), reads=[xs, dtd], writes=[xdtd])
                arow = arow_r.next()
                em.op("pool", lambda e: e.tensor_tensor(arow[:], tri[:].unsqueeze(1).to_broadcast([128, 32, 128]),
                                                        a[:].unsqueeze(2).to_broadcast([128, 32, 128]), ALU.mult), reads=[tri, a], writes=[arow])
                for g in range(4):
                    for h2 in range(2):
                        em.op("pe", lambda e: e.matmul(par[:, h2 * 512:(h2 + 1) * 512], ones32[:],
                                                       arow[:, g * 8 + h2 * 4:g * 8 + h2 * 4 + 4, :].rearrange("p h l -> p (h l)"), start=True, stop=True),
                              reads=[ones32, arow], writes=[par])
                    em.op("pe", lambda e: e.matmul(pmisc[:, 128:256], bT[:, g, :], cT[:, g, :], start=True, stop=True), reads=[bT, cT], writes=[pmisc])
                    cbm = cbm_r.next()
                    em.op("dve", lambda e: e.tensor_tensor(cbm[:], pmisc[:, 128:256], tri[:], ALU.mult), reads=[pmisc, tri], writes=[cbm])
                    seg = seg_r.next(); ear = ear_r.next(); Mh = Mh_r.next(); Cs = Cs_r.next()
                    for hh in range(8):
                        h = g * 8 + hh
                        em.op("dve", lambda e: e.tensor_scalar(seg[:, hh, :], par[:, hh * 128:(hh + 1) * 128], acs[:, h:h + 1], 0.0, ALU.subtract, ALU.min),
                              reads=[par, acs], writes=[seg])
                    em.op("act", lambda e: e.activation(out=seg[:], in_=seg[:], func=AF.Exp), reads=[seg], writes=[seg])
                    em.op("pool", lambda e: e.tensor_tensor(Mh[:], seg[:], cbm[:].unsqueeze(1).to_broadcast([128, 8, 128]), ALU.mult), reads=[seg, cbm], writes=[Mh])
                    em.op("act", lambda e: e.activation(out=ear[:], in_=par[:].rearrange("p (h l) -> p h l", l=128), func=AF.Exp), reads=[par], writes=[ear])
                    em.op("dve", lambda e: e.tensor_tensor(Cs[:], ear[:], cT[:, g, :].unsqueeze(1).to_broadcast([128, 8, 128]), ALU.mult), reads=[ear, cT], writes=[Cs])
                    for hh in range(8):
                        h = g * 8 + hh
                        em.op("pe", lambda e: e.matmul(py[:, hh * 64:(hh + 1) * 64], Mh[:, hh, :], xdt[:, h, :], start=True, stop=False), reads=[Mh, xdt], writes=[py])
                        em.op("pe", lambda e: e.matmul(py[:, hh * 64:(hh + 1) * 64], Cs[:, hh, :], stb[g][:, hh, :], start=False, stop=True), reads=[Cs, stb[g]], writes=[py])
                    tt = tt_r.next()
                    em.op("pool", lambda e: e.tensor_tensor(tt[:], xs[:, g * 8:(g + 1) * 8, :], dsk[:, g * 8:(g + 1) * 8].unsqueeze(2).to_broadcast([128, 8, 64]), ALU.mult),
                          reads=[xs, dsk], writes=[tt])
                    em.op("dve", lambda e: e.tensor_tensor(yz[:, g, :], py[:], tt[:].rearrange("p h d -> p (h d)"), ALU.add), reads=[py, tt], writes=[yz])
                    em.op("pool", lambda e: e.tensor_tensor(yz[:, g, :], yz[:, g, :], zs[:, g * 512:(g + 1) * 512], ALU.mult), reads=[yz, zs], writes=[yz])
                    em.op("act", lambda e: e.activation(out=junk[:], in_=yz[:, g, :], func=AF.Square, accum_out=ss4[:, g:g + 1]), reads=[yz], writes=[junk, ss4])
                    em.op("pe", lambda e: e.matmul(psn[:], btok[:, g * 128:(g + 1) * 128], xdtd[:, g * 8:(g + 1) * 8, :].rearrange("p h d -> p (h d)"), start=True, stop=True),
                          reads=[btok, xdtd], writes=[psn])
                    em.op("pool", lambda e: e.tensor_tensor(st32[g][:], st32[g][:], cdec[:, g * 8:(g + 1) * 8].unsqueeze(2).to_broadcast([128, 8, 64]), ALU.mult),
                          reads=[st32[g], cdec], writes=[st32[g]])
                    em.op("dve", lambda e: e.tensor_tensor(st32[g][:], st32[g][:], psn[:].rearrange("p (h d) -> p h d", d=64), ALU.add), reads=[st32[g], psn], writes=[st32[g]])
                    em.op("act", lambda e: e.copy(stb[g][:], st32[g][:]), reads=[st32[g]], writes=[stb[g]])
                em.op("dve", lambda e: e.tensor_scalar(rstd4[:], ss4[:], 1.0 / 512.0, None, ALU.mult), reads=[ss4], writes=[rstd4])
                em.op("act", lambda e: e.activation(out=rstd4[:], in_=rstd4[:], func=AF.Sqrt, bias=eps5[:], scale=1.0), reads=[rstd4, eps5], writes=[rstd4])
                em.op("dve", lambda e: e.reciprocal(rstd4[:], rstd4[:]), reads=[rstd4], writes=[rstd4])
                for g in range(4):
                    eng = "dve"
                    em.op(eng, lambda e: e.scalar_tensor_tensor(yn[:, g * 512:(g + 1) * 512], yz[:, g, :], rstd4[:, g:g + 1], nw[:, g * 512:(g + 1) * 512], ALU.mult, ALU.mult),
                          reads=[yz, rstd4, nw], writes=[yn])
                c4 = c % 4
                for q in range(4):
                    for k in range(4):
                        em.op("pe", lambda e: e.transpose(ptb[:, k * 128:(k + 1) * 128], yn[:, (q * 4 + k) * 128:(q * 4 + k + 1) * 128], identb[:]), reads=[yn, identb], writes=[ptb])
                    em.op("act", lambda e: e.copy(ynT[:, q * 4:(q + 1) * 4, c4 * 128:(c4 + 1) * 128], ptb[:].rearrange("p (k t) -> p k t", t=128)), reads=[ptb], writes=[ynT])
                if c4 == 3:
                    yo = yo_r.next()
                    for oc in range(8):
                        for k in range(16):
                            em.op("pe", lambda e: e.matmul(pout[:], wout[:, k, oc * 128:(oc + 1) * 128], ynT[:, k, :], start=(k == 0), stop=(k == 15)), reads=[wout, ynT], writes=[pout])
                        em.op("dve", lambda e: e.tensor_copy(yo[:, oc, :], pout[:]), reads=[pout], writes=[yo])
                    i = c // 4
                    em.dma("sp", ydst[:, :, i * 512:(i + 1) * 512].rearrange("c p t -> p c t"), yo[:], reads=[yo])

    def rope_phase():
        with em.phase():
            rc = em.sb([64, 2], F32, "rc")
            em.dma("sp", rc[:], k_ropec, writes=[rc])
            posi = em.sb([64, T], I32, "posi")
            em.dma("sp", posi[:], pos_in[0:1, :].to_broadcast([64, T]), writes=[posi])
            ang = em.sb([64, T], F32, "ang")
            em.op("dve", lambda e: e.tensor_copy(ang[:], posi[:]), reads=[posi], writes=[ang])
            em.op("dve", lambda e: e.tensor_scalar(ang[:], ang[:], rc[:, 0:1], None, ALU.mult), reads=[ang, rc], writes=[ang])
            MAGIC = 12582912.0
            C1 = 6.28125
            C2 = 2.0 * math.pi - 6.28125
            u = em.sb([64, T], F32, "u"); kk = em.sb([64, T], F32, "kk"); r = em.sb([64, T], F32, "r")
            for which, shift in ((0, math.pi / 2.0), (1, 0.0)):
                em.op("dve", lambda e: e.tensor_scalar_add(u[:], ang[:], shift), reads=[ang], writes=[u])
                em.op("dve", lambda e: e.tensor_scalar(kk[:], u[:], 1.0 / (2.0 * math.pi), MAGIC, ALU.mult, ALU.add), reads=[u], writes=[kk])
                em.op("dve", lambda e: e.tensor_scalar_add(kk[:], kk[:], -MAGIC), reads=[kk], writes=[kk])
                em.op("dve", lambda e: e.scalar_tensor_tensor(r[:], kk[:], -C1, u[:], ALU.mult, ALU.add), reads=[kk, u], writes=[r])
                em.op("dve", lambda e: e.scalar_tensor_tensor(r[:], kk[:], -C2, r[:], ALU.mult, ALU.add), reads=[kk, r], writes=[r])
                em.op("dve", lambda e: e.tensor_scalar(r[:], r[:], math.pi, -math.pi, ALU.min, ALU.max), reads=[r], writes=[r])
                em.op("act", lambda e: e.activation(out=r[:], in_=r[:], func=AF.Sin), reads=[r], writes=[r])
                if which == 1:
                    em.op("dve", lambda e: e.tensor_scalar(r[:], r[:], rc[:, 1:2], None, ALU.mult), reads=[r, rc], writes=[r])
                em.dma("sp", rope_d[which], r[:], reads=[r])

    def kv_phase(xsrc):
        with em.phase():
            hb = em.sb([128, 8, T], BF16, "hb")
            xt_r = em.rot(3, [128, 512], F32, "kvx")
            for i in range(NT):
                load_mod_tile(xsrc, i, xt_r, lambda c: hb[:, c, i * 512:(i + 1) * 512], kvmod[:, 8:16], kvmod[:, 0:8], hb)
            kvw = em.sb([128, 8, 1536], BF16, "kvw")
            em.dma("pool", kvw[:], kv_w.rearrange("(k p) f -> p k f", p=128), writes=[kvw])
            kvws = em.sb([128, 8, 768], BF16, "kvws")
            em.dma("pool", kvws[:], kv_w_sw.rearrange("(k p) f -> p k f", p=128), writes=[kvws])
            cos_r = em.rot(2, [64, 512], F32, "cos"); sin_r = em.rot(2, [64, 512], F32, "sin")
            P = [em.ps([128, 512], F32) for _ in range(6)]
            pdr = Rot(P[0:2]); psr = Rot(P[2:4])
            w1 = []; w2 = []; biasT = []
            cp = em.sb([32, 64], F32, "cp")
            em.dma("sp", cp[:], cmp_pos, writes=[cp])
            em.op("pe", lambda e: e.transpose(P[4][0:64, 0:32], cp[0:32, 0:64], ident[0:32, 0:32]), reads=[cp, ident], writes=[P[4]])
            cposT = em.sb([64, 32], BF16, "cposT")
            em.op("dve", lambda e: e.tensor_copy(cposT[:], P[4][0:64, 0:32]), reads=[P[4]], writes=[cposT])
            for m in range(2):
                a = em.sb([64, 32, 256], BF16, "w1")
                em.dma("pool", a[:], phi_w1[m].rearrange("(j d) f -> d j f", d=64), writes=[a])
                b = em.sb([128, 2, 64], BF16, "w2")
                em.dma("pool", b[:], phi_w2[m].rearrange("(c p) d -> p c d", p=128), writes=[b])
                w1.append(a); w2.append(b)
                bt = em.sb([128, 2], F32, "biasT")
                for hc in range(2):
                    for j in range(32):
                        em.op("pe", lambda e: e.matmul(P[5][:, hc:hc + 1], a[:, j, hc * 128:(hc + 1) * 128], cposT[:, j:j + 1], start=(j == 0), stop=(j == 31)),
                              reads=[a, cposT], writes=[P[5]])
                em.op("dve", lambda e: e.tensor_copy(bt[:], P[5][:, 0:2]), reads=[P[5]], writes=[bt])
                biasT.append(bt)
            kt_r = em.rot(2, [64, T], BF16, "kt")
            t1_r = em.rot(2, [64, 512], F32, "kt1")
            t2_r = em.rot(2, [64, 512], F32, "kt2")
            hid_r = em.rot(2, [128, 2, 256], BF16, "hid")
            u_r = em.rot(2, [128, 255], F32, "gu")
            u2_r = em.rot(2, [128, 255], F32, "gu2")
            kc_r = em.rot(2, [64, 256], BF16, "kc")
            vc_r = em.rot(2, [128, 64], BF16, "vc")

            def compress(srcT, m, g):
                hid = hid_r.next()
                for hc in range(2):
                    ph = P[4]
                    for j in range(32):
                        em.op("pe", lambda e: e.matmul(ph[:, 0:255], w1[m][:, j, hc * 128:(hc + 1) * 128], srcT[:, j:j + 16 * 254 + 1:16], start=(j == 0), stop=(j == 31)),
                              reads=[w1[m], srcT], writes=[ph])
                    u = u_r.next(); u2 = u2_r.next()
                    em.op("act", lambda e: e.activation(out=u[:], in_=ph[:, 0:255], func=AF.Identity, bias=biasT[m][:, hc:hc + 1], scale=1.0), reads=[ph, biasT[m]], writes=[u])
                    em.op("pool", lambda e: e.tensor_tensor(u2[:], u[:], u[:], ALU.mult), reads=[u], writes=[u2])
                    em.op("dve", lambda e: e.tensor_scalar(u2[:], u2[:], 0.044715, 1.0, ALU.mult, ALU.add), reads=[u2], writes=[u2])
                    em.op("pool", lambda e: e.tensor_tensor(u2[:], u2[:], u[:], ALU.mult), reads=[u2, u], writes=[u2])
                    em.op("act", lambda e: e.activation(out=u2[:], in_=u2[:], func=AF.Sigmoid, scale=1.5957691216057308), reads=[u2], writes=[u2])
                    em.op("dve", lambda e: e.tensor_tensor(hid[:, hc, 0:255], u[:], u2[:], ALU.mult), reads=[u, u2], writes=[hid])
                if m == 0:
                    pk = P[5]
                    for hc in range(2):
                        em.op("pe", lambda e: e.matmul(pk[0:64, 0:255], w2[0][:, hc, :], hid[:, hc, 0:255], start=(hc == 0), stop=(hc == 1)), reads=[w2[0], hid], writes=[pk])
                    kc = kc_r.next()
                    em.op("dve", lambda e: e.memset(kc[:], 0.0), writes=[kc])
                    em.op("dve", lambda e: e.tensor_copy(kc[:, 0:255], pk[0:64, 0:255]), reads=[pk], writes=[kc])
                    em.dma("sp", KC_d[g], kc[:], reads=[kc])
                else:
                    for ncn in range(2):
                        nn = 128 if ncn == 0 else 127
                        pv = P[5]
                        for hc in range(2):
                            em.op("pe", lambda e: e.matmul(pv[0:nn, 0:64], hid[:, hc, ncn * 128:ncn * 128 + nn], w2[1][:, hc, :], start=(hc == 0), stop=(hc == 1)), reads=[hid, w2[1]], writes=[pv])
                        vc = vc_r.next()
                        em.op("dve", lambda e: e.memset(vc[:], 0.0), writes=[vc])
                        em.op("dve", lambda e: e.tensor_copy(vc[0:nn, :], pv[0:nn, 0:64]), reads=[pv], writes=[vc])
                        em.dma("sp", VC_d[g, ncn], vc[:], reads=[vc])

            for si, slot in enumerate((0, 2, 4)):
                for g in range(4):
                    kt = kt_r.next()
                    for i in range(NT):
                        sl = slice(i * 512, (i + 1) * 512)
                        pd = pdr.next(); psw = psr.next()
                        for k in range(8):
                            em.op("pe", lambda e: e.matmul(pd[0:64, :], kvw[:, k, slot * 256 + g * 64:slot * 256 + g * 64 + 64], hb[:, k, sl], start=(k == 0), stop=(k == 7)), reads=[kvw, hb], writes=[pd])
                        for k in range(8):
                            em.op("pe", lambda e: e.matmul(psw[0:64, :], kvws[:, k, si * 256 + g * 64:si * 256 + g * 64 + 64], hb[:, k, sl], start=(k == 0), stop=(k == 7)), reads=[kvws, hb], writes=[psw])
                        t1 = t1_r.next(); t2 = t2_r.next()
                        cos = cos_r.next(); sin = sin_r.next()
                        em.dma("sp", cos[:], rope_d[0, :, sl], writes=[cos])
                        em.dma("sp", sin[:], rope_d[1, :, sl], writes=[sin])
                        em.op("dve", lambda e: e.tensor_tensor(t1[:], pd[0:64, :], cos[:], ALU.mult), reads=[pd, cos], writes=[t1])
                        em.op("dve", lambda e: e.tensor_tensor(t2[:], psw[0:64, :], sin[:], ALU.mult), reads=[psw, sin], writes=[t2])
                        em.op("pool", lambda e: e.tensor_tensor(kt[:, sl], t1[:], t2[:], ALU.add), reads=[t1, t2], writes=[kt])
                    em.dma("sp", KT_d[si, g], kt[:], reads=[kt])
                    if slot == 0:
                        compress(kt, 0, g)
            for g in range(4):
                vt = kt_r.next()
                for i in range(NT):
                    sl = slice(i * 512, (i + 1) * 512)
                    pd = pdr.next()
                    for k in range(8):
                        em.op("pe", lambda e: e.matmul(pd[0:64, :], kvw[:, k, 256 + g * 64:256 + g * 64 + 64], hb[:, k, sl], start=(k == 0), stop=(k == 7)), reads=[kvw, hb], writes=[pd])
                    em.op("act", lambda e: e.copy(vt[:, sl], pd[0:64, :]), reads=[pd], writes=[vt])
                compress(vt, 1, g)
            vt_r = em.rot(2, [128, 512], BF16, "vtok")
            for s in range(NS):
                ss = slice(s * 128, (s + 1) * 128)
                pv = pdr.next()
                for half, slot in enumerate((3, 5)):
                    for k in range(8):
                        em.op("pe", lambda e: e.matmul(pv[:, half * 256:(half + 1) * 256], hb[:, k, ss], kvw[:, k, slot * 256:(slot + 1) * 256], start=(k == 0), stop=(k == 7)), reads=[hb, kvw], writes=[pv])
                vtk = vt_r.next()
                em.op("act", lambda e: e.copy(vtk[:], pv[:]), reads=[pv], writes=[vtk])
                em.dma("sp", VT_d[ss, :], vtk[:], reads=[vtk])

    def nsa_phase(l, xsrc, ydst):
        jl = l - 2
        sc1 = mods[:, l, 8:16]
        sh = mods[:, l, 0:8]
        with em.phase():
            eall = em.sb([64, 32, 128], BF16, "eall"); em.dma("sp", eall[:], k_eall, writes=[eall])
            cz = em.sb([128, 4, 512], BF16, "cz"); em.dma("sp", cz[:], k_cz, writes=[cz])
            wm = em.sb([128, 8, 512], BF16, "wm"); em.dma("sp", wm[:], k_wm, writes=[wm])
            ovaug = em.sb([128, 2, 65], F32, "ovaug"); em.dma("sp", ovaug[:], k_ovaug.rearrange("c p j -> p c j"), writes=[ovaug])
            qw = em.sb([128, 8, 1072], BF16, "qw"); em.dma("pool", qw[:], nsa_q_w[jl].rearrange("(k p) f -> p k f", p=128), writes=[qw])
            qws = em.sb([128, 8, 1024], BF16, "qws"); em.dma("pool", qws[:], nsa_q_w_sw[jl].rearrange("(k p) f -> p k f", p=128), writes=[qws])
            ow = em.sb([64, 16, D], BF16, "ow"); em.dma("pool", ow[:], nsa_o_w[jl].rearrange("(h d) o -> d h o", d=64), writes=[ow])
            KC = em.sb([64, 4, 256], BF16, "KC"); em.dma("sp", KC[:], KC_d.rearrange("g d n -> d g n"), writes=[KC])
            VC = em.sb([128, 4, 2, 64], BF16, "VC"); em.dma("sp", VC[:], VC_d.rearrange("g c p d -> p g c d"), writes=[VC])
            P = [em.ps([128, 512], F32) for _ in range(8)]
            pS_r = Rot(P[0:2]); pM = P[2]; pO = P[3]; pD = P[4]; pI = P[5]; pOut = P[6]; pQ = P[7]; pM_r = Rot([P[2], P[5]])
            xt_r = em.rot(2, [128, 512], F32, "ax")
            hbt_r = em.rot(1, [128, 8, 512], BF16, "ahb")
            cos_r = em.rot(1, [64, 512], F32, "acos"); sin_r = em.rot(1, [64, 512], F32, "asin")
            mc_r = em.rot(1, [128, 2, 512], F32, "amc")
            oT_r = em.rot(1, [64, 16, 512], BF16, "oT")
            KS_r = em.rot(1, [64, T], BF16, "KS"); VS_r = em.rot(1, [128, NS, 64], BF16, "VS")
            KW_r = em.rot(1, [64, 1024], BF16, "KW"); VW_r = em.rot(1, [128, 8, 64], BF16, "VW")
            qt_b = [em.sb([64, 512], BF16, "qt") for _ in range(4)]
            sig_r = em.rot(2, [64, 512], F32, "sig")
            gwr_b = [em.sb([128, 8, 3, 64], BF16, "gwr") for _ in range(4)]
            oc_b = [em.sb([64, 512], F32, "ocomb") for _ in range(4)]
            t1_r = em.rot(1, [64, 512], F32, "at1"); t2_r = em.rot(1, [64, 512], F32, "at2")
            e32_r = em.rot(1, [128, 512], F32, "e32")
            p32 = [em.sb([128, 512], F32, "p32") for _ in range(2)]
            pbc = [em.sb([128, 512], BF16, "pbc") for _ in range(2)]
            eb_r = em.rot(3, [128, 512], BF16, "eb"); pb_r = em.rot(4, [128, 512], BF16, "pb")
            rden_r = em.rot(2, [64, 512], F32, "rden")
            impg = em.sb([128, 4, 64], F32, "impg")
            rd_r = em.rot(2, [128, 1], F32, "rd")
            selc_r = em.rot(2, [128, 4, 64], F32, "selc")
            sc_r = em.rot(2, [128, 64], F32, "sc"); rep_r = em.rot(2, [128, 64], F32, "rep")
            m8a = em.sb([128, 8]); m8b = em.sb([128, 8])
            selT = em.sb([64, 512], BF16, "selT")
            yo_r = em.rot(2, [128, 512], F32, "ayo")

            cur_hbt = [None]

            def finish_branch(r, b, first):
                rden = rden_r.next(); sig = sig_r.next()
                for k in range(8):
                    em.op("pe", lambda e: e.matmul(pQ[0:64, :], gwr_b[r][:, k, b, :], cur_hbt[0][:, k, :], start=(k == 0), stop=(k == 7)), reads=[gwr_b[r], cur_hbt[0]], writes=[pQ])
                em.op("act", lambda e: e.activation(out=sig[:], in_=pQ[0:64, :], func=AF.Sigmoid), reads=[pQ], writes=[sig])
                em.op("dve", lambda e: e.tensor_scalar_max(rden[:], pD[0:64, :], TINY), reads=[pD], writes=[rden])
                em.op("dve", lambda e: e.reciprocal(rden[:], rden[:]), reads=[rden], writes=[rden])
                em.op("pool", lambda e: e.tensor_tensor(rden[:], rden[:], sig[:], ALU.mult), reads=[rden, sig], writes=[rden])
                if first:
                    em.op("dve", lambda e: e.tensor_tensor(oc_b[r][:], pO[0:64, :], rden[:], ALU.mult), reads=[pO, rden], writes=[oc_b[r]])
                else:
                    em.op("dve", lambda e: e.tensor_tensor(rden[:], pO[0:64, :], rden[:], ALU.mult), reads=[pO, rden], writes=[rden])
                    em.op("pool", lambda e: e.tensor_tensor(oc_b[r][:], oc_b[r][:], rden[:], ALU.add), reads=[oc_b[r], rden], writes=[oc_b[r]])

            for i in range(NT):
                sl = slice(i * 512, (i + 1) * 512)
                hbt = hbt_r.next()
                cur_hbt[0] = hbt
                for c in range(8):
                    xt = xt_r.next()
                    em.dma("sp", xt[:], xsrc[c, :, sl], writes=[xt])
                    eng = "dve" if c % 2 == 0 else "pool"
                    em.op(eng, lambda e: e.tensor_scalar(hbt[:, c, :], xt[:], sc1[:, c:c + 1], sh[:, c:c + 1], ALU.mult, ALU.add),
                          reads=[xt, mods], writes=[hbt])
                cos = cos_r.next(); sin = sin_r.next(); mc = mc_r.next()
                em.dma("sp", cos[:], rope_d[0, :, sl], writes=[cos])
                em.dma("sp", sin[:], rope_d[1, :, sl], writes=[sin])
                em.op("dve", lambda e: e.tensor_scalar_mul(cos[:], cos[:], ATTN_SCALE), reads=[cos], writes=[cos])
                em.op("dve", lambda e: e.tensor_scalar_mul(sin[:], sin[:], ATTN_SCALE), reads=[sin], writes=[sin])
                em.dma("sp", mc[:], k_mcmp[:, :, sl].rearrange("c p t -> p c t"), writes=[mc])
                oT = oT_r.next()
                nkt = 4 * (i + 1)
                w0 = max(0, 4 * i - 4)
                for g in range(4):
                    KS = KS_r.next(); VS = VS_r.next(); KW = KW_r.next(); VW = VW_r.next()
                    em.dma("sp", KS[:, 0:nkt * 128], KT_d[1, g, :, 0:nkt * 128], writes=[KS])
                    em.dma("sp", VS[:, 0:nkt, :], VT_d[0:nkt * 128, g * 64:(g + 1) * 64].rearrange("(k p) d -> p k d", p=128), writes=[VS])
                    nwk = nkt - w0
                    em.dma("sp", KW[:, 0:nwk * 128], KT_d[2, g, :, w0 * 128:nkt * 128], writes=[KW])
                    em.dma("sp", VW[:, 0:nwk, :], VT_d[w0 * 128:nkt * 128, 256 + g * 64:256 + (g + 1) * 64].rearrange("(k p) d -> p k d", p=128), writes=[VW])
                    for r in range(4):
                        h = g * 4 + r
                        pd = pS_r.next(); psw = pS_r.next()
                        for k in range(8):
                            em.op("pe", lambda e: e.matmul(pd[0:64, :], qw[:, k, h * 64:(h + 1) * 64], hbt[:, k, :], start=(k == 0), stop=(k == 7)), reads=[qw, hbt], writes=[pd])
                        for k in range(8):
                            em.op("pe", lambda e: e.matmul(psw[0:64, :], qws[:, k, h * 64:(h + 1) * 64], hbt[:, k, :], start=(k == 0), stop=(k == 7)), reads=[qws, hbt], writes=[psw])
                        t1 = t1_r.next(); t2 = t2_r.next()
                        em.op("dve", lambda e: e.tensor_tensor(t1[:], pd[0:64, :], cos[:], ALU.mult), reads=[pd, cos], writes=[t1])
                        em.op("dve", lambda e: e.tensor_tensor(t2[:], psw[0:64, :], sin[:], ALU.mult), reads=[psw, sin], writes=[t2])
                        em.op("pool", lambda e: e.tensor_tensor(qt_b[r][:], t1[:], t2[:], ALU.add), reads=[t1, t2], writes=[qt_b[r]])
                        gwr = gwr_b[r]
                        em.op("pool", lambda e: e.tensor_copy(gwr[:], qw[:, :, 1024 + h * 3:1024 + h * 3 + 3].unsqueeze(3).to_broadcast([128, 8, 3, 64])), reads=[qw], writes=[gwr])
                        for cn in range(2):
                            pS = pS_r.next()
                            em.op("pe", lambda e: e.matmul(pS[:], KC[:, g, cn * 128:(cn + 1) * 128], qt_b[r][:], start=True, stop=True), reads=[KC, qt_b[r]], writes=[pS])
                            e32 = e32_r.next()
                            em.op("act", lambda e: e.activation(out=e32[:], in_=pS[:], func=AF.Exp), reads=[pS], writes=[e32])
                            em.op("dve", lambda e: e.tensor_tensor(p32[cn][:], e32[:], mc[:, cn, :], ALU.mult), reads=[e32, mc], writes=[p32[cn]])
                            em.op("pool", lambda e: e.tensor_copy(pbc[cn][:], p32[cn][:]), reads=[p32[cn]], writes=[pbc[cn]])
                        for cn in range(2):
                            em.op("pe", lambda e: e.matmul(pO[0:64, :], VC[:, g, cn, :], pbc[cn][:], start=(cn == 0), stop=(cn == 1)), reads=[VC, pbc[cn]], writes=[pO])
                        for cn in range(2):
                            em.op("pe", lambda e: e.matmul(pD[0:64, :], onesb[:], pbc[cn][:], start=(cn == 0), stop=(cn == 1)), reads=[onesb, pbc[cn]], writes=[pD])
                        finish_branch(r, 0, True)
                        for s in range(4):
                            for cn in range(2):
                                em.op("pe", lambda e: e.matmul(pI[:, 0:65], p32[cn][:, s * 128:(s + 1) * 128], ovaug[:, cn, :], start=(cn == 0), stop=(cn == 1)), reads=[p32[cn], ovaug], writes=[pI])
                            rd = rd_r.next()
                            em.op("dve", lambda e: e.tensor_scalar_max(rd[:], pI[:, 64:65], TINY), reads=[pI], writes=[rd])
                            em.op("dve", lambda e: e.reciprocal(rd[:], rd[:]), reads=[rd], writes=[rd])
                            if r == 0:
                                em.op("dve", lambda e: e.tensor_scalar(impg[:, s, :], pI[:, 0:64], rd[:, 0:1], None, ALU.mult), reads=[pI, rd], writes=[impg])
                            else:
                                em.op("dve", lambda e: e.scalar_tensor_tensor(impg[:, s, :], pI[:, 0:64], rd[:, 0:1], impg[:, s, :], ALU.mult, ALU.add), reads=[pI, rd, impg], writes=[impg])
                    for s in range(4):
                        selc = selc_r.next()
                        t0 = i * 512 + s * 128
                        em.dma("sp", selc[:], k_selc[t0:t0 + 128], writes=[selc])
                        sc = sc_r.next(); rep = rep_r.next()
                        em.op("dve", lambda e: e.tensor_tensor(sc[:], impg[:, s, :], selc[:, 0, :], ALU.mult), reads=[impg, selc], writes=[sc])
                        em.op("dve", lambda e: e.tensor_tensor(sc[:], sc[:], selc[:, 1, :], ALU.add), reads=[sc, selc], writes=[sc])
                        em.op("dve", lambda e: e.tensor_tensor(sc[:], sc[:], selc[:, 2, :], ALU.mult), reads=[sc, selc], writes=[sc])
                        em.op("dve", lambda e: e.tensor_tensor(sc[:], sc[:], selc[:, 3, :], ALU.add), reads=[sc, selc], writes=[sc])
                        em.op("dve", lambda e: e.max(m8a[:], sc[:]), reads=[sc], writes=[m8a])
                        em.op("dve", lambda e: e.match_replace(rep[:], m8a[:], sc[:], -3e30), reads=[sc, m8a], writes=[rep])
                        em.op("dve", lambda e: e.max(m8b[:], rep[:]), reads=[rep], writes=[m8b])
                        em.op("dve", lambda e: e.tensor_scalar(rep[:], sc[:], m8b[:, 7:8], None, ALU.is_ge), reads=[sc, m8b], writes=[rep])
                        em.op("dve", lambda e: e.tensor_tensor(rep[:], rep[:], selc[:, 2, :], ALU.mult), reads=[rep, selc], writes=[rep])
                        em.op("pe", lambda e: e.transpose(pI[0:64, 128:256], rep[:], ident[:]), reads=[rep, ident], writes=[pI])
                        em.op("act", lambda e: e.copy(selT[:, s * 128:(s + 1) * 128], pI[0:64, 128:256]), reads=[pI], writes=[selT])
                    for r in range(4):
                        h = g * 4 + r

                        def slc_stage(kt):
                            pS = pS_r.next(); pM = pM_r.next()
                            em.op("pe", lambda e: e.matmul(pS[:], KS[:, kt * 128:(kt + 1) * 128], qt_b[r][:], start=True, stop=True), reads=[KS, qt_b[r]], writes=[pS])
                            em.op("pe", lambda e: e.matmul(pM[:], eall[:, kt, :], selT[:], start=True, stop=True), reads=[eall, selT], writes=[pM])
                            eb = eb_r.next(); pb = pb_r.next()
                            em.op("act", lambda e: e.activation(out=eb[:], in_=pS[:], func=AF.Exp), reads=[pS], writes=[eb])
                            em.op("dve", lambda e: e.tensor_tensor(pb[:], eb[:], pM[:], ALU.mult), reads=[eb, pM], writes=[pb])
                            if kt >= 4 * i:
                                em.op("pool", lambda e: e.tensor_tensor(pb[:], pb[:], cz[:, kt - 4 * i, :], ALU.mult), reads=[pb, cz], writes=[pb])
                            return pb

                        def win_stage(kw):
                            kt = w0 + kw
                            pS = pS_r.next()
                            em.op("pe", lambda e: e.matmul(pS[:], KW[:, kw * 128:(kw + 1) * 128], qt_b[r][:], start=True, stop=True), reads=[KW, qt_b[r]], writes=[pS])
                            eb = eb_r.next(); pb = pb_r.next()
                            em.op("act", lambda e: e.activation(out=eb[:], in_=pS[:], func=AF.Exp), reads=[pS], writes=[eb])
                            em.op("pool", lambda e: e.tensor_tensor(pb[:], eb[:], wm[:, 4 * i - kt + 3, :], ALU.mult), reads=[eb, wm], writes=[pb])
                            return pb

                        cur_pb = slc_stage(0)
                        for kt in range(nkt):
                            nxt_pb = slc_stage(kt + 1) if kt + 1 < nkt else None
                            pb = cur_pb
                            em.op("pe", lambda e: e.matmul(pO[0:64, :], VS[:, kt, :], pb[:], start=(kt == 0), stop=(kt == nkt - 1)), reads=[VS, pb], writes=[pO])
                            em.op("pe", lambda e: e.matmul(pD[0:64, :], onesb[:], pb[:], start=(kt == 0), stop=(kt == nkt - 1)), reads=[onesb, pb], writes=[pD])
                            cur_pb = nxt_pb
                        finish_branch(r, 1, False)
                        cur_pb = win_stage(0)
                        for kw in range(nwk):
                            nxt_pb = win_stage(kw + 1) if kw + 1 < nwk else None
                            pb = cur_pb
                            em.op("pe", lambda e: e.matmul(pO[0:64, :], VW[:, kw, :], pb[:], start=(kw == 0), stop=(kw == nwk - 1)), reads=[VW, pb], writes=[pO])
                            em.op("pe", lambda e: e.matmul(pD[0:64, :], onesb[:], pb[:], start=(kw == 0), stop=(kw == nwk - 1)), reads=[onesb, pb], writes=[pD])
                            cur_pb = nxt_pb
                        finish_branch(r, 2, False)
                        em.op("act", lambda e: e.copy(oT[:, h, :], oc_b[r][:]), reads=[oc_b[r]], writes=[oT])
                for oc in range(8):
                    yo = yo_r.next()
                    for h in range(16):
                        em.op("pe", lambda e: e.matmul(pOut[:], ow[:, h, oc * 128:(oc + 1) * 128], oT[:, h, :], start=(h == 0), stop=(h == 15)), reads=[ow, oT], writes=[pOut])
                    em.op("dve", lambda e: e.tensor_copy(yo[:], pOut[:]), reads=[pOut], writes=[yo])
                    em.dma("sp", ydst[oc, :, sl], yo[:], reads=[yo])

    cur = 0
    if on("rope"):
        rope_phase()
    for l in range(DEPTH):
        if on("mix%d" % l):
            if l < 2:
                mamba_phase(l, xs_d[cur], ymix_d)
            else:
                nsa_phase(l, xs_d[cur], ymix_d)
        if on("ln%da" % l):
            post_norm(xs_d[cur], ymix_d, xs_d[1 - cur], mods[:, l, 16:24], 2 * l, False)
        cur = 1 - cur
        if on("moe%d" % l):
            moe_phase(l, xs_d[cur], ymix_d)
        if on("ln%db" % l):
            post_norm(xs_d[cur], ymix_d, xs_d[1 - cur], mods[:, l, 40:48], 2 * l + 1, l == DEPTH - 1)
        cur = 1 - cur
        if l == 1 and on("kv"):
            kv_phase(xs_d[cur])
    em.barrier()
    em.close()
    return nc, em


def make_in_maps(inputs, T, cores):
    cst = host_consts(T)
    f = lambda a: np.ascontiguousarray(np.asarray(a, dtype=np.float32))
    shared = {
        "ada_w": f(inputs["ada_w"]), "ada_b": f(inputs["ada_b"]),
        "ln_g": f(inputs["ln_g"]).reshape(8, D), "ln_b": f(inputs["ln_b"]).reshape(8, D),
        "ssm_in_w": f(inputs["ssm_in_w"]), "ssm_conv_w": f(inputs["ssm_conv_w"]), "ssm_conv_b": f(inputs["ssm_conv_b"]),
        "ssm_dt_bias": f(inputs["ssm_dt_bias"]), "ssm_a_log": f(inputs["ssm_a_log"]), "ssm_d": f(inputs["ssm_d"]),
        "ssm_norm_w": f(inputs["ssm_norm_w"]), "ssm_out_w": f(inputs["ssm_out_w"]),
        "kv_ada_w": f(inputs["kv_ada_w"]), "kv_ada_b": f(inputs["kv_ada_b"]).reshape(1, 2 * D),
        "kv_w": f(inputs["kv_w"]),
        "cmp_pos": f(inputs["cmp_pos"]),
        "phi_k_w1": f(inputs["phi_k_w1"]), "phi_k_w2": f(inputs["phi_k_w2"]),
        "phi_v_w1": f(inputs["phi_v_w1"]), "phi_v_w2": f(inputs["phi_v_w2"]),
        "nsa_q_w": f(inputs["nsa_q_w"]), "nsa_o_w": f(inputs["nsa_o_w"]),
        "router_w": f(inputs["router_w"]), "router_b": f(inputs["router_b"]),
        "moe_w_up": f(inputs["moe_w_up"]), "moe_b_up": f(inputs["moe_b_up"]),
        "moe_w_down": f(inputs["moe_w_down"]), "moe_b_down": f(inputs["moe_b_down"]),
    }
    kvw = shared["kv_w"]
    shared["kv_w_sw"] = np.concatenate([swap_halves(kvw[:, s * 256:(s + 1) * 256], 256) for s in (0, 2, 4)], axis=1)
    shared["nsa_q_w_sw"] = np.stack([swap_halves(shared["nsa_q_w"][j], 1024) for j in range(2)], axis=0)
    for k, v in cst.items():
        shared["k_" + k] = v
    maps = []
    for b in cores:
        m = dict(shared)
        m["x"] = f(inputs["x"][b][:T])
        m["c"] = np.ascontiguousarray(f(inputs["c"][b]).reshape(8, 128).T)
        m["pos"] = np.ascontiguousarray(np.asarray(inputs["pos"][b][:T], dtype=np.int32).reshape(1, T))
        maps.append(m)
    return maps


_CACHE = {}


def kernel(**inputs):
    T = 4096
    if T not in _CACHE:
        _CACHE[T] = build(T)[0]
    nc = _CACHE[T]
    maps = make_in_maps(inputs, T, list(range(8)))
    res = run_bass_kernel_spmd(nc, maps, core_ids=list(range(8)))
    out = np.stack([np.asarray(r["y"], dtype=np.float32) for r in res.results], axis=0)
    return out
```

```python
import contextlib
import os as _os_cap
import math
import numpy as np
import ml_dtypes
import concourse.bass as bass
import concourse.mybir as mybir
from concourse.bass_utils import run_bass_kernel_spmd

F32 = mybir.dt.float32
BF16 = mybir.dt.bfloat16
I32 = mybir.dt.int32
AF = mybir.ActivationFunctionType
ALU = mybir.AluOpType

D = 1024
DEPTH = 4
ALPHA = (2.0 * DEPTH) ** 0.25
LN_EPS = 1e-5
EPS_A = LN_EPS / (ALPHA * ALPHA)
ATTN_SCALE = 0.125
TINY = 1e-30
SEM_LIMIT = 30000


class Buf:
    __slots__ = ("t", "name", "w", "rd")

    def __init__(self, t, name):
        self.t = t
        self.name = name
        self.w = None
        self.rd = {}

    def __getitem__(self, idx):
        return self.t[idx]


class Rot:
    def __init__(self, bufs):
        self.bufs = bufs
        self.i = 0

    def next(self):
        b = self.bufs[self.i % len(self.bufs)]
        self.i += 1
        return b


class Em:
    ENG = ("pe", "act", "dve", "pool", "sp")

    def __init__(self, nc, n_dma_sems=12, same_engine_sync=True):
        self.nc = nc
        self.es = contextlib.ExitStack()
        self.eng = {"pe": nc.tensor, "act": nc.scalar, "dve": nc.vector, "pool": nc.gpsimd, "sp": nc.sync}
        self.sem = {}
        self.cnt = {}
        self.cur = {}
        self.gen = {}
        self.owner = {}
        for e in self.ENG:
            self.gen[e] = 0
            self._new_sem(e)
        self.known = {e: {} for e in self.ENG}
        self.dma_pool = {}
        self.dma_idx = {}
        self.dma_uses = {}
        self.dma_gen = 0
        for q in ("sp", "pool", "act"):
            self.dma_pool[q] = []
            for i in range(int(_os_cap.environ.get("PCAP", "4")) if q == "pool" else n_dma_sems):
                self.dma_pool[q].append(self._new_dma_sem(q))
            self.dma_idx[q] = 0
        self.same = same_engine_sync
        self.phase_stack = None
        self.uid = 0
        self.n_wait = 0
        self.n_ins = 0

    def _new_sem(self, e):
        k = "%s_%d" % (e, self.gen[e])
        self.gen[e] += 1
        self.sem[k] = self.es.enter_context(self.nc.semaphore("s_" + k))
        self.cnt[k] = 0
        self.cur[e] = k
        self.owner[k] = e

    def _new_dma_sem(self, q):
        k = "d_%s_%d" % (q, self.dma_gen)
        self.dma_gen += 1
        self.sem[k] = self.es.enter_context(self.nc.semaphore(k))
        self.dma_uses[k] = 0
        self.owner[k] = "dma"
        return k

    def _stack(self, persist):
        return self.es if (persist or self.phase_stack is None) else self.phase_stack

    def sb(self, shape, dtype=F32, name=None, persist=False):
        self.uid += 1
        nm = "%s_%d" % (name or "t", self.uid)
        t = self._stack(persist).enter_context(self.nc.sbuf_tensor(nm, list(shape), dtype))
        return Buf(t, nm)

    def rot(self, n, shape, dtype=F32, name=None):
        return Rot([self.sb(shape, dtype, name) for _ in range(n)])

    def ps(self, shape, dtype=F32, name=None, persist=False):
        self.uid += 1
        nm = "%s_%d" % (name or "p", self.uid)
        t = self._stack(persist).enter_context(self.nc.psum_tensor(nm, list(shape), dtype))
        return Buf(t, nm)

    def dram(self, name, shape, dtype, kind="Internal"):
        t = self.nc.dram_tensor(name, list(shape), dtype, kind=kind)
        return t.ap()

    @contextlib.contextmanager
    def phase(self):
        assert self.phase_stack is None
        self.barrier()
        self.phase_stack = contextlib.ExitStack()
        try:
            with self.phase_stack:
                yield
                self.barrier()
        finally:
            self.phase_stack = None

    def _need(self, e, dep):
        if dep is None:
            return
        k, v = dep
        if self.owner[k] == e and (e == "pe" or (e != "pool" and not self.same)):
            return
        if self.known[e].get(k, 0) >= v:
            return
        self.eng[e].wait_ge(self.sem[k], v)
        self.known[e][k] = v
        self.n_wait += 1

    def _deps(self, e, reads, writes):
        for b in reads:
            self._need(e, b.w)
        for b in writes:
            self._need(e, b.w)
            for k, v in b.rd.items():
                self._need(e, (k, v))

    def _record(self, tok, reads, writes):
        k, v = tok
        for b in reads:
            if b.rd.get(k, 0) < v:
                b.rd[k] = v
        for b in writes:
            b.w = tok
            b.rd = {}

    def op(self, e, ins_fn, reads=(), writes=()):
        self._deps(e, reads, writes)
        ins = ins_fn(self.eng[e])
        k = self.cur[e]
        self.cnt[k] += 1
        ins.then_inc(self.sem[k], 1)
        self._record((k, self.cnt[k]), reads, writes)
        self.n_ins += 1
        if self.cnt[k] >= SEM_LIMIT:
            self._new_sem(e)
        return ins

    def dma(self, q, out, in_, reads=(), writes=(), **kw):
        self._deps(q, reads, writes)
        pool = self.dma_pool[q]
        slot = self.dma_idx[q] % len(pool)
        k = pool[slot]
        self.dma_idx[q] += 1
        if 16 * (self.dma_uses[k] + 1) > SEM_LIMIT:
            k = self._new_dma_sem(q)
            pool[slot] = k
        if self.dma_uses[k] > 0:
            self._need(q, (k, 16 * self.dma_uses[k]))
        self.dma_uses[k] += 1
        ins = self.eng[q].dma_start(out=out, in_=in_, **kw)
        ins.then_inc(self.sem[k], 16)
        self._record((k, 16 * self.dma_uses[k]), reads, writes)
        self.n_ins += 1
        return ins

    def barrier(self):
        for e in self.ENG:
            for k, c in self.cnt.items():
                if c > 0:
                    self._need(e, (k, c))
            for k, u in self.dma_uses.items():
                if u > 0:
                    self._need(e, (k, 16 * u))

    def close(self):
        self.es.close()


def host_consts(T):
    c = {}
    c["ident"] = np.eye(128, dtype=np.float32)
    s = np.arange(128)
    c["tri"] = (s[:, None] <= s[None, :]).astype(np.float32)
    d = np.arange(64)
    inv = (10000.0 ** (-(np.arange(32, dtype=np.float32)) / np.float32(32))).astype(np.float32)
    c["ropec"] = np.stack([inv[d % 32], np.where(d < 32, -1.0, 1.0).astype(np.float32)], axis=1).astype(np.float32)
    n = np.arange(256)
    t = np.arange(T)
    m = ((16 * n[:, None] + 31) <= t[None, :]) & (n[:, None] < T // 16 - 1)
    c["mcmp"] = m.reshape(2, 128, T).astype(np.float32)
    n_cmp = T // 16 - 1
    n_slc = T // 64
    cs = np.arange(256)[:, None] * 16
    js = np.arange(64)[None, :] * 64
    ov = np.maximum(np.minimum(cs + 32, js + 64) - np.maximum(cs, js), 0).astype(np.float32) / 32.0
    ov[n_cmp:, :] = 0.0
    ov[:, n_slc:] = 0.0
    c["ovaug"] = np.concatenate([ov, np.ones((256, 1), np.float32)], axis=1).reshape(2, 128, 65)
    tb = (t // 64)[:, None]
    j = np.arange(64)[None, :]
    forced = ((j == 0) | (j == tb) | (j == tb - 1)).astype(np.float32)
    cb = (j <= tb).astype(np.float32)
    c["selc"] = np.stack([1.0 - forced, forced * (1e9 + 1024.0 * j), cb, (cb - 1.0) * 1e30], axis=1).astype(np.float32)
    E = np.zeros((64, 32, 128), np.float32)
    for kt in range(32):
        E[2 * kt, kt, :64] = 1.0
        E[2 * kt + 1, kt, 64:] = 1.0
    c["eall"] = E.astype(ml_dtypes.bfloat16)
    mm = np.arange(128)[:, None, None]
    dd = np.arange(4)[None, :, None]
    nn = np.arange(512)[None, None, :]
    c["cz"] = ((128 * dd + mm) <= nn).astype(ml_dtypes.bfloat16)
    rr = np.arange(8)[None, :, None] - 3
    diff = 128 * rr + nn - mm
    c["wm"] = ((diff >= 0) & (diff < 512)).astype(ml_dtypes.bfloat16)
    c["stri"] = (s[:, None] < s[None, :]).astype(np.float32)
    c["iota"] = np.broadcast_to(np.arange(128, dtype=np.float32)[None, :], (128, 128)).copy()
    c["base8"] = (np.arange(8, dtype=np.float32)[None, :] * 128 + np.arange(128, dtype=np.float32)[:, None]).astype(np.float32)
    return c


def swap_halves(w, ncols):
    w = w[:, :ncols].reshape(w.shape[0], ncols // 64, 2, 32)
    return np.ascontiguousarray(w[:, :, ::-1, :].reshape(w.shape[0], ncols))


def build(T, stages=None, dbg=False):
    NT = T // 512
    NS = T // 128
    nc = bass.Bass("TRN2", target_bir_lowering=False)
    import os as _os0
    em = Em(nc, same_engine_sync=(_os0.environ.get('SES', '1') == '1'))
    on = lambda s: stages is None or s in stages

    em.declared = []
    any_moe = stages is None or any(st.startswith("moe") for st in stages)

    def din(name, shape, dt=F32):
        if name in ("moe_w_up", "moe_w_down") and not any_moe:
            return None
        em.declared.append(name)
        return em.dram(name, shape, dt, kind="ExternalInput")

    x_in = din("x", [T, D])
    c_in = din("c", [128, 8])
    pos_in = din("pos", [1, T], I32)
    ada_w = din("ada_w", [4, D, 6 * D])
    ada_b = din("ada_b", [4, 6 * D])
    ln_g = din("ln_g", [8, D])
    ln_b = din("ln_b", [8, D])
    ssm_in_w = din("ssm_in_w", [2, D, 5152])
    ssm_conv_w = din("ssm_conv_w", [2, 4, 3072])
    ssm_conv_b = din("ssm_conv_b", [2, 3072])
    ssm_dt_bias = din("ssm_dt_bias", [2, 32])
    ssm_a_log = din("ssm_a_log", [2, 32])
    ssm_d = din("ssm_d", [2, 32])
    ssm_norm_w = din("ssm_norm_w", [2, 2048])
    ssm_out_w = din("ssm_out_w", [2, 2048, D])
    kv_ada_w = din("kv_ada_w", [D, 2 * D])
    kv_ada_b = din("kv_ada_b", [1, 2 * D])
    kv_w = din("kv_w", [D, 1536])
    kv_w_sw = din("kv_w_sw", [D, 768])
    cmp_pos = din("cmp_pos", [32, 64])
    phi_w1 = [din("phi_k_w1", [2048, 256]), din("phi_v_w1", [2048, 256])]
    phi_w2 = [din("phi_k_w2", [256, 64]), din("phi_v_w2", [256, 64])]
    nsa_q_w = din("nsa_q_w", [2, D, 1072])
    nsa_q_w_sw = din("nsa_q_w_sw", [2, D, 1024])
    nsa_o_w = din("nsa_o_w", [2, D, D])
    router_w = din("router_w", [4, D, 32])
    router_b = din("router_b", [4, 32])
    moe_w_up = din("moe_w_up", [4, 32, D, 2 * D])
    moe_b_up = din("moe_b_up", [4, 32, 2 * D])
    moe_w_down = din("moe_w_down", [4, 32, D, D])
    moe_b_down = din("moe_b_down", [4, 32, D])
    k_ident = din("k_ident", [128, 128])
    k_tri = din("k_tri", [128, 128])
    k_ropec = din("k_ropec", [64, 2])
    k_mcmp = din("k_mcmp", [2, 128, T])
    k_ovaug = din("k_ovaug", [2, 128, 65])
    k_selc = din("k_selc", [T, 4, 64])
    k_eall = din("k_eall", [64, 32, 128], BF16)
    k_cz = din("k_cz", [128, 4, 512], BF16)
    k_wm = din("k_wm", [128, 8, 512], BF16)
    k_stri = din("k_stri", [128, 128])
    k_iota = din("k_iota", [128, 128])
    k_base8 = din("k_base8", [128, 8])

    y_out = em.dram("y", [T, D], F32, kind="ExternalOutput")
    sk = "ExternalOutput" if dbg else "Internal"
    xs_d = [em.dram("xA", [8, 128, T], F32, kind=sk), em.dram("xB", [8, 128, T], F32, kind=sk)]
    ymix_d = em.dram("ymix", [8, 128, T], F32, kind=sk)
    zs_d = em.dram("zs_tok", [T, 2048], BF16, kind=sk)
    dt_d = em.dram("dt_tok", [T, 32], F32, kind=sk)
    xbc_d = em.dram("xbcT", [24, 128, T], BF16, kind=sk)
    rope_d = em.dram("rope", [2, 64, T], F32, kind=sk)
    KT_d = em.dram("KT", [3, 4, 64, T], BF16, kind=sk)
    VT_d = em.dram("VT", [T, 512], BF16, kind=sk)
    KC_d = em.dram("KC", [4, 64, 256], BF16, kind=sk)
    VC_d = em.dram("VC", [4, 2, 128, 64], BF16, kind=sk)
    NBLK = (4 * T + 32 * 512) // 512
    RROWS = NBLK * 512
    hs_d = em.dram("h_sorted", [RROWS, D], BF16)
    ys_d = em.dram("y_sorted", [RROWS, D], F32)

    ident = em.sb([128, 128], F32, "ident", persist=True)
    identb = em.sb([128, 128], BF16, "identb", persist=True)
    ones32 = em.sb([128, 128], F32, "ones32", persist=True)
    onesb = em.sb([128, 64], BF16, "onesb", persist=True)
    mods = em.sb([128, 4, 48], F32, "mods", persist=True)
    kvmod = em.sb([128, 16], F32, "kvmod", persist=True)
    lng = em.sb([128, 8, 8], F32, "lng", persist=True)
    lnb = em.sb([128, 8, 8], F32, "lnb", persist=True)
    epsb = em.sb([128, 1], F32, "epsb", persist=True)
    eps5 = em.sb([128, 1], F32, "eps5", persist=True)

    em.dma("sp", ident[:], k_ident, writes=[ident])
    em.op("dve", lambda e: e.tensor_copy(identb[:], ident[:]), reads=[ident], writes=[identb])
    em.op("dve", lambda e: e.memset(ones32[:], 1.0), writes=[ones32])
    em.op("dve", lambda e: e.memset(onesb[:], 1.0), writes=[onesb])
    em.op("dve", lambda e: e.memset(epsb[:], EPS_A), writes=[epsb])
    em.op("dve", lambda e: e.memset(eps5[:], LN_EPS), writes=[eps5])

    def rowsT(dst_ap, src_rows_ap, R, C, pbuf, dstbuf, tmp=None, tmp_ap=None):
        if tmp is None:
            tmp = em.sb([R, C * 128], F32, "rowsT")
            tmp_ap = tmp[:]
        em.dma("sp", tmp_ap[0:R, 0:C * 128], src_rows_ap, writes=[tmp])
        for c in range(C):
            em.op("pe", lambda e: e.transpose(pbuf[:, c * R:(c + 1) * R], tmp_ap[0:R, c * 128:(c + 1) * 128], ident[0:R, 0:R]),
                  reads=[tmp, ident], writes=[pbuf])
        em.op("dve", lambda e: e.tensor_copy(dst_ap, pbuf[:, 0:C * R].rearrange("p (c r) -> p c r", r=R)),
              reads=[pbuf], writes=[dstbuf])

    if on("mod"):
        with em.phase():
            pm = em.ps([128, 512], F32)
            cT = em.sb([128, 8])
            cact = em.sb([128, 8])
            em.dma("sp", cT[:], c_in, writes=[cT])
            em.op("act", lambda e: e.activation(out=cact[:], in_=cT[:], func=AF.Silu), reads=[cT], writes=[cact])
            rowsT(lng[:], ln_g, 8, 8, pm, lng)
            rowsT(lnb[:], ln_b, 8, 8, pm, lnb)
            wrot = em.rot(2, [128, 8, 512], F32, "adaw")
            pmod = em.ps([128, 64], F32)
            bT = em.sb([128, 48, 4])
            rowsT(bT[:], ada_b, 4, 48, pm, bT)
            bTk = em.sb([128, 16, 1])
            rowsT(bTk[:], kv_ada_b, 1, 16, pm, bTk)
            for i in range(5):
                ncol = 48 if i < 4 else 16
                for cb in range(ncol // 4):
                    wk = wrot.next()
                    src = ada_w[i][:, cb * 512:(cb + 1) * 512] if i < 4 else kv_ada_w[:, cb * 512:(cb + 1) * 512]
                    em.dma("sp", wk[:], src.rearrange("(k p) f -> p k f", p=128), writes=[wk])
                    for o4 in range(4):
                        oc = cb * 4 + o4
                        for k in range(8):
                            em.op("pe", lambda e: e.matmul(pmod[:, oc:oc + 1], wk[:, k, o4 * 128:(o4 + 1) * 128], cact[:, k:k + 1],
                                                           start=(k == 0), stop=(k == 7)),
                                  reads=[wk, cact], writes=[pmod])
                dst = mods[:, i, :] if i < 4 else kvmod[:]
                dbuf = mods if i < 4 else kvmod
                bsl = bT[:, :, i] if i < 4 else bTk[:, :, 0]
                em.op("dve", lambda e: e.tensor_tensor(dst, pmod[:, 0:ncol], bsl, ALU.add),
                      reads=[pmod, bT, bTk], writes=[dbuf])
            for i in range(4):
                for c0 in (8, 32):
                    em.op("dve", lambda e: e.tensor_scalar_add(mods[:, i, c0:c0 + 8], mods[:, i, c0:c0 + 8], 1.0),
                          reads=[mods], writes=[mods])
                for c0 in (16, 40):
                    em.op("dve", lambda e: e.tensor_scalar(mods[:, i, c0:c0 + 8], mods[:, i, c0:c0 + 8], 1.0, 1.0 / ALPHA,
                                                           ALU.add, ALU.mult), reads=[mods], writes=[mods])
            em.op("dve", lambda e: e.tensor_scalar_add(kvmod[:, 8:16], kvmod[:, 8:16], 1.0), reads=[kvmod], writes=[kvmod])

    if dbg and on("mod"):
        dbg_mods = em.dram("dbg_mods", [128, 4, 48], F32, kind="ExternalOutput")
        em.dma("sp", dbg_mods, mods[:], reads=[mods])

    if on("in"):
        with em.phase():
            xin = em.rot(2, [128, 4, D], F32, "xin")
            xo = em.rot(2, [128, 8, 512], F32, "xo")
            pp = Rot([em.ps([128, 512], F32) for _ in range(4)])
            for i in range(NT):
                a = xin.next()
                em.dma("sp", a[:], x_in[i * 512:(i + 1) * 512, :].rearrange("(s p) f -> p s f", p=128), writes=[a])
                o = xo.next()
                for c in range(8):
                    p = pp.next()
                    for s in range(4):
                        em.op("pe", lambda e: e.transpose(p[:, s * 128:(s + 1) * 128], a[:, s, c * 128:(c + 1) * 128], ident[:]),
                              reads=[a, ident], writes=[p])
                    eng = "act" if c % 2 else "dve"
                    if eng == "act":
                        em.op("act", lambda e: e.copy(o[:, c, :], p[:]), reads=[p], writes=[o])
                    else:
                        em.op("dve", lambda e: e.tensor_copy(o[:, c, :], p[:]), reads=[p], writes=[o])
                em.dma("sp", xs_d[0][:, :, i * 512:(i + 1) * 512].rearrange("c p t -> p c t"), o[:], reads=[o])

    def load_mod_tile(xsrc, i, xt_r, dst, sc1, sh, dst_buf):
        for c in range(8):
            xt = xt_r.next()
            em.dma("sp", xt[:], xsrc[c, :, i * 512:(i + 1) * 512], writes=[xt])
            eng = "dve" if c % 2 == 0 else "pool"
            em.op(eng, lambda e: e.tensor_scalar(dst(c), xt[:], sc1[:, c:c + 1], sh[:, c:c + 1], ALU.mult, ALU.add),
                  reads=[xt, mods, kvmod], writes=[dst_buf])

    def post_norm(xsrc, ysrc, xdst, g1a, r, final):
        with em.phase():
            xt_r = em.rot(2, [128, 8, 512], F32, "lnx")
            yt_r = em.rot(2, [128, 8, 512], F32, "lny")
            z_r = em.rot(2, [128, 8, 512], F32, "lnz")
            sq_r = em.rot(1, [128, 8, 512], F32, "lnsq")
            xo_r = em.rot(2, [128, 8, 512], F32, "lno")
            psum_s = em.ps([128, 512], F32)
            psum_q = em.ps([128, 512], F32)
            mean = em.sb([128, 512]); msq = em.sb([128, 512]); var = em.sb([128, 512]); rstd = em.sb([128, 512])
            if final:
                pT = [em.ps([128, 512], F32), em.ps([128, 512], F32)]
                ot_r = em.rot(2, [128, 4, D], F32, "lnot")
            for i in range(NT):
                sl = slice(i * 512, (i + 1) * 512)
                xt = xt_r.next(); yt = yt_r.next(); z = z_r.next(); sq = sq_r.next(); xo = xo_r.next()
                em.dma("sp", xt[:], xsrc[:, :, sl].rearrange("c p t -> p c t"), writes=[xt])
                em.dma("sp", yt[:], ysrc[:, :, sl].rearrange("c p t -> p c t"), writes=[yt])
                for c in range(8):
                    eng = "dve"
                    em.op(eng, lambda e: e.scalar_tensor_tensor(z[:, c, :], yt[:, c, :], g1a[:, c:c + 1], xt[:, c, :], ALU.mult, ALU.add),
                          reads=[yt, xt, mods], writes=[z])
                em.op("act", lambda e: e.activation(out=sq[:], in_=z[:], func=AF.Square), reads=[z], writes=[sq])
                for c in range(8):
                    em.op("pe", lambda e: e.matmul(psum_s[:], ones32[:], z[:, c, :], start=(c == 0), stop=(c == 7)),
                          reads=[ones32, z], writes=[psum_s])
                for c in range(8):
                    em.op("pe", lambda e: e.matmul(psum_q[:], ones32[:], sq[:, c, :], start=(c == 0), stop=(c == 7)),
                          reads=[ones32, sq], writes=[psum_q])
                em.op("act", lambda e: e.mul(mean[:], psum_s[:], 1.0 / D), reads=[psum_s], writes=[mean])
                em.op("pool", lambda e: e.tensor_tensor(msq[:], mean[:], mean[:], ALU.mult), reads=[mean], writes=[msq])
                em.op("dve", lambda e: e.scalar_tensor_tensor(var[:], psum_q[:], 1.0 / D, msq[:], ALU.mult, ALU.subtract),
                      reads=[psum_q, msq], writes=[var])
                em.op("act", lambda e: e.activation(out=var[:], in_=var[:], func=AF.Sqrt, bias=epsb[:], scale=1.0),
                      reads=[var, epsb], writes=[var])
                em.op("dve", lambda e: e.reciprocal(rstd[:], var[:]), reads=[var], writes=[rstd])
                for c in range(8):
                    em.op("pool", lambda e: e.tensor_tensor(z[:, c, :], z[:, c, :], mean[:], ALU.subtract), reads=[z, mean], writes=[z])
                    em.op("dve", lambda e: e.tensor_tensor(z[:, c, :], z[:, c, :], rstd[:], ALU.mult), reads=[z, rstd], writes=[z])
                    em.op("act", lambda e: e.activation(out=xo[:, c, :], in_=z[:, c, :], func=AF.Identity,
                                                        bias=lnb[:, c, r:r + 1], scale=lng[:, c, r:r + 1]),
                          reads=[z, lng, lnb], writes=[xo])
                if not final:
                    em.dma("sp", xdst[:, :, sl].rearrange("c p t -> p c t"), xo[:], reads=[xo])
                else:
                    ot = ot_r.next()
                    for s in range(4):
                        for c in range(8):
                            p = pT[c // 4]
                            em.op("pe", lambda e: e.transpose(p[:, (c % 4) * 128:(c % 4 + 1) * 128], xo[:, c, s * 128:(s + 1) * 128], ident[:]),
                                  reads=[xo, ident], writes=[p])
                        em.op("act", lambda e: e.copy(ot[:, s, 0:512], pT[0][:]), reads=[pT[0]], writes=[ot])
                        em.op("dve", lambda e: e.tensor_copy(ot[:, s, 512:1024], pT[1][:]), reads=[pT[1]], writes=[ot])
                    em.dma("sp", y_out[sl, :].rearrange("(s p) f -> p s f", p=128), ot[:], reads=[ot])

    def moe_phase_dense(l, xsrc, ydst):
        TB = min(1024, T)
        NJ = TB // 512
        sc1 = mods[:, l, 32:40]
        sh = mods[:, l, 24:32]
        with em.phase():
            rw = em.sb([128, 8, 32], F32, "rw")
            em.dma("sp", rw[:], router_w[l].rearrange("(k p) e -> p k e", p=128), writes=[rw])
            rb = em.sb([128, 32], F32, "rb")
            em.dma("sp", rb[:], router_b[l:l + 1, :].to_broadcast([128, 32]), writes=[rb])
            P = [em.ps([128, 512], F32) for _ in range(7)]
            bup = em.sb([128, 16, 32], F32, "bup")
            bdn = em.sb([32, D], BF16, "bdn")
            em.dma("pool", bdn[:], moe_b_down[l], writes=[bdn])
            acc = em.sb([128, 8, TB], F32, "acc")
            hb = em.sb([128, 8, TB], BF16, "hb")
            gT = em.sb([32, TB], BF16, "gT")
            h32_r = em.rot(1, [128, 8, 512], F32, "mh32")
            xt_r = em.rot(2, [128, 512], F32, "mx")
            rowsT(bup[:], moe_b_up[l], 32, 16, P[0], bup, tmp=h32_r.bufs[0], tmp_ap=h32_r.bufs[0][:].rearrange("p c t -> p (c t)"))
            wu_r = em.rot(2, [128, 8, 2048], BF16, "wu")
            wd_r = em.rot(2, [128, 8, 1024], BF16, "wd")
            hg_r = em.rot(2, [128, 8, 512], BF16, "hg")
            gsb_r = em.rot(1, [128, 512], F32, "gsb")
            glu_r = em.rot(1, [128, 512], F32, "glu")
            sig_r = em.rot(1, [128, 512], F32, "sig")
            lin_r = em.rot(1, [128, 512], F32, "lin")
            t1_r = em.rot(1, [128, 512], F32, "t1")
            sm = {k: em.sb([128, 32], F32, "sm" + k) for k in ("lg", "mask", "e", "g")}
            m8 = em.sb([128, 8]); nm = em.sb([128, 1]); ssum = em.sb([128, 1]); rs = em.sb([128, 1])
            pup = Rot(P[0:4]); pdn = Rot(P[4:6]); pg = P[6]
            for tb in range(T // TB):
                for j in range(NJ):
                    h32 = h32_r.next()
                    ti = tb * NJ + j
                    load_mod_tile(xsrc, ti, xt_r, lambda c: h32[:, c, :], sc1, sh, h32)
                    em.op("act", lambda e: e.copy(hb[:, :, j * 512:(j + 1) * 512], h32[:]), reads=[h32], writes=[hb])
                    for s in range(4):
                        pl = P[4]
                        for c in range(8):
                            em.op("pe", lambda e: e.matmul(pl[:, 0:32], h32[:, c, s * 128:(s + 1) * 128], rw[:, c, :], start=(c == 0), stop=(c == 7)),
                                  reads=[h32, rw], writes=[pl])
                        lg, mask, ee, g = sm["lg"], sm["mask"], sm["e"], sm["g"]
                        em.op("dve", lambda e: e.tensor_tensor(lg[:], pl[:, 0:32], rb[:], ALU.add), reads=[pl, rb], writes=[lg])
                        em.op("dve", lambda e: e.max(m8[:], lg[:]), reads=[lg], writes=[m8])
                        em.op("dve", lambda e: e.tensor_scalar(mask[:], lg[:], m8[:, 3:4], None, ALU.is_ge), reads=[lg, m8], writes=[mask])
                        em.op("dve", lambda e: e.tensor_scalar_mul(nm[:], m8[:, 0:1], -1.0), reads=[m8], writes=[nm])
                        em.op("act", lambda e: e.activation(out=ee[:], in_=lg[:], func=AF.Exp, bias=nm[:], scale=1.0), reads=[lg, nm], writes=[ee])
                        em.op("dve", lambda e: e.tensor_tensor(ee[:], ee[:], mask[:], ALU.mult), reads=[ee, mask], writes=[ee])
                        em.op("dve", lambda e: e.reduce_sum(ssum[:], ee[:], axis=mybir.AxisListType.X), reads=[ee], writes=[ssum])
                        em.op("dve", lambda e: e.reciprocal(rs[:], ssum[:]), reads=[ssum], writes=[rs])
                        em.op("dve", lambda e: e.tensor_scalar_mul(g[:], ee[:], rs[:, 0:1]), reads=[ee, rs], writes=[g])
                        pt = P[5]
                        em.op("pe", lambda e: e.transpose(pt[0:32, 0:128], g[:], ident[:]), reads=[g, ident], writes=[pt])
                        em.op("act", lambda e: e.copy(gT[:, j * 512 + s * 128: j * 512 + (s + 1) * 128], pt[0:32, 0:128]), reads=[pt], writes=[gT])
                for j in range(NJ):
                    for oc in range(8):
                        po = pdn.next()
                        em.op("pe", lambda e: e.matmul(po[:], bdn[:, oc * 128:(oc + 1) * 128], gT[:, j * 512:(j + 1) * 512], start=True, stop=True),
                              reads=[bdn, gT], writes=[po])
                        em.op("act", lambda e: e.copy(acc[:, oc, j * 512:(j + 1) * 512], po[:]), reads=[po], writes=[acc])
                def issue_w(ex_):
                    wu_ = wu_r.next(); wd_ = wd_r.next()
                    em.dma("pool", wu_[:], moe_w_up[l, ex_].rearrange("(k p) f -> p k f", p=128), writes=[wu_])
                    em.dma("pool", wd_[:], moe_w_down[l, ex_].rearrange("(k p) f -> p k f", p=128), writes=[wd_])
                    return wu_, wd_
                nxt = issue_w(0)
                for ex in range(32):
                    wu, wd = nxt
                    if ex + 1 < 32:
                        nxt = issue_w(ex + 1)
                    for j in range(NJ):
                        js = slice(j * 512, (j + 1) * 512)
                        gsb = gsb_r.next()
                        em.op("pe", lambda e: e.matmul(pg[:], identb[0:32, ex:ex + 1].to_broadcast([32, 128]), gT[:, js], start=True, stop=True), reads=[identb, gT], writes=[pg])
                        em.op("act", lambda e: e.copy(gsb[:], pg[:]), reads=[pg], writes=[gsb])
                        hg = hg_r.next()
                        for c in range(8):
                            p1 = pup.next(); p2 = pup.next()
                            for k in range(8):
                                em.op("pe", lambda e: e.matmul(p1[:], wu[:, k, c * 128:(c + 1) * 128], hb[:, k, js], start=(k == 0), stop=(k == 7)),
                                      reads=[wu, hb], writes=[p1])
                            for k in range(8):
                                em.op("pe", lambda e: e.matmul(p2[:], wu[:, k, 1024 + c * 128:1024 + (c + 1) * 128], hb[:, k, js], start=(k == 0), stop=(k == 7)),
                                      reads=[wu, hb], writes=[p2])
                            glu = glu_r.next(); sig = sig_r.next(); lin = lin_r.next(); t1 = t1_r.next()
                            em.op("dve", lambda e: e.tensor_scalar(glu[:], p1[:], bup[:, c, ex:ex + 1], 7.0, ALU.add, ALU.min), reads=[p1, bup], writes=[glu])
                            em.op("act", lambda e: e.activation(out=sig[:], in_=glu[:], func=AF.Sigmoid, scale=1.702), reads=[glu], writes=[sig])
                            em.op("dve", lambda e: e.tensor_scalar(lin[:], p2[:], bup[:, 8 + c, ex:ex + 1], 7.0, ALU.add, ALU.min), reads=[p2, bup], writes=[lin])
                            em.op("pool", lambda e: e.tensor_scalar(lin[:], lin[:], -7.0, 1.0, ALU.max, ALU.add), reads=[lin], writes=[lin])
                            em.op("pool", lambda e: e.tensor_tensor(t1[:], glu[:], sig[:], ALU.mult), reads=[glu, sig], writes=[t1])
                            em.op("pool", lambda e: e.tensor_tensor(lin[:], lin[:], gsb[:], ALU.mult), reads=[lin, gsb], writes=[lin])
                            em.op("dve", lambda e: e.tensor_tensor(hg[:, c, :], t1[:], lin[:], ALU.mult), reads=[t1, lin], writes=[hg])
                        for oc in range(8):
                            po = pdn.next()
                            for k in range(8):
                                em.op("pe", lambda e: e.matmul(po[:], wd[:, k, oc * 128:(oc + 1) * 128], hg[:, k, :], start=(k == 0), stop=(k == 7)),
                                      reads=[wd, hg], writes=[po])
                            em.op("dve", lambda e: e.tensor_tensor(acc[:, oc, js], po[:], acc[:, oc, js], ALU.add), reads=[po, acc], writes=[acc])
                em.dma("sp", ydst[:, :, tb * TB:(tb + 1) * TB].rearrange("c p t -> p c t"), acc[:], reads=[acc])

    U32 = mybir.dt.uint32
    hs_zeroed = [False]

    def idma(kind, out, in_, idx_ap, reads=(), writes=(), bound=None):
        q = "pool"
        em._deps(q, reads, writes)
        pool = em.dma_pool[q]
        slot = em.dma_idx[q] % len(pool)
        k = pool[slot]
        em.dma_idx[q] += 1
        if 16 * (em.dma_uses[k] + 1) > SEM_LIMIT:
            k = em._new_dma_sem(q)
            pool[slot] = k
        if em.dma_uses[k] > 0:
            em._need(q, (k, 16 * em.dma_uses[k]))
        em.dma_uses[k] += 1
        off = bass.IndirectOffsetOnAxis(ap=idx_ap, axis=0)
        if kind == "g":
            ins = nc.gpsimd.indirect_dma_start(out=out, out_offset=None, in_=in_, in_offset=off)
        else:
            ins = nc.gpsimd.indirect_dma_start(out=out, out_offset=off, in_=in_, in_offset=None)
        ins.then_inc(em.sem[k], 16)
        em._record((k, 16 * em.dma_uses[k]), reads, writes)
        em.n_ins += 1

    def moe_phase(l, xsrc, ydst):
        sc1 = mods[:, l, 32:40]
        sh = mods[:, l, 24:32]
        NQ = NS * 4
        MAGIC = 12582912.0
        desti = em.sb([128, NS, 4], I32, "desti", persist=True) if not hasattr(em, "_moe_p") else em._moe_p[0]
        eidi = em.sb([128, NS, 4], I32, "eidi", persist=True) if not hasattr(em, "_moe_p") else em._moe_p[1]
        g4 = em.sb([128, NS, 4], F32, "g4", persist=True) if not hasattr(em, "_moe_p") else em._moe_p[2]
        widx = em.sb([128, NBLK, 8], I32, "widx", persist=True) if not hasattr(em, "_moe_p") else em._moe_p[3]
        blki = em.sb([128, NBLK], I32, "blki", persist=True) if not hasattr(em, "_moe_p") else em._moe_p[4]
        em._moe_p = (desti, eidi, g4, widx, blki)

        with em.phase():
            rw = em.sb([128, 8, 32], F32, "rw")
            em.dma("sp", rw[:], router_w[l].rearrange("(k p) e -> p k e", p=128), writes=[rw])
            rb = em.sb([128, 32], F32, "rb")
            em.dma("sp", rb[:], router_b[l:l + 1, :].to_broadcast([128, 32]), writes=[rb])
            stri = em.sb([128, 128], F32, "stri"); em.dma("sp", stri[:], k_stri, writes=[stri])
            iota = em.sb([128, 128], F32, "iota"); em.dma("sp", iota[:], k_iota, writes=[iota])
            base8 = em.sb([128, 8], F32, "base8"); em.dma("sp", base8[:], k_base8, writes=[base8])
            if not hs_zeroed[0]:
                zt = em.sb([128, 4, D], BF16, "zt")
                em.op("dve", lambda e: e.memset(zt[:], 0.0), writes=[zt])
                for k in range(NBLK):
                    em.dma("sp", hs_d[k * 512:(k + 1) * 512, :].rearrange("(j p) f -> p j f", p=128), zt[:], reads=[zt])
                hs_zeroed[0] = True
            P = [em.ps([128, 512], F32) for _ in range(3)]
            ptb = [em.ps([128, 1024], BF16) for _ in range(2)]
            h32_r = em.rot(1, [128, 8, 512], F32, "mh32")
            hbt_r = em.rot(1, [128, 8, 512], BF16, "mhb")
            xt_r = em.rot(2, [128, 512], F32, "mx")
            htok = [em.sb([128, D], BF16, "htok") for _ in range(NS)]
            eidf = em.sb([128, NS, 4], F32, "eidf")
            rks = em.sb([128, NS, 4], F32, "rks")
            carry = em.sb([128, 32], F32, "carry")
            em.op("dve", lambda e: e.memset(carry[:], 0.0), writes=[carry])
            lg = em.sb([128, 32]); mask = em.sb([128, 32]); rank = em.sb([128, 32])
            m8 = em.sb([128, 8]); mi = em.sb([128, 8], U32); nm = em.sb([128, 1]); e4 = em.sb([128, 4]); ssum = em.sb([128, 1]); rs = em.sb([128, 1])
            oh4 = em.sb([128, 4, 32]);
            for j in range(NT):
                h32 = h32_r.next(); hbt = hbt_r.next()
                load_mod_tile(xsrc, j, xt_r, lambda c: h32[:, c, :], sc1, sh, h32)
                em.op("act", lambda e: e.copy(hbt[:], h32[:]), reads=[h32], writes=[hbt])
                for s_ in range(4):
                    ti = j * 4 + s_
                    ss = slice(s_ * 128, (s_ + 1) * 128)
                    pl = P[0]
                    for c in range(8):
                        em.op("pe", lambda e: e.matmul(pl[:, 0:32], h32[:, c, ss], rw[:, c, :], start=(c == 0), stop=(c == 7)), reads=[h32, rw], writes=[pl])
                    em.op("dve", lambda e: e.tensor_tensor(lg[:], pl[:, 0:32], rb[:], ALU.add), reads=[pl, rb], writes=[lg])
                    em.op("dve", lambda e: e.max(m8[:], lg[:]), reads=[lg], writes=[m8])
                    em.op("dve", lambda e: e.max_index(mi[:], m8[:], lg[:]), reads=[lg, m8], writes=[mi])
                    em.op("dve", lambda e: e.tensor_copy(eidf[:, ti, :], mi[:, 0:4]), reads=[mi], writes=[eidf])
                    em.op("dve", lambda e: e.tensor_scalar(mask[:], lg[:], m8[:, 3:4], None, ALU.is_ge), reads=[lg, m8], writes=[mask])
                    em.op("dve", lambda e: e.tensor_scalar_mul(nm[:], m8[:, 0:1], -1.0), reads=[m8], writes=[nm])
                    em.op("act", lambda e: e.activation(out=e4[:], in_=m8[:, 0:4], func=AF.Exp, bias=nm[:], scale=1.0), reads=[m8, nm], writes=[e4])
                    em.op("dve", lambda e: e.reduce_sum(ssum[:], e4[:], axis=mybir.AxisListType.X), reads=[e4], writes=[ssum])
                    em.op("dve", lambda e: e.reciprocal(rs[:], ssum[:]), reads=[ssum], writes=[rs])
                    em.op("dve", lambda e: e.tensor_scalar_mul(g4[:, ti, :], e4[:], rs[:, 0:1]), reads=[e4, rs], writes=[g4])
                    pr = P[1]
                    em.op("pe", lambda e: e.matmul(pr[:, 0:32], stri[:], mask[:], start=True, stop=True), reads=[stri, mask], writes=[pr])
                    em.op("pe", lambda e: e.matmul(pr[:, 32:64], ones32[:], mask[:], start=True, stop=True), reads=[ones32, mask], writes=[pr])
                    em.op("dve", lambda e: e.tensor_tensor(rank[:], pr[:, 0:32], carry[:], ALU.add), reads=[pr, carry], writes=[rank])
                    em.op("dve", lambda e: e.tensor_tensor(carry[:], carry[:], pr[:, 32:64], ALU.add), reads=[pr, carry], writes=[carry])
                    em.op("dve", lambda e: e.tensor_tensor(oh4[:], iota[:, 0:32].unsqueeze(1).to_broadcast([128, 4, 32]),
                                                           eidf[:, ti, :].unsqueeze(2).to_broadcast([128, 4, 32]), ALU.is_equal), reads=[iota, eidf], writes=[oh4])
                    em.op("dve", lambda e: e.tensor_tensor(oh4[:], oh4[:], rank[:].unsqueeze(1).to_broadcast([128, 4, 32]), ALU.mult), reads=[oh4, rank], writes=[oh4])
                    em.op("dve", lambda e: e.reduce_sum(rks[:, ti, :], oh4[:], axis=mybir.AxisListType.X), reads=[oh4], writes=[rks])
                    pt = ptb[ti % 2]
                    for c in range(8):
                        em.op("pe", lambda e: e.transpose(pt[:, c * 128:(c + 1) * 128], hbt[:, c, ss], identb[:]), reads=[hbt, identb], writes=[pt])
                    em.op("act", lambda e: e.copy(htok[ti][:], pt[:]), reads=[pt], writes=[htok[ti]])
            cnt = carry
            nb = em.sb([128, 32]); pad = em.sb([128, 32]); ca = em.sb([128, 32]); cb2 = em.sb([128, 32]); poff = em.sb([128, 32])
            em.op("dve", lambda e: e.tensor_scalar(nb[:], cnt[:], 511.0, 1.0 / 512.0, ALU.add, ALU.mult), reads=[cnt], writes=[nb])
            em.op("dve", lambda e: e.tensor_scalar(nb[:], nb[:], -0.5 + 1.0 / 1024.0, MAGIC, ALU.add, ALU.add), reads=[nb], writes=[nb])
            em.op("dve", lambda e: e.tensor_scalar_add(nb[:], nb[:], -MAGIC), reads=[nb], writes=[nb])
            em.op("dve", lambda e: e.tensor_scalar_mul(pad[:], nb[:], 512.0), reads=[nb], writes=[pad])
            em.op("dve", lambda e: e.tensor_copy(ca[:], pad[:]), reads=[pad], writes=[ca])
            src_, dst_ = ca, cb2
            for shf in (1, 2, 4, 8, 16):
                em.op("dve", lambda e: e.tensor_copy(dst_[:, 0:shf], src_[:, 0:shf]), reads=[src_], writes=[dst_])
                em.op("dve", lambda e: e.tensor_tensor(dst_[:, shf:32], src_[:, shf:32], src_[:, 0:32 - shf], ALU.add), reads=[src_, dst_], writes=[dst_])
                src_, dst_ = dst_, src_
            cum = src_
            em.op("dve", lambda e: e.tensor_tensor(poff[:], cum[:], pad[:], ALU.subtract), reads=[cum, pad], writes=[poff])
            oh = em.sb([128, NQ, 32], F32, "ohall")
            pofft = em.sb([128, NQ], F32, "pofft")
            em.op("dve", lambda e: e.tensor_tensor(oh[:], iota[:, 0:32].unsqueeze(1).to_broadcast([128, NQ, 32]),
                                                   eidf[:].rearrange("p a b -> p (a b)").unsqueeze(2).to_broadcast([128, NQ, 32]), ALU.is_equal), reads=[iota, eidf], writes=[oh])
            em.op("dve", lambda e: e.tensor_tensor(oh[:], oh[:], poff[:].unsqueeze(1).to_broadcast([128, NQ, 32]), ALU.mult), reads=[oh, poff], writes=[oh])
            em.op("dve", lambda e: e.reduce_sum(pofft[:], oh[:], axis=mybir.AxisListType.X), reads=[oh], writes=[pofft])
            em.op("dve", lambda e: e.tensor_tensor(pofft[:], pofft[:], rks[:].rearrange("p a b -> p (a b)"), ALU.add), reads=[pofft, rks], writes=[pofft])
            em.op("dve", lambda e: e.tensor_copy(desti[:].rearrange("p a b -> p (a b)"), pofft[:]), reads=[pofft], writes=[desti])
            em.op("dve", lambda e: e.tensor_copy(eidi[:], eidf[:]), reads=[eidf], writes=[eidi])
            cmpk = em.sb([128, NBLK, 32], F32, "cmpk")
            kst = em.sb([128, NBLK], F32, "kst")
            blkf = em.sb([128, NBLK], F32, "blkf")
            wxf = em.sb([128, NBLK, 8], F32, "wxf")
            em.op("dve", lambda e: e.tensor_scalar_mul(kst[:], iota[:, 0:NBLK], 512.0), reads=[iota], writes=[kst])
            em.op("dve", lambda e: e.tensor_tensor(cmpk[:], cum[:].unsqueeze(1).to_broadcast([128, NBLK, 32]),
                                                   kst[:].unsqueeze(2).to_broadcast([128, NBLK, 32]), ALU.is_le), reads=[cum, kst], writes=[cmpk])
            em.op("dve", lambda e: e.reduce_sum(blkf[:], cmpk[:], axis=mybir.AxisListType.X), reads=[cmpk], writes=[blkf])
            em.op("dve", lambda e: e.tensor_scalar_min(blkf[:], blkf[:], 31.0), reads=[blkf], writes=[blkf])
            em.op("dve", lambda e: e.tensor_scalar_add(kst[:], blkf[:], 32.0 * l), reads=[blkf], writes=[kst])
            em.op("dve", lambda e: e.tensor_copy(blki[:], kst[:]), reads=[kst], writes=[blki])
            em.op("dve", lambda e: e.tensor_scalar(blkf[:], blkf[:], 1024.0, 32768.0 * l, ALU.mult, ALU.add), reads=[blkf], writes=[blkf])
            em.op("dve", lambda e: e.tensor_tensor(wxf[:], blkf[:].unsqueeze(2).to_broadcast([128, NBLK, 8]),
                                                   base8[:].unsqueeze(1).to_broadcast([128, NBLK, 8]), ALU.add), reads=[blkf, base8], writes=[wxf])
            em.op("dve", lambda e: e.tensor_copy(widx[:], wxf[:]), reads=[wxf], writes=[widx])
            for ti in range(NS):
                for sl_ in range(4):
                    idma("s", hs_d[:, :], htok[ti][:, :], desti[:, ti, sl_:sl_ + 1], reads=[htok[ti], desti])

        import os as _os
        if _os.environ.get("MOE_CUT") == "A":
            return
        wup_rows = moe_w_up.rearrange("l e r f -> (l e r) f")
        wdn_rows = moe_w_down.rearrange("l e r f -> (l e r) f")
        with em.phase():
            P = [em.ps([128, 512], F32) for _ in range(6)]
            pup = Rot(P[0:4]); pdn = Rot(P[4:6])
            ptb = em.ps([128, 512], BF16)
            hblk_r = em.rot(2, [128, 4, D], BF16, "hblk")
            hT_r = em.rot(2, [128, 8, 512], BF16, "hT")
            wu_r = em.rot(2, [128, 8, 2048], BF16, "wu")
            wd_r = em.rot(2, [128, 8, 1024], BF16, "wd")
            stgu_r = em.rot(2, [128, 2048], F32, "stgu")
            stgd_r = em.rot(2, [128, 1024], F32, "stgd")
            bub_r = em.rot(2, [2, 3072], BF16, "bub")
            hg_r = em.rot(1, [128, 8, 512], BF16, "hg")
            glu_r = em.rot(1, [128, 512], F32, "glu"); sig_r = em.rot(1, [128, 512], F32, "sig")
            lin_r = em.rot(1, [128, 512], F32, "lin"); t1_r = em.rot(1, [128, 512], F32, "t1")
            yrow_r = em.rot(2, [128, D], F32, "yrow")
            onesrow = em.sb([1, 512], BF16, "onesrow")
            em.op("dve", lambda e: e.memset(onesrow[:], 1.0), writes=[onesrow])

            def gather_piece(k, c):
                su = stgu_r.next(); sd = stgd_r.next()
                if _os_cap.environ.get("MOE_NOGATHER") and k > 0:
                    return su, sd
                idma("g", su[:, :], wup_rows, widx[:, k, c:c + 1], reads=[widx], writes=[su])
                idma("g", sd[:, :], wdn_rows, widx[:, k, c:c + 1], reads=[widx], writes=[sd])
                return su, sd

            def cast_piece(wu_, wd_, c, su, sd):
                em.op("act", lambda e: e.copy(wu_[:, c, :], su[:]), reads=[su], writes=[wu_])
                em.op("dve", lambda e: e.tensor_copy(wd_[:, c, :], sd[:]), reads=[sd], writes=[wd_])

            def bias_row(k):
                bb_ = bub_r.next()
                idma("g", bb_[0:2, 0:2048], moe_b_up.rearrange("l e f -> (l e) f"), blki[0:2, k:k + 1], reads=[blki], writes=[bb_])
                idma("g", bb_[0:2, 2048:3072], moe_b_down.rearrange("l e f -> (l e) f"), blki[0:2, k:k + 1], reads=[blki], writes=[bb_])
                return bb_

            wu = wu_r.next(); wd = wd_r.next()
            for c in range(8):
                su, sd = gather_piece(0, c)
                cast_piece(wu, wd, c, su, sd)
            bb = bias_row(0)
            def load_hblk(k_):
                hb_ = hblk_r.next()
                em.dma("sp", hb_[:], hs_d[k_ * 512:(k_ + 1) * 512, :].rearrange("(j p) f -> p j f", p=128), writes=[hb_])
                return hb_
            hblk_n = load_hblk(0)
            for k in range(NBLK):
                hblk = hblk_n; hT = hT_r.next()
                more = k + 1 < NBLK
                if more:
                    hblk_n = load_hblk(k + 1)
                if more:
                    wu_n = wu_r.next(); wd_n = wd_r.next()
                    bb_n = bias_row(k + 1)
                    pend = gather_piece(k + 1, 0)
                for c in range(8):
                    for j in range(4):
                        em.op("pe", lambda e: e.transpose(ptb[:, j * 128:(j + 1) * 128], hblk[:, j, c * 128:(c + 1) * 128], identb[:]), reads=[hblk, identb], writes=[ptb])
                    if c % 2:
                        em.op("act", lambda e: e.copy(hT[:, c, :], ptb[:]), reads=[ptb], writes=[hT])
                    else:
                        em.op("dve", lambda e: e.tensor_copy(hT[:, c, :], ptb[:]), reads=[ptb], writes=[hT])
                hg = hg_r.next()
                for c in range(8):
                    if more and c + 1 < 8:
                        nxt_piece = gather_piece(k + 1, c + 1)
                    p1 = pup.next(); p2 = pup.next()
                    for k8 in range(8):
                        em.op("pe", lambda e: e.matmul(p1[:], wu[:, k8, c * 128:(c + 1) * 128], hT[:, k8, :], start=(k8 == 0), stop=False), reads=[wu, hT], writes=[p1])
                    em.op("pe", lambda e: e.matmul(p1[:], bb[0:1, c * 128:(c + 1) * 128], onesrow[:], start=False, stop=True), reads=[bb, onesrow], writes=[p1])
                    for k8 in range(8):
                        em.op("pe", lambda e: e.matmul(p2[:], wu[:, k8, 1024 + c * 128:1024 + (c + 1) * 128], hT[:, k8, :], start=(k8 == 0), stop=False), reads=[wu, hT], writes=[p2])
                    em.op("pe", lambda e: e.matmul(p2[:], bb[0:1, 1024 + c * 128:1024 + (c + 1) * 128], onesrow[:], start=False, stop=True), reads=[bb, onesrow], writes=[p2])
                    glu = glu_r.next(); sig = sig_r.next(); lin = lin_r.next(); t1 = t1_r.next()
                    em.op("dve", lambda e: e.tensor_scalar_min(glu[:], p1[:], 7.0), reads=[p1], writes=[glu])
                    em.op("act", lambda e: e.activation(out=sig[:], in_=glu[:], func=AF.Sigmoid, scale=1.702), reads=[glu], writes=[sig])
                    em.op("dve", lambda e: e.tensor_scalar(lin[:], p2[:], 7.0, -7.0, ALU.min, ALU.max), reads=[p2], writes=[lin])
                    em.op("dve", lambda e: e.tensor_tensor(t1[:], glu[:], sig[:], ALU.mult), reads=[glu, sig], writes=[t1])
                    em.op("dve", lambda e: e.scalar_tensor_tensor(hg[:, c, :], lin[:], 1.0, t1[:], ALU.add, ALU.mult), reads=[lin, t1], writes=[hg])
                    if more:
                        cast_piece(wu_n, wd_n, c, *pend)
                        if c + 1 < 8:
                            pend = nxt_piece
                for j in range(4):
                    yrow = yrow_r.next()
                    for half in range(2):
                        po = pdn.next()
                        for k8 in range(8):
                            em.op("pe", lambda e: e.matmul(po[:], hg[:, k8, j * 128:(j + 1) * 128], wd[:, k8, half * 512:(half + 1) * 512], start=(k8 == 0), stop=False), reads=[hg, wd], writes=[po])
                        em.op("pe", lambda e: e.matmul(po[:], onesrow[0:1, 0:128], bb[0:1, 2048 + half * 512:2048 + (half + 1) * 512], start=False, stop=True), reads=[onesrow, bb], writes=[po])
                        if half:
                            em.op("act", lambda e: e.copy(yrow[:, 512:1024], po[:]), reads=[po], writes=[yrow])
                        else:
                            em.op("dve", lambda e: e.tensor_copy(yrow[:, 0:512], po[:]), reads=[po], writes=[yrow])
                    em.dma("sp", ys_d[k * 512 + j * 128:k * 512 + (j + 1) * 128, :], yrow[:], reads=[yrow])
                if more:
                    wu, wd, bb = wu_n, wd_n, bb_n

        if _os.environ.get("MOE_CUT") == "B":
            return
        with em.phase():
            pT = [em.ps([128, 512], F32) for _ in range(2)]
            yr_r = em.rot(3, [128, D], F32, "cyr")
            acc_r = em.rot(2, [128, D], F32, "cacc")
            yo_r = em.rot(2, [128, 8, 512], F32, "cyo")
            yo = None
            for ti in range(NS):
                acc = acc_r.next()
                for sl_ in range(4):
                    yr = yr_r.next()
                    idma("g", yr[:, :], ys_d[:, :], desti[:, ti, sl_:sl_ + 1], reads=[desti], writes=[yr])
                    if sl_ == 0:
                        em.op("dve", lambda e: e.tensor_scalar(acc[:], yr[:], g4[:, ti, 0:1], None, ALU.mult), reads=[yr, g4], writes=[acc])
                    else:
                        em.op("dve", lambda e: e.scalar_tensor_tensor(acc[:], yr[:], g4[:, ti, sl_:sl_ + 1], acc[:], ALU.mult, ALU.add), reads=[yr, g4, acc], writes=[acc])
                s_ = ti % 4
                if s_ == 0:
                    yo = yo_r.next()
                for c in range(8):
                    p = pT[c % 2]
                    em.op("pe", lambda e: e.transpose(p[:, 0:128], acc[:, c * 128:(c + 1) * 128], ident[:]), reads=[acc, ident], writes=[p])
                    if c % 2:
                        em.op("act", lambda e: e.copy(yo[:, c, s_ * 128:(s_ + 1) * 128], p[:, 0:128]), reads=[p], writes=[yo])
                    else:
                        em.op("dve", lambda e: e.tensor_copy(yo[:, c, s_ * 128:(s_ + 1) * 128], p[:, 0:128]), reads=[p], writes=[yo])
                if s_ == 3:
                    j = ti // 4
                    em.dma("sp", ydst[:, :, j * 512:(j + 1) * 512].rearrange("c p t -> p c t"), yo[:], reads=[yo])

    def mamba_phase(l, xsrc, ydst):
        sc1 = mods[:, l, 8:16]
        sh = mods[:, l, 0:8]
        inw = ssm_in_w[l]

        def load_hb(hb):
            xt_r = em.rot(3, [128, 512], F32, "mbx")
            for i in range(NT):
                load_mod_tile(xsrc, i, xt_r, lambda c: hb[:, c, i * 512:(i + 1) * 512], sc1, sh, hb)

        with em.phase():
            hb = em.sb([128, 8, T], BF16, "hb")
            load_hb(hb)
            wz = em.sb([128, 8, 2048], BF16, "wz")
            em.dma("pool", wz[:], inw[:, 0:2048].rearrange("(k p) f -> p k f", p=128), writes=[wz])
            wdt = em.sb([128, 8, 32], BF16, "wdt")
            em.dma("pool", wdt[:], inw[:, 5120:5152].rearrange("(k p) f -> p k f", p=128), writes=[wdt])
            dtb = em.sb([128, 32], F32, "dtb")
            em.dma("sp", dtb[:], ssm_dt_bias[l:l + 1, :].to_broadcast([128, 32]), writes=[dtb])
            pz = Rot([em.ps([128, 512], F32) for _ in range(4)])
            pd = em.ps([128, 32], F32)
            zs_r = em.rot(2, [128, 2048], BF16, "zs")
            d_r = {k: em.rot(2, [128, 32], F32, "d" + k) for k in ("x", "a", "e", "r")}
            for s in range(NS):
                ss = slice(s * 128, (s + 1) * 128)
                zs = zs_r.next()
                for q in range(4):
                    p = pz.next()
                    for k in range(8):
                        em.op("pe", lambda e: e.matmul(p[:], hb[:, k, ss], wz[:, k, q * 512:(q + 1) * 512], start=(k == 0), stop=(k == 7)),
                              reads=[hb, wz], writes=[p])
                    em.op("act", lambda e: e.activation(out=zs[:, q * 512:(q + 1) * 512], in_=p[:], func=AF.Silu), reads=[p], writes=[zs])
                em.dma("sp", zs_d[ss, :], zs[:], reads=[zs])
                for k in range(8):
                    em.op("pe", lambda e: e.matmul(pd[:], hb[:, k, ss], wdt[:, k, :], start=(k == 0), stop=(k == 7)), reads=[hb, wdt], writes=[pd])
                dx = d_r["x"].next(); da = d_r["a"].next(); de = d_r["e"].next(); dr = d_r["r"].next()
                em.op("dve", lambda e: e.tensor_tensor(dx[:], pd[:], dtb[:], ALU.add), reads=[pd, dtb], writes=[dx])
                em.op("dve", lambda e: e.tensor_scalar_mul(da[:], dx[:], -1.0), reads=[dx], writes=[da])
                em.op("dve", lambda e: e.tensor_tensor(da[:], da[:], dx[:], ALU.max), reads=[dx, da], writes=[da])
                em.op("act", lambda e: e.activation(out=de[:], in_=da[:], func=AF.Exp, scale=-1.0), reads=[da], writes=[de])
                em.op("act", lambda e: e.activation(out=de[:], in_=de[:], func=AF.Ln, bias=ones32[:, 0:1], scale=1.0), reads=[de, ones32], writes=[de])
                em.op("dve", lambda e: e.scalar_tensor_tensor(dr[:], dx[:], 0.0, de[:], ALU.max, ALU.add), reads=[dx, de], writes=[dr])
                em.dma("sp", dt_d[ss, :], dr[:], reads=[dr])

        with em.phase():
            hb = em.sb([128, 8, T], BF16, "hb")
            load_hb(hb)
            wx = em.sb([128, 8, 3072], BF16, "wx")
            em.dma("pool", wx[:], inw[:, 2048:5120].rearrange("(k p) f -> p k f", p=128), writes=[wx])
            pm = em.ps([128, 512], F32)
            cw = em.sb([128, 24, 4], F32, "cw")
            cbias = em.sb([128, 24, 1], F32, "cb")
            pp = Rot([em.ps([128, 512], F32) for _ in range(4)])
            xpad_r = em.rot(2, [128, T + 3], F32, "xpad")
            rowsT(cw[:], ssm_conv_w[l], 4, 24, pm, cw, tmp=xpad_r.bufs[0], tmp_ap=xpad_r.bufs[0][:])
            rowsT(cbias[:], ssm_conv_b[l:l + 1, :], 1, 24, pm, cbias, tmp=xpad_r.bufs[1], tmp_ap=xpad_r.bufs[1][:])
            acc_r = em.rot(2, [128, T], F32, "cacc")
            ob_r = em.rot(1, [128, T], BF16, "cob")
            for ch in range(24):
                xp = xpad_r.next(); ac = acc_r.next(); ob = ob_r.next()
                em.op("pool", lambda e: e.memset(xp[:, 0:3], 0.0), writes=[xp])
                for i in range(NT):
                    p = pp.next()
                    for k in range(8):
                        em.op("pe", lambda e: e.matmul(p[:], wx[:, k, ch * 128:(ch + 1) * 128], hb[:, k, i * 512:(i + 1) * 512], start=(k == 0), stop=(k == 7)),
                              reads=[wx, hb], writes=[p])
                    if i % 2:
                        em.op("act", lambda e: e.copy(xp[:, 3 + i * 512:3 + (i + 1) * 512], p[:]), reads=[p], writes=[xp])
                    else:
                        em.op("dve", lambda e: e.tensor_copy(xp[:, 3 + i * 512:3 + (i + 1) * 512], p[:]), reads=[p], writes=[xp])
                em.op("dve", lambda e: e.tensor_scalar(ac[:], xp[:, 0:T], cw[:, ch, 0:1], None, ALU.mult), reads=[xp, cw], writes=[ac])
                for j in range(1, 4):
                    eng = "dve"
                    em.op(eng, lambda e: e.scalar_tensor_tensor(ac[:], xp[:, j:j + T], cw[:, ch, j:j + 1], ac[:], ALU.mult, ALU.add),
                          reads=[xp, cw, ac], writes=[ac])
                em.op("act", lambda e: e.activation(out=ob[:], in_=ac[:], func=AF.Silu, bias=cbias[:, ch, :], scale=1.0), reads=[ac, cbias], writes=[ob])
                em.dma("sp", xbc_d[ch], ob[:], reads=[ob])

        with em.phase():
            tri = em.sb([128, 128], F32, "tri")
            em.dma("sp", tri[:], k_tri, writes=[tri])
            aneg = em.sb([128, 32], F32, "aneg")
            em.dma("sp", aneg[:], ssm_a_log[l:l + 1, :].to_broadcast([128, 32]), writes=[aneg])
            em.op("act", lambda e: e.activation(out=aneg[:], in_=aneg[:], func=AF.Exp), reads=[aneg], writes=[aneg])
            em.op("dve", lambda e: e.tensor_scalar_mul(aneg[:], aneg[:], -1.0), reads=[aneg], writes=[aneg])
            dsk = em.sb([128, 32], F32, "dsk")
            em.dma("sp", dsk[:], ssm_d[l:l + 1, :].to_broadcast([128, 32]), writes=[dsk])
            nw = em.sb([128, 2048], F32, "nw")
            em.dma("sp", nw[:], ssm_norm_w[l:l + 1, :].to_broadcast([128, 2048]), writes=[nw])
            wout = em.sb([128, 16, D], BF16, "wout")
            em.dma("pool", wout[:], ssm_out_w[l].rearrange("(k p) o -> p k o", p=128), writes=[wout])
            st32 = [em.sb([128, 8, 64], F32, "st32") for _ in range(4)]
            stb = [em.sb([128, 8, 64], BF16, "stb") for _ in range(4)]
            for g in range(4):
                em.op("dve", lambda e: e.memset(st32[g][:], 0.0), writes=[st32[g]])
                em.op("dve", lambda e: e.memset(stb[g][:], 0.0), writes=[stb[g]])
            ynT = em.sb([128, 16, 512], BF16, "ynT")
            ptb = em.ps([128, 512], BF16)
            pmisc = em.ps([128, 512], F32)
            par = em.ps([128, 1024], F32)
            py = em.ps([128, 512], F32)
            psn = em.ps([128, 512], F32)
            pout = em.ps([128, 512], F32)
            xsT_r = em.rot(2, [128, 16, 128], BF16, "xsT")
            bT_r = em.rot(2, [128, 4, 128], BF16, "bT")
            cT_r = em.rot(2, [128, 4, 128], BF16, "cT")
            zs_r = em.rot(2, [128, 2048], BF16, "zsl")
            dt_r = em.rot(2, [128, 32], F32, "dtl")
            xs_r = em.rot(2, [128, 32, 64], BF16, "xs")
            bt_r = em.rot(2, [128, 512], BF16, "btok")
            xdt_r = em.rot(1, [128, 32, 64], BF16, "xdt")
            xdtd_r = em.rot(1, [128, 32, 64], BF16, "xdtd")
            arow_r = em.rot(1, [128, 32, 128], F32, "arow")
            seg_r = em.rot(2, [128, 8, 128], F32, "seg")
            ear_r = em.rot(2, [128, 8, 128], F32, "ear")
            Mh_r = em.rot(2, [128, 8, 128], BF16, "Mh")
            Cs_r = em.rot(2, [128, 8, 128], BF16, "Cs")
            cbm_r = em.rot(2, [128, 128], F32, "cbm")
            yz = em.sb([128, 4, 512], F32, "yz")
            tt_r = em.rot(2, [128, 8, 64], F32, "tt")
            junk = em.sb([128, 512], F32, "junk")
            yn = em.sb([128, 2048], BF16, "yn")
            yo_r = em.rot(1, [128, 8, 512], F32, "yo")
            sA = {k: em.sb([128, 32], F32, "s" + k) for k in ("a", "acs", "tot", "d1", "dend", "cdec", "dtd")}
            ss4 = em.sb([128, 4], F32); rstd4 = em.sb([128, 4], F32)
            for c in range(NS):
                cs_ = slice(c * 128, (c + 1) * 128)
                xsT = xsT_r.next(); bT = bT_r.next(); cT = cT_r.next(); zs = zs_r.next(); dt = dt_r.next()
                em.dma("sp", xsT[:], xbc_d[0:16, :, cs_].rearrange("k p t -> p k t"), writes=[xsT])
                em.dma("sp", bT[:], xbc_d[16:20, :, cs_].rearrange("k p t -> p k t"), writes=[bT])
                em.dma("sp", cT[:], xbc_d[20:24, :, cs_].rearrange("k p t -> p k t"), writes=[cT])
                em.dma("sp", zs[:], zs_d[cs_, :], writes=[zs])
                em.dma("sp", dt[:], dt_d[cs_, :], writes=[dt])
                xs = xs_r.next(); btok = bt_r.next()
                xsf = xs[:].rearrange("p h d -> p (h d)")
                for q in range(4):
                    for k in range(4):
                        em.op("pe", lambda e: e.transpose(ptb[:, k * 128:(k + 1) * 128], xsT[:, q * 4 + k, :], identb[:]), reads=[xsT, identb], writes=[ptb])
                    em.op("act", lambda e: e.copy(xsf[:, q * 512:(q + 1) * 512], ptb[:]), reads=[ptb], writes=[xs])
                for g in range(4):
                    em.op("pe", lambda e: e.transpose(ptb[:, g * 128:(g + 1) * 128], bT[:, g, :], identb[:]), reads=[bT, identb], writes=[ptb])
                em.op("dve", lambda e: e.tensor_copy(btok[:], ptb[:]), reads=[ptb], writes=[btok])
                a, acs, tot, d1, dend, cdec, dtd = (sA[k] for k in ("a", "acs", "tot", "d1", "dend", "cdec", "dtd"))
                em.op("dve", lambda e: e.tensor_tensor(a[:], dt[:], aneg[:], ALU.mult), reads=[dt, aneg], writes=[a])
                em.op("pe", lambda e: e.matmul(pmisc[:, 0:32], tri[:], a[:], start=True, stop=True), reads=[tri, a], writes=[pmisc])
                em.op("pe", lambda e: e.matmul(pmisc[:, 32:64], ones32[:], a[:], start=True, stop=True), reads=[ones32, a], writes=[pmisc])
                em.op("dve", lambda e: e.tensor_copy(acs[:], pmisc[:, 0:32]), reads=[pmisc], writes=[acs])
                em.op("dve", lambda e: e.tensor_copy(tot[:], pmisc[:, 32:64]), reads=[pmisc], writes=[tot])
                em.op("dve", lambda e: e.tensor_tensor(d1[:], tot[:], acs[:], ALU.subtract), reads=[tot, acs], writes=[d1])
                em.op("act", lambda e: e.activation(out=dend[:], in_=d1[:], func=AF.Exp), reads=[d1], writes=[dend])
                em.op("act", lambda e: e.activation(out=cdec[:], in_=tot[:], func=AF.Exp), reads=[tot], writes=[cdec])
                em.op("dve", lambda e: e.tensor_tensor(dtd[:], dt[:], dend[:], ALU.mult), reads=[dt, dend], writes=[dtd])
                xdt = xdt_r.next(); xdtd = xdtd_r.next()
                em.op("pool", lambda e: e.tensor_tensor(xdt[:], xs[:], dt[:].unsqueeze(2).to_broadcast([128, 32, 64]), ALU.mult), reads=[xs, dt], writes=[xdt])
                em.op("dve", lambda e: e.tensor_tensor(xdtd[:], xs[:], dtd[:].unsqueeze(2).to_broadcast([128, 32, 64]), ALU.mult), reads=[xs, dtd], writes=[xdtd])
                arow = arow_r.next()
                em.op("pool", lambda e: e.tensor_tensor(arow[:], tri[:].unsqueeze(1).to_broadcast([128, 32, 128]),
                                                        a[:].unsqueeze(2).to_broadcast([128, 32, 128]), ALU.mult), reads=[tri, a], writes=[arow])
                for g in range(4):
                    for h2 in range(2):
                        em.op("pe", lambda e: e.matmul(par[:, h2 * 512:(h2 + 1) * 512], ones32[:],
                                                       arow[:, g * 8 + h2 * 4:g * 8 + h2 * 4 + 4, :].rearrange("p h l -> p (h l)"), start=True, stop=True),
                              reads=[ones32, arow], writes=[par])
                    em.op("pe", lambda e: e.matmul(pmisc[:, 128:256], bT[:, g, :], cT[:, g, :], start=True, stop=True), reads=[bT, cT], writes=[pmisc])
                    cbm = cbm_r.next()
                    em.op("dve", lambda e: e.tensor_tensor(cbm[:], pmisc[:, 128:256], tri[:], ALU.mult), reads=[pmisc, tri], writes=[cbm])
                    seg = seg_r.next(); ear = ear_r.next(); Mh = Mh_r.next(); Cs = Cs_r.next()
                    for hh in range(8):
                        h = g * 8 + hh
                        em.op("dve", lambda e: e.tensor_scalar(seg[:, hh, :], par[:, hh * 128:(hh + 1) * 128], acs[:, h:h + 1], 0.0, ALU.subtract, ALU.min),
                              reads=[par, acs], writes=[seg])
                    em.op("act", lambda e: e.activation(out=seg[:], in_=seg[:], func=AF.Exp), reads=[seg], writes=[seg])
                    em.op("pool", lambda e: e.tensor_tensor(Mh[:], seg[:], cbm[:].unsqueeze(1).to_broadcast([128, 8, 128]), ALU.mult), reads=[seg, cbm], writes=[Mh])
                    em.op("act", lambda e: e.activation(out=ear[:], in_=par[:].rearrange("p (h l) -> p h l", l=128), func=AF.Exp), reads=[par], writes=[ear])
                    em.op("dve", lambda e: e.tensor_tensor(Cs[:], ear[:], cT[:, g, :].unsqueeze(1).to_broadcast([128, 8, 128]), ALU.mult), reads=[ear, cT], writes=[Cs])
                    for hh in range(8):
                        h = g * 8 + hh
                        em.op("pe", lambda e: e.matmul(py[:, hh * 64:(hh + 1) * 64], Mh[:, hh, :], xdt[:, h, :], start=True, stop=False), reads=[Mh, xdt], writes=[py])
                        em.op("pe", lambda e: e.matmul(py[:, hh * 64:(hh + 1) * 64], Cs[:, hh, :], stb[g][:, hh, :], start=False, stop=True), reads=[Cs, stb[g]], writes=[py])
                    tt = tt_r.next()
                    em.op("pool", lambda e: e.tensor_tensor(tt[:], xs[:, g * 8:(g + 1) * 8, :], dsk[:, g * 8:(g + 1) * 8].unsqueeze(2).to_broadcast([128, 8, 64]), ALU.mult),
                          reads=[xs, dsk], writes=[tt])
                    em.op("dve", lambda e: e.tensor_tensor(yz[:, g, :], py[:], tt[:].rearrange("p h d -> p (h d)"), ALU.add), reads=[py, tt], writes=[yz])
                    em.op("pool", lambda e: e.tensor_tensor(yz[:, g, :], yz[:, g, :], zs[:, g * 512:(g + 1) * 512], ALU.mult), reads=[yz, zs], writes=[yz])
                    em.op("act", lambda e: e.activation(out=junk[:], in_=yz[:, g, :], func=AF.Square, accum_out=ss4[:, g:g + 1]), reads=[yz], writes=[junk, ss4])
                    em.op("pe", lambda e: e.matmul(psn[:], btok[:, g * 128:(g + 1) * 128], xdtd[:, g * 8:(g + 1) * 8, :].rearrange("p h d -> p (h d)"), start=True, stop=True),
                          reads=[btok, xdtd], writes=[psn])
                    em.op("pool", lambda e: e.tensor_tensor(st32[g][:], st32[g][:], cdec[:, g * 8:(g + 1) * 8].unsqueeze(2).to_broadcast([128, 8, 64]), ALU.mult),
                          reads=[st32[g], cdec], writes=[st32[g]])
                    em.op("dve", lambda e: e.tensor_tensor(st32[g][:], st32[g][:], psn[:].rearrange("p (h d) -> p h d", d=64), ALU.add), reads=[st32[g], psn], writes=[st32[g]])
                    em.op("act", lambda e: e.copy(stb[g][:], st32[g][:]), reads=[st32[g]], writes=[stb[g]])
                em.op("dve", lambda e: e.tensor_scalar(rstd4[:], ss4[:], 1.0 / 512.0, None, ALU.mult), reads=[ss4], writes=[rstd4])
                em.op("act", lambda e: e.activation(out=rstd4[:], in_=rstd4[:], func=AF.Sqrt, bias=eps5[:], scale=1.0), reads=[rstd4, eps5], writes=[rstd4])
                em.op("dve", lambda e: e.reciprocal(rstd4[:], rstd4[:]), reads=[rstd4], writes=[rstd4])
                for g in range(4):
                    eng = "dve"
                    em.op(eng, lambda e: e.scalar_tensor_tensor(yn[:, g * 512:(g + 1) * 512], yz[:, g, :], rstd4[:, g:g + 1], nw[:, g * 512:(g + 1) * 512], ALU.mult, ALU.mult),
                          reads=[yz, rstd4, nw], writes=[yn])
                c4 = c % 4
                for q in range(4):
                    for k in range(4):
                        em.op("pe", lambda e: e.transpose(ptb[:, k * 128:(k + 1) * 128], yn[:, (q * 4 + k) * 128:(q * 4 + k + 1) * 128], identb[:]), reads=[yn, identb], writes=[ptb])
                    em.op("act", lambda e: e.copy(ynT[:, q * 4:(q + 1) * 4, c4 * 128:(c4 + 1) * 128], ptb[:].rearrange("p (k t) -> p k t", t=128)), reads=[ptb], writes=[ynT])
                if c4 == 3:
                    yo = yo_r.next()
                    for oc in range(8):
                        for k in range(16):
                            em.op("pe", lambda e: e.matmul(pout[:], wout[:, k, oc * 128:(oc + 1) * 128], ynT[:, k, :], start=(k == 0), stop=(k == 15)), reads=[wout, ynT], writes=[pout])
                        em.op("dve", lambda e: e.tensor_copy(yo[:, oc, :], pout[:]), reads=[pout], writes=[yo])
                    i = c // 4
                    em.dma("sp", ydst[:, :, i * 512:(i + 1) * 512].rearrange("c p t -> p c t"), yo[:], reads=[yo])

    def rope_phase():
        with em.phase():
            rc = em.sb([64, 2], F32, "rc")
            em.dma("sp", rc[:], k_ropec, writes=[rc])
            posi = em.sb([64, T], I32, "posi")
            em.dma("sp", posi[:], pos_in[0:1, :].to_broadcast([64, T]), writes=[posi])
            ang = em.sb([64, T], F32, "ang")
            em.op("dve", lambda e: e.tensor_copy(ang[:], posi[:]), reads=[posi], writes=[ang])
            em.op("dve", lambda e: e.tensor_scalar(ang[:], ang[:], rc[:, 0:1], None, ALU.mult), reads=[ang, rc], writes=[ang])
            MAGIC = 12582912.0
            C1 = 6.28125
            C2 = 2.0 * math.pi - 6.28125
            u = em.sb([64, T], F32, "u"); kk = em.sb([64, T], F32, "kk"); r = em.sb([64, T], F32, "r")
            for which, shift in ((0, math.pi / 2.0), (1, 0.0)):
                em.op("dve", lambda e: e.tensor_scalar_add(u[:], ang[:], shift), reads=[ang], writes=[u])
                em.op("dve", lambda e: e.tensor_scalar(kk[:], u[:], 1.0 / (2.0 * math.pi), MAGIC, ALU.mult, ALU.add), reads=[u], writes=[kk])
                em.op("dve", lambda e: e.tensor_scalar_add(kk[:], kk[:], -MAGIC), reads=[kk], writes=[kk])
                em.op("dve", lambda e: e.scalar_tensor_tensor(r[:], kk[:], -C1, u[:], ALU.mult, ALU.add), reads=[kk, u], writes=[r])
                em.op("dve", lambda e: e.scalar_tensor_tensor(r[:], kk[:], -C2, r[:], ALU.mult, ALU.add), reads=[kk, r], writes=[r])
                em.op("dve", lambda e: e.tensor_scalar(r[:], r[:], math.pi, -math.pi, ALU.min, ALU.max), reads=[r], writes=[r])
                em.op("act", lambda e: e.activation(out=r[:], in_=r[:], func=AF.Sin), reads=[r], writes=[r])
                if which == 1:
                    em.op("dve", lambda e: e.tensor_scalar(r[:], r[:], rc[:, 1:2], None, ALU.mult), reads=[r, rc], writes=[r])
                em.dma("sp", rope_d[which], r[:], reads=[r])

    def kv_phase(xsrc):
        with em.phase():
            hb = em.sb([128, 8, T], BF16, "hb")
            xt_r = em.rot(3, [128, 512], F32, "kvx")
            for i in range(NT):
                load_mod_tile(xsrc, i, xt_r, lambda c: hb[:, c, i * 512:(i + 1) * 512], kvmod[:, 8:16], kvmod[:, 0:8], hb)
            kvw = em.sb([128, 8, 1536], BF16, "kvw")
            em.dma("pool", kvw[:], kv_w.rearrange("(k p) f -> p k f", p=128), writes=[kvw])
            kvws = em.sb([128, 8, 768], BF16, "kvws")
            em.dma("pool", kvws[:], kv_w_sw.rearrange("(k p) f -> p k f", p=128), writes=[kvws])
            cos_r = em.rot(2, [64, 512], F32, "cos"); sin_r = em.rot(2, [64, 512], F32, "sin")
            P = [em.ps([128, 512], F32) for _ in range(6)]
            pdr = Rot(P[0:2]); psr = Rot(P[2:4])
            w1 = []; w2 = []; biasT = []
            cp = em.sb([32, 64], F32, "cp")
            em.dma("sp", cp[:], cmp_pos, writes=[cp])
            em.op("pe", lambda e: e.transpose(P[4][0:64, 0:32], cp[0:32, 0:64], ident[0:32, 0:32]), reads=[cp, ident], writes=[P[4]])
            cposT = em.sb([64, 32], BF16, "cposT")
            em.op("dve", lambda e: e.tensor_copy(cposT[:], P[4][0:64, 0:32]), reads=[P[4]], writes=[cposT])
            for m in range(2):
                a = em.sb([64, 32, 256], BF16, "w1")
                em.dma("pool", a[:], phi_w1[m].rearrange("(j d) f -> d j f", d=64), writes=[a])
                b = em.sb([128, 2, 64], BF16, "w2")
                em.dma("pool", b[:], phi_w2[m].rearrange("(c p) d -> p c d", p=128), writes=[b])
                w1.append(a); w2.append(b)
                bt = em.sb([128, 2], F32, "biasT")
                for hc in range(2):
                    for j in range(32):
                        em.op("pe", lambda e: e.matmul(P[5][:, hc:hc + 1], a[:, j, hc * 128:(hc + 1) * 128], cposT[:, j:j + 1], start=(j == 0), stop=(j == 31)),
                              reads=[a, cposT], writes=[P[5]])
                em.op("dve", lambda e: e.tensor_copy(bt[:], P[5][:, 0:2]), reads=[P[5]], writes=[bt])
                biasT.append(bt)
            kt_r = em.rot(2, [64, T], BF16, "kt")
            t1_r = em.rot(2, [64, 512], F32, "kt1")
            t2_r = em.rot(2, [64, 512], F32, "kt2")
            hid_r = em.rot(2, [128, 2, 256], BF16, "hid")
            u_r = em.rot(2, [128, 255], F32, "gu")
            u2_r = em.rot(2, [128, 255], F32, "gu2")
            kc_r = em.rot(2, [64, 256], BF16, "kc")
            vc_r = em.rot(2, [128, 64], BF16, "vc")

            def compress(srcT, m, g):
                hid = hid_r.next()
                for hc in range(2):
                    ph = P[4]
                    for j in range(32):
                        em.op("pe", lambda e: e.matmul(ph[:, 0:255], w1[m][:, j, hc * 128:(hc + 1) * 128], srcT[:, j:j + 16 * 254 + 1:16], start=(j == 0), stop=(j == 31)),
                              reads=[w1[m], srcT], writes=[ph])
                    u = u_r.next(); u2 = u2_r.next()
                    em.op("act", lambda e: e.activation(out=u[:], in_=ph[:, 0:255], func=AF.Identity, bias=biasT[m][:, hc:hc + 1], scale=1.0), reads=[ph, biasT[m]], writes=[u])
                    em.op("pool", lambda e: e.tensor_tensor(u2[:], u[:], u[:], ALU.mult), reads=[u], writes=[u2])
                    em.op("dve", lambda e: e.tensor_scalar(u2[:], u2[:], 0.044715, 1.0, ALU.mult, ALU.add), reads=[u2], writes=[u2])
                    em.op("pool", lambda e: e.tensor_tensor(u2[:], u2[:], u[:], ALU.mult), reads=[u2, u], writes=[u2])
                    em.op("act", lambda e: e.activation(out=u2[:], in_=u2[:], func=AF.Sigmoid, scale=1.5957691216057308), reads=[u2], writes=[u2])
                    em.op("dve", lambda e: e.tensor_tensor(hid[:, hc, 0:255], u[:], u2[:], ALU.mult), reads=[u, u2], writes=[hid])
                if m == 0:
                    pk = P[5]
                    for hc in range(2):
                        em.op("pe", lambda e: e.matmul(pk[0:64, 0:255], w2[0][:, hc, :], hid[:, hc, 0:255], start=(hc == 0), stop=(hc == 1)), reads=[w2[0], hid], writes=[pk])
                    kc = kc_r.next()
                    em.op("dve", lambda e: e.memset(kc[:], 0.0), writes=[kc])
                    em.op("dve", lambda e: e.tensor_copy(kc[:, 0:255], pk[0:64, 0:255]), reads=[pk], writes=[kc])
                    em.dma("sp", KC_d[g], kc[:], reads=[kc])
                else:
                    for ncn in range(2):
                        nn = 128 if ncn == 0 else 127
                        pv = P[5]
                        for hc in range(2):
                            em.op("pe", lambda e: e.matmul(pv[0:nn, 0:64], hid[:, hc, ncn * 128:ncn * 128 + nn], w2[1][:, hc, :], start=(hc == 0), stop=(hc == 1)), reads=[hid, w2[1]], writes=[pv])
                        vc = vc_r.next()
                        em.op("dve", lambda e: e.memset(vc[:], 0.0), writes=[vc])
                        em.op("dve", lambda e: e.tensor_copy(vc[0:nn, :], pv[0:nn, 0:64]), reads=[pv], writes=[vc])
                        em.dma("sp", VC_d[g, ncn], vc[:], reads=[vc])

            for si, slot in enumerate((0, 2, 4)):
                for g in range(4):
                    kt = kt_r.next()
                    for i in range(NT):
                        sl = slice(i * 512, (i + 1) * 512)
                        pd = pdr.next(); psw = psr.next()
                        for k in range(8):
                            em.op("pe", lambda e: e.matmul(pd[0:64, :], kvw[:, k, slot * 256 + g * 64:slot * 256 + g * 64 + 64], hb[:, k, sl], start=(k == 0), stop=(k == 7)), reads=[kvw, hb], writes=[pd])
                        for k in range(8):
                            em.op("pe", lambda e: e.matmul(psw[0:64, :], kvws[:, k, si * 256 + g * 64:si * 256 + g * 64 + 64], hb[:, k, sl], start=(k == 0), stop=(k == 7)), reads=[kvws, hb], writes=[psw])
                        t1 = t1_r.next(); t2 = t2_r.next()
                        cos = cos_r.next(); sin = sin_r.next()
                        em.dma("sp", cos[:], rope_d[0, :, sl], writes=[cos])
                        em.dma("sp", sin[:], rope_d[1, :, sl], writes=[sin])
                        em.op("dve", lambda e: e.tensor_tensor(t1[:], pd[0:64, :], cos[:], ALU.mult), reads=[pd, cos], writes=[t1])
                        em.op("dve", lambda e: e.tensor_tensor(t2[:], psw[0:64, :], sin[:], ALU.mult), reads=[psw, sin], writes=[t2])
                        em.op("pool", lambda e: e.tensor_tensor(kt[:, sl], t1[:], t2[:], ALU.add), reads=[t1, t2], writes=[kt])
                    em.dma("sp", KT_d[si, g], kt[:], reads=[kt])
                    if slot == 0:
                        compress(kt, 0, g)
            for g in range(4):
                vt = kt_r.next()
                for i in range(NT):
                    sl = slice(i * 512, (i + 1) * 512)
                    pd = pdr.next()
                    for k in range(8):
                        em.op("pe", lambda e: e.matmul(pd[0:64, :], kvw[:, k, 256 + g * 64:256 + g * 64 + 64], hb[:, k, sl], start=(k == 0), stop=(k == 7)), reads=[kvw, hb], writes=[pd])
                    em.op("act", lambda e: e.copy(vt[:, sl], pd[0:64, :]), reads=[pd], writes=[vt])
                compress(vt, 1, g)
            vt_r = em.rot(2, [128, 512], BF16, "vtok")
            for s in range(NS):
                ss = slice(s * 128, (s + 1) * 128)
                pv = pdr.next()
                for half, slot in enumerate((3, 5)):
                    for k in range(8):
                        em.op("pe", lambda e: e.matmul(pv[:, half * 256:(half + 1) * 256], hb[:, k, ss], kvw[:, k, slot * 256:(slot + 1) * 256], start=(k == 0), stop=(k == 7)), reads=[hb, kvw], writes=[pv])
                vtk = vt_r.next()
                em.op("act", lambda e: e.copy(vtk[:], pv[:]), reads=[pv], writes=[vtk])
                em.dma("sp", VT_d[ss, :], vtk[:], reads=[vtk])

    def nsa_phase(l, xsrc, ydst):
        jl = l - 2
        sc1 = mods[:, l, 8:16]
        sh = mods[:, l, 0:8]
        with em.phase():
            eall = em.sb([64, 32, 128], BF16, "eall"); em.dma("sp", eall[:], k_eall, writes=[eall])
            cz = em.sb([128, 4, 512], BF16, "cz"); em.dma("sp", cz[:], k_cz, writes=[cz])
            wm = em.sb([128, 8, 512], BF16, "wm"); em.dma("sp", wm[:], k_wm, writes=[wm])
            ovaug = em.sb([128, 2, 65], F32, "ovaug"); em.dma("sp", ovaug[:], k_ovaug.rearrange("c p j -> p c j"), writes=[ovaug])
            qw = em.sb([128, 8, 1072], BF16, "qw"); em.dma("pool", qw[:], nsa_q_w[jl].rearrange("(k p) f -> p k f", p=128), writes=[qw])
            qws = em.sb([128, 8, 1024], BF16, "qws"); em.dma("pool", qws[:], nsa_q_w_sw[jl].rearrange("(k p) f -> p k f", p=128), writes=[qws])
            ow = em.sb([64, 16, D], BF16, "ow"); em.dma("pool", ow[:], nsa_o_w[jl].rearrange("(h d) o -> d h o", d=64), writes=[ow])
            KC = em.sb([64, 4, 256], BF16, "KC"); em.dma("sp", KC[:], KC_d.rearrange("g d n -> d g n"), writes=[KC])
            VC = em.sb([128, 4, 2, 64], BF16, "VC"); em.dma("sp", VC[:], VC_d.rearrange("g c p d -> p g c d"), writes=[VC])
            P = [em.ps([128, 512], F32) for _ in range(8)]
            pS_r = Rot(P[0:2]); pM = P[2]; pO = P[3]; pD = P[4]; pI = P[5]; pOut = P[6]; pQ = P[7]; pM_r = Rot([P[2], P[5]])
            xt_r = em.rot(2, [128, 512], F32, "ax")
            hbt_r = em.rot(1, [128, 8, 512], BF16, "ahb")
            cos_r = em.rot(1, [64, 512], F32, "acos"); sin_r = em.rot(1, [64, 512], F32, "asin")
            mc_r = em.rot(1, [128, 2, 512], F32, "amc")
            oT_r = em.rot(1, [64, 16, 512], BF16, "oT")
            KS_r = em.rot(1, [64, T], BF16, "KS"); VS_r = em.rot(1, [128, NS, 64], BF16, "VS")
            KW_r = em.rot(1, [64, 1024], BF16, "KW"); VW_r = em.rot(1, [128, 8, 64], BF16, "VW")
            qt_b = [em.sb([64, 512], BF16, "qt") for _ in range(4)]
            sig_r = em.rot(2, [64, 512], F32, "sig")
            gwr_b = [em.sb([128, 8, 3, 64], BF16, "gwr") for _ in range(4)]
            oc_b = [em.sb([64, 512], F32, "ocomb") for _ in range(4)]
            t1_r = em.rot(1, [64, 512], F32, "at1"); t2_r = em.rot(1, [64, 512], F32, "at2")
            e32_r = em.rot(1, [128, 512], F32, "e32")
            p32 = [em.sb([128, 512], F32, "p32") for _ in range(2)]
            pbc = [em.sb([128, 512], BF16, "pbc") for _ in range(2)]
            eb_r = em.rot(3, [128, 512], BF16, "eb"); pb_r = em.rot(4, [128, 512], BF16, "pb")
            rden_r = em.rot(2, [64, 512], F32, "rden")
            impg = em.sb([128, 4, 64], F32, "impg")
            rd_r = em.rot(2, [128, 1], F32, "rd")
            selc_r = em.rot(2, [128, 4, 64], F32, "selc")
            sc_r = em.rot(2, [128, 64], F32, "sc"); rep_r = em.rot(2, [128, 64], F32, "rep")
            m8a = em.sb([128, 8]); m8b = em.sb([128, 8])
            selT = em.sb([64, 512], BF16, "selT")
            yo_r = em.rot(2, [128, 512], F32, "ayo")

            cur_hbt = [None]

            def finish_branch(r, b, first):
                rden = rden_r.next(); sig = sig_r.next()
                for k in range(8):
                    em.op("pe", lambda e: e.matmul(pQ[0:64, :], gwr_b[r][:, k, b, :], cur_hbt[0][:, k, :], start=(k == 0), stop=(k == 7)), reads=[gwr_b[r], cur_hbt[0]], writes=[pQ])
                em.op("act", lambda e: e.activation(out=sig[:], in_=pQ[0:64, :], func=AF.Sigmoid), reads=[pQ], writes=[sig])
                em.op("dve", lambda e: e.tensor_scalar_max(rden[:], pD[0:64, :], TINY), reads=[pD], writes=[rden])
                em.op("dve", lambda e: e.reciprocal(rden[:], rden[:]), reads=[rden], writes=[rden])
                em.op("pool", lambda e: e.tensor_tensor(rden[:], rden[:], sig[:], ALU.mult), reads=[rden, sig], writes=[rden])
                if first:
                    em.op("dve", lambda e: e.tensor_tensor(oc_b[r][:], pO[0:64, :], rden[:], ALU.mult), reads=[pO, rden], writes=[oc_b[r]])
                else:
                    em.op("dve", lambda e: e.tensor_tensor(rden[:], pO[0:64, :], rden[:], ALU.mult), reads=[pO, rden], writes=[rden])
                    em.op("pool", lambda e: e.tensor_tensor(oc_b[r][:], oc_b[r][:], rden[:], ALU.add), reads=[oc_b[r], rden], writes=[oc_b[r]])

            for i in range(NT):
                sl = slice(i * 512, (i + 1) * 512)
                hbt = hbt_r.next()
                cur_hbt[0] = hbt
                for c in range(8):
                    xt = xt_r.next()
                    em.dma("sp", xt[:], xsrc[c, :, sl], writes=[xt])
                    eng = "dve" if c % 2 == 0 else "pool"
                    em.op(eng, lambda e: e.tensor_scalar(hbt[:, c, :], xt[:], sc1[:, c:c + 1], sh[:, c:c + 1], ALU.mult, ALU.add),
                          reads=[xt, mods], writes=[hbt])
                cos = cos_r.next(); sin = sin_r.next(); mc = mc_r.next()
                em.dma("sp", cos[:], rope_d[0, :, sl], writes=[cos])
                em.dma("sp", sin[:], rope_d[1, :, sl], writes=[sin])
                em.op("dve", lambda e: e.tensor_scalar_mul(cos[:], cos[:], ATTN_SCALE), reads=[cos], writes=[cos])
                em.op("dve", lambda e: e.tensor_scalar_mul(sin[:], sin[:], ATTN_SCALE), reads=[sin], writes=[sin])
                em.dma("sp", mc[:], k_mcmp[:, :, sl].rearrange("c p t -> p c t"), writes=[mc])
                oT = oT_r.next()
                nkt = 4 * (i + 1)
                w0 = max(0, 4 * i - 4)
                for g in range(4):
                    KS = KS_r.next(); VS = VS_r.next(); KW = KW_r.next(); VW = VW_r.next()
                    em.dma("sp", KS[:, 0:nkt * 128], KT_d[1, g, :, 0:nkt * 128], writes=[KS])
                    em.dma("sp", VS[:, 0:nkt, :], VT_d[0:nkt * 128, g * 64:(g + 1) * 64].rearrange("(k p) d -> p k d", p=128), writes=[VS])
                    nwk = nkt - w0
                    em.dma("sp", KW[:, 0:nwk * 128], KT_d[2, g, :, w0 * 128:nkt * 128], writes=[KW])
                    em.dma("sp", VW[:, 0:nwk, :], VT_d[w0 * 128:nkt * 128, 256 + g * 64:256 + (g + 1) * 64].rearrange("(k p) d -> p k d", p=128), writes=[VW])
                    for r in range(4):
                        h = g * 4 + r
                        pd = pS_r.next(); psw = pS_r.next()
                        for k in range(8):
                            em.op("pe", lambda e: e.matmul(pd[0:64, :], qw[:, k, h * 64:(h + 1) * 64], hbt[:, k, :], start=(k == 0), stop=(k == 7)), reads=[qw, hbt], writes=[pd])
                        for k in range(8):
                            em.op("pe", lambda e: e.matmul(psw[0:64, :], qws[:, k, h * 64:(h + 1) * 64], hbt[:, k, :], start=(k == 0), stop=(k == 7)), reads=[qws, hbt], writes=[psw])
                        t1 = t1_r.next(); t2 = t2_r.next()
                        em.op("dve", lambda e: e.tensor_tensor(t1[:], pd[0:64, :], cos[:], ALU.mult), reads=[pd, cos], writes=[t1])
                        em.op("dve", lambda e: e.tensor_tensor(t2[:], psw[0:64, :], sin[:], ALU.mult), reads=[psw, sin], writes=[t2])
                        em.op("pool", lambda e: e.tensor_tensor(qt_b[r][:], t1[:], t2[:], ALU.add), reads=[t1, t2], writes=[qt_b[r]])
                        gwr = gwr_b[r]
                        em.op("pool", lambda e: e.tensor_copy(gwr[:], qw[:, :, 1024 + h * 3:1024 + h * 3 + 3].unsqueeze(3).to_broadcast([128, 8, 3, 64])), reads=[qw], writes=[gwr])
                        for cn in range(2):
                            pS = pS_r.next()
                            em.op("pe", lambda e: e.matmul(pS[:], KC[:, g, cn * 128:(cn + 1) * 128], qt_b[r][:], start=True, stop=True), reads=[KC, qt_b[r]], writes=[pS])
                            e32 = e32_r.next()
                            em.op("act", lambda e: e.activation(out=e32[:], in_=pS[:], func=AF.Exp), reads=[pS], writes=[e32])
                            em.op("dve", lambda e: e.tensor_tensor(p32[cn][:], e32[:], mc[:, cn, :], ALU.mult), reads=[e32, mc], writes=[p32[cn]])
                            em.op("pool", lambda e: e.tensor_copy(pbc[cn][:], p32[cn][:]), reads=[p32[cn]], writes=[pbc[cn]])
                        for cn in range(2):
                            em.op("pe", lambda e: e.matmul(pO[0:64, :], VC[:, g, cn, :], pbc[cn][:], start=(cn == 0), stop=(cn == 1)), reads=[VC, pbc[cn]], writes=[pO])
                        for cn in range(2):
                            em.op("pe", lambda e: e.matmul(pD[0:64, :], onesb[:], pbc[cn][:], start=(cn == 0), stop=(cn == 1)), reads=[onesb, pbc[cn]], writes=[pD])
                        finish_branch(r, 0, True)
                        for s in range(4):
                            for cn in range(2):
                                em.op("pe", lambda e: e.matmul(pI[:, 0:65], p32[cn][:, s * 128:(s + 1) * 128], ovaug[:, cn, :], start=(cn == 0), stop=(cn == 1)), reads=[p32[cn], ovaug], writes=[pI])
                            rd = rd_r.next()
                            em.op("dve", lambda e: e.tensor_scalar_max(rd[:], pI[:, 64:65], TINY), reads=[pI], writes=[rd])
                            em.op("dve", lambda e: e.reciprocal(rd[:], rd[:]), reads=[rd], writes=[rd])
                            if r == 0:
                                em.op("dve", lambda e: e.tensor_scalar(impg[:, s, :], pI[:, 0:64], rd[:, 0:1], None, ALU.mult), reads=[pI, rd], writes=[impg])
                            else:
                                em.op("dve", lambda e: e.scalar_tensor_tensor(impg[:, s, :], pI[:, 0:64], rd[:, 0:1], impg[:, s, :], ALU.mult, ALU.add), reads=[pI, rd, impg], writes=[impg])
                    for s in range(4):
                        selc = selc_r.next()
                        t0 = i * 512 + s * 128
                        em.dma("sp", selc[:], k_selc[t0:t0 + 128], writes=[selc])
                        sc = sc_r.next(); rep = rep_r.next()
                        em.op("dve", lambda e: e.tensor_tensor(sc[:], impg[:, s, :], selc[:, 0, :], ALU.mult), reads=[impg, selc], writes=[sc])
                        em.op("dve", lambda e: e.tensor_tensor(sc[:], sc[:], selc[:, 1, :], ALU.add), reads=[sc, selc], writes=[sc])
                        em.op("dve", lambda e: e.tensor_tensor(sc[:], sc[:], selc[:, 2, :], ALU.mult), reads=[sc, selc], writes=[sc])
                        em.op("dve", lambda e: e.tensor_tensor(sc[:], sc[:], selc[:, 3, :], ALU.add), reads=[sc, selc], writes=[sc])
                        em.op("dve", lambda e: e.max(m8a[:], sc[:]), reads=[sc], writes=[m8a])
                        em.op("dve", lambda e: e.match_replace(rep[:], m8a[:], sc[:], -3e30), reads=[sc, m8a], writes=[rep])
                        em.op("dve", lambda e: e.max(m8b[:], rep[:]), reads=[rep], writes=[m8b])
                        em.op("dve", lambda e: e.tensor_scalar(rep[:], sc[:], m8b[:, 7:8], None, ALU.is_ge), reads=[sc, m8b], writes=[rep])
                        em.op("dve", lambda e: e.tensor_tensor(rep[:], rep[:], selc[:, 2, :], ALU.mult), reads=[rep, selc], writes=[rep])
                        em.op("pe", lambda e: e.transpose(pI[0:64, 128:256], rep[:], ident[:]), reads=[rep, ident], writes=[pI])
                        em.op("act", lambda e: e.copy(selT[:, s * 128:(s + 1) * 128], pI[0:64, 128:256]), reads=[pI], writes=[selT])
                    for r in range(4):
                        h = g * 4 + r

                        def slc_stage(kt):
                            pS = pS_r.next(); pM = pM_r.next()
                            em.op("pe", lambda e: e.matmul(pS[:], KS[:, kt * 128:(kt + 1) * 128], qt_b[r][:], start=True, stop=True), reads=[KS, qt_b[r]], writes=[pS])
                            em.op("pe", lambda e: e.matmul(pM[:], eall[:, kt, :], selT[:], start=True, stop=True), reads=[eall, selT], writes=[pM])
                            eb = eb_r.next(); pb = pb_r.next()
                            em.op("act", lambda e: e.activation(out=eb[:], in_=pS[:], func=AF.Exp), reads=[pS], writes=[eb])
                            em.op("dve", lambda e: e.tensor_tensor(pb[:], eb[:], pM[:], ALU.mult), reads=[eb, pM], writes=[pb])
                            if kt >= 4 * i:
                                em.op("pool", lambda e: e.tensor_tensor(pb[:], pb[:], cz[:, kt - 4 * i, :], ALU.mult), reads=[pb, cz], writes=[pb])
                            return pb

                        def win_stage(kw):
                            kt = w0 + kw
                            pS = pS_r.next()
                            em.op("pe", lambda e: e.matmul(pS[:], KW[:, kw * 128:(kw + 1) * 128], qt_b[r][:], start=True, stop=True), reads=[KW, qt_b[r]], writes=[pS])
                            eb = eb_r.next(); pb = pb_r.next()
                            em.op("act", lambda e: e.activation(out=eb[:], in_=pS[:], func=AF.Exp), reads=[pS], writes=[eb])
                            em.op("pool", lambda e: e.tensor_tensor(pb[:], eb[:], wm[:, 4 * i - kt + 3, :], ALU.mult), reads=[eb, wm], writes=[pb])
                            return pb

                        cur_pb = slc_stage(0)
                        for kt in range(nkt):
                            nxt_pb = slc_stage(kt + 1) if kt + 1 < nkt else None
                            pb = cur_pb
                            em.op("pe", lambda e: e.matmul(pO[0:64, :], VS[:, kt, :], pb[:], start=(kt == 0), stop=(kt == nkt - 1)), reads=[VS, pb], writes=[pO])
                            em.op("pe", lambda e: e.matmul(pD[0:64, :], onesb[:], pb[:], start=(kt == 0), stop=(kt == nkt - 1)), reads=[onesb, pb], writes=[pD])
                            cur_pb = nxt_pb
                        finish_branch(r, 1, False)
                        cur_pb = win_stage(0)
                        for kw in range(nwk):
                            nxt_pb = win_stage(kw + 1) if kw + 1 < nwk else None
                            pb = cur_pb
                            em.op("pe", lambda e: e.matmul(pO[0:64, :], VW[:, kw, :], pb[:], start=(kw == 0), stop=(kw == nwk - 1)), reads=[VW, pb], writes=[pO])
                            em.op("pe", lambda e: e.matmul(pD[0:64, :], onesb[:], pb[:], start=(kw == 0), stop=(kw == nwk - 1)), reads=[onesb, pb], writes=[pD])
                            cur_pb = nxt_pb
                        finish_branch(r, 2, False)
                        em.op("act", lambda e: e.copy(oT[:, h, :], oc_b[r][:]), reads=[oc_b[r]], writes=[oT])
                for oc in range(8):
                    yo = yo_r.next()
                    for h in range(16):
                        em.op("pe", lambda e: e.matmul(pOut[:], ow[:, h, oc * 128:(oc + 1) * 128], oT[:, h, :], start=(h == 0), stop=(h == 15)), reads=[ow, oT], writes=[pOut])
                    em.op("dve", lambda e: e.tensor_copy(yo[:], pOut[:]), reads=[pOut], writes=[yo])
                    em.dma("sp", ydst[oc, :, sl], yo[:], reads=[yo])

    cur = 0
    if on("rope"):
        rope_phase()
    for l in range(DEPTH):
        if on("mix%d" % l):
            if l < 2:
                mamba_phase(l, xs_d[cur], ymix_d)
            else:
                nsa_phase(l, xs_d[cur], ymix_d)
        if on("ln%da" % l):
            post_norm(xs_d[cur], ymix_d, xs_d[1 - cur], mods[:, l, 16:24], 2 * l, False)
        cur = 1 - cur
        if on("moe%d" % l):
            moe_phase(l, xs_d[cur], ymix_d)
        if on("ln%db" % l):
            post_norm(xs_d[cur], ymix_d, xs_d[1 - cur], mods[:, l, 40:48], 2 * l + 1, l == DEPTH - 1)
        cur = 1 - cur
        if l == 1 and on("kv"):
            kv_phase(xs_d[cur])
    em.barrier()
    em.close()
    return nc, em


def make_in_maps(inputs, T, cores):
    cst = host_consts(T)
    f = lambda a: np.ascontiguousarray(np.asarray(a, dtype=np.float32))
    shared = {
        "ada_w": f(inputs["ada_w"]), "ada_b": f(inputs["ada_b"]),
        "ln_g": f(inputs["ln_g"]).reshape(8, D), "ln_b": f(inputs["ln_b"]).reshape(8, D),
        "ssm_in_w": f(inputs["ssm_in_w"]), "ssm_conv_w": f(inputs["ssm_conv_w"]), "ssm_conv_b": f(inputs["ssm_conv_b"]),
        "ssm_dt_bias": f(inputs["ssm_dt_bias"]), "ssm_a_log": f(inputs["ssm_a_log"]), "ssm_d": f(inputs["ssm_d"]),
        "ssm_norm_w": f(inputs["ssm_norm_w"]), "ssm_out_w": f(inputs["ssm_out_w"]),
        "kv_ada_w": f(inputs["kv_ada_w"]), "kv_ada_b": f(inputs["kv_ada_b"]).reshape(1, 2 * D),
        "kv_w": f(inputs["kv_w"]),
        "cmp_pos": f(inputs["cmp_pos"]),
        "phi_k_w1": f(inputs["phi_k_w1"]), "phi_k_w2": f(inputs["phi_k_w2"]),
        "phi_v_w1": f(inputs["phi_v_w1"]), "phi_v_w2": f(inputs["phi_v_w2"]),
        "nsa_q_w": f(inputs["nsa_q_w"]), "nsa_o_w": f(inputs["nsa_o_w"]),
        "router_w": f(inputs["router_w"]), "router_b": f(inputs["router_b"]),
        "moe_w_up": f(inputs["moe_w_up"]), "moe_b_up": f(inputs["moe_b_up"]),
        "moe_w_down": f(inputs["moe_w_down"]), "moe_b_down": f(inputs["moe_b_down"]),
    }
    kvw = shared["kv_w"]
    shared["kv_w_sw"] = np.concatenate([swap_halves(kvw[:, s * 256:(s + 1) * 256], 256) for s in (0, 2, 4)], axis=1)
    shared["nsa_q_w_sw"] = np.stack([swap_halves(shared["nsa_q_w"][j], 1024) for j in range(2)], axis=0)
    for k, v in cst.items():
        shared["k_" + k] = v
    maps = []
    for b in cores:
        m = dict(shared)
        m["x"] = f(inputs["x"][b][:T])
        m["c"] = np.ascontiguousarray(f(inputs["c"][b]).reshape(8, 128).T)
        m["pos"] = np.ascontiguousarray(np.asarray(inputs["pos"][b][:T], dtype=np.int32).reshape(1, T))
        maps.append(m)
    return maps


_CACHE = {}


def kernel(**inputs):
    T = 4096
    if T not in _CACHE:
        _CACHE[T] = build(T)[0]
    nc = _CACHE[T]
    maps = make_in_maps(inputs, T, list(range(8)))
    res = run_bass_kernel_spmd(nc, maps, core_ids=list(range(8)))
    out = np.stack([np.asarray(r["y"], dtype=np.float32) for r in res.results], axis=0)
    return out
```

```python
import contextlib
import os as _os_cap
import math
import numpy as np
import ml_dtypes
import concourse.bass as bass
import concourse.mybir as mybir
from concourse.bass_utils import run_bass_kernel_spmd

F32 = mybir.dt.float32
BF16 = mybir.dt.bfloat16
I32 = mybir.dt.int32
AF = mybir.ActivationFunctionType
ALU = mybir.AluOpType

D = 1024
DEPTH = 4
ALPHA = (2.0 * DEPTH) ** 0.25
LN_EPS = 1e-5
EPS_A = LN_EPS / (ALPHA * ALPHA)
ATTN_SCALE = 0.125
TINY = 1e-30
SEM_LIMIT = 30000


class Buf:
    __slots__ = ("t", "name", "w", "rd")

    def __init__(self, t, name):
        self.t = t
        self.name = name
        self.w = None
        self.rd = {}

    def __getitem__(self, idx):
        return self.t[idx]


class Rot:
    def __init__(self, bufs):
        self.bufs = bufs
        self.i = 0

    def next(self):
        b = self.bufs[self.i % len(self.bufs)]
        self.i += 1
        return b


class Em:
    ENG = ("pe", "act", "dve", "pool", "sp")

    def __init__(self, nc, n_dma_sems=12, same_engine_sync=True):
        self.nc = nc
        self.es = contextlib.ExitStack()
        self.eng = {"pe": nc.tensor, "act": nc.scalar, "dve": nc.vector, "pool": nc.gpsimd, "sp": nc.sync}
        self.sem = {}
        self.cnt = {}
        self.cur = {}
        self.gen = {}
        self.owner = {}
        for e in self.ENG:
            self.gen[e] = 0
            self._new_sem(e)
        self.known = {e: {} for e in self.ENG}
        self.dma_pool = {}
        self.dma_idx = {}
        self.dma_uses = {}
        self.dma_gen = 0
        for q in ("sp", "pool", "act"):
            self.dma_pool[q] = []
            for i in range(int(_os_cap.environ.get("PCAP", "4")) if q == "pool" else n_dma_sems):
                self.dma_pool[q].append(self._new_dma_sem(q))
            self.dma_idx[q] = 0
        self.same = same_engine_sync
        self.phase_stack = None
        self.uid = 0
        self.n_wait = 0
        self.n_ins = 0

    def _new_sem(self, e):
        k = "%s_%d" % (e, self.gen[e])
        self.gen[e] += 1
        self.sem[k] = self.es.enter_context(self.nc.semaphore("s_" + k))
        self.cnt[k] = 0
        self.cur[e] = k
        self.owner[k] = e

    def _new_dma_sem(self, q):
        k = "d_%s_%d" % (q, self.dma_gen)
        self.dma_gen += 1
        self.sem[k] = self.es.enter_context(self.nc.semaphore(k))
        self.dma_uses[k] = 0
        self.owner[k] = "dma"
        return k

    def _stack(self, persist):
        return self.es if (persist or self.phase_stack is None) else self.phase_stack

    def sb(self, shape, dtype=F32, name=None, persist=False):
        self.uid += 1
        nm = "%s_%d" % (name or "t", self.uid)
        t = self._stack(persist).enter_context(self.nc.sbuf_tensor(nm, list(shape), dtype))
        return Buf(t, nm)

    def rot(self, n, shape, dtype=F32, name=None):
        return Rot([self.sb(shape, dtype, name) for _ in range(n)])

    def ps(self, shape, dtype=F32, name=None, persist=False):
        self.uid += 1
        nm = "%s_%d" % (name or "p", self.uid)
        t = self._stack(persist).enter_context(self.nc.psum_tensor(nm, list(shape), dtype))
        return Buf(t, nm)

    def dram(self, name, shape, dtype, kind="Internal"):
        t = self.nc.dram_tensor(name, list(shape), dtype, kind=kind)
        return t.ap()

    @contextlib.contextmanager
    def phase(self):
        assert self.phase_stack is None
        self.barrier()
        self.phase_stack = contextlib.ExitStack()
        try:
            with self.phase_stack:
                yield
                self.barrier()
        finally:
            self.phase_stack = None

    def _need(self, e, dep):
        if dep is None:
            return
        k, v = dep
        if self.owner[k] == e and (e == "pe" or (e != "pool" and not self.same)):
            return
        if self.known[e].get(k, 0) >= v:
            return
        self.eng[e].wait_ge(self.sem[k], v)
        self.known[e][k] = v
        self.n_wait += 1

    def _deps(self, e, reads, writes):
        for b in reads:
            self._need(e, b.w)
        for b in writes:
            self._need(e, b.w)
            for k, v in b.rd.items():
                self._need(e, (k, v))

    def _record(self, tok, reads, writes):
        k, v = tok
        for b in reads:
            if b.rd.get(k, 0) < v:
                b.rd[k] = v
        for b in writes:
            b.w = tok
            b.rd = {}

    def op(self, e, ins_fn, reads=(), writes=()):
        self._deps(e, reads, writes)
        ins = ins_fn(self.eng[e])
        k = self.cur[e]
        self.cnt[k] += 1
        ins.then_inc(self.sem[k], 1)
        self._record((k, self.cnt[k]), reads, writes)
        self.n_ins += 1
        if self.cnt[k] >= SEM_LIMIT:
            self._new_sem(e)
        return ins

    def dma(self, q, out, in_, reads=(), writes=(), **kw):
        self._deps(q, reads, writes)
        pool = self.dma_pool[q]
        slot = self.dma_idx[q] % len(pool)
        k = pool[slot]
        self.dma_idx[q] += 1
        if 16 * (self.dma_uses[k] + 1) > SEM_LIMIT:
            k = self._new_dma_sem(q)
            pool[slot] = k
        if self.dma_uses[k] > 0:
            self._need(q, (k, 16 * self.dma_uses[k]))
        self.dma_uses[k] += 1
        ins = self.eng[q].dma_start(out=out, in_=in_, **kw)
        ins.then_inc(self.sem[k], 16)
        self._record((k, 16 * self.dma_uses[k]), reads, writes)
        self.n_ins += 1
        return ins

    def barrier(self):
        for e in self.ENG:
            for k, c in self.cnt.items():
                if c > 0:
                    self._need(e, (k, c))
            for k, u in self.dma_uses.items():
                if u > 0:
                    self._need(e, (k, 16 * u))

    def close(self):
        self.es.close()


def host_consts(T):
    c = {}
    c["ident"] = np.eye(128, dtype=np.float32)
    s = np.arange(128)
    c["tri"] = (s[:, None] <= s[None, :]).astype(np.float32)
    d = np.arange(64)
    inv = (10000.0 ** (-(np.arange(32, dtype=np.float32)) / np.float32(32))).astype(np.float32)
    c["ropec"] = np.stack([inv[d % 32], np.where(d < 32, -1.0, 1.0).astype(np.float32)], axis=1).astype(np.float32)
    n = np.arange(256)
    t = np.arange(T)
    m = ((16 * n[:, None] + 31) <= t[None, :]) & (n[:, None] < T // 16 - 1)
    c["mcmp"] = m.reshape(2, 128, T).astype(np.float32)
    n_cmp = T // 16 - 1
    n_slc = T // 64
    cs = np.arange(256)[:, None] * 16
    js = np.arange(64)[None, :] * 64
    ov = np.maximum(np.minimum(cs + 32, js + 64) - np.maximum(cs, js), 0).astype(np.float32) / 32.0
    ov[n_cmp:, :] = 0.0
    ov[:, n_slc:] = 0.0
    c["ovaug"] = np.concatenate([ov, np.ones((256, 1), np.float32)], axis=1).reshape(2, 128, 65)
    tb = (t // 64)[:, None]
    j = np.arange(64)[None, :]
    forced = ((j == 0) | (j == tb) | (j == tb - 1)).astype(np.float32)
    cb = (j <= tb).astype(np.float32)
    c["selc"] = np.stack([1.0 - forced, forced * (1e9 + 1024.0 * j), cb, (cb - 1.0) * 1e30], axis=1).astype(np.float32)
    E = np.zeros((64, 32, 128), np.float32)
    for kt in range(32):
        E[2 * kt, kt, :64] = 1.0
        E[2 * kt + 1, kt, 64:] = 1.0
    c["eall"] = E.astype(ml_dtypes.bfloat16)
    mm = np.arange(128)[:, None, None]
    dd = np.arange(4)[None, :, None]
    nn = np.arange(512)[None, None, :]
    c["cz"] = ((128 * dd + mm) <= nn).astype(ml_dtypes.bfloat16)
    rr = np.arange(8)[None, :, None] - 3
    diff = 128 * rr + nn - mm
    c["wm"] = ((diff >= 0) & (diff < 512)).astype(ml_dtypes.bfloat16)
    c["stri"] = (s[:, None] < s[None, :]).astype(np.float32)
    c["iota"] = np.broadcast_to(np.arange(128, dtype=np.float32)[None, :], (128, 128)).copy()
    c["base8"] = (np.arange(8, dtype=np.float32)[None, :] * 128 + np.arange(128, dtype=np.float32)[:, None]).astype(np.float32)
    return c


def swap_halves(w, ncols):
    w = w[:, :ncols].reshape(w.shape[0], ncols // 64, 2, 32)
    return np.ascontiguousarray(w[:, :, ::-1, :].reshape(w.shape[0], ncols))


def build(T, stages=None, dbg=False):
    NT = T // 512
    NS = T // 128
    nc = bass.Bass("TRN2", target_bir_lowering=False)
    import os as _os0
    em = Em(nc, same_engine_sync=(_os0.environ.get('SES', '1') == '1'))
    on = lambda s: stages is None or s in stages

    em.declared = []
    any_moe = stages is None or any(st.startswith("moe") for st in stages)

    def din(name, shape, dt=F32):
        if name in ("moe_w_up", "moe_w_down") and not any_moe:
            return None
        em.declared.append(name)
        return em.dram(name, shape, dt, kind="ExternalInput")

    x_in = din("x", [T, D])
    c_in = din("c", [128, 8])
    pos_in = din("pos", [1, T], I32)
    ada_w = din("ada_w", [4, D, 6 * D])
    ada_b = din("ada_b", [4, 6 * D])
    ln_g = din("ln_g", [8, D])
    ln_b = din("ln_b", [8, D])
    ssm_in_w = din("ssm_in_w", [2, D, 5152])
    ssm_conv_w = din("ssm_conv_w", [2, 4, 3072])
    ssm_conv_b = din("ssm_conv_b", [2, 3072])
    ssm_dt_bias = din("ssm_dt_bias", [2, 32])
    ssm_a_log = din("ssm_a_log", [2, 32])
    ssm_d = din("ssm_d", [2, 32])
    ssm_norm_w = din("ssm_norm_w", [2, 2048])
    ssm_out_w = din("ssm_out_w", [2, 2048, D])
    kv_ada_w = din("kv_ada_w", [D, 2 * D])
    kv_ada_b = din("kv_ada_b", [1, 2 * D])
    kv_w = din("kv_w", [D, 1536])
    kv_w_sw = din("kv_w_sw", [D, 768])
    cmp_pos = din("cmp_pos", [32, 64])
    phi_w1 = [din("phi_k_w1", [2048, 256]), din("phi_v_w1", [2048, 256])]
    phi_w2 = [din("phi_k_w2", [256, 64]), din("phi_v_w2", [256, 64])]
    nsa_q_w = din("nsa_q_w", [2, D, 1072])
    nsa_q_w_sw = din("nsa_q_w_sw", [2, D, 1024])
    nsa_o_w = din("nsa_o_w", [2, D, D])
    router_w = din("router_w", [4, D, 32])
    router_b = din("router_b", [4, 32])
    moe_w_up = din("moe_w_up", [4, 32, D, 2 * D])
    moe_b_up = din("moe_b_up", [4, 32, 2 * D])
    moe_w_down = din("moe_w_down", [4, 32, D, D])
    moe_b_down = din("moe_b_down", [4, 32, D])
    k_ident = din("k_ident", [128, 128])
    k_tri = din("k_tri", [128, 128])
    k_ropec = din("k_ropec", [64, 2])
    k_mcmp = din("k_mcmp", [2, 128, T])
    k_ovaug = din("k_ovaug", [2, 128, 65])
    k_selc = din("k_selc", [T, 4, 64])
    k_eall = din("k_eall", [64, 32, 128], BF16)
    k_cz = din("k_cz", [128, 4, 512], BF16)
    k_wm = din("k_wm", [128, 8, 512], BF16)
    k_stri = din("k_stri", [128, 128])
    k_iota = din("k_iota", [128, 128])
    k_base8 = din("k_base8", [128, 8])

    y_out = em.dram("y", [T, D], F32, kind="ExternalOutput")
    sk = "ExternalOutput" if dbg else "Internal"
    xs_d = [em.dram("xA", [8, 128, T], F32, kind=sk), em.dram("xB", [8, 128, T], F32, kind=sk)]
    ymix_d = em.dram("ymix", [8, 128, T], F32, kind=sk)
    zs_d = em.dram("zs_tok", [T, 2048], BF16, kind=sk)
    dt_d = em.dram("dt_tok", [T, 32], F32, kind=sk)
    xbc_d = em.dram("xbcT", [24, 128, T], BF16, kind=sk)
    rope_d = em.dram("rope", [2, 64, T], F32, kind=sk)
    KT_d = em.dram("KT", [3, 4, 64, T], BF16, kind=sk)
    VT_d = em.dram("VT", [T, 512], BF16, kind=sk)
    KC_d = em.dram("KC", [4, 64, 256], BF16, kind=sk)
    VC_d = em.dram("VC", [4, 2, 128, 64], BF16, kind=sk)
    NBLK = (4 * T + 32 * 512) // 512
    RROWS = NBLK * 512
    hs_d = em.dram("h_sorted", [RROWS, D], BF16)
    ys_d = em.dram("y_sorted", [RROWS, D], F32)

    ident = em.sb([128, 128], F32, "ident", persist=True)
    identb = em.sb([128, 128], BF16, "identb", persist=True)
    ones32 = em.sb([128, 128], F32, "ones32", persist=True)
    onesb = em.sb([128, 64], BF16, "onesb", persist=True)
    mods = em.sb([128, 4, 48], F32, "mods", persist=True)
    kvmod = em.sb([128, 16], F32, "kvmod", persist=True)
    lng = em.sb([128, 8, 8], F32, "lng", persist=True)
    lnb = em.sb([128, 8, 8], F32, "lnb", persist=True)
    epsb = em.sb([128, 1], F32, "epsb", persist=True)
    eps5 = em.sb([128, 1], F32, "eps5", persist=True)

    em.dma("sp", ident[:], k_ident, writes=[ident])
    em.op("dve", lambda e: e.tensor_copy(identb[:], ident[:]), reads=[ident], writes=[identb])
    em.op("dve", lambda e: e.memset(ones32[:], 1.0), writes=[ones32])
    em.op("dve", lambda e: e.memset(onesb[:], 1.0), writes=[onesb])
    em.op("dve", lambda e: e.memset(epsb[:], EPS_A), writes=[epsb])
    em.op("dve", lambda e: e.memset(eps5[:], LN_EPS), writes=[eps5])

    def rowsT(dst_ap, src_rows_ap, R, C, pbuf, dstbuf, tmp=None, tmp_ap=None):
        if tmp is None:
            tmp = em.sb([R, C * 128], F32, "rowsT")
            tmp_ap = tmp[:]
        em.dma("sp", tmp_ap[0:R, 0:C * 128], src_rows_ap, writes=[tmp])
        for c in range(C):
            em.op("pe", lambda e: e.transpose(pbuf[:, c * R:(c + 1) * R], tmp_ap[0:R, c * 128:(c + 1) * 128], ident[0:R, 0:R]),
                  reads=[tmp, ident], writes=[pbuf])
        em.op("dve", lambda e: e.tensor_copy(dst_ap, pbuf[:, 0:C * R].rearrange("p (c r) -> p c r", r=R)),
              reads=[pbuf], writes=[dstbuf])

    if on("mod"):
        with em.phase():
            pm = em.ps([128, 512], F32)
            cT = em.sb([128, 8])
            cact = em.sb([128, 8])
            em.dma("sp", cT[:], c_in, writes=[cT])
            em.op("act", lambda e: e.activation(out=cact[:], in_=cT[:], func=AF.Silu), reads=[cT], writes=[cact])
            rowsT(lng[:], ln_g, 8, 8, pm, lng)
            rowsT(lnb[:], ln_b, 8, 8, pm, lnb)
            wrot = em.rot(2, [128, 8, 512], F32, "adaw")
            pmod = em.ps([128, 64], F32)
            bT = em.sb([128, 48, 4])
            rowsT(bT[:], ada_b, 4, 48, pm, bT)
            bTk = em.sb([128, 16, 1])
            rowsT(bTk[:], kv_ada_b, 1, 16, pm, bTk)
            for i in range(5):
                ncol = 48 if i < 4 else 16
                for cb in range(ncol // 4):
                    wk = wrot.next()
                    src = ada_w[i][:, cb * 512:(cb + 1) * 512] if i < 4 else kv_ada_w[:, cb * 512:(cb + 1) * 512]
                    em.dma("sp", wk[:], src.rearrange("(k p) f -> p k f", p=128), writes=[wk])
                    for o4 in range(4):
                        oc = cb * 4 + o4
                        for k in range(8):
                            em.op("pe", lambda e: e.matmul(pmod[:, oc:oc + 1], wk[:, k, o4 * 128:(o4 + 1) * 128], cact[:, k:k + 1],
                                                           start=(k == 0), stop=(k == 7)),
                                  reads=[wk, cact], writes=[pmod])
                dst = mods[:, i, :] if i < 4 else kvmod[:]
                dbuf = mods if i < 4 else kvmod
                bsl = bT[:, :, i] if i < 4 else bTk[:, :, 0]
                em.op("dve", lambda e: e.tensor_tensor(dst, pmod[:, 0:ncol], bsl, ALU.add),
                      reads=[pmod, bT, bTk], writes=[dbuf])
            for i in range(4):
                for c0 in (8, 32):
                    em.op("dve", lambda e: e.tensor_scalar_add(mods[:, i, c0:c0 + 8], mods[:, i, c0:c0 + 8], 1.0),
                          reads=[mods], writes=[mods])
                for c0 in (16, 40):
                    em.op("dve", lambda e: e.tensor_scalar(mods[:, i, c0:c0 + 8], mods[:, i, c0:c0 + 8], 1.0, 1.0 / ALPHA,
                                                           ALU.add, ALU.mult), reads=[mods], writes=[mods])
            em.op("dve", lambda e: e.tensor_scalar_add(kvmod[:, 8:16], kvmod[:, 8:16], 1.0), reads=[kvmod], writes=[kvmod])

    if dbg and on("mod"):
        dbg_mods = em.dram("dbg_mods", [128, 4, 48], F32, kind="ExternalOutput")
        em.dma("sp", dbg_mods, mods[:], reads=[mods])

    if on("in"):
        with em.phase():
            xin = em.rot(2, [128, 4, D], F32, "xin")
            xo = em.rot(2, [128, 8, 512], F32, "xo")
            pp = Rot([em.ps([128, 512], F32) for _ in range(4)])
            for i in range(NT):
                a = xin.next()
                em.dma("sp", a[:], x_in[i * 512:(i + 1) * 512, :].rearrange("(s p) f -> p s f", p=128), writes=[a])
                o = xo.next()
                for c in range(8):
                    p = pp.next()
                    for s in range(4):
                        em.op("pe", lambda e: e.transpose(p[:, s * 128:(s + 1) * 128], a[:, s, c * 128:(c + 1) * 128], ident[:]),
                              reads=[a, ident], writes=[p])
                    eng = "act" if c % 2 else "dve"
                    if eng == "act":
                        em.op("act", lambda e: e.copy(o[:, c, :], p[:]), reads=[p], writes=[o])
                    else:
                        em.op("dve", lambda e: e.tensor_copy(o[:, c, :], p[:]), reads=[p], writes=[o])
                em.dma("sp", xs_d[0][:, :, i * 512:(i + 1) * 512].rearrange("c p t -> p c t"), o[:], reads=[o])

    def load_mod_tile(xsrc, i, xt_r, dst, sc1, sh, dst_buf):
        for c in range(8):
            xt = xt_r.next()
            em.dma("sp", xt[:], xsrc[c, :, i * 512:(i + 1) * 512], writes=[xt])
            eng = "dve" if c % 2 == 0 else "pool"
            em.op(eng, lambda e: e.tensor_scalar(dst(c), xt[:], sc1[:, c:c + 1], sh[:, c:c + 1], ALU.mult, ALU.add),
                  reads=[xt, mods, kvmod], writes=[dst_buf])

    def post_norm(xsrc, ysrc, xdst, g1a, r, final):
        with em.phase():
            xt_r = em.rot(2, [128, 8, 512], F32, "lnx")
            yt_r = em.rot(2, [128, 8, 512], F32, "lny")
            z_r = em.rot(2, [128, 8, 512], F32, "lnz")
            sq_r = em.rot(1, [128, 8, 512], F32, "lnsq")
            xo_r = em.rot(2, [128, 8, 512], F32, "lno")
            psum_s = em.ps([128, 512], F32)
            psum_q = em.ps([128, 512], F32)
            mean = em.sb([128, 512]); msq = em.sb([128, 512]); var = em.sb([128, 512]); rstd = em.sb([128, 512])
            if final:
                pT = [em.ps([128, 512], F32), em.ps([128, 512], F32)]
                ot_r = em.rot(2, [128, 4, D], F32, "lnot")
            for i in range(NT):
                sl = slice(i * 512, (i + 1) * 512)
                xt = xt_r.next(); yt = yt_r.next(); z = z_r.next(); sq = sq_r.next(); xo = xo_r.next()
                em.dma("sp", xt[:], xsrc[:, :, sl].rearrange("c p t -> p c t"), writes=[xt])
                em.dma("sp", yt[:], ysrc[:, :, sl].rearrange("c p t -> p c t"), writes=[yt])
                for c in range(8):
                    eng = "dve"
                    em.op(eng, lambda e: e.scalar_tensor_tensor(z[:, c, :], yt[:, c, :], g1a[:, c:c + 1], xt[:, c, :], ALU.mult, ALU.add),
                          reads=[yt, xt, mods], writes=[z])
                em.op("act", lambda e: e.activation(out=sq[:], in_=z[:], func=AF.Square), reads=[z], writes=[sq])
                for c in range(8):
                    em.op("pe", lambda e: e.matmul(psum_s[:], ones32[:], z[:, c, :], start=(c == 0), stop=(c == 7)),
                          reads=[ones32, z], writes=[psum_s])
                for c in range(8):
                    em.op("pe", lambda e: e.matmul(psum_q[:], ones32[:], sq[:, c, :], start=(c == 0), stop=(c == 7)),
                          reads=[ones32, sq], writes=[psum_q])
                em.op("act", lambda e: e.mul(mean[:], psum_s[:], 1.0 / D), reads=[psum_s], writes=[mean])
                em.op("pool", lambda e: e.tensor_tensor(msq[:], mean[:], mean[:], ALU.mult), reads=[mean], writes=[msq])
                em.op("dve", lambda e: e.scalar_tensor_tensor(var[:], psum_q[:], 1.0 / D, msq[:], ALU.mult, ALU.subtract),
                      reads=[psum_q, msq], writes=[var])
                em.op("act", lambda e: e.activation(out=var[:], in_=var[:], func=AF.Sqrt, bias=epsb[:], scale=1.0),
                      reads=[var, epsb], writes=[var])
                em.op("dve", lambda e: e.reciprocal(rstd[:], var[:]), reads=[var], writes=[rstd])
                for c in range(8):
                    em.op("pool", lambda e: e.tensor_tensor(z[:, c, :], z[:, c, :], mean[:], ALU.subtract), reads=[z, mean], writes=[z])
                    em.op("dve", lambda e: e.tensor_tensor(z[:, c, :], z[:, c, :], rstd[:], ALU.mult), reads=[z, rstd], writes=[z])
                    em.op("act", lambda e: e.activation(out=xo[:, c, :], in_=z[:, c, :], func=AF.Identity,
                                                        bias=lnb[:, c, r:r + 1], scale=lng[:, c, r:r + 1]),
                          reads=[z, lng, lnb], writes=[xo])
                if not final:
                    em.dma("sp", xdst[:, :, sl].rearrange("c p t -> p c t"), xo[:], reads=[xo])
                else:
                    ot = ot_r.next()
                    for s in range(4):
                        for c in range(8):
                            p = pT[c // 4]
                            em.op("pe", lambda e: e.transpose(p[:, (c % 4) * 128:(c % 4 + 1) * 128], xo[:, c, s * 128:(s + 1) * 128], ident[:]),
                                  reads=[xo, ident], writes=[p])
                        em.op("act", lambda e: e.copy(ot[:, s, 0:512], pT[0][:]), reads=[pT[0]], writes=[ot])
                        em.op("dve", lambda e: e.tensor_copy(ot[:, s, 512:1024], pT[1][:]), reads=[pT[1]], writes=[ot])
                    em.dma("sp", y_out[sl, :].rearrange("(s p) f -> p s f", p=128), ot[:], reads=[ot])

    def moe_phase_dense(l, xsrc, ydst):
        TB = min(1024, T)
        NJ = TB // 512
        sc1 = mods[:, l, 32:40]
        sh = mods[:, l, 24:32]
        with em.phase():
            rw = em.sb([128, 8, 32], F32, "rw")
            em.dma("sp", rw[:], router_w[l].rearrange("(k p) e -> p k e", p=128), writes=[rw])
            rb = em.sb([128, 32], F32, "rb")
            em.dma("sp", rb[:], router_b[l:l + 1, :].to_broadcast([128, 32]), writes=[rb])
            P = [em.ps([128, 512], F32) for _ in range(7)]
            bup = em.sb([128, 16, 32], F32, "bup")
            bdn = em.sb([32, D], BF16, "bdn")
            em.dma("pool", bdn[:], moe_b_down[l], writes=[bdn])
            acc = em.sb([128, 8, TB], F32, "acc")
            hb = em.sb([128, 8, TB], BF16, "hb")
            gT = em.sb([32, TB], BF16, "gT")
            h32_r = em.rot(1, [128, 8, 512], F32, "mh32")
            xt_r = em.rot(2, [128, 512], F32, "mx")
            rowsT(bup[:], moe_b_up[l], 32, 16, P[0], bup, tmp=h32_r.bufs[0], tmp_ap=h32_r.bufs[0][:].rearrange("p c t -> p (c t)"))
            wu_r = em.rot(2, [128, 8, 2048], BF16, "wu")
            wd_r = em.rot(2, [128, 8, 1024], BF16, "wd")
            hg_r = em.rot(2, [128, 8, 512], BF16, "hg")
            gsb_r = em.rot(1, [128, 512], F32, "gsb")
            glu_r = em.rot(1, [128, 512], F32, "glu")
            sig_r = em.rot(1, [128, 512], F32, "sig")
            lin_r = em.rot(1, [128, 512], F32, "lin")
            t1_r = em.rot(1, [128, 512], F32, "t1")
            sm = {k: em.sb([128, 32], F32, "sm" + k) for k in ("lg", "mask", "e", "g")}
            m8 = em.sb([128, 8]); nm = em.sb([128, 1]); ssum = em.sb([128, 1]); rs = em.sb([128, 1])
            pup = Rot(P[0:4]); pdn = Rot(P[4:6]); pg = P[6]
            for tb in range(T // TB):
                for j in range(NJ):
                    h32 = h32_r.next()
                    ti = tb * NJ + j
                    load_mod_tile(xsrc, ti, xt_r, lambda c: h32[:, c, :], sc1, sh, h32)
                    em.op("act", lambda e: e.copy(hb[:, :, j * 512:(j + 1) * 512], h32[:]), reads=[h32], writes=[hb])
                    for s in range(4):
                        pl = P[4]
                        for c in range(8):
                            em.op("pe", lambda e: e.matmul(pl[:, 0:32], h32[:, c, s * 128:(s + 1) * 128], rw[:, c, :], start=(c == 0), stop=(c == 7)),
                                  reads=[h32, rw], writes=[pl])
                        lg, mask, ee, g = sm["lg"], sm["mask"], sm["e"], sm["g"]
                        em.op("dve", lambda e: e.tensor_tensor(lg[:], pl[:, 0:32], rb[:], ALU.add), reads=[pl, rb], writes=[lg])
                        em.op("dve", lambda e: e.max(m8[:], lg[:]), reads=[lg], writes=[m8])
                        em.op("dve", lambda e: e.tensor_scalar(mask[:], lg[:], m8[:, 3:4], None, ALU.is_ge), reads=[lg, m8], writes=[mask])
                        em.op("dve", lambda e: e.tensor_scalar_mul(nm[:], m8[:, 0:1], -1.0), reads=[m8], writes=[nm])
                        em.op("act", lambda e: e.activation(out=ee[:], in_=lg[:], func=AF.Exp, bias=nm[:], scale=1.0), reads=[lg, nm], writes=[ee])
                        em.op("dve", lambda e: e.tensor_tensor(ee[:], ee[:], mask[:], ALU.mult), reads=[ee, mask], writes=[ee])
                        em.op("dve", lambda e: e.reduce_sum(ssum[:], ee[:], axis=mybir.AxisListType.X), reads=[ee], writes=[ssum])
                        em.op("dve", lambda e: e.reciprocal(rs[:], ssum[:]), reads=[ssum], writes=[rs])
                        em.op("dve", lambda e: e.tensor_scalar_mul(g[:], ee[:], rs[:, 0:1]), reads=[ee, rs], writes=[g])
                        pt = P[5]
                        em.op("pe", lambda e: e.transpose(pt[0:32, 0:128], g[:], ident[:]), reads=[g, ident], writes=[pt])
                        em.op("act", lambda e: e.copy(gT[:, j * 512 + s * 128: j * 512 + (s + 1) * 128], pt[0:32, 0:128]), reads=[pt], writes=[gT])
                for j in range(NJ):
                    for oc in range(8):
                        po = pdn.next()
                        em.op("pe", lambda e: e.matmul(po[:], bdn[:, oc * 128:(oc + 1) * 128], gT[:, j * 512:(j + 1) * 512], start=True, stop=True),
                              reads=[bdn, gT], writes=[po])
                        em.op("act", lambda e: e.copy(acc[:, oc, j * 512:(j + 1) * 512], po[:]), reads=[po], writes=[acc])
                def issue_w(ex_):
                    wu_ = wu_r.next(); wd_ = wd_r.next()
                    em.dma("pool", wu_[:], moe_w_up[l, ex_].rearrange("(k p) f -> p k f", p=128), writes=[wu_])
                    em.dma("pool", wd_[:], moe_w_down[l, ex_].rearrange("(k p) f -> p k f", p=128), writes=[wd_])
                    return wu_, wd_
                nxt = issue_w(0)
                for ex in range(32):
                    wu, wd = nxt
                    if ex + 1 < 32:
                        nxt = issue_w(ex + 1)
                    for j in range(NJ):
                        js = slice(j * 512, (j + 1) * 512)
                        gsb = gsb_r.next()
                        em.op("pe", lambda e: e.matmul(pg[:], identb[0:32, ex:ex + 1].to_broadcast([32, 128]), gT[:, js], start=True, stop=True), reads=[identb, gT], writes=[pg])
                        em.op("act", lambda e: e.copy(gsb[:], pg[:]), reads=[pg], writes=[gsb])
                        hg = hg_r.next()
                        for c in range(8):
                            p1 = pup.next(); p2 = pup.next()
                            for k in range(8):
                                em.op("pe", lambda e: e.matmul(p1[:], wu[:, k, c * 128:(c + 1) * 128], hb[:, k, js], start=(k == 0), stop=(k == 7)),
                                      reads=[wu, hb], writes=[p1])
                            for k in range(8):
                                em.op("pe", lambda e: e.matmul(p2[:], wu[:, k, 1024 + c * 128:1024 + (c + 1) * 128], hb[:, k, js], start=(k == 0), stop=(k == 7)),
                                      reads=[wu, hb], writes=[p2])
                            glu = glu_r.next(); sig = sig_r.next(); lin = lin_r.next(); t1 = t1_r.next()
                            em.op("dve", lambda e: e.tensor_scalar(glu[:], p1[:], bup[:, c, ex:ex + 1], 7.0, ALU.add, ALU.min), reads=[p1, bup], writes=[glu])
                            em.op("act", lambda e: e.activation(out=sig[:], in_=glu[:], func=AF.Sigmoid, scale=1.702), reads=[glu], writes=[sig])
                            em.op("dve", lambda e: e.tensor_scalar(lin[:], p2[:], bup[:, 8 + c, ex:ex + 1], 7.0, ALU.add, ALU.min), reads=[p2, bup], writes=[lin])
                            em.op("pool", lambda e: e.tensor_scalar(lin[:], lin[:], -7.0, 1.0, ALU.max, ALU.add), reads=[lin], writes=[lin])
                            em.op("pool", lambda e: e.tensor_tensor(t1[:], glu[:], sig[:], ALU.mult), reads=[glu, sig], writes=[t1])
                            em.op("pool", lambda e: e.tensor_tensor(lin[:], lin[:], gsb[:], ALU.mult), reads=[lin, gsb], writes=[lin])
                            em.op("dve", lambda e: e.tensor_tensor(hg[:, c, :], t1[:], lin[:], ALU.mult), reads=[t1, lin], writes=[hg])
                        for oc in range(8):
                            po = pdn.next()
                            for k in range(8):
                                em.op("pe", lambda e: e.matmul(po[:], wd[:, k, oc * 128:(oc + 1) * 128], hg[:, k, :], start=(k == 0), stop=(k == 7)),
                                      reads=[wd, hg], writes=[po])
                            em.op("dve", lambda e: e.tensor_tensor(acc[:, oc, js], po[:], acc[:, oc, js], ALU.add), reads=[po, acc], writes=[acc])
                em.dma("sp", ydst[:, :, tb * TB:(tb + 1) * TB].rearrange("c p t -> p c t"), acc[:], reads=[acc])

    U32 = mybir.dt.uint32
    hs_zeroed = [False]

    def idma(kind, out, in_, idx_ap, reads=(), writes=(), bound=None):
        q = "pool"
        em._deps(q, reads, writes)
        pool = em.dma_pool[q]
        slot = em.dma_idx[q] % len(pool)
        k = pool[slot]
        em.dma_idx[q] += 1
        if 16 * (em.dma_uses[k] + 1) > SEM_LIMIT:
            k = em._new_dma_sem(q)
            pool[slot] = k
        if em.dma_uses[k] > 0:
            em._need(q, (k, 16 * em.dma_uses[k]))
        em.dma_uses[k] += 1
        off = bass.IndirectOffsetOnAxis(ap=idx_ap, axis=0)
        if kind == "g":
            ins = nc.gpsimd.indirect_dma_start(out=out, out_offset=None, in_=in_, in_offset=off)
        else:
            ins = nc.gpsimd.indirect_dma_start(out=out, out_offset=off, in_=in_, in_offset=None)
        ins.then_inc(em.sem[k], 16)
        em._record((k, 16 * em.dma_uses[k]), reads, writes)
        em.n_ins += 1

    def moe_phase(l, xsrc, ydst):
        sc1 = mods[:, l, 32:40]
        sh = mods[:, l, 24:32]
        NQ = NS * 4
        MAGIC = 12582912.0
        desti = em.sb([128, NS, 4], I32, "desti", persist=True) if not hasattr(em, "_moe_p") else em._moe_p[0]
        eidi = em.sb([128, NS, 4], I32, "eidi", persist=True) if not hasattr(em, "_moe_p") else em._moe_p[1]
        g4 = em.sb([128, NS, 4], F32, "g4", persist=True) if not hasattr(em, "_moe_p") else em._moe_p[2]
        widx = em.sb([128, NBLK, 8], I32, "widx", persist=True) if not hasattr(em, "_moe_p") else em._moe_p[3]
        blki = em.sb([128, NBLK], I32, "blki", persist=True) if not hasattr(em, "_moe_p") else em._moe_p[4]
        em._moe_p = (desti, eidi, g4, widx, blki)

        with em.phase():
            rw = em.sb([128, 8, 32], F32, "rw")
            em.dma("sp", rw[:], router_w[l].rearrange("(k p) e -> p k e", p=128), writes=[rw])
            rb = em.sb([128, 32], F32, "rb")
            em.dma("sp", rb[:], router_b[l:l + 1, :].to_broadcast([128, 32]), writes=[rb])
            stri = em.sb([128, 128], F32, "stri"); em.dma("sp", stri[:], k_stri, writes=[stri])
            iota = em.sb([128, 128], F32, "iota"); em.dma("sp", iota[:], k_iota, writes=[iota])
            base8 = em.sb([128, 8], F32, "base8"); em.dma("sp", base8[:], k_base8, writes=[base8])
            if not hs_zeroed[0]:
                zt = em.sb([128, 4, D], BF16, "zt")
                em.op("dve", lambda e: e.memset(zt[:], 0.0), writes=[zt])
                for k in range(NBLK):
                    em.dma("sp", hs_d[k * 512:(k + 1) * 512, :].rearrange("(j p) f -> p j f", p=128), zt[:], reads=[zt])
                hs_zeroed[0] = True
            P = [em.ps([128, 512], F32) for _ in range(3)]
            ptb = [em.ps([128, 1024], BF16) for _ in range(2)]
            h32_r = em.rot(1, [128, 8, 512], F32, "mh32")
            hbt_r = em.rot(1, [128, 8, 512], BF16, "mhb")
            xt_r = em.rot(2, [128, 512], F32, "mx")
            htok = [em.sb([128, D], BF16, "htok") for _ in range(NS)]
            eidf = em.sb([128, NS, 4], F32, "eidf")
            rks = em.sb([128, NS, 4], F32, "rks")
            carry = em.sb([128, 32], F32, "carry")
            em.op("dve", lambda e: e.memset(carry[:], 0.0), writes=[carry])
            lg = em.sb([128, 32]); mask = em.sb([128, 32]); rank = em.sb([128, 32])
            m8 = em.sb([128, 8]); mi = em.sb([128, 8], U32); nm = em.sb([128, 1]); e4 = em.sb([128, 4]); ssum = em.sb([128, 1]); rs = em.sb([128, 1])
            oh4 = em.sb([128, 4, 32]);
            for j in range(NT):
                h32 = h32_r.next(); hbt = hbt_r.next()
                load_mod_tile(xsrc, j, xt_r, lambda c: h32[:, c, :], sc1, sh, h32)
                em.op("act", lambda e: e.copy(hbt[:], h32[:]), reads=[h32], writes=[hbt])
                for s_ in range(4):
                    ti = j * 4 + s_
                    ss = slice(s_ * 128, (s_ + 1) * 128)
                    pl = P[0]
                    for c in range(8):
                        em.op("pe", lambda e: e.matmul(pl[:, 0:32], h32[:, c, ss], rw[:, c, :], start=(c == 0), stop=(c == 7)), reads=[h32, rw], writes=[pl])
                    em.op("dve", lambda e: e.tensor_tensor(lg[:], pl[:, 0:32], rb[:], ALU.add), reads=[pl, rb], writes=[lg])
                    em.op("dve", lambda e: e.max(m8[:], lg[:]), reads=[lg], writes=[m8])
                    em.op("dve", lambda e: e.max_index(mi[:], m8[:], lg[:]), reads=[lg, m8], writes=[mi])
                    em.op("dve", lambda e: e.tensor_copy(eidf[:, ti, :], mi[:, 0:4]), reads=[mi], writes=[eidf])
                    em.op("dve", lambda e: e.tensor_scalar(mask[:], lg[:], m8[:, 3:4], None, ALU.is_ge), reads=[lg, m8], writes=[mask])
                    em.op("dve", lambda e: e.tensor_scalar_mul(nm[:], m8[:, 0:1], -1.0), reads=[m8], writes=[nm])
                    em.op("act", lambda e: e.activation(out=e4[:], in_=m8[:, 0:4], func=AF.Exp, bias=nm[:], scale=1.0), reads=[m8, nm], writes=[e4])
                    em.op("dve", lambda e: e.reduce_sum(ssum[:], e4[:], axis=mybir.AxisListType.X), reads=[e4], writes=[ssum])
                    em.op("dve", lambda e: e.reciprocal(rs[:], ssum[:]), reads=[ssum], writes=[rs])
                    em.op("dve", lambda e: e.tensor_scalar_mul(g4[:, ti, :], e4[:], rs[:, 0:1]), reads=[e4, rs], writes=[g4])
                    pr = P[1]
                    em.op("pe", lambda e: e.matmul(pr[:, 0:32], stri[:], mask[:], start=True, stop=True), reads=[stri, mask], writes=[pr])
                    em.op("pe", lambda e: e.matmul(pr[:, 32:64], ones32[:], mask[:], start=True, stop=True), reads=[ones32, mask], writes=[pr])
                    em.op("dve", lambda e: e.tensor_tensor(rank[:], pr[:, 0:32], carry[:], ALU.add), reads=[pr, carry], writes=[rank])
                    em.op("dve", lambda e: e.tensor_tensor(carry[:], carry[:], pr[:, 32:64], ALU.add), reads=[pr, carry], writes=[carry])
                    em.op("dve", lambda e: e.tensor_tensor(oh4[:], iota[:, 0:32].unsqueeze(1).to_broadcast([128, 4, 32]),
                                                           eidf[:, ti, :].unsqueeze(2).to_broadcast([128, 4, 32]), ALU.is_equal), reads=[iota, eidf], writes=[oh4])
                    em.op("dve", lambda e: e.tensor_tensor(oh4[:], oh4[:], rank[:].unsqueeze(1).to_broadcast([128, 4, 32]), ALU.mult), reads=[oh4, rank], writes=[oh4])
                    em.op("dve", lambda e: e.reduce_sum(rks[:, ti, :], oh4[:], axis=mybir.AxisListType.X), reads=[oh4], writes=[rks])
                    pt = ptb[ti % 2]
                    for c in range(8):
                        em.op("pe", lambda e: e.transpose(pt[:, c * 128:(c + 1) * 128], hbt[:, c, ss], identb[:]), reads=[hbt, identb], writes=[pt])
                    em.op("act", lambda e: e.copy(htok[ti][:], pt[:]), reads=[pt], writes=[htok[ti]])
            cnt = carry
            nb = em.sb([128, 32]); pad = em.sb([128, 32]); ca = em.sb([128, 32]); cb2 = em.sb([128, 32]); poff = em.sb([128, 32])
            em.op("dve", lambda e: e.tensor_scalar(nb[:], cnt[:], 511.0, 1.0 / 512.0, ALU.add, ALU.mult), reads=[cnt], writes=[nb])
            em.op("dve", lambda e: e.tensor_scalar(nb[:], nb[:], -0.5 + 1.0 / 1024.0, MAGIC, ALU.add, ALU.add), reads=[nb], writes=[nb])
            em.op("dve", lambda e: e.tensor_scalar_add(nb[:], nb[:], -MAGIC), reads=[nb], writes=[nb])
            em.op("dve", lambda e: e.tensor_scalar_mul(pad[:], nb[:], 512.0), reads=[nb], writes=[pad])
            em.op("dve", lambda e: e.tensor_copy(ca[:], pad[:]), reads=[pad], writes=[ca])
            src_, dst_ = ca, cb2
            for shf in (1, 2, 4, 8, 16):
                em.op("dve", lambda e: e.tensor_copy(dst_[:, 0:shf], src_[:, 0:shf]), reads=[src_], writes=[dst_])
                em.op("dve", lambda e: e.tensor_tensor(dst_[:, shf:32], src_[:, shf:32], src_[:, 0:32 - shf], ALU.add), reads=[src_, dst_], writes=[dst_])
                src_, dst_ = dst_, src_
            cum = src_
            em.op("dve", lambda e: e.tensor_tensor(poff[:], cum[:], pad[:], ALU.subtract), reads=[cum, pad], writes=[poff])
            oh = em.sb([128, NQ, 32], F32, "ohall")
            pofft = em.sb([128, NQ], F32, "pofft")
            em.op("dve", lambda e: e.tensor_tensor(oh[:], iota[:, 0:32].unsqueeze(1).to_broadcast([128, NQ, 32]),
                                                   eidf[:].rearrange("p a b -> p (a b)").unsqueeze(2).to_broadcast([128, NQ, 32]), ALU.is_equal), reads=[iota, eidf], writes=[oh])
            em.op("dve", lambda e: e.tensor_tensor(oh[:], oh[:], poff[:].unsqueeze(1).to_broadcast([128, NQ, 32]), ALU.mult), reads=[oh, poff], writes=[oh])
            em.op("dve", lambda e: e.reduce_sum(pofft[:], oh[:], axis=mybir.AxisListType.X), reads=[oh], writes=[pofft])
            em.op("dve", lambda e: e.tensor_tensor(pofft[:], pofft[:], rks[:].rearrange("p a b -> p (a b)"), ALU.add), reads=[pofft, rks], writes=[pofft])
            em.op("dve", lambda e: e.tensor_copy(desti[:].rearrange("p a b -> p (a b)"), pofft[:]), reads=[pofft], writes=[desti])
            em.op("dve", lambda e: e.tensor_copy(eidi[:], eidf[:]), reads=[eidf], writes=[eidi])
            cmpk = em.sb([128, NBLK, 32], F32, "cmpk")
            kst = em.sb([128, NBLK], F32, "kst")
            blkf = em.sb([128, NBLK], F32, "blkf")
            wxf = em.sb([128, NBLK, 8], F32, "wxf")
            em.op("dve", lambda e: e.tensor_scalar_mul(kst[:], iota[:, 0:NBLK], 512.0), reads=[iota], writes=[kst])
            em.op("dve", lambda e: e.tensor_tensor(cmpk[:], cum[:].unsqueeze(1).to_broadcast([128, NBLK, 32]),
                                                   kst[:].unsqueeze(2).to_broadcast([128, NBLK, 32]), ALU.is_le), reads=[cum, kst], writes=[cmpk])
            em.op("dve", lambda e: e.reduce_sum(blkf[:], cmpk[:], axis=mybir.AxisListType.X), reads=[cmpk], writes=[blkf])
            em.op("dve", lambda e: e.tensor_scalar_min(blkf[:], blkf[:], 31.0), reads=[blkf], writes=[blkf])
            em.op("dve", lambda e: e.tensor_scalar_add(kst[:], blkf[:], 32.0 * l), reads=[blkf], writes=[kst])
            em.op("dve", lambda e: e.tensor_copy(blki[:], kst[:]), reads=[kst], writes=[blki])
            em.op("dve", lambda e: e.tensor_scalar(blkf[:], blkf[:], 1024.0, 32768.0 * l, ALU.mult, ALU.add), reads=[blkf], writes=[blkf])
            em.op("dve", lambda e: e.tensor_tensor(wxf[:], blkf[:].unsqueeze(2).to_broadcast([128, NBLK, 8]),
                                                   base8[:].unsqueeze(1).to_broadcast([128, NBLK, 8]), ALU.add), reads=[blkf, base8], writes=[wxf])
            em.op("dve", lambda e: e.tensor_copy(widx[:], wxf[:]), reads=[wxf], writes=[widx])
            for ti in range(NS):
                for sl_ in range(4):
                    idma("s", hs_d[:, :], htok[ti][:, :], desti[:, ti, sl_:sl_ + 1], reads=[htok[ti], desti])

        import os as _os
        if _os.environ.get("MOE_CUT") == "A":
            return
        wup_rows = moe_w_up.rearrange("l e r f -> (l e r) f")
        wdn_rows = moe_w_down.rearrange("l e r f -> (l e r) f")
        with em.phase():
            P = [em.ps([128, 512], F32) for _ in range(6)]
            pup = Rot(P[0:4]); pdn = Rot(P[4:6])
            ptb = em.ps([128, 512], BF16)
            hblk_r = em.rot(2, [128, 4, D], BF16, "hblk")
            hT_r = em.rot(2, [128, 8, 512], BF16, "hT")
            wu_r = em.rot(2, [128, 8, 2048], BF16, "wu")
            wd_r = em.rot(2, [128, 8, 1024], BF16, "wd")
            stgu_r = em.rot(2, [128, 2048], F32, "stgu")
            stgd_r = em.rot(2, [128, 1024], F32, "stgd")
            bub_r = em.rot(2, [2, 3072], BF16, "bub")
            hg_r = em.rot(1, [128, 8, 512], BF16, "hg")
            glu_r = em.rot(1, [128, 512], F32, "glu"); sig_r = em.rot(1, [128, 512], F32, "sig")
            lin_r = em.rot(1, [128, 512], F32, "lin"); t1_r = em.rot(1, [128, 512], F32, "t1")
            yrow_r = em.rot(2, [128, D], F32, "yrow")
            onesrow = em.sb([1, 512], BF16, "onesrow")
            em.op("dve", lambda e: e.memset(onesrow[:], 1.0), writes=[onesrow])

            def gather_piece(k, c):
                su = stgu_r.next(); sd = stgd_r.next()
                if _os_cap.environ.get("MOE_NOGATHER") and k > 0:
                    return su, sd
                idma("g", su[:, :], wup_rows, widx[:, k, c:c + 1], reads=[widx], writes=[su])
                idma("g", sd[:, :], wdn_rows, widx[:, k, c:c + 1], reads=[widx], writes=[sd])
                return su, sd

            def cast_piece(wu_, wd_, c, su, sd):
                em.op("act", lambda e: e.copy(wu_[:, c, :], su[:]), reads=[su], writes=[wu_])
                em.op("dve", lambda e: e.tensor_copy(wd_[:, c, :], sd[:]), reads=[sd], writes=[wd_])

            def bias_row(k):
                bb_ = bub_r.next()
                idma("g", bb_[0:2, 0:2048], moe_b_up.rearrange("l e f -> (l e) f"), blki[0:2, k:k + 1], reads=[blki], writes=[bb_])
                idma("g", bb_[0:2, 2048:3072], moe_b_down.rearrange("l e f -> (l e) f"), blki[0:2, k:k + 1], reads=[blki], writes=[bb_])
                return bb_

            wu = wu_r.next(); wd = wd_r.next()
            for c in range(8):
                su, sd = gather_piece(0, c)
                cast_piece(wu, wd, c, su, sd)
            bb = bias_row(0)
            def load_hblk(k_):
                hb_ = hblk_r.next()
                em.dma("sp", hb_[:], hs_d[k_ * 512:(k_ + 1) * 512, :].rearrange("(j p) f -> p j f", p=128), writes=[hb_])
                return hb_
            hblk_n = load_hblk(0)
            for k in range(NBLK):
                hblk = hblk_n; hT = hT_r.next()
                more = k + 1 < NBLK
                if more:
                    hblk_n = load_hblk(k + 1)
                if more:
                    wu_n = wu_r.next(); wd_n = wd_r.next()
                    bb_n = bias_row(k + 1)
                    pend = gather_piece(k + 1, 0)
                for c in range(8):
                    for j in range(4):
                        em.op("pe", lambda e: e.transpose(ptb[:, j * 128:(j + 1) * 128], hblk[:, j, c * 128:(c + 1) * 128], identb[:]), reads=[hblk, identb], writes=[ptb])
                    if c % 2:
                        em.op("act", lambda e: e.copy(hT[:, c, :], ptb[:]), reads=[ptb], writes=[hT])
                    else:
                        em.op("dve", lambda e: e.tensor_copy(hT[:, c, :], ptb[:]), reads=[ptb], writes=[hT])
                hg = hg_r.next()
                for c in range(8):
                    if more and c + 1 < 8:
                        nxt_piece = gather_piece(k + 1, c + 1)
                    p1 = pup.next(); p2 = pup.next()
                    for k8 in range(8):
                        em.op("pe", lambda e: e.matmul(p1[:], wu[:, k8, c * 128:(c + 1) * 128], hT[:, k8, :], start=(k8 == 0), stop=False), reads=[wu, hT], writes=[p1])
                    em.op("pe", lambda e: e.matmul(p1[:], bb[0:1, c * 128:(c + 1) * 128], onesrow[:], start=False, stop=True), reads=[bb, onesrow], writes=[p1])
                    for k8 in range(8):
                        em.op("pe", lambda e: e.matmul(p2[:], wu[:, k8, 1024 + c * 128:1024 + (c + 1) * 128], hT[:, k8, :], start=(k8 == 0), stop=False), reads=[wu, hT], writes=[p2])
                    em.op("pe", lambda e: e.matmul(p2[:], bb[0:1, 1024 + c * 128:1024 + (c + 1) * 128], onesrow[:], start=False, stop=True), reads=[bb, onesrow], writes=[p2])
                    glu = glu_r.next(); sig = sig_r.next(); lin = lin_r.next(); t1 = t1_r.next()
                    em.op("dve", lambda e: e.tensor_scalar_min(glu[:], p1[:], 7.0), reads=[p1], writes=[glu])
                    em.op("act", lambda e: e.activation(out=sig[:], in_=glu[:], func=AF.Sigmoid, scale=1.702), reads=[glu], writes=[sig])
                    em.op("dve", lambda e: e.tensor_scalar(lin[:], p2[:], 7.0, -7.0, ALU.min, ALU.max), reads=[p2], writes=[lin])
                    em.op("dve", lambda e: e.tensor_tensor(t1[:], glu[:], sig[:], ALU.mult), reads=[glu, sig], writes=[t1])
                    em.op("dve", lambda e: e.scalar_tensor_tensor(hg[:, c, :], lin[:], 1.0, t1[:], ALU.add, ALU.mult), reads=[lin, t1], writes=[hg])
                    if more:
                        cast_piece(wu_n, wd_n, c, *pend)
                        if c + 1 < 8:
                            pend = nxt_piece
                for j in range(4):
                    yrow = yrow_r.next()
                    for half in range(2):
                        po = pdn.next()
                        for k8 in range(8):
                            em.op("pe", lambda e: e.matmul(po[:], hg[:, k8, j * 128:(j + 1) * 128], wd[:, k8, half * 512:(half + 1) * 512], start=(k8 == 0), stop=False), reads=[hg, wd], writes=[po])
                        em.op("pe", lambda e: e.matmul(po[:], onesrow[0:1, 0:128], bb[0:1, 2048 + half * 512:2048 + (half + 1) * 512], start=False, stop=True), reads=[onesrow, bb], writes=[po])
                        if half:
                            em.op("act", lambda e: e.copy(yrow[:, 512:1024], po[:]), reads=[po], writes=[yrow])
                        else:
                            em.op("dve", lambda e: e.tensor_copy(yrow[:, 0:512], po[:]), reads=[po], writes=[yrow])
                    em.dma("sp", ys_d[k * 512 + j * 128:k * 512 + (j + 1) * 128, :], yrow[:], reads=[yrow])
                if more:
                    wu, wd, bb = wu_n, wd_n, bb_n

        if _os.environ.get("MOE_CUT") == "B":
            return
        with em.phase():
            pT = [em.ps([128, 512], F32) for _ in range(2)]
            yr_r = em.rot(3, [128, D], F32, "cyr")
            acc_r = em.rot(2, [128, D], F32, "cacc")
            yo_r = em.rot(2, [128, 8, 512], F32, "cyo")
            yo = None
            for ti in range(NS):
                acc = acc_r.next()
                for sl_ in range(4):
                    yr = yr_r.next()
                    idma("g", yr[:, :], ys_d[:, :], desti[:, ti, sl_:sl_ + 1], reads=[desti], writes=[yr])
                    if sl_ == 0:
                        em.op("dve", lambda e: e.tensor_scalar(acc[:], yr[:], g4[:, ti, 0:1], None, ALU.mult), reads=[yr, g4], writes=[acc])
                    else:
                        em.op("dve", lambda e: e.scalar_tensor_tensor(acc[:], yr[:], g4[:, ti, sl_:sl_ + 1], acc[:], ALU.mult, ALU.add), reads=[yr, g4, acc], writes=[acc])
                s_ = ti % 4
                if s_ == 0:
                    yo = yo_r.next()
                for c in range(8):
                    p = pT[c % 2]
                    em.op("pe", lambda e: e.transpose(p[:, 0:128], acc[:, c * 128:(c + 1) * 128], ident[:]), reads=[acc, ident], writes=[p])
                    if c % 2:
                        em.op("act", lambda e: e.copy(yo[:, c, s_ * 128:(s_ + 1) * 128], p[:, 0:128]), reads=[p], writes=[yo])
                    else:
                        em.op("dve", lambda e: e.tensor_copy(yo[:, c, s_ * 128:(s_ + 1) * 128], p[:, 0:128]), reads=[p], writes=[yo])
                if s_ == 3:
                    j = ti // 4
                    em.dma("sp", ydst[:, :, j * 512:(j + 1) * 512].rearrange("c p t -> p c t"), yo[:], reads=[yo])

    def mamba_phase(l, xsrc, ydst):
        sc1 = mods[:, l, 8:16]
        sh = mods[:, l, 0:8]
        inw = ssm_in_w[l]

        def load_hb(hb):
            xt_r = em.rot(3, [128, 512], F32, "mbx")
            for i in range(NT):
                load_mod_tile(xsrc, i, xt_r, lambda c: hb[:, c, i * 512:(i + 1) * 512], sc1, sh, hb)

        with em.phase():
            hb = em.sb([128, 8, T], BF16, "hb")
            load_hb(hb)
            wz = em.sb([128, 8, 2048], BF16, "wz")
            em.dma("pool", wz[:], inw[:, 0:2048].rearrange("(k p) f -> p k f", p=128), writes=[wz])
            wdt = em.sb([128, 8, 32], BF16, "wdt")
            em.dma("pool", wdt[:], inw[:, 5120:5152].rearrange("(k p) f -> p k f", p=128), writes=[wdt])
            dtb = em.sb([128, 32], F32, "dtb")
            em.dma("sp", dtb[:], ssm_dt_bias[l:l + 1, :].to_broadcast([128, 32]), writes=[dtb])
            pz = Rot([em.ps([128, 512], F32) for _ in range(4)])
            pd = em.ps([128, 32], F32)
            zs_r = em.rot(2, [128, 2048], BF16, "zs")
            d_r = {k: em.rot(2, [128, 32], F32, "d" + k) for k in ("x", "a", "e", "r")}
            for s in range(NS):
                ss = slice(s * 128, (s + 1) * 128)
                zs = zs_r.next()
                for q in range(4):
                    p = pz.next()
                    for k in range(8):
                        em.op("pe", lambda e: e.matmul(p[:], hb[:, k, ss], wz[:, k, q * 512:(q + 1) * 512], start=(k == 0), stop=(k == 7)),
                              reads=[hb, wz], writes=[p])
                    em.op("act", lambda e: e.activation(out=zs[:, q * 512:(q + 1) * 512], in_=p[:], func=AF.Silu), reads=[p], writes=[zs])
                em.dma("sp", zs_d[ss, :], zs[:], reads=[zs])
                for k in range(8):
                    em.op("pe", lambda e: e.matmul(pd[:], hb[:, k, ss], wdt[:, k, :], start=(k == 0), stop=(k == 7)), reads=[hb, wdt], writes=[pd])
                dx = d_r["x"].next(); da = d_r["a"].next(); de = d_r["e"].next(); dr = d_r["r"].next()
                em.op("dve", lambda e: e.tensor_tensor(dx[:], pd[:], dtb[:], ALU.add), reads=[pd, dtb], writes=[dx])
                em.op("dve", lambda e: e.tensor_scalar_mul(da[:], dx[:], -1.0), reads=[dx], writes=[da])
                em.op("dve", lambda e: e.tensor_tensor(da[:], da[:], dx[:], ALU.max), reads=[dx, da], writes=[da])
                em.op("act", lambda e: e.activation(out=de[:], in_=da[:], func=AF.Exp, scale=-1.0), reads=[da], writes=[de])
                em.op("act", lambda e: e.activation(out=de[:], in_=de[:], func=AF.Ln, bias=ones32[:, 0:1], scale=1.0), reads=[de, ones32], writes=[de])
                em.op("dve", lambda e: e.scalar_tensor_tensor(dr[:], dx[:], 0.0, de[:], ALU.max, ALU.add), reads=[dx, de], writes=[dr])
                em.dma("sp", dt_d[ss, :], dr[:], reads=[dr])

        with em.phase():
            hb = em.sb([128, 8, T], BF16, "hb")
            load_hb(hb)
            wx = em.sb([128, 8, 3072], BF16, "wx")
            em.dma("pool", wx[:], inw[:, 2048:5120].rearrange("(k p) f -> p k f", p=128), writes=[wx])
            pm = em.ps([128, 512], F32)
            cw = em.sb([128, 24, 4], F32, "cw")
            cbias = em.sb([128, 24, 1], F32, "cb")
            pp = Rot([em.ps([128, 512], F32) for _ in range(4)])
            xpad_r = em.rot(2, [128, T + 3], F32, "xpad")
            rowsT(cw[:], ssm_conv_w[l], 4, 24, pm, cw, tmp=xpad_r.bufs[0], tmp_ap=xpad_r.bufs[0][:])
            rowsT(cbias[:], ssm_conv_b[l:l + 1, :], 1, 24, pm, cbias, tmp=xpad_r.bufs[1], tmp_ap=xpad_r.bufs[1][:])
            acc_r = em.rot(2, [128, T], F32, "cacc")
            ob_r = em.rot(1, [128, T], BF16, "cob")
            for ch in range(24):
                xp = xpad_r.next(); ac = acc_r.next(); ob = ob_r.next()
                em.op("pool", lambda e: e.memset(xp[:, 0:3], 0.0), writes=[xp])
                for i in range(NT):
                    p = pp.next()
                    for k in range(8):
                        em.op("pe", lambda e: e.matmul(p[:], wx[:, k, ch * 128:(ch + 1) * 128], hb[:, k, i * 512:(i + 1) * 512], start=(k == 0), stop=(k == 7)),
                              reads=[wx, hb], writes=[p])
                    if i % 2:
                        em.op("act", lambda e: e.copy(xp[:, 3 + i * 512:3 + (i + 1) * 512], p[:]), reads=[p], writes=[xp])
                    else:
                        em.op("dve", lambda e: e.tensor_copy(xp[:, 3 + i * 512:3 + (i + 1) * 512], p[:]), reads=[p], writes=[xp])
                em.op("dve", lambda e: e.tensor_scalar(ac[:], xp[:, 0:T], cw[:, ch, 0:1], None, ALU.mult), reads=[xp, cw], writes=[ac])
                for j in range(1, 4):
                    eng = "dve"
                    em.op(eng, lambda e: e.scalar_tensor_tensor(ac[:], xp[:, j:j + T], cw[:, ch, j:j + 1], ac[:], ALU.mult, ALU.add),
                          reads=[xp, cw, ac], writes=[ac])
                em.op("act", lambda e: e.activation(out=ob[:], in_=ac[:], func=AF.Silu, bias=cbias[:, ch, :], scale=1.0), reads=[ac, cbias], writes=[ob])
                em.dma("sp", xbc_d[ch], ob[:], reads=[ob])

        with em.phase():
            tri = em.sb([128, 128], F32, "tri")
            em.dma("sp", tri[:], k_tri, writes=[tri])
            aneg = em.sb([128, 32], F32, "aneg")
            em.dma("sp", aneg[:], ssm_a_log[l:l + 1, :].to_broadcast([128, 32]), writes=[aneg])
            em.op("act", lambda e: e.activation(out=aneg[:], in_=aneg[:], func=AF.Exp), reads=[aneg], writes=[aneg])
            em.op("dve", lambda e: e.tensor_scalar_mul(aneg[:], aneg[:], -1.0), reads=[aneg], writes=[aneg])
            dsk = em.sb([128, 32], F32, "dsk")
            em.dma("sp", dsk[:], ssm_d[l:l + 1, :].to_broadcast([128, 32]), writes=[dsk])
            nw = em.sb([128, 2048], F32, "nw")
            em.dma("sp", nw[:], ssm_norm_w[l:l + 1, :].to_broadcast([128, 2048]), writes=[nw])
            wout = em.sb([128, 16, D], BF16, "wout")
            em.dma("pool", wout[:], ssm_out_w[l].rearrange("(k p) o -> p k o", p=128), writes=[wout])
            st32 = [em.sb([128, 8, 64], F32, "st32") for _ in range(4)]
            stb = [em.sb([128, 8, 64], BF16, "stb") for _ in range(4)]
            for g in range(4):
                em.op("dve", lambda e: e.memset(st32[g][:], 0.0), writes=[st32[g]])
                em.op("dve", lambda e: e.memset(stb[g][:], 0.0), writes=[stb[g]])
            ynT = em.sb([128, 16, 512], BF16, "ynT")
            ptb = em.ps([128, 512], BF16)
            pmisc = em.ps([128, 512], F32)
            par = em.ps([128, 1024], F32)
            py = em.ps([128, 512], F32)
            psn = em.ps([128, 512], F32)
            pout = em.ps([128, 512], F32)
            xsT_r = em.rot(2, [128, 16, 128], BF16, "xsT")
            bT_r = em.rot(2, [128, 4, 128], BF16, "bT")
            cT_r = em.rot(2, [128, 4, 128], BF16, "cT")
            zs_r = em.rot(2, [128, 2048], BF16, "zsl")
            dt_r = em.rot(2, [128, 32], F32, "dtl")
            xs_r = em.rot(2, [128, 32, 64], BF16, "xs")
            bt_r = em.rot(2, [128, 512], BF16, "btok")
            xdt_r = em.rot(1, [128, 32, 64], BF16, "xdt")
            xdtd_r = em.rot(1, [128, 32, 64], BF16, "xdtd")
            arow_r = em.rot(1, [128, 32, 128], F32, "arow")
            seg_r = em.rot(2, [128, 8, 128], F32, "seg")
            ear_r = em.rot(2, [128, 8, 128], F32, "ear")
            Mh_r = em.rot(2, [128, 8, 128], BF16, "Mh")
            Cs_r = em.rot(2, [128, 8, 128], BF16, "Cs")
            cbm_r = em.rot(2, [128, 128], F32, "cbm")
            yz = em.sb([128, 4, 512], F32, "yz")
            tt_r = em.rot(2, [128, 8, 64], F32, "tt")
            junk = em.sb([128, 512], F32, "junk")
            yn = em.sb([128, 2048], BF16, "yn")
            yo_r = em.rot(1, [128, 8, 512], F32, "yo")
            sA = {k: em.sb([128, 32], F32, "s" + k) for k in ("a", "acs", "tot", "d1", "dend", "cdec", "dtd")}
            ss4 = em.sb([128, 4], F32); rstd4 = em.sb([128, 4], F32)
            for c in range(NS):
                cs_ = slice(c * 128, (c + 1) * 128)
                xsT = xsT_r.next(); bT = bT_r.next(); cT = cT_r.next(); zs = zs_r.next(); dt = dt_r.next()
                em.dma("sp", xsT[:], xbc_d[0:16, :, cs_].rearrange("k p t -> p k t"), writes=[xsT])
                em.dma("sp", bT[:], xbc_d[16:20, :, cs_].rearrange("k p t -> p k t"), writes=[bT])
                em.dma("sp", cT[:], xbc_d[20:24, :, cs_].rearrange("k p t -> p k t"), writes=[cT])
                em.dma("sp", zs[:], zs_d[cs_, :], writes=[zs])
                em.dma("sp", dt[:], dt_d[cs_, :], writes=[dt])
                xs = xs_r.next(); btok = bt_r.next()
                xsf = xs[:].rearrange("p h d -> p (h d)")
                for q in range(4):
                    for k in range(4):
                        em.op("pe", lambda e: e.transpose(ptb[:, k * 128:(k + 1) * 128], xsT[:, q * 4 + k, :], identb[:]), reads=[xsT, identb], writes=[ptb])
                    em.op("act", lambda e: e.copy(xsf[:, q * 512:(q + 1) * 512], ptb[:]), reads=[ptb], writes=[xs])
                for g in range(4):
                    em.op("pe", lambda e: e.transpose(ptb[:, g * 128:(g + 1) * 128], bT[:, g, :], identb[:]), reads=[bT, identb], writes=[ptb])
                em.op("dve", lambda e: e.tensor_copy(btok[:], ptb[:]), reads=[ptb], writes=[btok])
                a, acs, tot, d1, dend, cdec, dtd = (sA[k] for k in ("a", "acs", "tot", "d1", "dend", "cdec", "dtd"))
                em.op("dve", lambda e: e.tensor_tensor(a[:], dt[:], aneg[:], ALU.mult), reads=[dt, aneg], writes=[a])
                em.op("pe", lambda e: e.matmul(pmisc[:, 0:32], tri[:], a[:], start=True, stop=True), reads=[tri, a], writes=[pmisc])
                em.op("pe", lambda e: e.matmul(pmisc[:, 32:64], ones32[:], a[:], start=True, stop=True), reads=[ones32, a], writes=[pmisc])
                em.op("dve", lambda e: e.tensor_copy(acs[:], pmisc[:, 0:32]), reads=[pmisc], writes=[acs])
                em.op("dve", lambda e: e.tensor_copy(tot[:], pmisc[:, 32:64]), reads=[pmisc], writes=[tot])
                em.op("dve", lambda e: e.tensor_tensor(d1[:], tot[:], acs[:], ALU.subtract), reads=[tot, acs], writes=[d1])
                em.op("act", lambda e: e.activation(out=dend[:], in_=d1[:], func=AF.Exp), reads=[d1], writes=[dend])
                em.op("act", lambda e: e.activation(out=cdec[:], in_=tot[:], func=AF.Exp), reads=[tot], writes=[cdec])
                em.op("dve", lambda e: e.tensor_tensor(dtd[:], dt[:], dend[:], ALU.mult), reads=[dt, dend], writes=[dtd])
                xdt = xdt_r.next(); xdtd = xdtd_r.next()
                em.op("pool", lambda e: e.tensor_tensor(xdt[:], xs[:], dt[:].unsqueeze(2).to_broadcast([128, 32, 64]), ALU.mult), reads=[xs, dt], writes=[xdt])
                em.op("dve", lambda e: e.tensor_tensor(xdtd[:], xs[:], dtd[:].unsqueeze(2).to_broadcast([128, 32, 64]), ALU.mult), reads=[xs, dtd], writes=[xdtd])
                arow = arow_r.next()
                em.op("pool", lambda e: e.tensor_tensor(arow[:], tri[:].unsqueeze(1).to_broadcast([128, 32, 128]),
                                                        a[:].unsqueeze(2).to_broadcast([128, 32, 128]), ALU.mult), reads=[tri, a], writes=[arow])
                for g in range(4):
                    for h2 in range(2):
                        em.op("pe", lambda e: e.matmul(par[:, h2 * 512:(h2 + 1) * 512], ones32[:],
                                                       arow[:, g * 8 + h2 * 4:g * 8 + h2 * 4 + 4, :].rearrange("p h l -> p (h l)"), start=True, stop=True),
                              reads=[ones32, arow], writes=[par])
                    em.op("pe", lambda e: e.matmul(pmisc[:, 128:256], bT[:, g, :], cT[:, g, :], start=True, stop=True), reads=[bT, cT], writes=[pmisc])
                    cbm = cbm_r.next()
                    em.op("dve", lambda e: e.tensor_tensor(cbm[:], pmisc[:, 128:256], tri[:], ALU.mult), reads=[pmisc, tri], writes=[cbm])
                    seg = seg_r.next(); ear = ear_r.next(); Mh = Mh_r.next(); Cs = Cs_r.next()
                    for hh in range(8):
                        h = g * 8 + hh
                        em.op("dve", lambda e: e.tensor_scalar(seg[:, hh, :], par[:, hh * 128:(hh + 1) * 128], acs[:, h:h + 1], 0.0, ALU.subtract, ALU.min),
                              reads=[par, acs], writes=[seg])
                    em.op("act", lambda e: e.activation(out=seg[:], in_=seg[:], func=AF.Exp), reads=[seg], writes=[seg])
                    em.op("pool", lambda e: e.tensor_tensor(Mh[:], seg[:], cbm[:].unsqueeze(1).to_broadcast([128, 8, 128]), ALU.mult), reads=[seg, cbm], writes=[Mh])
                    em.op("act", lambda e: e.activation(out=ear[:], in_=par[:].rearrange("p (h l) -> p h l", l=128), func=AF.Exp), reads=[par], writes=[ear])
                    em.op("dve", lambda e: e.tensor_tensor(Cs[:], ear[:], cT[:, g, :].unsqueeze(1).to_broadcast([128, 8, 128]), ALU.mult), reads=[ear, cT], writes=[Cs])
                    for hh in range(8):
                        h = g * 8 + hh
                        em.op("pe", lambda e: e.matmul(py[:, hh * 64:(hh + 1) * 64], Mh[:, hh, :], xdt[:, h, :], start=True, stop=False), reads=[Mh, xdt], writes=[py])
                        em.op("pe", lambda e: e.matmul(py[:, hh * 64:(hh + 1) * 64], Cs[:, hh, :], stb[g][:, hh, :], start=False, stop=True), reads=[Cs, stb[g]], writes=[py])
                    tt = tt_r.next()
                    em.op("pool", lambda e: e.tensor_tensor(tt[:], xs[:, g * 8:(g + 1) * 8, :], dsk[:, g * 8:(g + 1) * 8].unsqueeze(2).to_broadcast([128, 8, 64]), ALU.mult),
                          reads=[xs, dsk], writes=[tt])
                    em.op("dve", lambda e: e.tensor_tensor(yz[:, g, :], py[:], tt[:].rearrange("p h d -> p (h d)"), ALU.add), reads=[py, tt], writes=[yz])
                    em.op("pool", lambda e: e.tensor_tensor(yz[:, g, :], yz[:, g, :], zs[:, g * 512:(g + 1) * 512], ALU.mult), reads=[yz, zs], writes=[yz])
                    em.op("act", lambda e: e.activation(out=junk[:], in_=yz[:, g, :], func=AF.Square, accum_out=ss4[:, g:g + 1]), reads=[yz], writes=[junk, ss4])
                    em.op("pe", lambda e: e.matmul(psn[:], btok[:, g * 128:(g + 1) * 128], xdtd[:, g * 8:(g + 1) * 8, :].rearrange("p h d -> p (h d)"), start=True, stop=True),
                          reads=[btok, xdtd], writes=[psn])
                    em.op("pool", lambda e: e.tensor_tensor(st32[g][:], st32[g][:], cdec[:, g * 8:(g + 1) * 8].unsqueeze(2).to_broadcast([128, 8, 64]), ALU.mult),
                          reads=[st32[g], cdec], writes=[st32[g]])
                    em.op("dve", lambda e: e.tensor_tensor(st32[g][:], st32[g][:], psn[:].rearrange("p (h d) -> p h d", d=64), ALU.add), reads=[st32[g], psn], writes=[st32[g]])
                    em.op("act", lambda e: e.copy(stb[g][:], st32[g][:]), reads=[st32[g]], writes=[stb[g]])
                em.op("dve", lambda e: e.tensor_scalar(rstd4[:], ss4[:], 1.0 / 512.0, None, ALU.mult), reads=[ss4], writes=[rstd4])
                em.op("act", lambda e: e.activation(out=rstd4[:], in_=rstd4[:], func=AF.Sqrt, bias=eps5[:], scale=1.0), reads=[rstd4, eps5], writes=[rstd4])
                em.op("dve", lambda e: e.reciprocal(rstd4[:], rstd4[:]), reads=[rstd4], writes=[rstd4])
                for g in range(4):
                    eng = "dve"
                    em.op(eng, lambda e: e.scalar_tensor_tensor(yn[:, g * 512:(g + 1) * 512], yz[:, g, :], rstd4[:, g:g + 1], nw[:, g * 512:(g + 1) * 512], ALU.mult, ALU.mult),
                          reads=[yz, rstd4, nw], writes=[yn])
                c4 = c % 4
                for q in range(4):
                    for k in range(4):
                        em.op("pe", lambda e: e.transpose(ptb[:, k * 128:(k + 1) * 128], yn[:, (q * 4 + k) * 128:(q * 4 + k + 1) * 128], identb[:]), reads=[yn, identb], writes=[ptb])
                    em.op("act", lambda e: e.copy(ynT[:, q * 4:(q + 1) * 4, c4 * 128:(c4 + 1) * 128], ptb[:].rearrange("p (k t) -> p k t", t=128)), reads=[ptb], writes=[ynT])
                if c4 == 3:
                    yo = yo_r.next()
                    for oc in range(8):
                        for k in range(16):
                            em.op("pe", lambda e: e.matmul(pout[:], wout[:, k, oc * 128:(oc + 1) * 128], ynT[:, k, :], start=(k == 0), stop=(k == 15)), reads=[wout, ynT], writes=[pout])
                        em.op("dve", lambda e: e.tensor_copy(yo[:, oc, :], pout[:]), reads=[pout], writes=[yo])
                    i = c // 4
                    em.dma("sp", ydst[:, :, i * 512:(i + 1) * 512].rearrange("c p t -> p c t"), yo[:], reads=[yo])

    def rope_phase():
        with em.phase():
            rc = em.sb([64, 2], F32, "rc")
            em.dma("sp", rc[:], k_ropec, writes=[rc])
            posi = em.sb([64, T], I32, "posi")
            em.dma("sp", posi[:], pos_in[0:1, :].to_broadcast([64, T]), writes=[posi])
            ang = em.sb([64, T], F32, "ang")
            em.op("dve", lambda e: e.tensor_copy(ang[:], posi[:]), reads=[posi], writes=[ang])
            em.op("dve", lambda e: e.tensor_scalar(ang[:], ang[:], rc[:, 0:1], None, ALU.mult), reads=[ang, rc], writes=[ang])
            MAGIC = 12582912.0
            C1 = 6.28125
            C2 = 2.0 * math.pi - 6.28125
            u = em.sb([64, T], F32, "u"); kk = em.sb([64, T], F32, "kk"); r = em.sb([64, T], F32, "r")
            for which, shift in ((0, math.pi / 2.0), (1, 0.0)):
                em.op("dve", lambda e: e.tensor_scalar_add(u[:], ang[:], shift), reads=[ang], writes=[u])
                em.op("dve", lambda e: e.tensor_scalar(kk[:], u[:], 1.0 / (2.0 * math.pi), MAGIC, ALU.mult, ALU.add), reads=[u], writes=[kk])
                em.op("dve", lambda e: e.tensor_scalar_add(kk[:], kk[:], -MAGIC), reads=[kk], writes=[kk])
                em.op("dve", lambda e: e.scalar_tensor_tensor(r[:], kk[:], -C1, u[:], ALU.mult, ALU.add), reads=[kk, u], writes=[r])
                em.op("dve", lambda e: e.scalar_tensor_tensor(r[:], kk[:], -C2, r[:], ALU.mult, ALU.add), reads=[kk, r], writes=[r])
                em.op("dve", lambda e: e.tensor_scalar(r[:], r[:], math.pi, -math.pi, ALU.min, ALU.max), reads=[r], writes=[r])
                em.op("act", lambda e: e.activation(out=r[:], in_=r[:], func=AF.Sin), reads=[r], writes=[r])
                if which == 1:
                    em.op("dve", lambda e: e.tensor_scalar(r[:], r[:], rc[:, 1:2], None, ALU.mult), reads=[r, rc], writes=[r])
                em.dma("sp", rope_d[which], r[:], reads=[r])

    def kv_phase(xsrc):
        with em.phase():
            hb = em.sb([128, 8, T], BF16, "hb")
            xt_r = em.rot(3, [128, 512], F32, "kvx")
            for i in range(NT):
                load_mod_tile(xsrc, i, xt_r, lambda c: hb[:, c, i * 512:(i + 1) * 512], kvmod[:, 8:16], kvmod[:, 0:8], hb)
            kvw = em.sb([128, 8, 1536], BF16, "kvw")
            em.dma("pool", kvw[:], kv_w.rearrange("(k p) f -> p k f", p=128), writes=[kvw])
            kvws = em.sb([128, 8, 768], BF16, "kvws")
            em.dma("pool", kvws[:], kv_w_sw.rearrange("(k p) f -> p k f", p=128), writes=[kvws])
            cos_r = em.rot(2, [64, 512], F32, "cos"); sin_r = em.rot(2, [64, 512], F32, "sin")
            P = [em.ps([128, 512], F32) for _ in range(6)]
            pdr = Rot(P[0:2]); psr = Rot(P[2:4])
            w1 = []; w2 = []; biasT = []
            cp = em.sb([32, 64], F32, "cp")
            em.dma("sp", cp[:], cmp_pos, writes=[cp])
            em.op("pe", lambda e: e.transpose(P[4][0:64, 0:32], cp[0:32, 0:64], ident[0:32, 0:32]), reads=[cp, ident], writes=[P[4]])
            cposT = em.sb([64, 32], BF16, "cposT")
            em.op("dve", lambda e: e.tensor_copy(cposT[:], P[4][0:64, 0:32]), reads=[P[4]], writes=[cposT])
            for m in range(2):
                a = em.sb([64, 32, 256], BF16, "w1")
                em.dma("pool", a[:], phi_w1[m].rearrange("(j d) f -> d j f", d=64), writes=[a])
                b = em.sb([128, 2, 64], BF16, "w2")
                em.dma("pool", b[:], phi_w2[m].rearrange("(c p) d -> p c d", p=128), writes=[b])
                w1.append(a); w2.append(b)
                bt = em.sb([128, 2], F32, "biasT")
                for hc in range(2):
                    for j in range(32):
                        em.op("pe", lambda e: e.matmul(P[5][:, hc:hc + 1], a[:, j, hc * 128:(hc + 1) * 128], cposT[:, j:j + 1], start=(j == 0), stop=(j == 31)),
                              reads=[a, cposT], writes=[P[5]])
                em.op("dve", lambda e: e.tensor_copy(bt[:], P[5][:, 0:2]), reads=[P[5]], writes=[bt])
                biasT.append(bt)
            kt_r = em.rot(2, [64, T], BF16, "kt")
            t1_r = em.rot(2, [64, 512], F32, "kt1")
            t2_r = em.rot(2, [64, 512], F32, "kt2")
            hid_r = em.rot(2, [128, 2, 256], BF16, "hid")
            u_r = em.rot(2, [128, 255], F32, "gu")
            u2_r = em.rot(2, [128, 255], F32, "gu2")
            kc_r = em.rot(2, [64, 256], BF16, "kc")
            vc_r = em.rot(2, [128, 64], BF16, "vc")

            def compress(srcT, m, g):
                hid = hid_r.next()
                for hc in range(2):
                    ph = P[4]
                    for j in range(32):
                        em.op("pe", lambda e: e.matmul(ph[:, 0:255], w1[m][:, j, hc * 128:(hc + 1) * 128], srcT[:, j:j + 16 * 254 + 1:16], start=(j == 0), stop=(j == 31)),
                              reads=[w1[m], srcT], writes=[ph])
                    u = u_r.next(); u2 = u2_r.next()
                    em.op("act", lambda e: e.activation(out=u[:], in_=ph[:, 0:255], func=AF.Identity, bias=biasT[m][:, hc:hc + 1], scale=1.0), reads=[ph, biasT[m]], writes=[u])
                    em.op("pool", lambda e: e.tensor_tensor(u2[:], u[:], u[:], ALU.mult), reads=[u], writes=[u2])
                    em.op("dve", lambda e: e.tensor_scalar(u2[:], u2[:], 0.044715, 1.0, ALU.mult, ALU.add), reads=[u2], writes=[u2])
                    em.op("pool", lambda e: e.tensor_tensor(u2[:], u2[:], u[:], ALU.mult), reads=[u2, u], writes=[u2])
                    em.op("act", lambda e: e.activation(out=u2[:], in_=u2[:], func=AF.Sigmoid, scale=1.5957691216057308), reads=[u2], writes=[u2])
                    em.op("dve", lambda e: e.tensor_tensor(hid[:, hc, 0:255], u[:], u2[:], ALU.mult), reads=[u, u2], writes=[hid])
                if m == 0:
                    pk = P[5]
                    for hc in range(2):
                        em.op("pe", lambda e: e.matmul(pk[0:64, 0:255], w2[0][:, hc, :], hid[:, hc, 0:255], start=(hc == 0), stop=(hc == 1)), reads=[w2[0], hid], writes=[pk])
                    kc = kc_r.next()
                    em.op("dve", lambda e: e.memset(kc[:], 0.0), writes=[kc])
                    em.op("dve", lambda e: e.tensor_copy(kc[:, 0:255], pk[0:64, 0:255]), reads=[pk], writes=[kc])
                    em.dma("sp", KC_d[g], kc[:], reads=[kc])
                else:
                    for ncn in range(2):
                        nn = 128 if ncn == 0 else 127
                        pv = P[5]
                        for hc in range(2):
                            em.op("pe", lambda e: e.matmul(pv[0:nn, 0:64], hid[:, hc, ncn * 128:ncn * 128 + nn], w2[1][:, hc, :], start=(hc == 0), stop=(hc == 1)), reads=[hid, w2[1]], writes=[pv])
                        vc = vc_r.next()
                        em.op("dve", lambda e: e.memset(vc[:], 0.0), writes=[vc])
                        em.op("dve", lambda e: e.tensor_copy(vc[0:nn, :], pv[0:nn, 0:64]), reads=[pv], writes=[vc])
                        em.dma("sp", VC_d[g, ncn], vc[:], reads=[vc])

            for si, slot in enumerate((0, 2, 4)):
                for g in range(4):
                    kt = kt_r.next()
                    for i in range(NT):
                        sl = slice(i * 512, (i + 1) * 512)
                        pd = pdr.next(); psw = psr.next()
                        for k in range(8):
                            em.op("pe", lambda e: e.matmul(pd[0:64, :], kvw[:, k, slot * 256 + g * 64:slot * 256 + g * 64 + 64], hb[:, k, sl], start=(k == 0), stop=(k == 7)), reads=[kvw, hb], writes=[pd])
                        for k in range(8):
                            em.op("pe", lambda e: e.matmul(psw[0:64, :], kvws[:, k, si * 256 + g * 64:si * 256 + g * 64 + 64], hb[:, k, sl], start=(k == 0), stop=(k == 7)), reads=[kvws, hb], writes=[psw])
                        t1 = t1_r.next(); t2 = t2_r.next()
                        cos = cos_r.next(); sin = sin_r.next()
                        em.dma("sp", cos[:], rope_d[0, :, sl], writes=[cos])
                        em.dma("sp", sin[:], rope_d[1, :, sl], writes=[sin])
                        em.op("dve", lambda e: e.tensor_tensor(t1[:], pd[0:64, :], cos[:], ALU.mult), reads=[pd, cos], writes=[t1])
                        em.op("dve", lambda e: e.tensor_tensor(t2[:], psw[0:64, :], sin[:], ALU.mult), reads=[psw, sin], writes=[t2])
                        em.op("pool", lambda e: e.tensor_tensor(kt[:, sl], t1[:], t2[:], ALU.add), reads=[t1, t2], writes=[kt])
                    em.dma("sp", KT_d[si, g], kt[:], reads=[kt])
                    if slot == 0:
                        compress(kt, 0, g)
            for g in range(4):
                vt = kt_r.next()
                for i in range(NT):
                    sl = slice(i * 512, (i + 1) * 512)
                    pd = pdr.next()
                    for k in range(8):
                        em.op("pe", lambda e: e.matmul(pd[0:64, :], kvw[:, k, 256 + g * 64:256 + g * 64 + 64], hb[:, k, sl], start=(k == 0), stop=(k == 7)), reads=[kvw, hb], writes=[pd])
                    em.op("act", lambda e: e.copy(vt[:, sl], pd[0:64, :]), reads=[pd], writes=[vt])
                compress(vt, 1, g)
            vt_r = em.rot(2, [128, 512], BF16, "vtok")
            for s in range(NS):
                ss = slice(s * 128, (s + 1) * 128)
                pv = pdr.next()
                for half, slot in enumerate((3, 5)):
                    for k in range(8):
                        em.op("pe", lambda e: e.matmul(pv[:, half * 256:(half + 1) * 256], hb[:, k, ss], kvw[:, k, slot * 256:(slot + 1) * 256], start=(k == 0), stop=(k == 7)), reads=[hb, kvw], writes=[pv])
                vtk = vt_r.next()
                em.op("act", lambda e: e.copy(vtk[:], pv[:]), reads=[pv], writes=[vtk])
                em.dma("sp", VT_d[ss, :], vtk[:], reads=[vtk])

    def nsa_phase(l, xsrc, ydst):
        jl = l - 2
        sc1 = mods[:, l, 8:16]
        sh = mods[:, l, 0:8]
        with em.phase():
            eall = em.sb([64, 32, 128], BF16, "eall"); em.dma("sp", eall[:], k_eall, writes=[eall])
            cz = em.sb([128, 4, 512], BF16, "cz"); em.dma("sp", cz[:], k_cz, writes=[cz])
            wm = em.sb([128, 8, 512], BF16, "wm"); em.dma("sp", wm[:], k_wm, writes=[wm])
            ovaug = em.sb([128, 2, 65], F32, "ovaug"); em.dma("sp", ovaug[:], k_ovaug.rearrange("c p j -> p c j"), writes=[ovaug])
            qw = em.sb([128, 8, 1072], BF16, "qw"); em.dma("pool", qw[:], nsa_q_w[jl].rearrange("(k p) f -> p k f", p=128), writes=[qw])
            qws = em.sb([128, 8, 1024], BF16, "qws"); em.dma("pool", qws[:], nsa_q_w_sw[jl].rearrange("(k p) f -> p k f", p=128), writes=[qws])
            ow = em.sb([64, 16, D], BF16, "ow"); em.dma("pool", ow[:], nsa_o_w[jl].rearrange("(h d) o -> d h o", d=64), writes=[ow])
            KC = em.sb([64, 4, 256], BF16, "KC"); em.dma("sp", KC[:], KC_d.rearrange("g d n -> d g n"), writes=[KC])
            VC = em.sb([128, 4, 2, 64], BF16, "VC"); em.dma("sp", VC[:], VC_d.rearrange("g c p d -> p g c d"), writes=[VC])
            P = [em.ps([128, 512], F32) for _ in range(8)]
            pS_r = Rot(P[0:2]); pM = P[2]; pO = P[3]; pD = P[4]; pI = P[5]; pOut = P[6]; pQ = P[7]; pM_r = Rot([P[2], P[5]])
            xt_r = em.rot(2, [128, 512], F32, "ax")
            hbt_r = em.rot(1, [128, 8, 512], BF16, "ahb")
            cos_r = em.rot(1, [64, 512], F32, "acos"); sin_r = em.rot(1, [64, 512], F32, "asin")
            mc_r = em.rot(1, [128, 2, 512], F32, "amc")
            oT_r = em.rot(1, [64, 16, 512], BF16, "oT")
            KS_r = em.rot(1, [64, T], BF16, "KS"); VS_r = em.rot(1, [128, NS, 64], BF16, "VS")
            KW_r = em.rot(1, [64, 1024], BF16, "KW"); VW_r = em.rot(1, [128, 8, 64], BF16, "VW")
            qt_b = [em.sb([64, 512], BF16, "qt") for _ in range(4)]
            sig_r = em.rot(2, [64, 512], F32, "sig")
            gsig = em.sb([128, 48], F32, "gsig")
            gT = em.sb([48, 512], BF16, "gT")
            oc_b = [em.sb([64, 512], F32, "ocomb") for _ in range(4)]
            t1_r = em.rot(1, [64, 512], F32, "at1"); t2_r = em.rot(1, [64, 512], F32, "at2")
            e32_r = em.rot(1, [128, 512], F32, "e32")
            p32 = [em.sb([128, 512], F32, "p32") for _ in range(2)]
            pbc = [em.sb([128, 512], BF16, "pbc") for _ in range(2)]
            eb_r = em.rot(3, [128, 512], BF16, "eb"); pb_r = em.rot(4, [128, 512], BF16, "pb")
            rden_r = em.rot(2, [64, 512], F32, "rden")
            impg = em.sb([128, 4, 64], F32, "impg")
            rd_r = em.rot(2, [128, 1], F32, "rd")
            selc_r = em.rot(2, [128, 4, 64], F32, "selc")
            sc_r = em.rot(2, [128, 64], F32, "sc"); rep_r = em.rot(2, [128, 64], F32, "rep")
            m8a = em.sb([128, 8]); m8b = em.sb([128, 8])
            selT = em.sb([64, 512], BF16, "selT")
            yo_r = em.rot(2, [128, 512], F32, "ayo")

            cur_hbt = [None]
            cur_g = [0]

            def finish_branch(r, b, first):
                rden = rden_r.next(); sig = sig_r.next()
                col = (cur_g[0] * 4 + r) * 3 + b
                em.op("pe", lambda e: e.matmul(pQ[0:64, :], identb[0:48, col:col + 1].to_broadcast([48, 64]), gT[:], start=True, stop=True), reads=[identb, gT], writes=[pQ])
                em.op("act", lambda e: e.copy(sig[:], pQ[0:64, :]), reads=[pQ], writes=[sig])
                em.op("dve", lambda e: e.tensor_scalar_max(rden[:], pD[0:64, :], TINY), reads=[pD], writes=[rden])
                em.op("dve", lambda e: e.reciprocal(rden[:], rden[:]), reads=[rden], writes=[rden])
                em.op("pool", lambda e: e.tensor_tensor(rden[:], rden[:], sig[:], ALU.mult), reads=[rden, sig], writes=[rden])
                if first:
                    em.op("dve", lambda e: e.tensor_tensor(oc_b[r][:], pO[0:64, :], rden[:], ALU.mult), reads=[pO, rden], writes=[oc_b[r]])
                else:
                    em.op("dve", lambda e: e.tensor_tensor(rden[:], pO[0:64, :], rden[:], ALU.mult), reads=[pO, rden], writes=[rden])
                    em.op("pool", lambda e: e.tensor_tensor(oc_b[r][:], oc_b[r][:], rden[:], ALU.add), reads=[oc_b[r], rden], writes=[oc_b[r]])

            for i in range(NT):
                sl = slice(i * 512, (i + 1) * 512)
                hbt = hbt_r.next()
                cur_hbt[0] = hbt
                for c in range(8):
                    xt = xt_r.next()
                    em.dma("sp", xt[:], xsrc[c, :, sl], writes=[xt])
                    eng = "dve" if c % 2 == 0 else "pool"
                    em.op(eng, lambda e: e.tensor_scalar(hbt[:, c, :], xt[:], sc1[:, c:c + 1], sh[:, c:c + 1], ALU.mult, ALU.add),
                          reads=[xt, mods], writes=[hbt])
                cos = cos_r.next(); sin = sin_r.next(); mc = mc_r.next()
                em.dma("sp", cos[:], rope_d[0, :, sl], writes=[cos])
                em.dma("sp", sin[:], rope_d[1, :, sl], writes=[sin])
                em.op("dve", lambda e: e.tensor_scalar_mul(cos[:], cos[:], ATTN_SCALE), reads=[cos], writes=[cos])
                em.op("dve", lambda e: e.tensor_scalar_mul(sin[:], sin[:], ATTN_SCALE), reads=[sin], writes=[sin])
                em.dma("sp", mc[:], k_mcmp[:, :, sl].rearrange("c p t -> p c t"), writes=[mc])
                for s_ in range(4):
                    for k in range(8):
                        em.op("pe", lambda e: e.matmul(pQ[:, 0:48], hbt[:, k, s_ * 128:(s_ + 1) * 128], qw[:, k, 1024:1072], start=(k == 0), stop=(k == 7)), reads=[hbt, qw], writes=[pQ])
                    em.op("act", lambda e: e.activation(out=gsig[:], in_=pQ[:, 0:48], func=AF.Sigmoid), reads=[pQ], writes=[gsig])
                    em.op("pe", lambda e: e.transpose(pQ[0:48, 128:256], gsig[:], ident[:]), reads=[gsig, ident], writes=[pQ])
                    em.op("dve", lambda e: e.tensor_copy(gT[:, s_ * 128:(s_ + 1) * 128], pQ[0:48, 128:256]), reads=[pQ], writes=[gT])
                oT = oT_r.next()
                nkt = 4 * (i + 1)
                w0 = max(0, 4 * i - 4)
                for g in range(4):
                    cur_g[0] = g
                    KS = KS_r.next(); VS = VS_r.next(); KW = KW_r.next(); VW = VW_r.next()
                    em.dma("sp", KS[:, 0:nkt * 128], KT_d[1, g, :, 0:nkt * 128], writes=[KS])
                    em.dma("sp", VS[:, 0:nkt, :], VT_d[0:nkt * 128, g * 64:(g + 1) * 64].rearrange("(k p) d -> p k d", p=128), writes=[VS])
                    nwk = nkt - w0
                    em.dma("sp", KW[:, 0:nwk * 128], KT_d[2, g, :, w0 * 128:nkt * 128], writes=[KW])
                    em.dma("sp", VW[:, 0:nwk, :], VT_d[w0 * 128:nkt * 128, 256 + g * 64:256 + (g + 1) * 64].rearrange("(k p) d -> p k d", p=128), writes=[VW])
                    for r in range(4):
                        h = g * 4 + r
                        pd = pS_r.next(); psw = pS_r.next()
                        for k in range(8):
                            em.op("pe", lambda e: e.matmul(pd[0:64, :], qw[:, k, h * 64:(h + 1) * 64], hbt[:, k, :], start=(k == 0), stop=(k == 7)), reads=[qw, hbt], writes=[pd])
                        for k in range(8):
                            em.op("pe", lambda e: e.matmul(psw[0:64, :], qws[:, k, h * 64:(h + 1) * 64], hbt[:, k, :], start=(k == 0), stop=(k == 7)), reads=[qws, hbt], writes=[psw])
                        t1 = t1_r.next(); t2 = t2_r.next()
                        em.op("dve", lambda e: e.tensor_tensor(t1[:], pd[0:64, :], cos[:], ALU.mult), reads=[pd, cos], writes=[t1])
                        em.op("dve", lambda e: e.tensor_tensor(t2[:], psw[0:64, :], sin[:], ALU.mult), reads=[psw, sin], writes=[t2])
                        em.op("pool", lambda e: e.tensor_tensor(qt_b[r][:], t1[:], t2[:], ALU.add), reads=[t1, t2], writes=[qt_b[r]])
                        for cn in range(2):
                            pS = pS_r.next()
                            em.op("pe", lambda e: e.matmul(pS[:], KC[:, g, cn * 128:(cn + 1) * 128], qt_b[r][:], start=True, stop=True), reads=[KC, qt_b[r]], writes=[pS])
                            e32 = e32_r.next()
                            em.op("act", lambda e: e.activation(out=e32[:], in_=pS[:], func=AF.Exp), reads=[pS], writes=[e32])
                            em.op("dve", lambda e: e.tensor_tensor(p32[cn][:], e32[:], mc[:, cn, :], ALU.mult), reads=[e32, mc], writes=[p32[cn]])
                            em.op("pool", lambda e: e.tensor_copy(pbc[cn][:], p32[cn][:]), reads=[p32[cn]], writes=[pbc[cn]])
                        for cn in range(2):
                            em.op("pe", lambda e: e.matmul(pO[0:64, :], VC[:, g, cn, :], pbc[cn][:], start=(cn == 0), stop=(cn == 1)), reads=[VC, pbc[cn]], writes=[pO])
                        for cn in range(2):
                            em.op("pe", lambda e: e.matmul(pD[0:64, :], onesb[:], pbc[cn][:], start=(cn == 0), stop=(cn == 1)), reads=[onesb, pbc[cn]], writes=[pD])
                        finish_branch(r, 0, True)
                        for s in range(4):
                            for cn in range(2):
                                em.op("pe", lambda e: e.matmul(pI[:, 0:65], p32[cn][:, s * 128:(s + 1) * 128], ovaug[:, cn, :], start=(cn == 0), stop=(cn == 1)), reads=[p32[cn], ovaug], writes=[pI])
                            rd = rd_r.next()
                            em.op("dve", lambda e: e.tensor_scalar_max(rd[:], pI[:, 64:65], TINY), reads=[pI], writes=[rd])
                            em.op("dve", lambda e: e.reciprocal(rd[:], rd[:]), reads=[rd], writes=[rd])
                            if r == 0:
                                em.op("dve", lambda e: e.tensor_scalar(impg[:, s, :], pI[:, 0:64], rd[:, 0:1], None, ALU.mult), reads=[pI, rd], writes=[impg])
                            else:
                                em.op("dve", lambda e: e.scalar_tensor_tensor(impg[:, s, :], pI[:, 0:64], rd[:, 0:1], impg[:, s, :], ALU.mult, ALU.add), reads=[pI, rd, impg], writes=[impg])
                    for s in range(4):
                        selc = selc_r.next()
                        t0 = i * 512 + s * 128
                        em.dma("sp", selc[:], k_selc[t0:t0 + 128], writes=[selc])
                        sc = sc_r.next(); rep = rep_r.next()
                        em.op("dve", lambda e: e.tensor_tensor(sc[:], impg[:, s, :], selc[:, 0, :], ALU.mult), reads=[impg, selc], writes=[sc])
                        em.op("dve", lambda e: e.tensor_tensor(sc[:], sc[:], selc[:, 1, :], ALU.add), reads=[sc, selc], writes=[sc])
                        em.op("dve", lambda e: e.tensor_tensor(sc[:], sc[:], selc[:, 2, :], ALU.mult), reads=[sc, selc], writes=[sc])
                        em.op("dve", lambda e: e.tensor_tensor(sc[:], sc[:], selc[:, 3, :], ALU.add), reads=[sc, selc], writes=[sc])
                        em.op("dve", lambda e: e.max(m8a[:], sc[:]), reads=[sc], writes=[m8a])
                        em.op("dve", lambda e: e.match_replace(rep[:], m8a[:], sc[:], -3e30), reads=[sc, m8a], writes=[rep])
                        em.op("dve", lambda e: e.max(m8b[:], rep[:]), reads=[rep], writes=[m8b])
                        em.op("dve", lambda e: e.tensor_scalar(rep[:], sc[:], m8b[:, 7:8], None, ALU.is_ge), reads=[sc, m8b], writes=[rep])
                        em.op("dve", lambda e: e.tensor_tensor(rep[:], rep[:], selc[:, 2, :], ALU.mult), reads=[rep, selc], writes=[rep])
                        em.op("pe", lambda e: e.transpose(pI[0:64, 128:256], rep[:], ident[:]), reads=[rep, ident], writes=[pI])
                        em.op("act", lambda e: e.copy(selT[:, s * 128:(s + 1) * 128], pI[0:64, 128:256]), reads=[pI], writes=[selT])
                    for r in range(4):
                        h = g * 4 + r

                        def slc_stage(kt):
                            pS = pS_r.next(); pM = pM_r.next()
                            em.op("pe", lambda e: e.matmul(pS[:], KS[:, kt * 128:(kt + 1) * 128], qt_b[r][:], start=True, stop=True), reads=[KS, qt_b[r]], writes=[pS])
                            em.op("pe", lambda e: e.matmul(pM[:], eall[:, kt, :], selT[:], start=True, stop=True), reads=[eall, selT], writes=[pM])
                            eb = eb_r.next(); pb = pb_r.next()
                            em.op("act", lambda e: e.activation(out=eb[:], in_=pS[:], func=AF.Exp), reads=[pS], writes=[eb])
                            em.op("dve", lambda e: e.tensor_tensor(pb[:], eb[:], pM[:], ALU.mult), reads=[eb, pM], writes=[pb])
                            if kt >= 4 * i:
                                em.op("pool", lambda e: e.tensor_tensor(pb[:], pb[:], cz[:, kt - 4 * i, :], ALU.mult), reads=[pb, cz], writes=[pb])
                            return pb

                        def win_stage(kw):
                            kt = w0 + kw
                            pS = pS_r.next()
                            em.op("pe", lambda e: e.matmul(pS[:], KW[:, kw * 128:(kw + 1) * 128], qt_b[r][:], start=True, stop=True), reads=[KW, qt_b[r]], writes=[pS])
                            eb = eb_r.next(); pb = pb_r.next()
                            em.op("act", lambda e: e.activation(out=eb[:], in_=pS[:], func=AF.Exp), reads=[pS], writes=[eb])
                            em.op("pool", lambda e: e.tensor_tensor(pb[:], eb[:], wm[:, 4 * i - kt + 3, :], ALU.mult), reads=[eb, wm], writes=[pb])
                            return pb

                        cur_pb = slc_stage(0)
                        for kt in range(nkt):
                            nxt_pb = slc_stage(kt + 1) if kt + 1 < nkt else None
                            pb = cur_pb
                            em.op("pe", lambda e: e.matmul(pO[0:64, :], VS[:, kt, :], pb[:], start=(kt == 0), stop=(kt == nkt - 1)), reads=[VS, pb], writes=[pO])
                            em.op("pe", lambda e: e.matmul(pD[0:64, :], onesb[:], pb[:], start=(kt == 0), stop=(kt == nkt - 1)), reads=[onesb, pb], writes=[pD])
                            cur_pb = nxt_pb
                        finish_branch(r, 1, False)
                        cur_pb = win_stage(0)
                        for kw in range(nwk):
                            nxt_pb = win_stage(kw + 1) if kw + 1 < nwk else None
                            pb = cur_pb
                            em.op("pe", lambda e: e.matmul(pO[0:64, :], VW[:, kw, :], pb[:], start=(kw == 0), stop=(kw == nwk - 1)), reads=[VW, pb], writes=[pO])
                            em.op("pe", lambda e: e.matmul(pD[0:64, :], onesb[:], pb[:], start=(kw == 0), stop=(kw == nwk - 1)), reads=[onesb, pb], writes=[pD])
                            cur_pb = nxt_pb
                        finish_branch(r, 2, False)
                        em.op("act", lambda e: e.copy(oT[:, h, :], oc_b[r][:]), reads=[oc_b[r]], writes=[oT])
                for oc in range(8):
                    yo = yo_r.next()
                    for h in range(16):
                        em.op("pe", lambda e: e.matmul(pOut[:], ow[:, h, oc * 128:(oc + 1) * 128], oT[:, h, :], start=(h == 0), stop=(h == 15)), reads=[ow, oT], writes=[pOut])
                    em.op("dve", lambda e: e.tensor_copy(yo[:], pOut[:]), reads=[pOut], writes=[yo])
                    em.dma("sp", ydst[oc, :, sl], yo[:], reads=[yo])

    cur = 0
    if on("rope"):
        rope_phase()
    for l in range(DEPTH):
        if on("mix%d" % l):
            if l < 2:
                mamba_phase(l, xs_d[cur], ymix_d)
            else:
                nsa_phase(l, xs_d[cur], ymix_d)
        if on("ln%da" % l):
            post_norm(xs_d[cur], ymix_d, xs_d[1 - cur], mods[:, l, 16:24], 2 * l, False)
        cur = 1 - cur
        if on("moe%d" % l):
            moe_phase(l, xs_d[cur], ymix_d)
        if on("ln%db" % l):
            post_norm(xs_d[cur], ymix_d, xs_d[1 - cur], mods[:, l, 40:48], 2 * l + 1, l == DEPTH - 1)
        cur = 1 - cur
        if l == 1 and on("kv"):
            kv_phase(xs_d[cur])
    em.barrier()
    em.close()
    return nc, em


def make_in_maps(inputs, T, cores):
    cst = host_consts(T)
    f = lambda a: np.ascontiguousarray(np.asarray(a, dtype=np.float32))
    shared = {
        "ada_w": f(inputs["ada_w"]), "ada_b": f(inputs["ada_b"]),
        "ln_g": f(inputs["ln_g"]).reshape(8, D), "ln_b": f(inputs["ln_b"]).reshape(8, D),
        "ssm_in_w": f(inputs["ssm_in_w"]), "ssm_conv_w": f(inputs["ssm_conv_w"]), "ssm_conv_b": f(inputs["ssm_conv_b"]),
        "ssm_dt_bias": f(inputs["ssm_dt_bias"]), "ssm_a_log": f(inputs["ssm_a_log"]), "ssm_d": f(inputs["ssm_d"]),
        "ssm_norm_w": f(inputs["ssm_norm_w"]), "ssm_out_w": f(inputs["ssm_out_w"]),
        "kv_ada_w": f(inputs["kv_ada_w"]), "kv_ada_b": f(inputs["kv_ada_b"]).reshape(1, 2 * D),
        "kv_w": f(inputs["kv_w"]),
        "cmp_pos": f(inputs["cmp_pos"]),
        "phi_k_w1": f(inputs["phi_k_w1"]), "phi_k_w2": f(inputs["phi_k_w2"]),
        "phi_v_w1": f(inputs["phi_v_w1"]), "phi_v_w2": f(inputs["phi_v_w2"]),
        "nsa_q_w": f(inputs["nsa_q_w"]), "nsa_o_w": f(inputs["nsa_o_w"]),
        "router_w": f(inputs["router_w"]), "router_b": f(inputs["router_b"]),
        "moe_w_up": f(inputs["moe_w_up"]), "moe_b_up": f(inputs["moe_b_up"]),
        "moe_w_down": f(inputs["moe_w_down"]), "moe_b_down": f(inputs["moe_b_down"]),
    }
    kvw = shared["kv_w"]
    shared["kv_w_sw"] = np.concatenate([swap_halves(kvw[:, s * 256:(s + 1) * 256], 256) for s in (0, 2, 4)], axis=1)
    shared["nsa_q_w_sw"] = np.stack([swap_halves(shared["nsa_q_w"][j], 1024) for j in range(2)], axis=0)
    for k, v in cst.items():
        shared["k_" + k] = v
    maps = []
    for b in cores:
        m = dict(shared)
        m["x"] = f(inputs["x"][b][:T])
        m["c"] = np.ascontiguousarray(f(inputs["c"][b]).reshape(8, 128).T)
        m["pos"] = np.ascontiguousarray(np.asarray(inputs["pos"][b][:T], dtype=np.int32).reshape(1, T))
        maps.append(m)
    return maps


_CACHE = {}


def kernel(**inputs):
    T = 4096
    if T not in _CACHE:
        _CACHE[T] = build(T)[0]
    nc = _CACHE[T]
    maps = make_in_maps(inputs, T, list(range(8)))
    res = run_bass_kernel_spmd(nc, maps, core_ids=list(range(8)))
    out = np.stack([np.asarray(r["y"], dtype=np.float32) for r in res.results], axis=0)
    return out
```
